# Optimizing a Trainium2 kernel written in Bass

```python
import math
import jax, jax.numpy as jnp
from jax import lax
import numpy as np

D_MODEL = 1024
BATCH = 4
SEQ = 8192
DEPTH = 1

CONV_DIM = 1024
CONV_K = 31
HG_HEADS = 8
HG_DK = 128
HG_DV = 128
HG_CHUNK = 64
HG_KDIM = HG_HEADS * HG_DK
HG_VDIM = HG_HEADS * HG_DV
OFF_CA = 0
OFF_CB = OFF_CA + CONV_DIM
OFF_Q = OFF_CB + CONV_DIM
OFF_F = OFF_Q + HG_KDIM
OFF_I = OFF_F + HG_KDIM
OFF_G = OFF_I + HG_VDIM
OFF_GC = OFF_G + HG_VDIM
OFF_GH = OFF_GC + D_MODEL
IN_COLS = OFF_GH + D_MODEL
N_EXPERTS = 32
TOP_K = 4
D_FF = 1024
SWIGLU_ALPHA = 1.702
SWIGLU_LIMIT = 7.0
MOE_BLOCK = 256
EPS = 1e-6
N_MOD = 6

kernel_name = "hybrid_conv_hgrn2_moe_adaln_block"


def rmsnorm(x, g):
    xf = x.astype(jnp.float32)
    y = xf * lax.rsqrt(jnp.mean(xf * xf, axis=-1, keepdims=True) + EPS) * g.astype(jnp.float32)
    return y.astype(x.dtype)


def layernorm(x, g, b):
    xf = x.astype(jnp.float32)
    mu = jnp.mean(xf, axis=-1, keepdims=True)
    var = jnp.mean(jnp.square(xf - mu), axis=-1, keepdims=True)
    y = (xf - mu) * lax.rsqrt(var + EPS) * g.astype(jnp.float32) + b.astype(jnp.float32)
    return y.astype(x.dtype)


def conformer_conv(a, b, dw, dw_bias, ln_g, ln_b, w_proj):
    u = a * jax.nn.sigmoid(b)
    kern = dw.astype(u.dtype).reshape(CONV_K, 1, CONV_DIM)
    u = lax.conv_general_dilated(u, kern, window_strides=(1,), padding=[(CONV_K - 1, 0)],
                                 dimension_numbers=("NWC", "WIO", "NWC"),
                                 feature_group_count=CONV_DIM)
    u = u + dw_bias.astype(u.dtype)
    u = jax.nn.silu(layernorm(u, ln_g, ln_b))
    return u @ w_proj


def hgrn2_step(state, inp):
    q, k, v, logf = inp
    G = jnp.cumsum(logf, axis=2)
    C = q.shape[2]
    causal = jnp.tril(jnp.ones((C, C), dtype=bool))
    diff = G[:, :, :, None, :] - G[:, :, None, :, :]
    decay = jnp.exp(jnp.where(causal[None, None, :, :, None], diff, -jnp.inf))
    A = jnp.einsum("bhtk,bhsk,bhtsk->bhts", q, k, decay)
    o = jnp.einsum("bhts,bhsv->bhtv", A, v) + jnp.einsum("bhtk,bhkv->bhtv", q * jnp.exp(G), state)
    G_last = G[:, :, -1, :]
    k_dec = k * jnp.exp(G_last[:, :, None, :] - G)
    state = jnp.exp(G_last)[..., None] * state + jnp.einsum("bhsk,bhsv->bhkv", k_dec, v)
    return state, o


def hgrn2(q, fz, iv, og, lb, norm_g, w_proj):
    B, S, _ = q.shape
    n_chunks = S // HG_CHUNK
    qf = q.astype(jnp.float32).reshape(B, S, HG_HEADS, HG_DK) * (HG_DK ** -0.5)
    z = fz.astype(jnp.float32).reshape(B, S, HG_HEADS, HG_DK)
    lbh = lb.reshape(HG_HEADS, HG_DK)
    logf = jnp.logaddexp(jnp.log(lbh), jnp.log1p(-lbh) + jax.nn.log_sigmoid(z))
    k = (1.0 - lbh) * jax.nn.sigmoid(-z)
    v = iv.astype(jnp.float32).reshape(B, S, HG_HEADS, HG_DV)

    def to_chunks(t):
        return t.reshape(B, n_chunks, HG_CHUNK, HG_HEADS, t.shape[-1]).transpose(1, 0, 3, 2, 4)

    s0 = jnp.zeros((B, HG_HEADS, HG_DK, HG_DV), jnp.float32)
    _, o = lax.scan(hgrn2_step, s0, (to_chunks(qf), to_chunks(k), to_chunks(v), to_chunks(logf)))
    o = o.transpose(1, 0, 3, 2, 4).reshape(B, S, HG_HEADS, HG_DV)
    o = o * lax.rsqrt(jnp.mean(o * o, axis=-1, keepdims=True) + EPS) * norm_g.astype(jnp.float32)
    o = o * jax.nn.silu(og.astype(jnp.float32).reshape(B, S, HG_HEADS, HG_DV))
    return o.reshape(B, S, HG_VDIM).astype(q.dtype) @ w_proj


def clamped_swiglu(u):
    gl, lin = u[:, :D_FF], u[:, D_FF:]
    gl = jnp.minimum(gl, SWIGLU_LIMIT)
    lin = jnp.clip(lin, -SWIGLU_LIMIT, SWIGLU_LIMIT)
    return gl * jax.nn.sigmoid(SWIGLU_ALPHA * gl) * (lin + 1.0)


def moe(h, w_router, b_router, w1, b1, w2, b2):
    B, S, D = h.shape
    T = B * S
    TK = T * TOP_K
    hf = h.reshape(T, D)
    logits = hf.astype(jnp.float32) @ w_router.astype(jnp.float32) + b_router.astype(jnp.float32)
    top_val, top_idx = lax.top_k(logits, TOP_K)
    top_w = jax.nn.softmax(top_val, axis=-1)
    flat_e = top_idx.reshape(TK).astype(jnp.int32)
    flat_tok = jnp.arange(TK, dtype=jnp.int32) // TOP_K
    flat_w = top_w.reshape(TK)
    order = jnp.argsort(flat_e)
    se, stok, sw = flat_e[order], flat_tok[order], flat_w[order]
    counts = jnp.bincount(flat_e, length=N_EXPERTS).astype(jnp.int32)
    padded = (counts + MOE_BLOCK - 1) // MOE_BLOCK * MOE_BLOCK
    start = jnp.cumsum(counts) - counts
    pend = jnp.cumsum(padded)
    pstart = pend - padded
    dest = pstart[se] + jnp.arange(TK, dtype=jnp.int32) - start[se]
    n_blocks = -(-(TK + N_EXPERTS * (MOE_BLOCK - 1)) // MOE_BLOCK)
    R = n_blocks * MOE_BLOCK
    row_tok = jnp.zeros((R,), jnp.int32).at[dest].set(stok)
    row_w = jnp.zeros((R,), jnp.float32).at[dest].set(sw)
    block_e = jnp.minimum(
        jnp.searchsorted(pend, jnp.arange(n_blocks, dtype=jnp.int32) * MOE_BLOCK, side="right"),
        N_EXPERTS - 1).astype(jnp.int32)

    def block_fn(args):
        tok, e = args
        xb = hf[tok]
        u = xb @ w1[e] + b1[e]
        return clamped_swiglu(u) @ w2[e] + b2[e]

    ys = lax.map(block_fn, (row_tok.reshape(n_blocks, MOE_BLOCK), block_e))
    out = jnp.zeros((T, D), jnp.float32).at[row_tok].add(
        ys.reshape(R, D).astype(jnp.float32) * row_w[:, None])
    return out.reshape(B, S, D).astype(h.dtype)


def setup_inputs(seed: int = 0) -> dict:
    key = jax.random.key(seed)
    ks = jax.random.split(key, 24)
    n = jax.random.normal
    f32 = jnp.float32
    D, L = D_MODEL, DEPTH
    return {
        "x": n(ks[0], (BATCH, SEQ, D), f32),
        "c": n(ks[1], (BATCH, D), f32),
        "w_ada": n(ks[2], (L, D, N_MOD * D), f32) * (0.5 * D ** -0.5),
        "b_ada": n(ks[3], (L, N_MOD * D), f32) * 0.01,
        "g_mix": 1.0 + 0.02 * n(ks[4], (L, D), f32),
        "w_in": n(ks[5], (L, D, IN_COLS), f32) * D ** -0.5,
        "conv_dw": n(ks[6], (L, CONV_K, CONV_DIM), f32) * CONV_K ** -0.5,
        "conv_dw_bias": n(ks[7], (L, CONV_DIM), f32) * 0.01,
        "conv_ln_g": 1.0 + 0.02 * n(ks[8], (L, CONV_DIM), f32),
        "conv_ln_b": n(ks[9], (L, CONV_DIM), f32) * 0.01,
        "w_conv_out": n(ks[10], (L, CONV_DIM, D), f32) * CONV_DIM ** -0.5,
        "lb_param": n(ks[11], (L + 1, HG_KDIM), f32) * 0.5,
        "hgrn_norm_g": 1.0 + 0.02 * n(ks[12], (L, HG_DV), f32),
        "w_hgrn_out": n(ks[13], (L, HG_VDIM, D), f32) * HG_VDIM ** -0.5,
        "w_out": n(ks[14], (L, D, D), f32) * D ** -0.5,
        "g_ffn": 1.0 + 0.02 * n(ks[15], (L, D), f32),
        "w_router": n(ks[16], (L, D, N_EXPERTS), f32) * D ** -0.5,
        "b_router": n(ks[17], (L, N_EXPERTS), f32) * 0.01,
        "w1": n(ks[18], (L, N_EXPERTS, D, 2 * D_FF), f32) * D ** -0.5,
        "b1": n(ks[19], (L, N_EXPERTS, 2 * D_FF), f32) * 0.01,
        "w2": n(ks[20], (L, N_EXPERTS, D_FF, D), f32) * D_FF ** -0.5,
        "b2": n(ks[21], (L, N_EXPERTS, D), f32) * 0.01,
        "g_final": 1.0 + 0.02 * n(ks[22], (D,), f32),
    }


def reference(x, c, w_ada, b_ada, g_mix, w_in, conv_dw, conv_dw_bias, conv_ln_g, conv_ln_b,
              w_conv_out, lb_param, hgrn_norm_g, w_hgrn_out, w_out, g_ffn, w_router, b_router,
              w1, b1, w2, b2, g_final):
    dt = x.dtype
    lb_all = jnp.cumsum(jax.nn.softmax(lb_param.astype(jnp.float32), axis=0), axis=0)
    c_act = jax.nn.silu(c.astype(jnp.float32))
    for l in range(DEPTH):
        mod = c_act @ w_ada[l].astype(jnp.float32) + b_ada[l].astype(jnp.float32)
        sh1, sc1, ga1, sh2, sc2, ga2 = [m[:, None, :].astype(dt) for m in jnp.split(mod, N_MOD, axis=-1)]

        h = rmsnorm(x, g_mix[l]) * (1.0 + sc1) + sh1
        p = h @ w_in[l]
        y_conv = conformer_conv(p[..., OFF_CA:OFF_CB], p[..., OFF_CB:OFF_Q], conv_dw[l],
                                conv_dw_bias[l], conv_ln_g[l], conv_ln_b[l], w_conv_out[l])
        y_hg = hgrn2(p[..., OFF_Q:OFF_F], p[..., OFF_F:OFF_I], p[..., OFF_I:OFF_G],
                     p[..., OFF_G:OFF_GC], lb_all[l], hgrn_norm_g[l], w_hgrn_out[l])
        merged = (jax.nn.sigmoid(p[..., OFF_GC:OFF_GH]) * y_conv
                  + jax.nn.sigmoid(p[..., OFF_GH:IN_COLS]) * y_hg)
        x = x + ga1 * (merged @ w_out[l])

        h2 = rmsnorm(x, g_ffn[l]) * (1.0 + sc2) + sh2
        x = x + ga2 * moe(h2, w_router[l], b_router[l], w1[l], b1[l], w2[l], b2[l])
    return rmsnorm(x, g_final)
```

```python
import os
import numpy as np
import concourse.bass as bass
import concourse.mybir as mybir
from concourse.bass_utils import run_bass_kernel_spmd

F32 = mybir.dt.float32
BF16 = mybir.dt.bfloat16
I32 = mybir.dt.int32
ALU = mybir.AluOpType
AF = mybir.ActivationFunctionType

P = 128
D = 1024
KC = 8
NTOK = 4096
TT = 256
NS = TT // P
NTILE = NTOK // TT
HALO = 30
UW = HALO + TT + 2
NE = 32
QT = 1024
NQ = NTOK // QT
EPS = 1e-6
BLK = 512
NB = 64
NR = NB * BLK
CW = 768
SPARSE = True
C_C, C_BADA, C_GMIX, C_DWB, C_LNG, C_LNB, C_LB0, C_LB1, C_GFFN, C_DW, C_B1, C_NG, RV = (
    0, 8, 56, 64, 72, 80, 88, 96, 104, 112, 360, 872, 876)
SAME_ENG_SYNC = True
CUT = int(os.environ.get('KCUT', '99'))
SUB = int(os.environ.get('KSUB', '99'))
EPOCH = 30000


class Op:
    __slots__ = ("eng", "fn", "deps", "marked", "sem", "val", "dma")


class Part:
    __slots__ = ("w", "r")

    def __init__(self):
        self.w = {}
        self.r = {}


class Buf:
    def __init__(self, ap, name="", nparts=1):
        self.ap = ap
        self.parts = [Part() for _ in range(nparts)]
        self.name = name

    def __getitem__(self, k):
        if len(self.parts) == 1:
            return self
        return (self, k)


def _expand(lst):
    out = {}
    for it in lst:
        if it is None:
            continue
        if isinstance(it, tuple):
            b, k = it
            if isinstance(k, int):
                ps = [b.parts[k]]
            elif isinstance(k, slice):
                ps = b.parts[k]
            else:
                ps = [b.parts[i] for i in k]
        else:
            ps = it.parts
        for p in ps:
            out[id(p)] = p
    return out


class Sched:
    ENG = ("sp", "act", "dve", "pool", "pe")

    def __init__(self, dma_sems, eng_sems):
        self.ops = {e: [] for e in self.ENG}
        self.pool = dma_sems
        self.esem = eng_sems
        self.dma_i = {q: 0 for q in dma_sems}
        self.dma_last = {}

    def op(self, eng, fn, r=(), w=(), dma=False):
        o = Op()
        o.eng, o.fn, o.dma, o.marked, o.deps, o.sem, o.val = eng, fn, dma, False, {}, None, 0
        rp = _expand(r)
        wp = _expand(w)
        for p in rp.values():
            for x in p.w.values():
                o.deps[id(x)] = x
        for p in wp.values():
            for x in p.r.values():
                o.deps[id(x)] = x
            for x in p.w.values():
                o.deps[id(x)] = x
        key = ("d", id(o)) if dma else eng
        for p in wp.values():
            p.w = {key: o}
            p.r = {}
        for k, p in rp.items():
            if k not in wp:
                p.r[key] = o
        if dma:
            pl = self.pool[eng]
            i = self.dma_i[eng] % len(pl)
            self.dma_i[eng] += 1
            prev = self.dma_last.get((eng, i))
            if prev is not None:
                o.deps[id(prev)] = prev
            o.sem = pl[i]
            o.val = (prev.val if prev is not None else 0) + 16
            self.dma_last[(eng, i)] = o
        for d in list(o.deps.values()):
            if (not d.dma) and d.eng == eng and (eng == "pe" or not SAME_ENG_SYNC):
                del o.deps[id(d)]
            else:
                d.marked = True
        self.ops[eng].append(o)
        return o

    def finish(self):
        o = Op()
        o.eng, o.fn, o.dma, o.marked, o.sem, o.val = "sp", (lambda e: e.nop()), False, False, None, 0
        o.deps = {id(x): x for x in self.dma_last.values()}
        self.ops["sp"].append(o)
        for eng in self.ENG:
            cnt = 0
            for q in self.ops[eng]:
                if (not q.dma) and q.marked:
                    q.sem = self.esem[eng][cnt // EPOCH]
                    q.val = cnt % EPOCH + 1
                    cnt += 1

    def run(self, eng, e):
        known = {}
        for o in self.ops[eng]:
            for d in o.deps.values():
                k = d.sem.num
                if known.get(k, 0) >= d.val:
                    continue
                e.wait_ge(d.sem, d.val)
                known[k] = d.val
            ins = o.fn(e)
            if o.dma:
                ins.then_inc(o.sem, 16)
            elif o.marked:
                ins.then_inc(o.sem, 1)


def build_nc(stage=99, dbg=False):
    DBG = []
    nc = bass.Bass("TRN2", target_bir_lowering=False)

    def dram(name, shape, dtype=F32, kind="ExternalInput"):
        return nc.dram_tensor(name, shape, dtype, kind=kind).ap()

    xm = dram("xm", [NTOK, D])
    xp = dram("xp", [NTOK, D])
    flag_d = dram("flag", [P, 1])
    pvec_d = dram("pvec", [P, RV])
    cst_d = dram("cst", [P, CW])
    gffn_d = dram("g_ffn", [1, D])
    w_ada = dram("w_ada", [D, 6 * D])
    b_ada = dram("b_ada", [1, 6 * D])
    w_in = dram("w_in", [D, 8 * D])
    wco_d = dram("w_conv_out", [D, D])
    who_d = dram("w_hgrn_out", [D, D])
    wout_d = dram("w_out", [D, D])
    wr_d = dram("w_router", [D, NE])
    br_d = dram("b_router", [1, NE])
    w1_2d = dram("w1", [NE * D, 2 * D])
    w2_2d = dram("w2", [NE * D, D])
    w1_d = w1_2d.rearrange("(e r) n -> e r n", e=NE)
    w2_d = w2_2d.rearrange("(e r) n -> e r n", e=NE)
    b2_d = dram("b2", [NE, D])
    gfin_d = dram("g_final", [1, D])
    y_d = dram("y", [NTOK, D], F32, "ExternalOutput")
    x1_d = dram("x1_scr", [NTOK, D], F32, "Internal")
    h2_d = dram("h2_scr", [P, KC * NTOK], BF16, "Internal")
    h2tm_d = dram("h2tm_scr", [NTOK, D], BF16, "Internal")
    dg_d = dram("dg_scr", [KC, P, 31, P], BF16, "Internal")
    wbf_d = dram("wbf_scr", [22, P, KC * 512], BF16, "Internal")
    xs_d = dram("xs_scr", [NR, D], BF16, "Internal")
    ys_d = dram("ys_scr", [NR, D], BF16, "Internal")

    import contextlib
    es = contextlib.ExitStack()
    with es:
        AW = 52500
        arena = es.enter_context(nc.sbuf_tensor("arena", [P, AW], F32))
        psf = [es.enter_context(nc.psum_tensor("psf%d" % i, [P, 512], F32)) for i in range(6)]
        pst = [es.enter_context(nc.psum_tensor("pst%d" % i, [P, 1024], BF16)) for i in range(2)]
        dsems = {"sp": [es.enter_context(nc.semaphore("dmah%d" % i)) for i in range(20)],
                 "pool": [es.enter_context(nc.semaphore("dmas%d" % i)) for i in range(20)]}
        esems = {e: [es.enter_context(nc.semaphore("e_%s%d" % (e, i))) for i in range(3)]
                 for e in Sched.ENG}
        S = Sched(dsems, esems)
        PS = [Buf(t[:, :], "psf%d" % i, 1) for i, t in enumerate(psf)]
        PT = [Buf(t[:, :], "pst%d" % i, 1) for i, t in enumerate(pst)]
        psr = [0]

        def nps():
            b = PS[psr[0] % 4]
            psr[0] += 1
            return b
        PSS = PS[4]
        PSM = PS[5]

        class Carver:
            def __init__(self, start):
                self.off = start

            def get(self, dtype, free_shape, name="", nparts=1):
                n = int(np.prod(free_shape))
                nbytes = n * (2 if dtype == BF16 else 4)
                nbytes = (nbytes + 63) // 64 * 64
                w0 = self.off // 4
                w1 = (self.off + nbytes) // 4
                assert w1 <= AW, ("arena overflow", name, self.off + nbytes)
                ap = arena[:, w0:w1]
                if dtype != F32:
                    ap = ap.bitcast(dtype)
                ap = ap[:, 0:n]
                if len(free_shape) == 2:
                    ap = ap.rearrange("p (a b) -> p a b", a=free_shape[0])
                elif len(free_shape) == 3:
                    ap = ap.rearrange("p (a b c) -> p a b c", a=free_shape[0], b=free_shape[1])
                self.off += nbytes
                return Buf(ap, name, nparts)

        cv = Carver(0)
        pv = cv.get(F32, [RV], "pv")
        cst = cv.get(F32, [CW], "cst")
        flag = cv.get(F32, [1], "flag")
        identb = cv.get(BF16, [P], "identb")
        onesb = cv.get(BF16, [P], "onesb")
        onesf = cv.get(F32, [P], "onesf")
        mask01 = cv.get(BF16, [P], "mask01")
        modT = cv.get(F32, [48], "modT")
        gsc1 = cv.get(F32, [8], "gsc1")
        gsc2 = cv.get(F32, [8], "gsc2")
        lb = cv.get(F32, [8], "lb")
        oml = cv.get(F32, [8], "oml")
        sc = cv.get(F32, [8], "sc")
        ga1_bc = cv.get(F32, [D], "ga1_bc")
        ga2_bc = cv.get(F32, [D], "ga2_bc")
        gfin_bc = cv.get(F32, [D], "gfin_bc")
        br_bc = cv.get(F32, [NE], "br_bc")
        gates = cv.get(F32, [NTOK // P, NE], "gates", NTOK // P)
        wr = cv.get(BF16, [KC, NE], "wr")
        b2b = cv.get(BF16, [D], "b2b")
        small = cv.get(F32, [64], "small", 64)
        gsc2_bc = cv.get(F32, [D], "gsc2_bc")
        sh2_bc = cv.get(F32, [D], "sh2_bc")
        lgts = cv.get(F32, [NTOK // P, NE], "lgts", NTOK // P)
        mx4 = cv.get(F32, [NTOK // P, 4], "mx4", NTOK // P)
        gk = cv.get(F32, [NTOK // P, 4], "gk", NTOK // P)
        desti = cv.get(I32, [NTOK // P, 4], "desti")
        widx = cv.get(I32, [NB, KC], "widx")
        bef = cv.get(F32, [NB], "bef")
        shared_end = cv.off
        ident_f = cst.ap[:, 0:128]
        maskf = cst.ap[:, 128:256]
        resetm = cst.ap[:, 256:512]
        ustrict = cst.ap[:, 512:640]
        iotab = cst.ap[:, 640:704]
        iotae = cst.ap[:, 704:736]
        basekp = cst.ap[:, 736:744]

        mv = Carver(shared_end)
        xt = mv.get(F32, [NS, D], "xt", NS)
        junk = mv.get(BF16, [D], "junk")
        xn = mv.get(BF16, [NS, D], "xn", NS)
        hT = mv.get(BF16, [KC, TT], "hT", 8)
        setup_off = mv.off
        wslot = [mv.get(BF16, [KC, 512], "wslot%d" % i) for i in range(3)]
        uX = [mv.get(BF16, [KC, UW], "uX%d" % i, 8) for i in range(2)]
        FA = mv.get(F32, [KC, TT], "FA", 8)
        FB = mv.get(F32, [KC, TT], "FB", 8)
        FC = mv.get(F32, [KC, TT], "FC", 8)
        sgb = mv.get(BF16, [KC, TT], "sgb", 8)
        m1 = mv.get(BF16, [KC, TT], "m1", 8)
        QE = mv.get(BF16, [KC, TT], "QE", 8)
        QO = mv.get(BF16, [KC, TT], "QO", 8)
        kT = mv.get(BF16, [KC, TT], "kT", 8)
        sog = mv.get(BF16, [KC, TT], "sog", 8)
        sgc = mv.get(BF16, [KC, TT], "sgc", 8)
        sgh = mv.get(BF16, [KC, TT], "sgh", 8)
        osq = mv.get(BF16, [KC, TT], "osq", 8)
        mT = mv.get(BF16, [KC, TT], "mT", 8)
        h2t = mv.get(BF16, [KC, TT], "h2t", 8)
        vtm = mv.get(BF16, [NS, D], "vtm", NS)
        KE = mv.get(BF16, [NS, KC, P], "KE", NS)
        KO = mv.get(BF16, [NS, KC, P], "KO", NS)
        AT = [mv.get(BF16, [4, P], "AT%d" % i, 4) for i in range(2)]
        SA = mv.get(BF16, [KC, P], "SA", 8)
        SB = mv.get(BF16, [KC, P], "SB", 8)
        tmpa = mv.get(F32, [512], "tmpa")
        st_mean = mv.get(F32, [TT], "st_mean")
        st_var = mv.get(F32, [TT], "st_var")
        st_t = mv.get(F32, [TT], "st_t")
        lgt = mv.get(F32, [NE], "lgt")
        dgs = [mv.get(BF16, [31, P], "dgs%d" % i) for i in range(2)]
        mx8 = mv.get(F32, [8], "mx8")
        egt = mv.get(F32, [NE], "egt")
        mixer_end = mv.off
        sv = Carver(setup_off)
        scb = sv.get(F32, [KC, P], "scb")
        wada = [sv.get(F32, [KC, 512], "wada%d" % i) for i in range(2)]
        bada_bc = sv.get(F32, [D], "bada_bc")
        dgtmp = sv.get(BF16, [31, P], "dgtmp")
        DgB = Buf(None, "dg_dram")

        ev = Carver(shared_end)
        h2q = ev.get(BF16, [KC, QT], "h2q")
        acc = ev.get(F32, [QT // P, D], "acc", 2 * QT // P)
        W1 = [ev.get(BF16, [KC, 512], "W1_%d" % i) for i in range(4)]
        W2 = ev.get(BF16, [KC, D], "W2")
        actT = [ev.get(BF16, [KC, 512], "actT%d" % i, 8) for i in range(2)]
        tg = [ev.get(F32, [512], "tg%d" % i) for i in range(2)]
        tsg = [ev.get(F32, [512], "tsg%d" % i) for i in range(2)]
        tl = [ev.get(F32, [512], "tl%d" % i) for i in range(2)]
        x1t = [ev.get(F32, [D], "x1t%d" % i) for i in range(2)]
        xo = ev.get(F32, [D], "xo")
        ejunk = ev.get(BF16, [D], "ejunk")
        gTb = ev.get(BF16, [P], "gTb")
        moe_end = ev.off
        print("arena bytes: shared", shared_end, "mixer", mixer_end, "moe", moe_end)

        def V(fn, r=(), w=()):
            return S.op("dve", fn, r, w)

        def A(fn, r=(), w=()):
            return S.op("act", fn, r, w)

        def G(fn, r=(), w=()):
            return S.op("pool", fn, r, w)

        def T(fn, r=(), w=()):
            return S.op("pe", fn, r, w)

        def DMA(q, fn, r=(), w=()):
            return S.op(q, fn, r, w, dma=True)

        def dump(name, buf, ap=None):
            if not dbg:
                return
            ap = buf.ap if ap is None else ap
            shp = list(ap.shape)
            dtn = nc.dram_tensor("dbg_" + name, shp, ap.dtype, kind="ExternalOutput").ap()
            DMA("sp", lambda e: e.dma_start(out=dtn, in_=ap), r=[buf])
            DBG.append("dbg_" + name)

        def barrier(bufs):
            last = {e: S.ops[e][-1] for e in ("act", "dve", "pool", "pe") if S.ops[e]}
            bb = Buf(None, "barrier")
            for e, o in last.items():
                bb.parts[0].w[e] = o
            for x in S.dma_last.values():
                bb.parts[0].w[("d", id(x))] = x
            for e in ("act", "dve", "pool", "pe", "sp"):
                S.op(e, (lambda en: en.nop()), r=[bb])

        DMA("sp", lambda e: e.dma_start(out=pv.ap, in_=pvec_d), w=[pv])
        DMA("sp", lambda e: e.dma_start(out=cst.ap, in_=cst_d), w=[cst])
        DMA("sp", lambda e: e.dma_start(out=flag.ap, in_=flag_d), w=[flag])
        DMA("sp", lambda e: e.dma_start(out=gfin_bc.ap, in_=gfin_d.broadcast_to([P, D])), w=[gfin_bc])
        DMA("sp", lambda e: e.dma_start(out=br_bc.ap, in_=br_d.broadcast_to([P, NE])), w=[br_bc])
        DMA("pool", lambda e: e.dma_start(out=wr.ap, in_=wr_d.rearrange("(k p) n -> p k n", p=P)), w=[wr])
        DMA("pool", lambda e: e.dma_start(out=b2b.ap[0:NE, :], in_=b2_d), w=[b2b])
        V(lambda e: e.tensor_copy(out=identb.ap, in_=ident_f), r=[cst], w=[identb])
        V(lambda e: e.tensor_copy(out=mask01.ap, in_=maskf), r=[cst], w=[mask01])
        V(lambda e: e.memset(onesb.ap, 1.0), w=[onesb])
        V(lambda e: e.memset(onesf.ap, 1.0), w=[onesf])
        V(lambda e: e.memset(gates.ap, 0.0), w=[gates])
        V(lambda e: e.memset(lgts.ap, 0.0), w=[lgts])
        V(lambda e: e.memset(mx4.ap, 0.0), w=[mx4])
        V(lambda e: e.memset(gk.ap, 0.0), w=[gk])
        A(lambda e: e.activation(out=sc.ap, in_=pv.ap[:, C_C:C_C + 8], func=AF.Silu), r=[pv], w=[sc])
        V(lambda e: e.tensor_copy(out=scb.ap, in_=sc.ap.unsqueeze(2).broadcast_to([P, KC, P])), r=[sc], w=[scb])
        V(lambda e: e.tensor_tensor(out=small.ap[:, 0:8], in0=pv.ap[:, C_LB0:C_LB0 + 8],
                                    in1=pv.ap[:, C_LB1:C_LB1 + 8], op=ALU.subtract), r=[pv], w=[small[slice(0, 8)]])
        A(lambda e: e.activation(out=lb.ap, in_=small.ap[:, 0:8], func=AF.Sigmoid), r=[small[slice(0, 8)]], w=[lb])
        V(lambda e: e.tensor_scalar(out=oml.ap, in0=lb.ap, scalar1=-1.0, scalar2=1.0, op0=ALU.mult, op1=ALU.add),
          r=[lb], w=[oml])
        wada_v = w_ada.rearrange("(k p) n -> p k n", p=P)
        for g in range(12):
            wb = wada[g % 2]
            DMA("sp", lambda e, g=g, wb=wb: e.dma_start(out=wb.ap, in_=wada_v[:, :, g * 512:(g + 1) * 512]), w=[wb])
            for cc in range(4):
                j = g * 4 + cc
                for k in range(KC):
                    T(lambda e, j=j, k=k, cc=cc, wb=wb: e.matmul(PSM.ap[:, j:j + 1], lhsT=wb.ap[:, k, cc * 128:(cc + 1) * 128],
                                                             rhs=sc.ap[:, k:k + 1], start=(k == 0), stop=(k == KC - 1)),
                      r=[wb, sc], w=[PSM])
            if g in (4, 5, 6, 7, 8, 9, 10, 11):
                ps = nps()
                dst = {2: ga1_bc, 3: sh2_bc, 4: gsc2_bc, 5: ga2_bc}[g // 2]
                hh = g % 2
                for k in range(KC):
                    T(lambda e, k=k, wb=wb, ps=ps: e.matmul(ps.ap, lhsT=scb.ap[:, k, :], rhs=wb.ap[:, k, :],
                                                          start=(k == 0), stop=(k == KC - 1)), r=[wb, scb], w=[ps])
                DMA("sp", lambda e, g=g: e.dma_start(out=bada_bc.ap[:, 0:512],
                                                    in_=b_ada[:, g * 512:(g + 1) * 512].broadcast_to([P, 512])), w=[bada_bc])
                V(lambda e, ps=ps, dst=dst, hh=hh: e.tensor_tensor(out=dst.ap[:, hh * 512:(hh + 1) * 512], in0=ps.ap,
                                                                 in1=bada_bc.ap[:, 0:512], op=ALU.add),
                  r=[ps, bada_bc], w=[dst])
        V(lambda e: e.tensor_tensor(out=modT.ap, in0=PSM.ap[:, 0:48], in1=pv.ap[:, C_BADA:C_BADA + 48], op=ALU.add),
          r=[PSM, pv], w=[modT])
        V(lambda e: e.scalar_tensor_tensor(out=gsc1.ap, in0=modT.ap[:, 8:16], scalar=1.0, in1=pv.ap[:, C_GMIX:C_GMIX + 8],
                                           op0=ALU.add, op1=ALU.mult), r=[modT, pv], w=[gsc1])
        DMA("sp", lambda e: e.dma_start(out=bada_bc.ap, in_=gffn_d.broadcast_to([P, D])), w=[bada_bc])
        V(lambda e: e.scalar_tensor_tensor(out=gsc2_bc.ap, in0=gsc2_bc.ap, scalar=1.0, in1=bada_bc.ap, op0=ALU.add, op1=ALU.mult),
          r=[gsc2_bc, bada_bc], w=[gsc2_bc])
        V(lambda e: e.scalar_tensor_tensor(out=gsc2.ap, in0=modT.ap[:, 32:40], scalar=1.0, in1=pv.ap[:, C_GFFN:C_GFFN + 8],
                                           op0=ALU.add, op1=ALU.mult), r=[modT, pv], w=[gsc2])

        for c in range(KC):
            V(lambda e, c=c: e.tensor_tensor(out=dgtmp.ap, in0=identb.ap.unsqueeze(1).broadcast_to([P, 31, P]),
                                             in1=pv.ap[:, C_DW + c * 31:C_DW + (c + 1) * 31].unsqueeze(2).broadcast_to([P, 31, P]), op=ALU.mult),
              r=[identb, pv], w=[dgtmp])
            DMA("sp", lambda e, c=c: e.dma_start(out=dg_d[c], in_=dgtmp.ap), r=[dgtmp], w=[DgB])
        dump("modT", modT)
        dump("ga1", ga1_bc)
        dump("ga2", ga2_bc)
        dump("lb", lb)
        dump("gsc1", gsc1)
        barrier(None)
        V(lambda e: e.memset(QE.ap, 0.0), w=[QE])
        V(lambda e: e.memset(QO.ap, 0.0), w=[QO])
        V(lambda e: e.memset(KE.ap, 0.0), w=[KE])
        V(lambda e: e.memset(KO.ap, 0.0), w=[KO])
        V(lambda e: e.memset(SA.ap, 0.0), w=[SA])
        V(lambda e: e.memset(uX[0].ap, 0.0), w=[uX[0]])
        V(lambda e: e.memset(uX[1].ap, 0.0), w=[uX[1]])
        win_v = w_in.rearrange("(k p) n -> p k n", p=P)
        wco_v = wco_d.rearrange("(k p) n -> p k n", p=P)
        who_v = who_d.rearrange("(k p) n -> p k n", p=P)
        wout_v = wout_d.rearrange("(k p) n -> p k n", p=P)
        wq = {"n": 0, "pending": []}

        WbB = Buf(None, "wbf_dram", 22)

        def wsrc32(kind, g):
            if kind == "in":
                return win_v[:, :, g * 512:(g + 1) * 512]
            v = {"co": wco_v, "ho": who_v, "out": wout_v}[kind]
            return v[:, :, g * 512:(g + 1) * 512]

        def wsrc(kind, g):
            return {"in": 0, "co": 16, "ho": 18, "out": 20}[kind] + g

        gl_all = [("in", g) for g in range(16)] + [("co", 0), ("co", 1), ("ho", 0), ("ho", 1), ("out", 0), ("out", 1)]
        for n_, (k_, g_) in enumerate(gl_all):
            sl_ = wslot[n_ % 3]
            DMA("pool", lambda e, sl_=sl_, k_=k_, g_=g_: e.dma_start(out=sl_.ap, in_=wsrc32(k_, g_)), w=[sl_])
            DMA("sp", lambda e, sl_=sl_, n_=n_: e.dma_start(out=wbf_d[n_].rearrange("p (k n) -> p k n", k=KC), in_=sl_.ap),
                r=[sl_], w=[WbB[n_]])

        def wissue(src):
            slot = wslot[wq["n"] % 3]
            wq["n"] += 1
            if os.environ.get("KWMIX") == "none" and wq["n"] > 3:
                pass
            else:
                DMA("pool", lambda e, slot=slot, src=src: e.dma_start(out=slot.ap, in_=wbf_d[src].rearrange("p (k n) -> p k n", k=KC)),
                    r=[WbB[src]], w=[slot])
            wq["pending"].append(slot)

        def wnext():
            return wq["pending"].pop(0)

        def tile_groups(prefix, lastp):
            gl = []
            if (not prefix) or lastp:
                gl += [("in", 2), ("in", 3), ("in", 0), ("in", 1)]
            if CUT <= 3:
                return gl
            gl += [("in", 6), ("in", 7)]
            if CUT <= 4:
                return gl
            if not prefix:
                gl += [("in", 4), ("in", 5)]
            gl += [("in", 8), ("in", 9)]
            if not prefix:
                gl += [("in", 10), ("in", 11), ("in", 12), ("in", 13), ("in", 14), ("in", 15),
                       ("co", 0), ("co", 1)]
                if CUT > 6:
                    gl += [("ho", 0), ("ho", 1), ("out", 0), ("out", 1)]
            return gl

        tiles = [(True, t == NTILE - 1, t) for t in range(NTILE)] + [(False, False, t) for t in range(NTILE)]
        allg = []
        for (pf, lp, t) in tiles:
            allg += [wsrc(k, g) for (k, g) in tile_groups(pf, lp)]
        gi = {"i": 0}

        def wget():
            while gi["i"] < len(allg) and len(wq["pending"]) < 3:
                wissue(allg[gi["i"]])
                gi["i"] += 1
            return wnext()

        def fm_group(ws, cbase, evac):
            for cc in range(4):
                ps = nps()
                for k in range(KC):
                    T(lambda e, ps=ps, k=k, cc=cc: e.matmul(ps.ap[:, 0:TT], lhsT=ws.ap[:, k, cc * 128:(cc + 1) * 128],
                                                          rhs=hT.ap[:, k, :], start=(k == 0), stop=(k == KC - 1)),
                      r=[ws, hT], w=[ps])
                evac(ps, cbase + cc)

        def rms_to_T(src, dstT, scale_ap, bias_ap, scale_b, bias_b):
            for j in range(NS):
                A(lambda e, j=j: e.activation(out=junk.ap, in_=src.ap[:, j, :], func=AF.Square,
                                              accum_out=small.ap[:, 8 + j:9 + j]), r=[src[j]], w=[junk, small[8 + j]])
            A(lambda e: e.activation(out=small.ap[:, 12:12 + NS], in_=small.ap[:, 8:8 + NS], func=AF.Sqrt,
                                     scale=1.0 / D, bias=EPS), r=[small[slice(8, 8 + NS)]], w=[small[slice(12, 12 + NS)]])
            V(lambda e: e.reciprocal(out=small.ap[:, 16:16 + NS], in_=small.ap[:, 12:12 + NS]), r=[small[slice(12, 12 + NS)]], w=[small[slice(16, 16 + NS)]])
            for j in range(NS):
                V(lambda e, j=j: e.tensor_scalar(out=xn.ap[:, j, :], in0=src.ap[:, j, :], scalar1=small.ap[:, 16 + j:17 + j],
                                                 scalar2=None, op0=ALU.mult), r=[src[j], small[16 + j]], w=[xn[j]])
            for k in range(KC):
                pt = PT[k % 2]
                for j in range(NS):
                    T(lambda e, pt=pt, j=j, k=k: e.transpose(pt.ap[:, j * P:(j + 1) * P], xn.ap[:, j, k * P:(k + 1) * P],
                                                            identb.ap), r=[xn[j], identb], w=[pt[j]])
                A(lambda e, pt=pt, k=k: e.activation(out=dstT.ap[:, k, :], in_=pt.ap[:, 0:TT], func=AF.Identity,
                                                     scale=scale_ap[:, k:k + 1], bias=bias_ap[:, k:k + 1]),
                  r=[pt[slice(0, NS)], scale_b, bias_b], w=[dstT[k]])

        def mixer_tile(prefix, lastp, t):
            src = xp if prefix else xm
            tok0 = t * TT
            ucur = uX[t % 2]
            uprev = uX[(t + 1) % 2]
            do_conv_in = (not prefix) or lastp
            DMA("sp", lambda e: e.dma_start(out=xt.ap, in_=src[tok0:tok0 + TT, :].rearrange("(j p) d -> p j d", p=P)), w=[xt])
            rms_to_T(xt, hT, gsc1.ap, modT.ap[:, 0:8], gsc1, modT)
            if CUT <= 1:
                return
            dm = dbg and (not prefix) and t == 0
            if dm:
                dump("hT", hT)
            if do_conv_in:
                V(lambda e: e.tensor_copy(out=ucur.ap[:, :, 0:HALO], in_=uprev.ap[:, :, TT:TT + HALO]), r=[uprev], w=[ucur])
                for half in range(2):
                    ws = wget()
                    fm_group(ws, half * 4, lambda ps, c: A(
                        lambda e, ps=ps, c=c: e.activation(out=sgb.ap[:, c, :], in_=ps.ap[:, 0:TT], func=AF.Sigmoid),
                        r=[ps], w=[sgb[c]]))
                if dm:
                    dump("sgb0", sgb)
                for half in range(2):
                    ws = wget()
                    if dm:
                        dump("ws%d" % half, ws)
                    fm_group(ws, half * 4, lambda ps, c: V(
                        lambda e, ps=ps, c=c: e.tensor_tensor(out=ucur.ap[:, c, HALO:HALO + TT], in0=ps.ap[:, 0:TT],
                                                              in1=sgb.ap[:, c, :], op=ALU.mult), r=[ps, sgb[c]], w=[ucur[c]]))
                if prefix:
                    V(lambda e: e.tensor_scalar(out=ucur.ap[:, :, TT:TT + HALO], in0=ucur.ap[:, :, TT:TT + HALO],
                                                scalar1=flag.ap[:, 0:1], scalar2=None, op0=ALU.mult), r=[ucur, flag], w=[ucur])
            if CUT <= 2:
                return
            if not prefix:
                if os.environ.get("KCONV") == "dve":
                    for c in range(KC):
                        V(lambda e, c=c: e.tensor_scalar(out=FA.ap[:, c, :], in0=ucur.ap[:, c, 0:TT],
                                                         scalar1=pv.ap[:, C_DW + c * 31:C_DW + c * 31 + 1],
                                                         scalar2=pv.ap[:, C_DWB + c:C_DWB + c + 1], op0=ALU.mult, op1=ALU.add),
                          r=[ucur[c], pv], w=[FA[c]])
                    for j in range(1, 31):
                        for c in range(KC):
                            V(lambda e, c=c, j=j: e.scalar_tensor_tensor(out=FA.ap[:, c, :], in0=ucur.ap[:, c, j:j + TT],
                                                                         scalar=pv.ap[:, C_DW + c * 31 + j:C_DW + c * 31 + j + 1],
                                                                         in1=FA.ap[:, c, :], op0=ALU.mult, op1=ALU.add),
                              r=[ucur[c], pv, FA[c]], w=[FA[c]])
                else:
                    for c in range(KC):
                        dg = dgs[c % 2]
                        DMA("sp", lambda e, c=c, dg=dg: e.dma_start(out=dg.ap, in_=dg_d[c]), r=[DgB], w=[dg])
                        ps = nps()
                        for j in range(31):
                            T(lambda e, c=c, j=j, dg=dg, ps=ps: e.matmul(ps.ap[:, 0:TT], lhsT=dg.ap[:, j, :], rhs=ucur.ap[:, c, j:j + TT],
                                                                       start=(j == 0), stop=(j == 30)), r=[dg, ucur[c]], w=[ps])
                        V(lambda e, c=c, ps=ps: e.tensor_scalar(out=FA.ap[:, c, :], in0=ps.ap[:, 0:TT], scalar1=pv.ap[:, C_DWB + c:C_DWB + c + 1],
                                                                scalar2=None, op0=ALU.add), r=[ps, pv], w=[FA[c]])
                A(lambda e: e.activation(out=FB.ap, in_=FA.ap, func=AF.Square), r=[FA], w=[FB])
                for k in range(KC):
                    T(lambda e, k=k: e.matmul(PSS.ap[:, 0:TT], lhsT=onesf.ap, rhs=FA.ap[:, k, :], start=(k == 0), stop=(k == KC - 1)),
                      r=[onesf, FA[k]], w=[PSS[slice(0, 2)]])
                for k in range(KC):
                    T(lambda e, k=k: e.matmul(PSS.ap[:, TT:2 * TT], lhsT=onesf.ap, rhs=FB.ap[:, k, :], start=(k == 0), stop=(k == KC - 1)),
                      r=[onesf, FB[k]], w=[PSS[slice(2, 4)]])
                V(lambda e: e.tensor_scalar(out=st_mean.ap, in0=PSS.ap[:, 0:TT], scalar1=1.0 / D, scalar2=None, op0=ALU.mult),
                  r=[PSS], w=[st_mean])
                V(lambda e: e.tensor_tensor(out=st_t.ap, in0=st_mean.ap, in1=st_mean.ap, op=ALU.mult), r=[st_mean], w=[st_t])
                V(lambda e: e.scalar_tensor_tensor(out=st_var.ap, in0=PSS.ap[:, TT:2 * TT], scalar=1.0 / D, in1=st_t.ap,
                                                   op0=ALU.mult, op1=ALU.subtract), r=[PSS, st_t], w=[st_var])
                A(lambda e: e.activation(out=st_var.ap, in_=st_var.ap, func=AF.Sqrt, bias=EPS), r=[st_var], w=[st_var])
                V(lambda e: e.reciprocal(out=st_var.ap, in_=st_var.ap), r=[st_var], w=[st_var])
                V(lambda e: e.tensor_tensor(out=FA.ap, in0=FA.ap, in1=st_mean.ap.unsqueeze(1).broadcast_to([P, KC, TT]),
                                            op=ALU.subtract), r=[FA, st_mean], w=[FA])
                V(lambda e: e.tensor_tensor(out=FA.ap, in0=FA.ap, in1=st_var.ap.unsqueeze(1).broadcast_to([P, KC, TT]),
                                            op=ALU.mult), r=[FA, st_var], w=[FA])
                for c in range(KC):
                    A(lambda e, c=c: e.activation(out=sgb.ap[:, c, :], in_=FA.ap[:, c, :], func=AF.Silu,
                                                  scale=pv.ap[:, C_LNG + c:C_LNG + c + 1], bias=pv.ap[:, C_LNB + c:C_LNB + c + 1]),
                      r=[FA[c], pv], w=[sgb[c]])
            if dm:
                dump("ucur", ucur)
                dump("u2T", sgb)
            if CUT <= 3:
                return
            for half in range(2):
                ws = wget()
                fm_group(ws, half * 4, lambda ps, c: A(
                    lambda e, ps=ps, c=c: e.activation(out=FA.ap[:, c, :], in_=ps.ap[:, 0:TT], func=AF.Sigmoid), r=[ps], w=[FA[c]]))
            for c in range(KC):
                V(lambda e, c=c: e.tensor_scalar(out=FA.ap[:, c, :], in0=FA.ap[:, c, :], scalar1=oml.ap[:, c:c + 1],
                                                 scalar2=lb.ap[:, c:c + 1], op0=ALU.mult, op1=ALU.add), r=[FA[c], oml, lb], w=[FA[c]])
            A(lambda e: e.activation(out=FB.ap, in_=FA.ap, func=AF.Ln), r=[FA], w=[FB])
            for c in range(KC):
                V(lambda e, c=c: e.tensor_tensor_scan(out=FC.ap[:, c, :], data0=resetm, data1=FB.ap[:, c, :], initial=0.0,
                                                      op0=ALU.mult, op1=ALU.add), r=[FB[c], cst], w=[FC[c]])
            A(lambda e: e.activation(out=FB.ap, in_=FC.ap, func=AF.Exp), r=[FC], w=[FB])
            A(lambda e: e.activation(out=FC.ap, in_=FC.ap, func=AF.Exp, scale=-1.0), r=[FC], w=[FC])
            V(lambda e: e.scalar_tensor_tensor(out=kT.ap, in0=FA.ap, scalar=1.0, in1=FC.ap, op0=ALU.subtract, op1=ALU.mult),
              r=[FA, FC], w=[kT])
            if CUT <= 4:
                return
            if not prefix:
                for half in range(2):
                    ws = wget()

                    def evq(ps, c):
                        for par, dst in ((0, QE), (1, QO)):
                            V(lambda e, ps=ps, c=c, par=par, dst=dst: e.scalar_tensor_tensor(
                                out=dst.ap[:, c, :].rearrange("p (b two s) -> p b two s", two=2, s=64)[:, :, par, :],
                                in0=ps.ap[:, 0:TT].rearrange("p (b two s) -> p b two s", two=2, s=64)[:, :, par, :],
                                scalar=-(128.0 ** -0.5),
                                in1=FB.ap[:, c, :].rearrange("p (b two s) -> p b two s", two=2, s=64)[:, :, par, :],
                                op0=ALU.mult, op1=ALU.mult), r=[ps, FB[c]], w=[dst[c]])
                    fm_group(ws, half * 4, evq)
            for half in range(2):
                ws = wget()
                for j in range(NS):
                    ps = nps()
                    for k in range(KC):
                        T(lambda e, ps=ps, k=k, j=j, ws=ws: e.matmul(ps.ap, lhsT=hT.ap[:, k, j * P:(j + 1) * P], rhs=ws.ap[:, k, :],
                                                                   start=(k == 0), stop=(k == KC - 1)), r=[hT, ws], w=[ps])
                    A(lambda e, ps=ps, j=j, half=half: e.activation(out=vtm.ap[:, j, half * 512:(half + 1) * 512], in_=ps.ap,
                                                                    func=AF.Identity), r=[ps], w=[vtm[j]])
            if not prefix:
                ws = wget()
                fm_group(ws, 0, lambda ps, c: A(lambda e, ps=ps, c=c: e.activation(out=sog.ap[:, c, :], in_=ps.ap[:, 0:TT], func=AF.Silu), r=[ps], w=[sog[c]]))
                ws = wget()
                fm_group(ws, 4, lambda ps, c: A(lambda e, ps=ps, c=c: e.activation(out=sog.ap[:, c, :], in_=ps.ap[:, 0:TT], func=AF.Silu), r=[ps], w=[sog[c]]))
                for dst in (sgc, sgh):
                    for half in range(2):
                        ws = wget()
                        fm_group(ws, half * 4, lambda ps, c, dst=dst: A(
                            lambda e, ps=ps, c=c, dst=dst: e.activation(out=dst.ap[:, c, :], in_=ps.ap[:, 0:TT], func=AF.Sigmoid),
                            r=[ps], w=[dst[c]]))
                for half in range(2):
                    ws = wget()
                    for cc in range(4):
                        c = half * 4 + cc
                        ps = nps()
                        for k in range(KC):
                            T(lambda e, ps=ps, k=k, cc=cc, ws=ws: e.matmul(ps.ap[:, 0:TT], lhsT=ws.ap[:, k, cc * P:(cc + 1) * P],
                                                                         rhs=sgb.ap[:, k, :], start=(k == 0), stop=(k == KC - 1)),
                              r=[ws, sgb], w=[ps])
                        V(lambda e, ps=ps, c=c: e.tensor_tensor(out=m1.ap[:, c, :], in0=ps.ap[:, 0:TT], in1=sgc.ap[:, c, :], op=ALU.mult),
                          r=[ps, sgc[c]], w=[m1[c]])
            if dm:
                dump("f", FA)
                dump("eG", FB)
                dump("kT", kT)
                dump("QE", QE)
                dump("QO", QO)
                dump("vtm", vtm)
                dump("m1", m1)
                dump("sog", sog)
            if CUT <= 5:
                return
            for b in range(NS):
                blk = slice(b * P, (b + 1) * P)
                pt = PT[b % 2]
                for h in range(KC):
                    T(lambda e, pt=pt, h=h, blk=blk: e.transpose(pt.ap[:, h * P:(h + 1) * P], kT.ap[:, h, blk], identb.ap),
                      r=[kT[h], identb], w=[pt[h]])
                A(lambda e, pt=pt, b=b: e.activation(out=KE.ap[0:64, b, :, :], in_=pt.ap[0:64, :].rearrange("p (h k) -> p h k", h=KC),
                                                     func=AF.Identity), r=[pt], w=[KE[b]])
                V(lambda e, pt=pt, b=b: e.tensor_copy(out=KO.ap[64:128, b, :, :], in_=pt.ap[64:128, :].rearrange("p (h k) -> p h k", h=KC)),
                  r=[pt], w=[KO[b]])
                if not prefix:
                    for hq in range(2):
                        psa = nps()
                        at = AT[hq]
                        for hh in range(4):
                            h = hq * 4 + hh
                            T(lambda e, psa=psa, h=h, hh=hh, blk=blk: e.matmul(psa.ap[:, hh * P:(hh + 1) * P], lhsT=kT.ap[:, h, blk],
                                                                             rhs=QE.ap[:, h, blk], start=True, stop=False),
                              r=[kT[h], QE[h]], w=[psa[hh]])
                            T(lambda e, psa=psa, h=h, hh=hh, blk=blk: e.matmul(psa.ap[:, hh * P:(hh + 1) * P], lhsT=kT.ap[:, h, blk],
                                                                             rhs=QO.ap[:, h, blk], start=False, stop=True),
                              r=[kT[h], QO[h]], w=[psa[hh]])
                        V(lambda e, psa=psa, at=at: e.tensor_tensor(out=at.ap, in0=psa.ap.rearrange("p (h t) -> p h t", h=4),
                                                                    in1=mask01.ap.unsqueeze(1).broadcast_to([P, 4, P]), op=ALU.mult),
                          r=[psa, mask01], w=[at])
                for (Sin, Sout, Kx, par) in ((SA, SB, KE, 0), (SB, SA, KO, 1)):
                    for hq in range(2):
                        PSx = PSS if hq == 0 else PSM
                        at = AT[hq]
                        for hh in range(4):
                            h = hq * 4 + hh
                            T(lambda e, h=h, hh=hh, Sin=Sin, PSx=PSx: e.matmul(PSx.ap[:, hh * P:(hh + 1) * P], lhsT=identb.ap, rhs=Sin.ap[:, h, :],
                                                                               start=True, stop=False), r=[identb, Sin[h]], w=[PSx])
                            T(lambda e, h=h, hh=hh, Kx=Kx, b=b, PSx=PSx: e.matmul(PSx.ap[:, hh * P:(hh + 1) * P], lhsT=Kx.ap[:, b, h, :],
                                                                                  rhs=vtm.ap[:, b, h * P:(h + 1) * P], start=False, stop=True),
                              r=[Kx[b], vtm[b]], w=[PSx])
                        if par == 1 and (not prefix):
                            pso = nps()
                            for hh in range(4):
                                h = hq * 4 + hh
                                T(lambda e, pso=pso, h=h, hh=hh, b=b, at=at: e.matmul(pso.ap[:, hh * P:(hh + 1) * P], lhsT=vtm.ap[:, b, h * P:(h + 1) * P],
                                                                                     rhs=at.ap[:, hh, :], start=True, stop=False),
                                  r=[vtm[b], at[hh]], w=[pso[hh]])
                                T(lambda e, pso=pso, h=h, hh=hh, blk=blk: e.matmul(pso.ap[:, hh * P:(hh + 1) * P], lhsT=SA.ap[:, h, :],
                                                                                 rhs=QE.ap[:, h, blk], start=False, stop=False),
                                  r=[SA[h], QE[h]], w=[pso[hh]])
                                T(lambda e, pso=pso, h=h, hh=hh, blk=blk: e.matmul(pso.ap[:, hh * P:(hh + 1) * P], lhsT=SB.ap[:, h, :],
                                                                                 rhs=QO.ap[:, h, blk], start=False, stop=True),
                                  r=[SB[h], QO[h]], w=[pso[hh]])
                            A(lambda e, pso=pso, hq=hq, blk=blk: e.activation(out=FA.ap[:, hq * 4:hq * 4 + 4, blk],
                                                                              in_=pso.ap.rearrange("p (h t) -> p h t", h=4), func=AF.Identity),
                              r=[pso], w=[FA[slice(hq * 4, hq * 4 + 4)]])
                    for hq in range(2):
                        PSx = PSS if hq == 0 else PSM
                        h0 = hq * 4
                        col = b * P + par * 64 + 63
                        V(lambda e, h0=h0, col=col, Sout=Sout, PSx=PSx: e.tensor_tensor(
                            out=Sout.ap[:, h0:h0 + 4, :], in0=PSx.ap.rearrange("p (h v) -> p h v", h=4),
                            in1=FB.ap[:, h0:h0 + 4, col:col + 1].broadcast_to([P, 4, P]), op=ALU.mult),
                          r=[PSx, FB[slice(h0, h0 + 4)]], w=[Sout[slice(h0, h0 + 4)]])
            if CUT <= 6:
                return
            if prefix:
                if lastp:
                    V(lambda e: e.tensor_scalar(out=SA.ap, in0=SA.ap, scalar1=flag.ap[:, 0:1], scalar2=None, op0=ALU.mult),
                      r=[SA, flag], w=[SA])
                return
            if dm:
                dump("oT", FA)
                dump("SA", SA)
            A(lambda e: e.activation(out=osq.ap, in_=FA.ap, func=AF.Square), r=[FA], w=[osq])
            for hp in range(4):
                ps = nps()
                for i in range(2):
                    h = hp * 2 + i
                    T(lambda e, ps=ps, h=h, i=i: e.matmul(ps.ap[:, i * TT:(i + 1) * TT], lhsT=onesb.ap, rhs=osq.ap[:, h, :], start=True, stop=True),
                      r=[onesb, osq[h]], w=[ps[slice(i * 2, i * 2 + 2)]])
                A(lambda e, ps=ps, hp=hp: e.activation(out=FC.ap[:, hp * 2:hp * 2 + 2, :], in_=ps.ap.rearrange("p (h t) -> p h t", h=2),
                                                       func=AF.Sqrt, scale=1.0 / 128.0, bias=EPS), r=[ps], w=[FC[slice(hp * 2, hp * 2 + 2)]])
            V(lambda e: e.reciprocal(out=FC.ap, in_=FC.ap), r=[FC], w=[FC])
            V(lambda e: e.tensor_tensor(out=FA.ap, in0=FA.ap, in1=FC.ap, op=ALU.mult), r=[FA, FC], w=[FA])
            V(lambda e: e.scalar_tensor_tensor(out=kT.ap, in0=FA.ap, scalar=pv.ap[:, C_NG:C_NG + 1], in1=sog.ap,
                                               op0=ALU.mult, op1=ALU.mult), r=[FA, pv, sog], w=[kT])
            for half in range(2):
                ws = wget()
                for cc in range(4):
                    c = half * 4 + cc
                    ps = nps()
                    for k in range(KC):
                        T(lambda e, ps=ps, k=k, cc=cc, ws=ws: e.matmul(ps.ap[:, 0:TT], lhsT=ws.ap[:, k, cc * P:(cc + 1) * P], rhs=kT.ap[:, k, :],
                                                                     start=(k == 0), stop=(k == KC - 1)), r=[ws, kT], w=[ps])
                    V(lambda e, ps=ps, c=c: e.tensor_tensor(out=mT.ap[:, c, :], in0=ps.ap[:, 0:TT], in1=sgh.ap[:, c, :], op=ALU.mult),
                      r=[ps, sgh[c]], w=[mT[c]])
            V(lambda e: e.tensor_tensor(out=mT.ap, in0=mT.ap, in1=m1.ap, op=ALU.add), r=[mT, m1], w=[mT])
            for half in range(2):
                ws = wget()
                for j in range(NS):
                    ps = nps()
                    for k in range(KC):
                        T(lambda e, ps=ps, k=k, j=j, ws=ws: e.matmul(ps.ap, lhsT=mT.ap[:, k, j * P:(j + 1) * P], rhs=ws.ap[:, k, :],
                                                                   start=(k == 0), stop=(k == KC - 1)), r=[mT, ws], w=[ps])
                    V(lambda e, ps=ps, half=half: e.tensor_tensor(out=tmpa.ap, in0=ps.ap, in1=ga1_bc.ap[:, half * 512:(half + 1) * 512], op=ALU.mult),
                      r=[ps, ga1_bc], w=[tmpa])
                    V(lambda e, j=j, half=half: e.tensor_tensor(out=xt.ap[:, j, half * 512:(half + 1) * 512], in0=tmpa.ap,
                                                                in1=xt.ap[:, j, half * 512:(half + 1) * 512], op=ALU.add), r=[tmpa, xt[j]], w=[xt[j]])
            if dm:
                dump("ogT", kT)
                dump("mT", mT)
                dump("x1", xt)
            if CUT <= 7:
                return
            DMA("sp", lambda e: e.dma_start(out=x1_d[tok0:tok0 + TT, :].rearrange("(j p) d -> p j d", p=P), in_=xt.ap), r=[xt])
            rms_to_T(xt, h2t, gsc2.ap, modT.ap[:, 24:32], gsc2, modT)
            if not SPARSE:
                DMA("sp", lambda e: e.dma_start(out=h2_d.rearrange("p (k t) -> p k t", k=KC)[:, :, tok0:tok0 + TT], in_=h2t.ap), r=[h2t])
            else:
                for j in range(NS):
                    for half in range(2):
                        hs = slice(half * 512, (half + 1) * 512)
                        V(lambda e, j=j, hs=hs: e.scalar_tensor_tensor(out=tmpa.ap, in0=xt.ap[:, j, hs], scalar=small.ap[:, 16 + j:17 + j],
                                                                       in1=gsc2_bc.ap[:, hs], op0=ALU.mult, op1=ALU.mult),
                          r=[xt[j], small[16 + j], gsc2_bc], w=[tmpa])
                        V(lambda e, j=j, hs=hs: e.tensor_tensor(out=xn.ap[:, j, hs], in0=tmpa.ap, in1=sh2_bc.ap[:, hs], op=ALU.add),
                          r=[tmpa, sh2_bc], w=[xn[j]])
                DMA("sp", lambda e: e.dma_start(out=h2tm_d[tok0:tok0 + TT, :].rearrange("(j p) d -> p j d", p=P), in_=xn.ap), r=[xn])
            for j in range(NS):
                st = t * NS + j
                for k in range(KC):
                    T(lambda e, k=k, j=j: e.matmul(PSM.ap[:, 0:NE], lhsT=h2t.ap[:, k, j * P:(j + 1) * P], rhs=wr.ap[:, k, :],
                                                   start=(k == 0), stop=(k == KC - 1)), r=[h2t, wr], w=[PSM])
                V(lambda e: e.tensor_tensor(out=lgt.ap, in0=PSM.ap[:, 0:NE], in1=br_bc.ap, op=ALU.add), r=[PSM, br_bc], w=[lgt])
                V(lambda e, st=st: e.tensor_copy(out=lgts.ap[:, st, :], in_=lgt.ap), r=[lgt], w=[lgts[st]])
                V(lambda e: e.max(out=mx8.ap, in_=lgt.ap), r=[lgt], w=[mx8])
                V(lambda e: e.tensor_scalar(out=small.ap[:, 24:25], in0=mx8.ap[:, 0:1], scalar1=-1.0, scalar2=None, op0=ALU.mult),
                  r=[mx8], w=[small[24]])
                A(lambda e: e.activation(out=egt.ap, in_=lgt.ap, func=AF.Exp, bias=small.ap[:, 24:25]), r=[lgt, small[24]], w=[egt])
                V(lambda e: e.scalar_tensor_tensor(out=egt.ap, in0=lgt.ap, scalar=mx8.ap[:, 3:4], in1=egt.ap, op0=ALU.is_ge, op1=ALU.mult),
                  r=[lgt, mx8, egt], w=[egt])
                V(lambda e: e.reduce_sum(out=small.ap[:, 25:26], in_=egt.ap, axis=mybir.AxisListType.X), r=[egt], w=[small[25]])
                V(lambda e: e.reciprocal(out=small.ap[:, 26:27], in_=small.ap[:, 25:26]), r=[small[25]], w=[small[26]])
                V(lambda e, st=st: e.tensor_scalar(out=gates.ap[:, st, :], in0=egt.ap, scalar1=small.ap[:, 26:27], scalar2=None, op0=ALU.mult),
                  r=[egt, small[26]], w=[gates[st]])
                V(lambda e, st=st: e.tensor_copy(out=mx4.ap[:, st, :], in_=mx8.ap[:, 0:4]), r=[mx8], w=[mx4[st]])
                A(lambda e, st=st: e.activation(out=gk.ap[:, st, :], in_=mx8.ap[:, 0:4], func=AF.Exp, bias=small.ap[:, 24:25]),
                  r=[mx8, small[24]], w=[gk[st]])
                V(lambda e, st=st: e.tensor_scalar(out=gk.ap[:, st, :], in0=gk.ap[:, st, :], scalar1=small.ap[:, 26:27], scalar2=None, op0=ALU.mult),
                  r=[gk[st], small[26]], w=[gk[st]])

        if stage in (1, 3):
            tiles = [(True, True, NTILE - 1)] + [(False, False, t) for t in range(2 if stage == 1 else 4)]
            allg = []
            for (pf, lp, t) in tiles:
                allg += [wsrc(k, g) for (k, g) in tile_groups(pf, lp)]
        if stage == 0:
            tiles = []
        if os.environ.get("KPRE") == "0":
            tiles = [x for x in tiles if not x[0]]
            allg = []
            for (pf, lp, t) in tiles:
                allg += [wsrc(k, g) for (k, g) in tile_groups(pf, lp)]
        if os.environ.get("KPRE") == "only":
            tiles = [x for x in tiles if x[0]]
            allg = []
            for (pf, lp, t) in tiles:
                allg += [wsrc(k, g) for (k, g) in tile_groups(pf, lp)]
        for (pf, lp, t) in tiles:
            mixer_tile(pf, lp, t)
        if dbg and tiles:
            dump("h2t", h2t)
            dump("gates", gates)

        barrier(None)


        nst = len([1 for (pf, lp, t) in tiles if not pf]) * NS
        NST = NTOK // P
        XsB = Buf(None, "xs_dram", 4 * NST + 1)
        YsB = Buf(None, "ys_dram", NB)
        if stage == 6:
            nst = 0
        if SPARSE and nst > 0:
            rv = Carver(shared_end)
            maskall = rv.get(F32, [NST, NE], "maskall")
            posall = rv.get(F32, [NST, NE], "posall", NST)
            eqt = rv.get(F32, [NST, NE], "eqt")
            cum = rv.get(F32, [NE], "cum")
            nblk = rv.get(F32, [NE], "nblk")
            pend = rv.get(F32, [NE], "pend")
            pstart = rv.get(F32, [NE], "pstart")
            destf = rv.get(F32, [NST, 4], "destf", 4)
            widxf = rv.get(F32, [NB, KC], "widxf")
            oobf = rv.get(F32, [NB], "oobf")
            oob2 = rv.get(F32, [NB], "oob2")
            zt = rv.get(BF16, [4096], "zt")
            hrow = [rv.get(BF16, [D], "hrow%d" % i) for i in range(2)]
            V(lambda e: e.memset(zt.ap, 0.0), w=[zt])
            xs_fill = xs_d.rearrange("(c p q) d -> c p (q d)", p=P, q=4)
            for ci in range(NR // (P * 4)):
                DMA("sp", lambda e, ci=ci: e.dma_start(out=xs_fill[ci], in_=zt.ap), r=[zt], w=[XsB])
            V(lambda e: e.tensor_scalar(out=maskall.ap, in0=gates.ap, scalar1=0.0, scalar2=None, op0=ALU.is_gt), r=[gates], w=[maskall])
            V(lambda e: e.memset(cum.ap, 0.0), w=[cum])
            for st in range(NST):
                T(lambda e, st=st: e.matmul(PSM.ap[:, 0:NE], lhsT=ustrict, rhs=maskall.ap[:, st, :], start=True, stop=False), r=[cst, maskall], w=[PSM])
                T(lambda e: e.matmul(PSM.ap[:, 0:NE], lhsT=onesf.ap, rhs=cum.ap, start=False, stop=True), r=[onesf, cum], w=[PSM])
                A(lambda e, st=st: e.activation(out=posall.ap[:, st, :], in_=PSM.ap[:, 0:NE], func=AF.Identity), r=[PSM], w=[posall[st]])
                V(lambda e, st=st: e.tensor_tensor(out=cum.ap, in0=cum.ap, in1=maskall.ap[:, st, :], op=ALU.add), r=[cum, maskall], w=[cum])
            T(lambda e: e.matmul(PSM.ap[:, 0:NE], lhsT=onesf.ap, rhs=cum.ap, start=True, stop=True), r=[onesf, cum], w=[PSM])
            V(lambda e: e.tensor_copy(out=cum.ap, in_=PSM.ap[:, 0:NE]), r=[PSM], w=[cum])
            V(lambda e: e.memset(nblk.ap, 0.0), w=[nblk])
            for jb in range(NTOK // BLK):
                V(lambda e, jb=jb: e.scalar_tensor_tensor(out=nblk.ap, in0=cum.ap, scalar=float(jb * BLK), in1=nblk.ap, op0=ALU.is_gt, op1=ALU.add),
                  r=[cum, nblk], w=[nblk])
            V(lambda e: e.tensor_scalar(out=nblk.ap, in0=nblk.ap, scalar1=float(BLK), scalar2=None, op0=ALU.mult), r=[nblk], w=[nblk])
            V(lambda e: e.tensor_tensor_scan(out=pend.ap, data0=onesf.ap[:, 0:NE], data1=nblk.ap, initial=0.0, op0=ALU.mult, op1=ALU.add),
              r=[onesf, nblk], w=[pend])
            V(lambda e: e.tensor_tensor(out=pstart.ap, in0=pend.ap, in1=nblk.ap, op=ALU.subtract), r=[pend, nblk], w=[pstart])
            V(lambda e: e.tensor_tensor(out=posall.ap, in0=posall.ap, in1=pstart.ap.unsqueeze(1).broadcast_to([P, NST, NE]), op=ALU.add),
              r=[posall, pstart], w=[posall])
            for k in range(4):
                V(lambda e, k=k: e.tensor_tensor(out=eqt.ap, in0=lgts.ap, in1=mx4.ap[:, :, k:k + 1].broadcast_to([P, NST, NE]), op=ALU.is_equal),
                  r=[lgts, mx4], w=[eqt])
                V(lambda e: e.tensor_tensor(out=eqt.ap, in0=eqt.ap, in1=posall.ap, op=ALU.mult), r=[eqt, posall], w=[eqt])
                V(lambda e, k=k: e.reduce_sum(out=destf.ap[:, :, k], in_=eqt.ap, axis=mybir.AxisListType.X), r=[eqt], w=[destf[k]])
            V(lambda e: e.tensor_copy(out=desti.ap, in_=destf.ap), r=[destf], w=[desti])
            V(lambda e: e.memset(bef.ap, 0.0), w=[bef])
            for ex in range(NE):
                V(lambda e, ex=ex: e.scalar_tensor_tensor(out=bef.ap, in0=iotab, scalar=pend.ap[:, ex:ex + 1], in1=bef.ap, op0=ALU.is_ge, op1=ALU.add),
                  r=[cst, pend, bef], w=[bef])
            V(lambda e: e.tensor_scalar(out=bef.ap, in0=bef.ap, scalar1=float(NE - 1), scalar2=None, op0=ALU.min), r=[bef], w=[bef])
            V(lambda e: e.scalar_tensor_tensor(out=widxf.ap, in0=bef.ap.unsqueeze(2).broadcast_to([P, NB, KC]), scalar=float(D),
                                               in1=basekp.unsqueeze(1).broadcast_to([P, NB, KC]), op0=ALU.mult, op1=ALU.add),
              r=[bef, cst], w=[widxf])
            V(lambda e: e.tensor_scalar(out=oobf.ap, in0=iotab, scalar1=pend.ap[:, NE - 1:NE], scalar2=65536.0, op0=ALU.is_ge, op1=ALU.mult),
              r=[cst, pend], w=[oobf])
            V(lambda e: e.tensor_tensor(out=oob2.ap[:, 2:NB], in0=bef.ap[:, 2:NB], in1=bef.ap[:, 0:NB - 2], op=ALU.is_equal), r=[bef], w=[oob2])
            V(lambda e: e.scalar_tensor_tensor(out=oobf.ap[:, 2:NB], in0=oob2.ap[:, 2:NB], scalar=65536.0, in1=oobf.ap[:, 2:NB],
                                               op0=ALU.mult, op1=ALU.add), r=[oob2, oobf], w=[oobf])
            V(lambda e: e.tensor_tensor(out=widxf.ap, in0=widxf.ap, in1=oobf.ap.unsqueeze(2).broadcast_to([P, NB, KC]), op=ALU.add),
              r=[widxf, oobf], w=[widxf])
            V(lambda e: e.tensor_copy(out=widx.ap, in_=widxf.ap), r=[widxf], w=[widx])
            for st in range(nst):
                hr = hrow[st % 2]
                DMA("sp", lambda e, st=st, hr=hr: e.dma_start(out=hr.ap, in_=h2tm_d[st * P:(st + 1) * P, :]), w=[hr])
                for k in range(4):
                    DMA("pool", lambda e, st=st, k=k, hr=hr: e.indirect_dma_start(
                        out=xs_d[:, :], out_offset=bass.IndirectOffsetOnAxis(ap=desti.ap[:, st, k:k + 1], axis=0),
                        in_=hr.ap, in_offset=None), r=[hr, desti, XsB[4 * NST]], w=[XsB[st * 4 + k]])
            if dbg:
                dump("desti", desti)
                dump("bef", bef)
                dump("cnt", cum)
            barrier(None)

            bv = Carver(shared_end)
            W1b = [bv.get(BF16, [KC, 2 * D], "W1b%d" % i, KC) for i in range(2)]
            W2b = [bv.get(BF16, [KC, D], "W2b%d" % i, KC) for i in range(2)]
            xrows = bv.get(BF16, [4, D], "xrows", 4)
            xbTs = [bv.get(BF16, [KC, BLK], "xbT%d" % i, KC) for i in range(2)]
            actB = [bv.get(BF16, [KC, BLK], "actB%d" % i, KC) for i in range(2)]
            tgB = [bv.get(F32, [BLK], "tgB%d" % i) for i in range(2)]
            tsB = [bv.get(F32, [BLK], "tsB%d" % i) for i in range(2)]
            tlB = [bv.get(F32, [BLK], "tlB%d" % i) for i in range(2)]
            ysb = [bv.get(BF16, [D], "ysb%d" % i, 2) for i in range(2)]
            oneh = bv.get(F32, [NE], "oneh")
            b1tmp = bv.get(F32, [16, NE], "b1tmp")
            b1sel = [bv.get(F32, [16], "b1sel%d" % i) for i in range(2)]
            print("arena bytes: blocks", bv.off)
            w1_flat = w1_2d
            w2_flat = w2_2d
            b1v = pv.ap[:, C_B1:C_B1 + NE * 16].rearrange("p (e i) -> p i e", i=16)
            ps6 = [0]

            def nps6():
                bk = PS[ps6[0] % 6]
                ps6[0] += 1
                return bk

            bc_cache = {}

            def bc_reg(e):
                if "r" not in bc_cache:
                    bc_cache["r"] = e.to_reg(NE * D - 1)
                return bc_cache["r"]

            def load_w1(b):
                wa = W1b[b % 2]
                for k in range(KC):
                    DMA("pool", lambda e, b=b, k=k, wa=wa: e.indirect_dma_start(
                        out=wa.ap[:, k, :], out_offset=None, in_=w1_flat[:, :],
                        in_offset=bass.IndirectOffsetOnAxis(ap=widx.ap[:, b, k:k + 1], axis=0),
                        bounds_check=bc_reg(e), oob_is_err=False), r=[widx], w=[wa[k]])

            def load_w2(b):
                wb2 = W2b[b % 2]
                for k in range(KC):
                    DMA("pool", lambda e, b=b, k=k, wb2=wb2: e.indirect_dma_start(
                        out=wb2.ap[:, k, :], out_offset=None, in_=w2_flat[:, :],
                        in_offset=bass.IndirectOffsetOnAxis(ap=widx.ap[:, b, k:k + 1], axis=0),
                        bounds_check=bc_reg(e), oob_is_err=False), r=[widx], w=[wb2[k]])

            def prep(b):
                xbT = xbTs[b % 2]
                DMA("sp", lambda e, b=b: e.dma_start(out=xrows.ap, in_=xs_d[b * BLK:(b + 1) * BLK, :].rearrange("(j p) d -> p j d", p=P)),
                    r=[XsB], w=[xrows])
                for k in range(KC):
                    pt = PT[k % 2]
                    for j in range(4):
                        T(lambda e, pt=pt, j=j, k=k: e.transpose(pt.ap[:, j * P:(j + 1) * P], xrows.ap[:, j, k * P:(k + 1) * P], identb.ap),
                          r=[xrows[j], identb], w=[pt])
                    if k % 2 == 0:
                        A(lambda e, pt=pt, k=k, xbT=xbT: e.activation(out=xbT.ap[:, k, :], in_=pt.ap[:, 0:BLK], func=AF.Identity), r=[pt], w=[xbT[k]])
                    else:
                        V(lambda e, pt=pt, k=k, xbT=xbT: e.tensor_copy(out=xbT.ap[:, k, :], in_=pt.ap[:, 0:BLK]), r=[pt], w=[xbT[k]])
                bs = b1sel[b % 2]
                V(lambda e, b=b: e.tensor_scalar(out=oneh.ap, in0=iotae, scalar1=bef.ap[:, b:b + 1], scalar2=None, op0=ALU.is_equal),
                  r=[cst, bef], w=[oneh])
                V(lambda e: e.tensor_tensor(out=b1tmp.ap, in0=b1v, in1=oneh.ap.unsqueeze(1).broadcast_to([P, 16, NE]), op=ALU.mult),
                  r=[pv, oneh], w=[b1tmp])
                V(lambda e, bs=bs: e.reduce_sum(out=bs.ap, in_=b1tmp.ap, axis=mybir.AxisListType.X), r=[b1tmp], w=[bs])
                V(lambda e, bs=bs: e.tensor_scalar(out=bs.ap[:, 8:16], in0=bs.ap[:, 8:16], scalar1=1.0, scalar2=None, op0=ALU.add), r=[bs], w=[bs])

            def w1_piece(b, i):
                wa, xbT, bs, aT = W1b[b % 2], xbTs[b % 2], b1sel[b % 2], actB[b % 2]
                psg = nps6()
                psl = nps6()
                for k in range(KC):
                    T(lambda e, k=k: e.matmul(psg.ap, lhsT=wa.ap[:, k, i * P:(i + 1) * P], rhs=xbT.ap[:, k, :],
                                              start=(k == 0), stop=(k == KC - 1)), r=[wa, xbT], w=[psg])
                for k in range(KC):
                    T(lambda e, k=k: e.matmul(psl.ap, lhsT=wa.ap[:, k, D + i * P:D + (i + 1) * P], rhs=xbT.ap[:, k, :],
                                              start=(k == 0), stop=(k == KC - 1)), r=[wa, xbT], w=[psl])
                a_, b_, c_ = tgB[i % 2], tsB[i % 2], tlB[i % 2]
                V(lambda e: e.tensor_scalar(out=a_.ap, in0=psg.ap, scalar1=bs.ap[:, i:i + 1], scalar2=7.0, op0=ALU.add, op1=ALU.min),
                  r=[psg, bs], w=[a_])
                A(lambda e: e.activation(out=b_.ap, in_=a_.ap, func=AF.Silu, scale=1.702), r=[a_], w=[b_])
                V(lambda e: e.tensor_scalar(out=c_.ap, in0=psl.ap, scalar1=bs.ap[:, 8 + i:9 + i], scalar2=8.0, op0=ALU.add, op1=ALU.min),
                  r=[psl, bs], w=[c_])
                V(lambda e: e.scalar_tensor_tensor(out=aT.ap[:, i, :], in0=c_.ap, scalar=-6.0, in1=b_.ap, op0=ALU.max, op1=ALU.mult),
                  r=[b_, c_], w=[aT[i]])

            def w2_piece(b, g):
                j4, half = g // 2, g % 2
                wb2, aT, yb = W2b[b % 2], actB[b % 2], ysb[j4 % 2]
                ps = nps6()
                for i in range(KC):
                    T(lambda e, i=i: e.matmul(ps.ap, lhsT=aT.ap[:, i, j4 * P:(j4 + 1) * P], rhs=wb2.ap[:, i, half * 512:(half + 1) * 512],
                                              start=(i == 0), stop=(i == KC - 1)), r=[aT, wb2], w=[ps])
                if half == 0:
                    A(lambda e: e.activation(out=yb.ap[:, 0:512], in_=ps.ap, func=AF.Identity, scale=1.0 / 1.702), r=[ps], w=[yb[0]])
                else:
                    A(lambda e: e.activation(out=yb.ap[:, 512:1024], in_=ps.ap, func=AF.Identity, scale=1.0 / 1.702), r=[ps], w=[yb[1]])
                    r0 = b * BLK + j4 * P
                    DMA("sp", lambda e: e.dma_start(out=ys_d[r0:r0 + P, :], in_=yb.ap), r=[yb], w=[YsB[b]])

            nblocks = NB if stage == 99 else int(os.environ.get("KNB", NB))
            if nblocks:
                load_w1(0)
                load_w2(0)
                prep(0)
                if nblocks > 1:
                    load_w1(1)
            for b in range(nblocks):
                for i in range(KC):
                    w1_piece(b, i)
                    if i == 1 and b + 1 < nblocks:
                        prep(b + 1)
                    if b > 0:
                        w2_piece(b - 1, i)
                if b + 2 < nblocks:
                    load_w1(b + 2)
                if b + 1 < nblocks:
                    load_w2(b + 1)
            if nblocks:
                for g in range(8):
                    w2_piece(nblocks - 1, g)
            barrier(None)


            cb = Carver(shared_end)
            Yk2 = [[cb.get(BF16, [D], "Yk%d_%d" % (s_, i)) for i in range(4)] for s_ in range(2)]
            accs = cb.get(F32, [D], "accs", 2)
            cx1 = [cb.get(F32, [D], "cx1%d" % i) for i in range(2)]
            cxo = cb.get(F32, [D], "cxo")
            cjunk = cb.get(BF16, [D], "cjunk")
            cgT = cb.get(BF16, [P], "cgT")
            for st in range(nst):
                r0 = st * P
                xb = cx1[st % 2]
                Yk = Yk2[st % 2]
                DMA("sp", lambda e, xb=xb, r0=r0: e.dma_start(out=xb.ap, in_=x1_d[r0:r0 + P, :]), w=[xb])
                for k in range(4):
                    DMA("pool", lambda e, st=st, k=k, Yk=Yk: e.indirect_dma_start(
                        out=Yk[k].ap, out_offset=None, in_=ys_d[:, :],
                        in_offset=bass.IndirectOffsetOnAxis(ap=desti.ap[:, st, k:k + 1], axis=0)), r=[desti, YsB], w=[Yk[k]])
                T(lambda e, st=st: e.transpose(PSM.ap[0:NE, 0:P], gates.ap[:, st, :], ident_f), r=[gates[st], cst], w=[PSM])
                V(lambda e: e.tensor_copy(out=cgT.ap[0:NE, :], in_=PSM.ap[0:NE, 0:P]), r=[PSM], w=[cgT])
                for half in range(2):
                    ps = nps()
                    T(lambda e, ps=ps, half=half: e.matmul(ps.ap, lhsT=cgT.ap[0:NE, :], rhs=b2b.ap[0:NE, half * 512:(half + 1) * 512],
                                                          start=True, stop=True), r=[cgT, b2b], w=[ps])
                    A(lambda e, ps=ps, half=half: e.activation(out=accs.ap[:, half * 512:(half + 1) * 512], in_=ps.ap, func=AF.Identity),
                      r=[ps], w=[accs[half]])
                for k in range(4):
                    V(lambda e, st=st, k=k, Yk=Yk: e.scalar_tensor_tensor(out=accs.ap, in0=Yk[k].ap, scalar=gk.ap[:, st, k:k + 1], in1=accs.ap,
                                                                   op0=ALU.mult, op1=ALU.add), r=[Yk[k], gk[st], accs], w=[accs])
                V(lambda e: e.tensor_tensor(out=cxo.ap, in0=accs.ap, in1=ga2_bc.ap, op=ALU.mult), r=[accs, ga2_bc], w=[cxo])
                V(lambda e, xb=xb: e.tensor_tensor(out=cxo.ap, in0=cxo.ap, in1=xb.ap, op=ALU.add), r=[cxo, xb], w=[cxo])
                A(lambda e: e.activation(out=cjunk.ap, in_=cxo.ap, func=AF.Square, accum_out=small.ap[:, 32:33]), r=[cxo], w=[cjunk, small[32]])
                A(lambda e: e.activation(out=small.ap[:, 33:34], in_=small.ap[:, 32:33], func=AF.Sqrt, scale=1.0 / D, bias=EPS), r=[small[32]], w=[small[33]])
                V(lambda e: e.reciprocal(out=small.ap[:, 34:35], in_=small.ap[:, 33:34]), r=[small[33]], w=[small[34]])
                V(lambda e, xb=xb: e.scalar_tensor_tensor(out=xb.ap, in0=cxo.ap, scalar=small.ap[:, 34:35], in1=gfin_bc.ap, op0=ALU.mult, op1=ALU.mult),
                  r=[cxo, small[34], gfin_bc], w=[xb])
                DMA("sp", lambda e, xb=xb, r0=r0: e.dma_start(out=y_d[r0:r0 + P, :], in_=xb.ap), r=[xb])

        w1_v = w1_d.rearrange("e (k p) n -> e p k n", p=P)
        w2_v = w2_d.rearrange("e (k p) n -> e p k n", p=P)
        h2_v = h2_d.rearrange("p (k t) -> p k t", k=KC)

        def load_w1(e_, i):
            DMA("pool", lambda en: en.dma_start(out=W1[i].ap[:, :, 0:256], in_=w1_v[e_][:, :, i * 256:(i + 1) * 256]), w=[W1[i]])
            DMA("pool", lambda en: en.dma_start(out=W1[i].ap[:, :, 256:512], in_=w1_v[e_][:, :, D + i * 256:D + (i + 1) * 256]), w=[W1[i]])

        def load_w2(e_):
            DMA("pool", lambda en: en.dma_start(out=W2.ap, in_=w2_v[e_]), w=[W2])

        seq = [(q, e_) for q in range(NQ) for e_ in range(NE)]
        if stage <= 1 or SPARSE:
            seq = []
        if stage in (2, 3):
            seq = [(0, e_) for e_ in range(NE)]
        if SPARSE:
            seq = []
        if seq:
            for i in range(4):
                load_w1(0, i)
            load_w2(0)
        for si, (q, e_) in enumerate(seq):
            nxt = seq[si + 1][1] if si + 1 < len(seq) else None
            if e_ == 0:
                DMA("sp", lambda en, q=q: en.dma_start(out=h2q.ap, in_=h2_v[:, :, q * QT:(q + 1) * QT]), w=[h2q])
                for j in range(QT // P):
                    st = q * (QT // P) + j
                    T(lambda en, st=st: en.transpose(PSM.ap[0:NE, 0:P], gates.ap[:, st, :], ident_f), r=[gates[st], cst], w=[PSM])
                    V(lambda en: en.tensor_copy(out=gTb.ap[0:NE, :], in_=PSM.ap[0:NE, 0:P]), r=[PSM], w=[gTb])
                    for half in range(2):
                        ps = nps()
                        T(lambda en, ps=ps, half=half: en.matmul(ps.ap, lhsT=gTb.ap[0:NE, :], rhs=b2b.ap[0:NE, half * 512:(half + 1) * 512],
                                                                start=True, stop=True), r=[gTb, b2b], w=[ps])
                        A(lambda en, ps=ps, j=j, half=half: en.activation(out=acc.ap[:, j, half * 512:(half + 1) * 512], in_=ps.ap, func=AF.Identity),
                          r=[ps], w=[acc[j * 2 + half]])
            for blk in range(QT // 512):
                tsl = slice(blk * 512, (blk + 1) * 512)
                aT = actT[blk % 2]
                for i in range(KC):
                    g4, sub = i // 2, i % 2
                    wb = W1[g4]
                    psg = nps()
                    psl = nps()
                    for k in range(KC):
                        T(lambda en, psg=psg, k=k, wb=wb, sub=sub, tsl=tsl: en.matmul(psg.ap, lhsT=wb.ap[:, k, sub * P:(sub + 1) * P], rhs=h2q.ap[:, k, tsl],
                                                                                 start=(k == 0), stop=(k == KC - 1)), r=[wb, h2q], w=[psg])
                    for k in range(KC):
                        T(lambda en, psl=psl, k=k, wb=wb, sub=sub, tsl=tsl: en.matmul(psl.ap, lhsT=wb.ap[:, k, 256 + sub * P:256 + (sub + 1) * P], rhs=h2q.ap[:, k, tsl],
                                                                                 start=(k == 0), stop=(k == KC - 1)), r=[wb, h2q], w=[psl])
                    if blk == QT // 512 - 1 and sub == 1 and nxt is not None:
                        load_w1(nxt, g4)
                    cg = C_B1 + e_ * 16 + i
                    cl = C_B1 + e_ * 16 + 8 + i
                    a_, b_, c_ = tg[i % 2], tsg[i % 2], tl[i % 2]
                    V(lambda en, psg=psg, cg=cg, a_=a_: en.tensor_scalar(out=a_.ap, in0=psg.ap, scalar1=pv.ap[:, cg:cg + 1], scalar2=7.0, op0=ALU.add, op1=ALU.min),
                      r=[psg, pv], w=[a_])
                    A(lambda en, a_=a_, b_=b_: en.activation(out=b_.ap, in_=a_.ap, func=AF.Sigmoid, scale=1.702), r=[a_], w=[b_])
                    V(lambda en, psl=psl, cl=cl, c_=c_: en.tensor_scalar(out=c_.ap, in0=psl.ap, scalar1=pv.ap[:, cl:cl + 1], scalar2=7.0, op0=ALU.add, op1=ALU.min),
                      r=[psl, pv], w=[c_])
                    G(lambda en, c_=c_: en.tensor_scalar(out=c_.ap, in0=c_.ap, scalar1=-7.0, scalar2=1.0, op0=ALU.max, op1=ALU.add), r=[c_], w=[c_])
                    G(lambda en, a_=a_, b_=b_: en.tensor_tensor(out=a_.ap, in0=a_.ap, in1=b_.ap, op=ALU.mult), r=[a_, b_], w=[a_])
                    V(lambda en, a_=a_, c_=c_, aT=aT, i=i: en.tensor_tensor(out=aT.ap[:, i, :], in0=a_.ap, in1=c_.ap, op=ALU.mult), r=[a_, c_], w=[aT[i]])
                for j4 in range(4):
                    j = blk * 4 + j4
                    st = q * (QT // P) + j
                    for half in range(2):
                        ps = nps()
                        for i in range(KC):
                            T(lambda en, ps=ps, i=i, j4=j4, half=half, aT=aT: en.matmul(ps.ap, lhsT=aT.ap[:, i, j4 * P:(j4 + 1) * P],
                                                                                     rhs=W2.ap[:, i, half * 512:(half + 1) * 512],
                                                                                     start=(i == 0), stop=(i == KC - 1)), r=[aT, W2], w=[ps])
                        V(lambda en, ps=ps, j=j, half=half, st=st, e_=e_: en.scalar_tensor_tensor(
                            out=acc.ap[:, j, half * 512:(half + 1) * 512], in0=ps.ap, scalar=gates.ap[:, st, e_:e_ + 1],
                            in1=acc.ap[:, j, half * 512:(half + 1) * 512], op0=ALU.mult, op1=ALU.add), r=[ps, gates[st], acc[j * 2 + half]], w=[acc[j * 2 + half]])
            if nxt is not None:
                load_w2(nxt)
            if e_ == NE - 1:
                for j in range(QT // P):
                    r0 = q * QT + j * P
                    xb = x1t[j % 2]
                    DMA("sp", lambda en, xb=xb, r0=r0: en.dma_start(out=xb.ap, in_=x1_d[r0:r0 + P, :]), w=[xb])
                    V(lambda en, j=j: en.tensor_tensor(out=xo.ap, in0=acc.ap[:, j, :], in1=ga2_bc.ap, op=ALU.mult), r=[acc[slice(2 * j, 2 * j + 2)], ga2_bc], w=[xo])
                    G(lambda en, xb=xb: en.tensor_tensor(out=xo.ap, in0=xo.ap, in1=xb.ap, op=ALU.add), r=[xo, xb], w=[xo])
                    A(lambda en: en.activation(out=ejunk.ap, in_=xo.ap, func=AF.Square, accum_out=small.ap[:, 32:33]), r=[xo], w=[ejunk, small[32]])
                    A(lambda en: en.activation(out=small.ap[:, 33:34], in_=small.ap[:, 32:33], func=AF.Sqrt, scale=1.0 / D, bias=EPS), r=[small[32]], w=[small[33]])
                    V(lambda en: en.reciprocal(out=small.ap[:, 34:35], in_=small.ap[:, 33:34]), r=[small[33]], w=[small[34]])
                    V(lambda en, xb=xb: en.scalar_tensor_tensor(out=xb.ap, in0=xo.ap, scalar=small.ap[:, 34:35], in1=gfin_bc.ap, op0=ALU.mult, op1=ALU.mult),
                      r=[xo, small[34], gfin_bc], w=[xb])
                    DMA("sp", lambda en, xb=xb, r0=r0: en.dma_start(out=y_d[r0:r0 + P, :], in_=xb.ap), r=[xb])

        S.finish()
        print("ops:", {e: len(v) for e, v in S.ops.items()})
        with nc.Block() as block:
            @block.sync
            def _(e):
                S.run("sp", e)

            @block.scalar
            def _(e):
                S.run("act", e)

            @block.vector
            def _(e):
                S.run("dve", e)

            @block.gpsimd
            def _(e):
                S.run("pool", e)

            @block.tensor
            def _(e):
                S.run("pe", e)
    nc._dbg_names = DBG
    return nc


def _fm(v, n):
    return np.ascontiguousarray(np.asarray(v, np.float32).reshape(n, P).T)


def _consts():
    cst = np.zeros((P, CW), np.float32)
    cst[:, 0:128] = np.eye(P, dtype=np.float32)
    s = np.arange(P)[:, None]
    t = np.arange(P)[None, :]
    cst[:, 128:256] = ((s // 64 == t // 64) & (s <= t)).astype(np.float32)
    rm = np.ones((P, 256), np.float32)
    rm[:, ::64] = 0.0
    cst[:, 256:512] = rm
    cst[:, 512:640] = (s < t).astype(np.float32)
    cst[:, 640:704] = np.arange(NB, dtype=np.float32)[None, :] * BLK
    cst[:, 704:736] = np.arange(NE, dtype=np.float32)[None, :]
    cst[:, 736:744] = np.arange(KC, dtype=np.float32)[None, :] * P + np.arange(P, dtype=np.float32)[:, None]
    return cst


def make_in_maps(x, c, w_ada, b_ada, g_mix, w_in, conv_dw, conv_dw_bias, conv_ln_g, conv_ln_b,
                 w_conv_out, lb_param, hgrn_norm_g, w_hgrn_out, w_out, g_ffn, w_router, b_router,
                 w1, b1, w2, b2, g_final, cores=range(8)):
    f = lambda a: np.ascontiguousarray(np.asarray(a, np.float32))
    x = f(x)
    cst = _consts()
    dwT = np.ascontiguousarray(f(conv_dw)[0].T.reshape(KC, P, 31).transpose(1, 0, 2).reshape(P, KC * 31))
    b1T = np.ascontiguousarray(f(b1)[0].reshape(NE, 16, P).transpose(2, 0, 1).reshape(P, NE * 16))
    common = {
        "cst": cst,
        "w_ada": f(w_ada)[0], "b_ada": f(b_ada)[0:1], "w_in": f(w_in)[0],
        "w_conv_out": f(w_conv_out)[0], "w_hgrn_out": f(w_hgrn_out)[0], "w_out": f(w_out)[0],
        "w_router": f(w_router)[0], "b_router": f(b_router)[0:1],
        "w1": f(w1)[0].reshape(NE * D, 2 * D), "w2": f(w2)[0].reshape(NE * D, D), "b2": f(b2)[0], "g_final": f(g_final).reshape(1, D),
        "g_ffn": f(g_ffn)[0:1],
    }
    in_maps = []
    for core in cores:
        b, half = core // 2, core % 2
        pvec = np.zeros((P, RV), np.float32)
        pvec[:, C_C:C_C + 8] = _fm(np.asarray(c)[b], 8)
        pvec[:, C_BADA:C_BADA + 48] = _fm(np.asarray(b_ada)[0], 48)
        pvec[:, C_GMIX:C_GMIX + 8] = _fm(np.asarray(g_mix)[0], 8)
        pvec[:, C_DWB:C_DWB + 8] = _fm(np.asarray(conv_dw_bias)[0], 8)
        pvec[:, C_LNG:C_LNG + 8] = _fm(np.asarray(conv_ln_g)[0], 8)
        pvec[:, C_LNB:C_LNB + 8] = _fm(np.asarray(conv_ln_b)[0], 8)
        pvec[:, C_LB0:C_LB0 + 8] = _fm(np.asarray(lb_param)[0], 8)
        pvec[:, C_LB1:C_LB1 + 8] = _fm(np.asarray(lb_param)[1], 8)
        pvec[:, C_GFFN:C_GFFN + 8] = _fm(np.asarray(g_ffn)[0], 8)
        pvec[:, C_DW:C_DW + 248] = dwT
        pvec[:, C_B1:C_B1 + 512] = b1T
        pvec[:, C_NG] = np.asarray(hgrn_norm_g, np.float32)[0]
        m = dict(common)
        m["xm"] = np.ascontiguousarray(x[b, half * NTOK:(half + 1) * NTOK])
        m["xp"] = np.ascontiguousarray(x[b, 0:NTOK]) if half == 1 else np.zeros((NTOK, D), np.float32)
        m["flag"] = np.full((P, 1), float(half), np.float32)
        m["pvec"] = pvec
        in_maps.append(m)
    return in_maps


_NC = None


def kernel(**inputs):
    global _NC
    in_maps = make_in_maps(**inputs)
    if _NC is None:
        _NC = build_nc()
    res = run_bass_kernel_spmd(_NC, in_maps, core_ids=list(range(8)))
    out = np.zeros((4, 2 * NTOK, D), np.float32)
    for core in range(8):
        b, half = core // 2, core % 2
        out[b, half * NTOK:(half + 1) * NTOK] = np.asarray(res.results[core]["y"], np.float32)
    return out
```

```python
import os
import numpy as np
import concourse.bass as bass
import concourse.mybir as mybir
from concourse.bass_utils import run_bass_kernel_spmd

F32 = mybir.dt.float32
BF16 = mybir.dt.bfloat16
I32 = mybir.dt.int32
ALU = mybir.AluOpType
AF = mybir.ActivationFunctionType

P = 128
D = 1024
KC = 8
NTOK = 4096
TT = 256
NS = TT // P
NTILE = NTOK // TT
HALO = 30
UW = HALO + TT + 2
NE = 32
QT = 1024
NQ = NTOK // QT
EPS = 1e-6
BLK = 512
NB = 64
NR = NB * BLK
CW = 768
SPARSE = True
C_C, C_BADA, C_GMIX, C_DWB, C_LNG, C_LNB, C_LB0, C_LB1, C_GFFN, C_DW, C_B1, C_NG, RV = (
    0, 8, 56, 64, 72, 80, 88, 96, 104, 112, 360, 872, 876)
SAME_ENG_SYNC = True
CUT = int(os.environ.get('KCUT', '99'))
SUB = int(os.environ.get('KSUB', '99'))
EPOCH = 30000


class Op:
    __slots__ = ("eng", "fn", "deps", "marked", "sem", "val", "dma")


class Part:
    __slots__ = ("w", "r")

    def __init__(self):
        self.w = {}
        self.r = {}


class Buf:
    def __init__(self, ap, name="", nparts=1):
        self.ap = ap
        self.parts = [Part() for _ in range(nparts)]
        self.name = name

    def __getitem__(self, k):
        if len(self.parts) == 1:
            return self
        return (self, k)


def _expand(lst):
    out = {}
    for it in lst:
        if it is None:
            continue
        if isinstance(it, tuple):
            b, k = it
            if isinstance(k, int):
                ps = [b.parts[k]]
            elif isinstance(k, slice):
                ps = b.parts[k]
            else:
                ps = [b.parts[i] for i in k]
        else:
            ps = it.parts
        for p in ps:
            out[id(p)] = p
    return out


class Sched:
    ENG = ("sp", "act", "dve", "pool", "pe")

    def __init__(self, dma_sems, eng_sems):
        self.ops = {e: [] for e in self.ENG}
        self.pool = dma_sems
        self.esem = eng_sems
        self.dma_i = {q: 0 for q in dma_sems}
        self.dma_last = {}

    def op(self, eng, fn, r=(), w=(), dma=False):
        o = Op()
        o.eng, o.fn, o.dma, o.marked, o.deps, o.sem, o.val = eng, fn, dma, False, {}, None, 0
        rp = _expand(r)
        wp = _expand(w)
        for p in rp.values():
            for x in p.w.values():
                o.deps[id(x)] = x
        for p in wp.values():
            for x in p.r.values():
                o.deps[id(x)] = x
            for x in p.w.values():
                o.deps[id(x)] = x
        key = ("d", id(o)) if dma else eng
        for p in wp.values():
            p.w = {key: o}
            p.r = {}
        for k, p in rp.items():
            if k not in wp:
                p.r[key] = o
        if dma:
            pl = self.pool[eng]
            i = self.dma_i[eng] % len(pl)
            self.dma_i[eng] += 1
            prev = self.dma_last.get((eng, i))
            if prev is not None:
                o.deps[id(prev)] = prev
            o.sem = pl[i]
            o.val = (prev.val if prev is not None else 0) + 16
            self.dma_last[(eng, i)] = o
        for d in list(o.deps.values()):
            if (not d.dma) and d.eng == eng and (eng == "pe" or not SAME_ENG_SYNC):
                del o.deps[id(d)]
            else:
                d.marked = True
        self.ops[eng].append(o)
        return o

    def finish(self):
        o = Op()
        o.eng, o.fn, o.dma, o.marked, o.sem, o.val = "sp", (lambda e: e.nop()), False, False, None, 0
        o.deps = {id(x): x for x in self.dma_last.values()}
        self.ops["sp"].append(o)
        for eng in self.ENG:
            cnt = 0
            for q in self.ops[eng]:
                if (not q.dma) and q.marked:
                    q.sem = self.esem[eng][cnt // EPOCH]
                    q.val = cnt % EPOCH + 1
                    cnt += 1

    def run(self, eng, e):
        known = {}
        for o in self.ops[eng]:
            for d in o.deps.values():
                k = d.sem.num
                if known.get(k, 0) >= d.val:
                    continue
                e.wait_ge(d.sem, d.val)
                known[k] = d.val
            ins = o.fn(e)
            if o.dma:
                ins.then_inc(o.sem, 16)
            elif o.marked:
                ins.then_inc(o.sem, 1)


def build_nc(stage=99, dbg=False):
    DBG = []
    nc = bass.Bass("TRN2", target_bir_lowering=False)

    def dram(name, shape, dtype=F32, kind="ExternalInput"):
        return nc.dram_tensor(name, shape, dtype, kind=kind).ap()

    xm = dram("xm", [NTOK, D])
    xp = dram("xp", [NTOK, D])
    flag_d = dram("flag", [P, 1])
    pvec_d = dram("pvec", [P, RV])
    cst_d = dram("cst", [P, CW])
    gffn_d = dram("g_ffn", [1, D])
    w_ada = dram("w_ada", [D, 6 * D])
    b_ada = dram("b_ada", [1, 6 * D])
    w_in = dram("w_in", [D, 8 * D])
    wco_d = dram("w_conv_out", [D, D])
    who_d = dram("w_hgrn_out", [D, D])
    wout_d = dram("w_out", [D, D])
    wr_d = dram("w_router", [D, NE])
    br_d = dram("b_router", [1, NE])
    w1_2d = dram("w1", [NE * D, 2 * D])
    w2_2d = dram("w2", [NE * D, D])
    w1_d = w1_2d.rearrange("(e r) n -> e r n", e=NE)
    w2_d = w2_2d.rearrange("(e r) n -> e r n", e=NE)
    b2_d = dram("b2", [NE, D])
    gfin_d = dram("g_final", [1, D])
    y_d = dram("y", [NTOK, D], F32, "ExternalOutput")
    x1_d = dram("x1_scr", [NTOK, D], F32, "Internal")
    h2_d = dram("h2_scr", [P, KC * NTOK], BF16, "Internal")
    h2tm_d = dram("h2tm_scr", [NTOK, D], BF16, "Internal")
    dg_d = dram("dg_scr", [KC, P, 31, P], BF16, "Internal")
    wbf_d = dram("wbf_scr", [22, P, KC * 512], BF16, "Internal")
    xs_d = dram("xs_scr", [NR, D], BF16, "Internal")
    ys_d = dram("ys_scr", [NR, D], F32, "Internal")

    import contextlib
    es = contextlib.ExitStack()
    with es:
        AW = 52500
        arena = es.enter_context(nc.sbuf_tensor("arena", [P, AW], F32))
        psf = [es.enter_context(nc.psum_tensor("psf%d" % i, [P, 512], F32)) for i in range(6)]
        pst = [es.enter_context(nc.psum_tensor("pst%d" % i, [P, 1024], BF16)) for i in range(2)]
        dsems = {"sp": [es.enter_context(nc.semaphore("dmah%d" % i)) for i in range(20)],
                 "pool": [es.enter_context(nc.semaphore("dmas%d" % i)) for i in range(20)]}
        esems = {e: [es.enter_context(nc.semaphore("e_%s%d" % (e, i))) for i in range(3)]
                 for e in Sched.ENG}
        S = Sched(dsems, esems)
        PS = [Buf(t[:, :], "psf%d" % i, 1) for i, t in enumerate(psf)]
        PT = [Buf(t[:, :], "pst%d" % i, 1) for i, t in enumerate(pst)]
        psr = [0]

        def nps():
            b = PS[psr[0] % 4]
            psr[0] += 1
            return b
        PSS = PS[4]
        PSM = PS[5]

        class Carver:
            def __init__(self, start):
                self.off = start

            def get(self, dtype, free_shape, name="", nparts=1):
                n = int(np.prod(free_shape))
                nbytes = n * (2 if dtype == BF16 else 4)
                nbytes = (nbytes + 63) // 64 * 64
                w0 = self.off // 4
                w1 = (self.off + nbytes) // 4
                assert w1 <= AW, ("arena overflow", name, self.off + nbytes)
                ap = arena[:, w0:w1]
                if dtype != F32:
                    ap = ap.bitcast(dtype)
                ap = ap[:, 0:n]
                if len(free_shape) == 2:
                    ap = ap.rearrange("p (a b) -> p a b", a=free_shape[0])
                elif len(free_shape) == 3:
                    ap = ap.rearrange("p (a b c) -> p a b c", a=free_shape[0], b=free_shape[1])
                self.off += nbytes
                return Buf(ap, name, nparts)

        cv = Carver(0)
        pv = cv.get(F32, [RV], "pv")
        cst = cv.get(F32, [CW], "cst")
        flag = cv.get(F32, [1], "flag")
        identb = cv.get(BF16, [P], "identb")
        onesb = cv.get(BF16, [P], "onesb")
        onesf = cv.get(F32, [P], "onesf")
        mask01 = cv.get(BF16, [P], "mask01")
        modT = cv.get(F32, [48], "modT")
        gsc1 = cv.get(F32, [8], "gsc1")
        gsc2 = cv.get(F32, [8], "gsc2")
        lb = cv.get(F32, [8], "lb")
        oml = cv.get(F32, [8], "oml")
        sc = cv.get(F32, [8], "sc")
        ga1_bc = cv.get(F32, [D], "ga1_bc")
        ga2_bc = cv.get(F32, [D], "ga2_bc")
        gfin_bc = cv.get(F32, [D], "gfin_bc")
        br_bc = cv.get(F32, [NE], "br_bc")
        gates = cv.get(F32, [NTOK // P, NE], "gates", NTOK // P)
        wr = cv.get(BF16, [KC, NE], "wr")
        b2b = cv.get(BF16, [D], "b2b")
        small = cv.get(F32, [64], "small", 64)
        gsc2_bc = cv.get(F32, [D], "gsc2_bc")
        sh2_bc = cv.get(F32, [D], "sh2_bc")
        lgts = cv.get(F32, [NTOK // P, NE], "lgts", NTOK // P)
        mx4 = cv.get(F32, [NTOK // P, 4], "mx4", NTOK // P)
        gk = cv.get(F32, [NTOK // P, 4], "gk", NTOK // P)
        desti = cv.get(I32, [NTOK // P, 4], "desti")
        widx = cv.get(I32, [NB, KC], "widx")
        bef = cv.get(F32, [NB], "bef")
        shared_end = cv.off
        ident_f = cst.ap[:, 0:128]
        maskf = cst.ap[:, 128:256]
        resetm = cst.ap[:, 256:512]
        ustrict = cst.ap[:, 512:640]
        iotab = cst.ap[:, 640:704]
        iotae = cst.ap[:, 704:736]
        basekp = cst.ap[:, 736:744]

        mv = Carver(shared_end)
        xt = mv.get(F32, [NS, D], "xt", NS)
        junk = mv.get(BF16, [D], "junk")
        xn = mv.get(BF16, [NS, D], "xn", NS)
        hT = mv.get(BF16, [KC, TT], "hT", 8)
        setup_off = mv.off
        wslot = [mv.get(BF16, [KC, 512], "wslot%d" % i) for i in range(3)]
        uX = [mv.get(BF16, [KC, UW], "uX%d" % i, 8) for i in range(2)]
        FA = mv.get(F32, [KC, TT], "FA", 8)
        FB = mv.get(F32, [KC, TT], "FB", 8)
        FC = mv.get(F32, [KC, TT], "FC", 8)
        sgb = mv.get(BF16, [KC, TT], "sgb", 8)
        m1 = mv.get(BF16, [KC, TT], "m1", 8)
        QE = mv.get(BF16, [KC, TT], "QE", 8)
        QO = mv.get(BF16, [KC, TT], "QO", 8)
        kT = mv.get(BF16, [KC, TT], "kT", 8)
        sog = mv.get(BF16, [KC, TT], "sog", 8)
        sgc = mv.get(BF16, [KC, TT], "sgc", 8)
        sgh = mv.get(BF16, [KC, TT], "sgh", 8)
        osq = mv.get(BF16, [KC, TT], "osq", 8)
        mT = mv.get(BF16, [KC, TT], "mT", 8)
        h2t = mv.get(BF16, [KC, TT], "h2t", 8)
        vtm = mv.get(BF16, [NS, D], "vtm", NS)
        KE = mv.get(BF16, [NS, KC, P], "KE", NS)
        KO = mv.get(BF16, [NS, KC, P], "KO", NS)
        AT = [mv.get(BF16, [4, P], "AT%d" % i, 4) for i in range(2)]
        SA = mv.get(BF16, [KC, P], "SA", 8)
        SB = mv.get(BF16, [KC, P], "SB", 8)
        tmpa = mv.get(F32, [512], "tmpa")
        st_mean = mv.get(F32, [TT], "st_mean")
        st_var = mv.get(F32, [TT], "st_var")
        st_t = mv.get(F32, [TT], "st_t")
        lgt = mv.get(F32, [NE], "lgt")
        dgs = [mv.get(BF16, [31, P], "dgs%d" % i) for i in range(2)]
        zt1 = mv.get(BF16, [D], "zt1")
        mx8 = mv.get(F32, [8], "mx8")
        egt = mv.get(F32, [NE], "egt")
        mixer_end = mv.off
        sv = Carver(setup_off)
        scb = sv.get(F32, [KC, P], "scb")
        wada = [sv.get(F32, [KC, 512], "wada%d" % i) for i in range(2)]
        bada_bc = sv.get(F32, [D], "bada_bc")
        dgtmp = sv.get(BF16, [31, P], "dgtmp")
        DgB = Buf(None, "dg_dram")

        ev = Carver(shared_end)
        h2q = ev.get(BF16, [KC, QT], "h2q")
        acc = ev.get(F32, [QT // P, D], "acc", 2 * QT // P)
        W1 = [ev.get(BF16, [KC, 512], "W1_%d" % i) for i in range(4)]
        W2 = ev.get(BF16, [KC, D], "W2")
        actT = [ev.get(BF16, [KC, 512], "actT%d" % i, 8) for i in range(2)]
        tg = [ev.get(F32, [512], "tg%d" % i) for i in range(2)]
        tsg = [ev.get(F32, [512], "tsg%d" % i) for i in range(2)]
        tl = [ev.get(F32, [512], "tl%d" % i) for i in range(2)]
        x1t = [ev.get(F32, [D], "x1t%d" % i) for i in range(2)]
        xo = ev.get(F32, [D], "xo")
        ejunk = ev.get(BF16, [D], "ejunk")
        gTb = ev.get(BF16, [P], "gTb")
        moe_end = ev.off
        print("arena bytes: shared", shared_end, "mixer", mixer_end, "moe", moe_end)

        def V(fn, r=(), w=()):
            return S.op("dve", fn, r, w)

        def A(fn, r=(), w=()):
            return S.op("act", fn, r, w)

        def G(fn, r=(), w=()):
            return S.op("pool", fn, r, w)

        def T(fn, r=(), w=()):
            return S.op("pe", fn, r, w)

        def DMA(q, fn, r=(), w=()):
            return S.op(q, fn, r, w, dma=True)

        def dump(name, buf, ap=None):
            if not dbg:
                return
            ap = buf.ap if ap is None else ap
            shp = list(ap.shape)
            dtn = nc.dram_tensor("dbg_" + name, shp, ap.dtype, kind="ExternalOutput").ap()
            DMA("sp", lambda e: e.dma_start(out=dtn, in_=ap), r=[buf])
            DBG.append("dbg_" + name)

        def barrier(bufs):
            last = {e: S.ops[e][-1] for e in ("act", "dve", "pool", "pe") if S.ops[e]}
            bb = Buf(None, "barrier")
            for e, o in last.items():
                bb.parts[0].w[e] = o
            for x in S.dma_last.values():
                bb.parts[0].w[("d", id(x))] = x
            for e in ("act", "dve", "pool", "pe", "sp"):
                S.op(e, (lambda en: en.nop()), r=[bb])

        XsB = Buf(None, "xs_dram", 4 * (NTOK // P) + 1)
        V(lambda e: e.memset(zt1.ap, 0.0), w=[zt1])
        xs_fill = xs_d.rearrange("(c p q) d -> c p q d", p=P, q=16)
        for ci in range(NR // (P * 16)):
            DMA("sp", lambda e, ci=ci: e.dma_start(out=xs_fill[ci], in_=zt1.ap.unsqueeze(1).broadcast_to([P, 16, D])), r=[zt1], w=[XsB])
        DMA("sp", lambda e: e.dma_start(out=pv.ap, in_=pvec_d), w=[pv])
        DMA("sp", lambda e: e.dma_start(out=cst.ap, in_=cst_d), w=[cst])
        DMA("sp", lambda e: e.dma_start(out=flag.ap, in_=flag_d), w=[flag])
        DMA("sp", lambda e: e.dma_start(out=gfin_bc.ap, in_=gfin_d.broadcast_to([P, D])), w=[gfin_bc])
        DMA("sp", lambda e: e.dma_start(out=br_bc.ap, in_=br_d.broadcast_to([P, NE])), w=[br_bc])
        DMA("pool", lambda e: e.dma_start(out=wr.ap, in_=wr_d.rearrange("(k p) n -> p k n", p=P)), w=[wr])
        DMA("pool", lambda e: e.dma_start(out=b2b.ap[0:NE, :], in_=b2_d), w=[b2b])
        V(lambda e: e.tensor_copy(out=identb.ap, in_=ident_f), r=[cst], w=[identb])
        V(lambda e: e.tensor_copy(out=mask01.ap, in_=maskf), r=[cst], w=[mask01])
        V(lambda e: e.memset(onesb.ap, 1.0), w=[onesb])
        V(lambda e: e.memset(onesf.ap, 1.0), w=[onesf])
        V(lambda e: e.memset(gates.ap, 0.0), w=[gates])
        V(lambda e: e.memset(lgts.ap, 0.0), w=[lgts])
        V(lambda e: e.memset(mx4.ap, 0.0), w=[mx4])
        V(lambda e: e.memset(gk.ap, 0.0), w=[gk])
        A(lambda e: e.activation(out=sc.ap, in_=pv.ap[:, C_C:C_C + 8], func=AF.Silu), r=[pv], w=[sc])
        V(lambda e: e.tensor_copy(out=scb.ap, in_=sc.ap.unsqueeze(2).broadcast_to([P, KC, P])), r=[sc], w=[scb])
        V(lambda e: e.tensor_tensor(out=small.ap[:, 0:8], in0=pv.ap[:, C_LB0:C_LB0 + 8],
                                    in1=pv.ap[:, C_LB1:C_LB1 + 8], op=ALU.subtract), r=[pv], w=[small[slice(0, 8)]])
        A(lambda e: e.activation(out=lb.ap, in_=small.ap[:, 0:8], func=AF.Sigmoid), r=[small[slice(0, 8)]], w=[lb])
        V(lambda e: e.tensor_scalar(out=oml.ap, in0=lb.ap, scalar1=-1.0, scalar2=1.0, op0=ALU.mult, op1=ALU.add),
          r=[lb], w=[oml])
        wada_v = w_ada.rearrange("(k p) n -> p k n", p=P)
        for g in range(12):
            wb = wada[g % 2]
            DMA("sp", lambda e, g=g, wb=wb: e.dma_start(out=wb.ap, in_=wada_v[:, :, g * 512:(g + 1) * 512]), w=[wb])
            for cc in range(4):
                j = g * 4 + cc
                for k in range(KC):
                    T(lambda e, j=j, k=k, cc=cc, wb=wb: e.matmul(PSM.ap[:, j:j + 1], lhsT=wb.ap[:, k, cc * 128:(cc + 1) * 128],
                                                             rhs=sc.ap[:, k:k + 1], start=(k == 0), stop=(k == KC - 1)),
                      r=[wb, sc], w=[PSM])
            if g in (4, 5, 6, 7, 8, 9, 10, 11):
                ps = nps()
                dst = {2: ga1_bc, 3: sh2_bc, 4: gsc2_bc, 5: ga2_bc}[g // 2]
                hh = g % 2
                for k in range(KC):
                    T(lambda e, k=k, wb=wb, ps=ps: e.matmul(ps.ap, lhsT=scb.ap[:, k, :], rhs=wb.ap[:, k, :],
                                                          start=(k == 0), stop=(k == KC - 1)), r=[wb, scb], w=[ps])
                DMA("sp", lambda e, g=g: e.dma_start(out=bada_bc.ap[:, 0:512],
                                                    in_=b_ada[:, g * 512:(g + 1) * 512].broadcast_to([P, 512])), w=[bada_bc])
                V(lambda e, ps=ps, dst=dst, hh=hh: e.tensor_tensor(out=dst.ap[:, hh * 512:(hh + 1) * 512], in0=ps.ap,
                                                                 in1=bada_bc.ap[:, 0:512], op=ALU.add),
                  r=[ps, bada_bc], w=[dst])
        V(lambda e: e.tensor_tensor(out=modT.ap, in0=PSM.ap[:, 0:48], in1=pv.ap[:, C_BADA:C_BADA + 48], op=ALU.add),
          r=[PSM, pv], w=[modT])
        V(lambda e: e.scalar_tensor_tensor(out=gsc1.ap, in0=modT.ap[:, 8:16], scalar=1.0, in1=pv.ap[:, C_GMIX:C_GMIX + 8],
                                           op0=ALU.add, op1=ALU.mult), r=[modT, pv], w=[gsc1])
        DMA("sp", lambda e: e.dma_start(out=bada_bc.ap, in_=gffn_d.broadcast_to([P, D])), w=[bada_bc])
        V(lambda e: e.scalar_tensor_tensor(out=gsc2_bc.ap, in0=gsc2_bc.ap, scalar=1.0, in1=bada_bc.ap, op0=ALU.add, op1=ALU.mult),
          r=[gsc2_bc, bada_bc], w=[gsc2_bc])
        V(lambda e: e.scalar_tensor_tensor(out=gsc2.ap, in0=modT.ap[:, 32:40], scalar=1.0, in1=pv.ap[:, C_GFFN:C_GFFN + 8],
                                           op0=ALU.add, op1=ALU.mult), r=[modT, pv], w=[gsc2])

        for c in range(KC):
            V(lambda e, c=c: e.tensor_tensor(out=dgtmp.ap, in0=identb.ap.unsqueeze(1).broadcast_to([P, 31, P]),
                                             in1=pv.ap[:, C_DW + c * 31:C_DW + (c + 1) * 31].unsqueeze(2).broadcast_to([P, 31, P]), op=ALU.mult),
              r=[identb, pv], w=[dgtmp])
            DMA("sp", lambda e, c=c: e.dma_start(out=dg_d[c], in_=dgtmp.ap), r=[dgtmp], w=[DgB])
        dump("modT", modT)
        dump("ga1", ga1_bc)
        dump("ga2", ga2_bc)
        dump("lb", lb)
        dump("gsc1", gsc1)
        barrier(None)
        V(lambda e: e.memset(QE.ap, 0.0), w=[QE])
        V(lambda e: e.memset(QO.ap, 0.0), w=[QO])
        V(lambda e: e.memset(KE.ap, 0.0), w=[KE])
        V(lambda e: e.memset(KO.ap, 0.0), w=[KO])
        V(lambda e: e.memset(SA.ap, 0.0), w=[SA])
        V(lambda e: e.memset(uX[0].ap, 0.0), w=[uX[0]])
        V(lambda e: e.memset(uX[1].ap, 0.0), w=[uX[1]])
        win_v = w_in.rearrange("(k p) n -> p k n", p=P)
        wco_v = wco_d.rearrange("(k p) n -> p k n", p=P)
        who_v = who_d.rearrange("(k p) n -> p k n", p=P)
        wout_v = wout_d.rearrange("(k p) n -> p k n", p=P)
        wq = {"n": 0, "pending": []}

        WbB = Buf(None, "wbf_dram", 22)

        def wsrc32(kind, g):
            if kind == "in":
                return win_v[:, :, g * 512:(g + 1) * 512]
            v = {"co": wco_v, "ho": who_v, "out": wout_v}[kind]
            return v[:, :, g * 512:(g + 1) * 512]

        def wsrc(kind, g):
            return {"in": 0, "co": 16, "ho": 18, "out": 20}[kind] + g

        gl_all = [("in", g) for g in (6, 7, 8, 9, 2, 3, 0, 1, 4, 5, 10, 11, 12, 13, 14, 15)] + \
                 [("co", 0), ("co", 1), ("ho", 0), ("ho", 1), ("out", 0), ("out", 1)]
        for n_, (k_, g_) in enumerate(gl_all):
            sl_ = wslot[n_ % 3]
            gid_ = wsrc(k_, g_)
            DMA("pool", lambda e, sl_=sl_, k_=k_, g_=g_: e.dma_start(out=sl_.ap, in_=wsrc32(k_, g_)), w=[sl_])
            DMA("sp", lambda e, sl_=sl_, gid_=gid_: e.dma_start(out=wbf_d[gid_].rearrange("p (k n) -> p k n", k=KC), in_=sl_.ap),
                r=[sl_], w=[WbB[gid_]])

        def wissue(src):
            slot = wslot[wq["n"] % 3]
            wq["n"] += 1
            if os.environ.get("KWMIX") == "none" and wq["n"] > 3:
                pass
            else:
                DMA("pool", lambda e, slot=slot, src=src: e.dma_start(out=slot.ap, in_=wbf_d[src].rearrange("p (k n) -> p k n", k=KC)),
                    r=[WbB[src]], w=[slot])
            wq["pending"].append(slot)

        def wnext():
            return wq["pending"].pop(0)

        def tile_groups(prefix, lastp):
            gl = []
            if (not prefix) or lastp:
                gl += [("in", 2), ("in", 3), ("in", 0), ("in", 1)]
            if CUT <= 3:
                return gl
            gl += [("in", 6), ("in", 7)]
            if CUT <= 4:
                return gl
            if not prefix:
                gl += [("in", 4), ("in", 5)]
            gl += [("in", 8), ("in", 9)]
            if not prefix:
                gl += [("in", 10), ("in", 11), ("in", 12), ("in", 13), ("in", 14), ("in", 15),
                       ("co", 0), ("co", 1)]
                if CUT > 6:
                    gl += [("ho", 0), ("ho", 1), ("out", 0), ("out", 1)]
            return gl

        tiles = [(True, t == NTILE - 1, t) for t in range(NTILE)] + [(False, False, t) for t in range(NTILE)]
        allg = []
        for (pf, lp, t) in tiles:
            allg += [wsrc(k, g) for (k, g) in tile_groups(pf, lp)]
        gi = {"i": 0}

        def wget():
            while gi["i"] < len(allg) and len(wq["pending"]) < 3:
                wissue(allg[gi["i"]])
                gi["i"] += 1
            return wnext()

        def fm_group(ws, cbase, evac):
            for cc in range(4):
                ps = nps()
                for k in range(KC):
                    T(lambda e, ps=ps, k=k, cc=cc: e.matmul(ps.ap[:, 0:TT], lhsT=ws.ap[:, k, cc * 128:(cc + 1) * 128],
                                                          rhs=hT.ap[:, k, :], start=(k == 0), stop=(k == KC - 1)),
                      r=[ws, hT], w=[ps])
                evac(ps, cbase + cc)

        def rms_to_T(src, dstT, scale_ap, bias_ap, scale_b, bias_b):
            for j in range(NS):
                A(lambda e, j=j: e.activation(out=junk.ap, in_=src.ap[:, j, :], func=AF.Square,
                                              accum_out=small.ap[:, 8 + j:9 + j]), r=[src[j]], w=[junk, small[8 + j]])
            A(lambda e: e.activation(out=small.ap[:, 12:12 + NS], in_=small.ap[:, 8:8 + NS], func=AF.Sqrt,
                                     scale=1.0 / D, bias=EPS), r=[small[slice(8, 8 + NS)]], w=[small[slice(12, 12 + NS)]])
            V(lambda e: e.reciprocal(out=small.ap[:, 16:16 + NS], in_=small.ap[:, 12:12 + NS]), r=[small[slice(12, 12 + NS)]], w=[small[slice(16, 16 + NS)]])
            for j in range(NS):
                V(lambda e, j=j: e.tensor_scalar(out=xn.ap[:, j, :], in0=src.ap[:, j, :], scalar1=small.ap[:, 16 + j:17 + j],
                                                 scalar2=None, op0=ALU.mult), r=[src[j], small[16 + j]], w=[xn[j]])
            for k in range(KC):
                pt = PT[k % 2]
                for j in range(NS):
                    T(lambda e, pt=pt, j=j, k=k: e.transpose(pt.ap[:, j * P:(j + 1) * P], xn.ap[:, j, k * P:(k + 1) * P],
                                                            identb.ap), r=[xn[j], identb], w=[pt[j]])
                A(lambda e, pt=pt, k=k: e.activation(out=dstT.ap[:, k, :], in_=pt.ap[:, 0:TT], func=AF.Identity,
                                                     scale=scale_ap[:, k:k + 1], bias=bias_ap[:, k:k + 1]),
                  r=[pt[slice(0, NS)], scale_b, bias_b], w=[dstT[k]])

        def mixer_tile(prefix, lastp, t):
            src = xp if prefix else xm
            tok0 = t * TT
            ucur = uX[t % 2]
            uprev = uX[(t + 1) % 2]
            do_conv_in = (not prefix) or lastp
            DMA("sp", lambda e: e.dma_start(out=xt.ap, in_=src[tok0:tok0 + TT, :].rearrange("(j p) d -> p j d", p=P)), w=[xt])
            rms_to_T(xt, hT, gsc1.ap, modT.ap[:, 0:8], gsc1, modT)
            if CUT <= 1:
                return
            dm = dbg and (not prefix) and t == 0
            if dm:
                dump("hT", hT)
            if do_conv_in:
                V(lambda e: e.tensor_copy(out=ucur.ap[:, :, 0:HALO], in_=uprev.ap[:, :, TT:TT + HALO]), r=[uprev], w=[ucur])
                for half in range(2):
                    ws = wget()
                    fm_group(ws, half * 4, lambda ps, c: A(
                        lambda e, ps=ps, c=c: e.activation(out=sgb.ap[:, c, :], in_=ps.ap[:, 0:TT], func=AF.Sigmoid),
                        r=[ps], w=[sgb[c]]))
                if dm:
                    dump("sgb0", sgb)
                for half in range(2):
                    ws = wget()
                    if dm:
                        dump("ws%d" % half, ws)
                    fm_group(ws, half * 4, lambda ps, c: V(
                        lambda e, ps=ps, c=c: e.tensor_tensor(out=ucur.ap[:, c, HALO:HALO + TT], in0=ps.ap[:, 0:TT],
                                                              in1=sgb.ap[:, c, :], op=ALU.mult), r=[ps, sgb[c]], w=[ucur[c]]))
                if prefix:
                    V(lambda e: e.tensor_scalar(out=ucur.ap[:, :, TT:TT + HALO], in0=ucur.ap[:, :, TT:TT + HALO],
                                                scalar1=flag.ap[:, 0:1], scalar2=None, op0=ALU.mult), r=[ucur, flag], w=[ucur])
            if CUT <= 2:
                return
            if not prefix:
                if os.environ.get("KCONV") == "dve":
                    for c in range(KC):
                        V(lambda e, c=c: e.tensor_scalar(out=FA.ap[:, c, :], in0=ucur.ap[:, c, 0:TT],
                                                         scalar1=pv.ap[:, C_DW + c * 31:C_DW + c * 31 + 1],
                                                         scalar2=pv.ap[:, C_DWB + c:C_DWB + c + 1], op0=ALU.mult, op1=ALU.add),
                          r=[ucur[c], pv], w=[FA[c]])
                    for j in range(1, 31):
                        for c in range(KC):
                            V(lambda e, c=c, j=j: e.scalar_tensor_tensor(out=FA.ap[:, c, :], in0=ucur.ap[:, c, j:j + TT],
                                                                         scalar=pv.ap[:, C_DW + c * 31 + j:C_DW + c * 31 + j + 1],
                                                                         in1=FA.ap[:, c, :], op0=ALU.mult, op1=ALU.add),
                              r=[ucur[c], pv, FA[c]], w=[FA[c]])
                else:
                    for c in range(KC):
                        dg = dgs[c % 2]
                        DMA("sp", lambda e, c=c, dg=dg: e.dma_start(out=dg.ap, in_=dg_d[c]), r=[DgB], w=[dg])
                        ps = nps()
                        for j in range(31):
                            T(lambda e, c=c, j=j, dg=dg, ps=ps: e.matmul(ps.ap[:, 0:TT], lhsT=dg.ap[:, j, :], rhs=ucur.ap[:, c, j:j + TT],
                                                                       start=(j == 0), stop=(j == 30)), r=[dg, ucur[c]], w=[ps])
                        V(lambda e, c=c, ps=ps: e.tensor_scalar(out=FA.ap[:, c, :], in0=ps.ap[:, 0:TT], scalar1=pv.ap[:, C_DWB + c:C_DWB + c + 1],
                                                                scalar2=None, op0=ALU.add), r=[ps, pv], w=[FA[c]])
                A(lambda e: e.activation(out=osq.ap, in_=FA.ap, func=AF.Identity), r=[FA], w=[osq])
                A(lambda e: e.activation(out=mT.ap, in_=FA.ap, func=AF.Square), r=[FA], w=[mT])
                for k in range(KC):
                    T(lambda e, k=k: e.matmul(PSS.ap[:, 0:TT], lhsT=onesb.ap, rhs=osq.ap[:, k, :], start=(k == 0), stop=(k == KC - 1)),
                      r=[onesb, osq[k]], w=[PSS[slice(0, 2)]])
                for k in range(KC):
                    T(lambda e, k=k: e.matmul(PSS.ap[:, TT:2 * TT], lhsT=onesb.ap, rhs=mT.ap[:, k, :], start=(k == 0), stop=(k == KC - 1)),
                      r=[onesb, mT[k]], w=[PSS[slice(2, 4)]])
                V(lambda e: e.tensor_scalar(out=st_mean.ap, in0=PSS.ap[:, 0:TT], scalar1=1.0 / D, scalar2=None, op0=ALU.mult),
                  r=[PSS], w=[st_mean])
                V(lambda e: e.tensor_tensor(out=st_t.ap, in0=st_mean.ap, in1=st_mean.ap, op=ALU.mult), r=[st_mean], w=[st_t])
                V(lambda e: e.scalar_tensor_tensor(out=st_var.ap, in0=PSS.ap[:, TT:2 * TT], scalar=1.0 / D, in1=st_t.ap,
                                                   op0=ALU.mult, op1=ALU.subtract), r=[PSS, st_t], w=[st_var])
                A(lambda e: e.activation(out=st_var.ap, in_=st_var.ap, func=AF.Sqrt, bias=EPS), r=[st_var], w=[st_var])
                V(lambda e: e.reciprocal(out=st_var.ap, in_=st_var.ap), r=[st_var], w=[st_var])
                V(lambda e: e.tensor_tensor(out=FA.ap, in0=FA.ap, in1=st_mean.ap.unsqueeze(1).broadcast_to([P, KC, TT]),
                                            op=ALU.subtract), r=[FA, st_mean], w=[FA])
                V(lambda e: e.tensor_tensor(out=FA.ap, in0=FA.ap, in1=st_var.ap.unsqueeze(1).broadcast_to([P, KC, TT]),
                                            op=ALU.mult), r=[FA, st_var], w=[FA])
                for c in range(KC):
                    A(lambda e, c=c: e.activation(out=sgb.ap[:, c, :], in_=FA.ap[:, c, :], func=AF.Silu,
                                                  scale=pv.ap[:, C_LNG + c:C_LNG + c + 1], bias=pv.ap[:, C_LNB + c:C_LNB + c + 1]),
                      r=[FA[c], pv], w=[sgb[c]])
            if dm:
                dump("ucur", ucur)
                dump("u2T", sgb)
            if CUT <= 3:
                return
            for half in range(2):
                ws = wget()
                fm_group(ws, half * 4, lambda ps, c: A(
                    lambda e, ps=ps, c=c: e.activation(out=FA.ap[:, c, :], in_=ps.ap[:, 0:TT], func=AF.Sigmoid), r=[ps], w=[FA[c]]))
            for c in range(KC):
                V(lambda e, c=c: e.tensor_scalar(out=FA.ap[:, c, :], in0=FA.ap[:, c, :], scalar1=oml.ap[:, c:c + 1],
                                                 scalar2=lb.ap[:, c:c + 1], op0=ALU.mult, op1=ALU.add), r=[FA[c], oml, lb], w=[FA[c]])
            A(lambda e: e.activation(out=FB.ap, in_=FA.ap, func=AF.Ln), r=[FA], w=[FB])
            for c in range(KC):
                V(lambda e, c=c: e.tensor_tensor_scan(out=FC.ap[:, c, :], data0=resetm, data1=FB.ap[:, c, :], initial=0.0,
                                                      op0=ALU.mult, op1=ALU.add), r=[FB[c], cst], w=[FC[c]])
            A(lambda e: e.activation(out=FB.ap, in_=FC.ap, func=AF.Exp), r=[FC], w=[FB])
            A(lambda e: e.activation(out=FC.ap, in_=FC.ap, func=AF.Exp, scale=-1.0), r=[FC], w=[FC])
            V(lambda e: e.scalar_tensor_tensor(out=kT.ap, in0=FA.ap, scalar=1.0, in1=FC.ap, op0=ALU.subtract, op1=ALU.mult),
              r=[FA, FC], w=[kT])
            if CUT <= 4:
                return
            if not prefix:
                for half in range(2):
                    ws = wget()

                    def evq(ps, c):
                        for par, dst in ((0, QE), (1, QO)):
                            V(lambda e, ps=ps, c=c, par=par, dst=dst: e.scalar_tensor_tensor(
                                out=dst.ap[:, c, :].rearrange("p (b two s) -> p b two s", two=2, s=64)[:, :, par, :],
                                in0=ps.ap[:, 0:TT].rearrange("p (b two s) -> p b two s", two=2, s=64)[:, :, par, :],
                                scalar=-(128.0 ** -0.5),
                                in1=FB.ap[:, c, :].rearrange("p (b two s) -> p b two s", two=2, s=64)[:, :, par, :],
                                op0=ALU.mult, op1=ALU.mult), r=[ps, FB[c]], w=[dst[c]])
                    fm_group(ws, half * 4, evq)
            for half in range(2):
                ws = wget()
                for j in range(NS):
                    ps = nps()
                    for k in range(KC):
                        T(lambda e, ps=ps, k=k, j=j, ws=ws: e.matmul(ps.ap, lhsT=hT.ap[:, k, j * P:(j + 1) * P], rhs=ws.ap[:, k, :],
                                                                   start=(k == 0), stop=(k == KC - 1)), r=[hT, ws], w=[ps])
                    A(lambda e, ps=ps, j=j, half=half: e.activation(out=vtm.ap[:, j, half * 512:(half + 1) * 512], in_=ps.ap,
                                                                    func=AF.Identity), r=[ps], w=[vtm[j]])
            if not prefix:
                ws = wget()
                fm_group(ws, 0, lambda ps, c: A(lambda e, ps=ps, c=c: e.activation(out=sog.ap[:, c, :], in_=ps.ap[:, 0:TT], func=AF.Silu), r=[ps], w=[sog[c]]))
                ws = wget()
                fm_group(ws, 4, lambda ps, c: A(lambda e, ps=ps, c=c: e.activation(out=sog.ap[:, c, :], in_=ps.ap[:, 0:TT], func=AF.Silu), r=[ps], w=[sog[c]]))
                for dst in (sgc, sgh):
                    for half in range(2):
                        ws = wget()
                        fm_group(ws, half * 4, lambda ps, c, dst=dst: A(
                            lambda e, ps=ps, c=c, dst=dst: e.activation(out=dst.ap[:, c, :], in_=ps.ap[:, 0:TT], func=AF.Sigmoid),
                            r=[ps], w=[dst[c]]))
                for half in range(2):
                    ws = wget()
                    for cc in range(4):
                        c = half * 4 + cc
                        ps = nps()
                        for k in range(KC):
                            T(lambda e, ps=ps, k=k, cc=cc, ws=ws: e.matmul(ps.ap[:, 0:TT], lhsT=ws.ap[:, k, cc * P:(cc + 1) * P],
                                                                         rhs=sgb.ap[:, k, :], start=(k == 0), stop=(k == KC - 1)),
                              r=[ws, sgb], w=[ps])
                        V(lambda e, ps=ps, c=c: e.tensor_tensor(out=m1.ap[:, c, :], in0=ps.ap[:, 0:TT], in1=sgc.ap[:, c, :], op=ALU.mult),
                          r=[ps, sgc[c]], w=[m1[c]])
            if dm:
                dump("f", FA)
                dump("eG", FB)
                dump("kT", kT)
                dump("QE", QE)
                dump("QO", QO)
                dump("vtm", vtm)
                dump("m1", m1)
                dump("sog", sog)
            if CUT <= 5:
                return
            for b in range(NS):
                blk = slice(b * P, (b + 1) * P)
                pt = PT[b % 2]
                for h in range(KC):
                    T(lambda e, pt=pt, h=h, blk=blk: e.transpose(pt.ap[:, h * P:(h + 1) * P], kT.ap[:, h, blk], identb.ap),
                      r=[kT[h], identb], w=[pt[h]])
                A(lambda e, pt=pt, b=b: e.activation(out=KE.ap[0:64, b, :, :], in_=pt.ap[0:64, :].rearrange("p (h k) -> p h k", h=KC),
                                                     func=AF.Identity), r=[pt], w=[KE[b]])
                V(lambda e, pt=pt, b=b: e.tensor_copy(out=KO.ap[64:128, b, :, :], in_=pt.ap[64:128, :].rearrange("p (h k) -> p h k", h=KC)),
                  r=[pt], w=[KO[b]])
                if not prefix:
                    for hq in range(2):
                        psa = nps()
                        at = AT[hq]
                        for hh in range(4):
                            h = hq * 4 + hh
                            T(lambda e, psa=psa, h=h, hh=hh, blk=blk: e.matmul(psa.ap[:, hh * P:(hh + 1) * P], lhsT=kT.ap[:, h, blk],
                                                                             rhs=QE.ap[:, h, blk], start=True, stop=False),
                              r=[kT[h], QE[h]], w=[psa[hh]])
                            T(lambda e, psa=psa, h=h, hh=hh, blk=blk: e.matmul(psa.ap[:, hh * P:(hh + 1) * P], lhsT=kT.ap[:, h, blk],
                                                                             rhs=QO.ap[:, h, blk], start=False, stop=True),
                              r=[kT[h], QO[h]], w=[psa[hh]])
                        V(lambda e, psa=psa, at=at: e.tensor_tensor(out=at.ap, in0=psa.ap.rearrange("p (h t) -> p h t", h=4),
                                                                    in1=mask01.ap.unsqueeze(1).broadcast_to([P, 4, P]), op=ALU.mult),
                          r=[psa, mask01], w=[at])
                for (Sin, Sout, Kx, par) in ((SA, SB, KE, 0), (SB, SA, KO, 1)):
                    for hq in range(2):
                        PSx = PSS if hq == 0 else PSM
                        at = AT[hq]
                        for hh in range(4):
                            h = hq * 4 + hh
                            T(lambda e, h=h, hh=hh, Sin=Sin, PSx=PSx: e.matmul(PSx.ap[:, hh * P:(hh + 1) * P], lhsT=identb.ap, rhs=Sin.ap[:, h, :],
                                                                               start=True, stop=False), r=[identb, Sin[h]], w=[PSx])
                            T(lambda e, h=h, hh=hh, Kx=Kx, b=b, PSx=PSx: e.matmul(PSx.ap[:, hh * P:(hh + 1) * P], lhsT=Kx.ap[:, b, h, :],
                                                                                  rhs=vtm.ap[:, b, h * P:(h + 1) * P], start=False, stop=True),
                              r=[Kx[b], vtm[b]], w=[PSx])
                        if par == 1 and (not prefix):
                            pso = nps()
                            for hh in range(4):
                                h = hq * 4 + hh
                                T(lambda e, pso=pso, h=h, hh=hh, b=b, at=at: e.matmul(pso.ap[:, hh * P:(hh + 1) * P], lhsT=vtm.ap[:, b, h * P:(h + 1) * P],
                                                                                     rhs=at.ap[:, hh, :], start=True, stop=False),
                                  r=[vtm[b], at[hh]], w=[pso[hh]])
                                T(lambda e, pso=pso, h=h, hh=hh, blk=blk: e.matmul(pso.ap[:, hh * P:(hh + 1) * P], lhsT=SA.ap[:, h, :],
                                                                                 rhs=QE.ap[:, h, blk], start=False, stop=False),
                                  r=[SA[h], QE[h]], w=[pso[hh]])
                                T(lambda e, pso=pso, h=h, hh=hh, blk=blk: e.matmul(pso.ap[:, hh * P:(hh + 1) * P], lhsT=SB.ap[:, h, :],
                                                                                 rhs=QO.ap[:, h, blk], start=False, stop=True),
                                  r=[SB[h], QO[h]], w=[pso[hh]])
                            A(lambda e, pso=pso, hq=hq, blk=blk: e.activation(out=FA.ap[:, hq * 4:hq * 4 + 4, blk],
                                                                              in_=pso.ap.rearrange("p (h t) -> p h t", h=4), func=AF.Identity),
                              r=[pso], w=[FA[slice(hq * 4, hq * 4 + 4)]])
                    for hq in range(2):
                        PSx = PSS if hq == 0 else PSM
                        h0 = hq * 4
                        col = b * P + par * 64 + 63
                        V(lambda e, h0=h0, col=col, Sout=Sout, PSx=PSx: e.tensor_tensor(
                            out=Sout.ap[:, h0:h0 + 4, :], in0=PSx.ap.rearrange("p (h v) -> p h v", h=4),
                            in1=FB.ap[:, h0:h0 + 4, col:col + 1].broadcast_to([P, 4, P]), op=ALU.mult),
                          r=[PSx, FB[slice(h0, h0 + 4)]], w=[Sout[slice(h0, h0 + 4)]])
            if CUT <= 6:
                return
            if prefix:
                if lastp:
                    V(lambda e: e.tensor_scalar(out=SA.ap, in0=SA.ap, scalar1=flag.ap[:, 0:1], scalar2=None, op0=ALU.mult),
                      r=[SA, flag], w=[SA])
                return
            if dm:
                dump("oT", FA)
                dump("SA", SA)
            A(lambda e: e.activation(out=osq.ap, in_=FA.ap, func=AF.Square), r=[FA], w=[osq])
            for hp in range(4):
                ps = nps()
                for i in range(2):
                    h = hp * 2 + i
                    T(lambda e, ps=ps, h=h, i=i: e.matmul(ps.ap[:, i * TT:(i + 1) * TT], lhsT=onesb.ap, rhs=osq.ap[:, h, :], start=True, stop=True),
                      r=[onesb, osq[h]], w=[ps[slice(i * 2, i * 2 + 2)]])
                A(lambda e, ps=ps, hp=hp: e.activation(out=FC.ap[:, hp * 2:hp * 2 + 2, :], in_=ps.ap.rearrange("p (h t) -> p h t", h=2),
                                                       func=AF.Sqrt, scale=1.0 / 128.0, bias=EPS), r=[ps], w=[FC[slice(hp * 2, hp * 2 + 2)]])
            V(lambda e: e.reciprocal(out=FC.ap, in_=FC.ap), r=[FC], w=[FC])
            V(lambda e: e.tensor_tensor(out=FA.ap, in0=FA.ap, in1=FC.ap, op=ALU.mult), r=[FA, FC], w=[FA])
            V(lambda e: e.scalar_tensor_tensor(out=kT.ap, in0=FA.ap, scalar=pv.ap[:, C_NG:C_NG + 1], in1=sog.ap,
                                               op0=ALU.mult, op1=ALU.mult), r=[FA, pv, sog], w=[kT])
            for half in range(2):
                ws = wget()
                for cc in range(4):
                    c = half * 4 + cc
                    ps = nps()
                    for k in range(KC):
                        T(lambda e, ps=ps, k=k, cc=cc, ws=ws: e.matmul(ps.ap[:, 0:TT], lhsT=ws.ap[:, k, cc * P:(cc + 1) * P], rhs=kT.ap[:, k, :],
                                                                     start=(k == 0), stop=(k == KC - 1)), r=[ws, kT], w=[ps])
                    V(lambda e, ps=ps, c=c: e.tensor_tensor(out=mT.ap[:, c, :], in0=ps.ap[:, 0:TT], in1=sgh.ap[:, c, :], op=ALU.mult),
                      r=[ps, sgh[c]], w=[mT[c]])
            V(lambda e: e.tensor_tensor(out=mT.ap, in0=mT.ap, in1=m1.ap, op=ALU.add), r=[mT, m1], w=[mT])
            for half in range(2):
                ws = wget()
                for j in range(NS):
                    ps = nps()
                    for k in range(KC):
                        T(lambda e, ps=ps, k=k, j=j, ws=ws: e.matmul(ps.ap, lhsT=mT.ap[:, k, j * P:(j + 1) * P], rhs=ws.ap[:, k, :],
                                                                   start=(k == 0), stop=(k == KC - 1)), r=[mT, ws], w=[ps])
                    V(lambda e, ps=ps, half=half: e.tensor_tensor(out=tmpa.ap, in0=ps.ap, in1=ga1_bc.ap[:, half * 512:(half + 1) * 512], op=ALU.mult),
                      r=[ps, ga1_bc], w=[tmpa])
                    V(lambda e, j=j, half=half: e.tensor_tensor(out=xt.ap[:, j, half * 512:(half + 1) * 512], in0=tmpa.ap,
                                                                in1=xt.ap[:, j, half * 512:(half + 1) * 512], op=ALU.add), r=[tmpa, xt[j]], w=[xt[j]])
            if dm:
                dump("ogT", kT)
                dump("mT", mT)
                dump("x1", xt)
            if CUT <= 7:
                return
            DMA("sp", lambda e: e.dma_start(out=x1_d[tok0:tok0 + TT, :].rearrange("(j p) d -> p j d", p=P), in_=xt.ap), r=[xt])
            rms_to_T(xt, h2t, gsc2.ap, modT.ap[:, 24:32], gsc2, modT)
            if not SPARSE:
                DMA("sp", lambda e: e.dma_start(out=h2_d.rearrange("p (k t) -> p k t", k=KC)[:, :, tok0:tok0 + TT], in_=h2t.ap), r=[h2t])
            else:
                for j in range(NS):
                    for half in range(2):
                        hs = slice(half * 512, (half + 1) * 512)
                        V(lambda e, j=j, hs=hs: e.scalar_tensor_tensor(out=tmpa.ap, in0=xt.ap[:, j, hs], scalar=small.ap[:, 16 + j:17 + j],
                                                                       in1=gsc2_bc.ap[:, hs], op0=ALU.mult, op1=ALU.mult),
                          r=[xt[j], small[16 + j], gsc2_bc], w=[tmpa])
                        V(lambda e, j=j, hs=hs: e.tensor_tensor(out=xn.ap[:, j, hs], in0=tmpa.ap, in1=sh2_bc.ap[:, hs], op=ALU.add),
                          r=[tmpa, sh2_bc], w=[xn[j]])
                DMA("sp", lambda e: e.dma_start(out=h2tm_d[tok0:tok0 + TT, :].rearrange("(j p) d -> p j d", p=P), in_=xn.ap), r=[xn])
            for j in range(NS):
                st = t * NS + j
                for k in range(KC):
                    T(lambda e, k=k, j=j: e.matmul(PSM.ap[:, 0:NE], lhsT=h2t.ap[:, k, j * P:(j + 1) * P], rhs=wr.ap[:, k, :],
                                                   start=(k == 0), stop=(k == KC - 1)), r=[h2t, wr], w=[PSM])
                V(lambda e: e.tensor_tensor(out=lgt.ap, in0=PSM.ap[:, 0:NE], in1=br_bc.ap, op=ALU.add), r=[PSM, br_bc], w=[lgt])
                V(lambda e, st=st: e.tensor_copy(out=lgts.ap[:, st, :], in_=lgt.ap), r=[lgt], w=[lgts[st]])
                V(lambda e: e.max(out=mx8.ap, in_=lgt.ap), r=[lgt], w=[mx8])
                V(lambda e: e.tensor_scalar(out=small.ap[:, 24:25], in0=mx8.ap[:, 0:1], scalar1=-1.0, scalar2=None, op0=ALU.mult),
                  r=[mx8], w=[small[24]])
                A(lambda e: e.activation(out=egt.ap, in_=lgt.ap, func=AF.Exp, bias=small.ap[:, 24:25]), r=[lgt, small[24]], w=[egt])
                V(lambda e: e.scalar_tensor_tensor(out=egt.ap, in0=lgt.ap, scalar=mx8.ap[:, 3:4], in1=egt.ap, op0=ALU.is_ge, op1=ALU.mult),
                  r=[lgt, mx8, egt], w=[egt])
                V(lambda e: e.reduce_sum(out=small.ap[:, 25:26], in_=egt.ap, axis=mybir.AxisListType.X), r=[egt], w=[small[25]])
                V(lambda e: e.reciprocal(out=small.ap[:, 26:27], in_=small.ap[:, 25:26]), r=[small[25]], w=[small[26]])
                V(lambda e, st=st: e.tensor_scalar(out=gates.ap[:, st, :], in0=egt.ap, scalar1=small.ap[:, 26:27], scalar2=None, op0=ALU.mult),
                  r=[egt, small[26]], w=[gates[st]])
                V(lambda e, st=st: e.tensor_copy(out=mx4.ap[:, st, :], in_=mx8.ap[:, 0:4]), r=[mx8], w=[mx4[st]])
                A(lambda e, st=st: e.activation(out=gk.ap[:, st, :], in_=mx8.ap[:, 0:4], func=AF.Exp, bias=small.ap[:, 24:25]),
                  r=[mx8, small[24]], w=[gk[st]])
                V(lambda e, st=st: e.tensor_scalar(out=gk.ap[:, st, :], in0=gk.ap[:, st, :], scalar1=small.ap[:, 26:27], scalar2=None, op0=ALU.mult),
                  r=[gk[st], small[26]], w=[gk[st]])

        if stage in (1, 3):
            tiles = [(True, True, NTILE - 1)] + [(False, False, t) for t in range(2 if stage == 1 else 4)]
            allg = []
            for (pf, lp, t) in tiles:
                allg += [wsrc(k, g) for (k, g) in tile_groups(pf, lp)]
        if stage == 0:
            tiles = []
        if os.environ.get("KPRE") == "0":
            tiles = [x for x in tiles if not x[0]]
            allg = []
            for (pf, lp, t) in tiles:
                allg += [wsrc(k, g) for (k, g) in tile_groups(pf, lp)]
        if os.environ.get("KPRE") == "only":
            tiles = [x for x in tiles if x[0]]
            allg = []
            for (pf, lp, t) in tiles:
                allg += [wsrc(k, g) for (k, g) in tile_groups(pf, lp)]
        for (pf, lp, t) in tiles:
            mixer_tile(pf, lp, t)
        if dbg and tiles:
            dump("h2t", h2t)
            dump("gates", gates)

        barrier(None)


        nst = len([1 for (pf, lp, t) in tiles if not pf]) * NS
        NST = NTOK // P
        YsB = Buf(None, "ys_dram", NB)
        if stage == 6:
            nst = 0
        if SPARSE and nst > 0:
            rv = Carver(shared_end)
            maskall = rv.get(F32, [NST, NE], "maskall")
            posall = rv.get(F32, [NST, NE], "posall", NST)
            eqt = rv.get(F32, [NST, NE], "eqt")
            cum = rv.get(F32, [NE], "cum")
            nblk = rv.get(F32, [NE], "nblk")
            pend = rv.get(F32, [NE], "pend")
            pstart = rv.get(F32, [NE], "pstart")
            destf = rv.get(F32, [NST, 4], "destf", 4)
            widxf = rv.get(F32, [NB, KC], "widxf")
            oobf = rv.get(F32, [NB], "oobf")
            zt = rv.get(BF16, [4096], "zt")
            hrow = [rv.get(BF16, [D], "hrow%d" % i) for i in range(2)]
            V(lambda e: e.tensor_scalar(out=maskall.ap, in0=gates.ap, scalar1=0.0, scalar2=None, op0=ALU.is_gt), r=[gates], w=[maskall])
            V(lambda e: e.memset(cum.ap, 0.0), w=[cum])
            for st in range(NST):
                T(lambda e, st=st: e.matmul(PSM.ap[:, 0:NE], lhsT=ustrict, rhs=maskall.ap[:, st, :], start=True, stop=False), r=[cst, maskall], w=[PSM])
                T(lambda e: e.matmul(PSM.ap[:, 0:NE], lhsT=onesf.ap, rhs=cum.ap, start=False, stop=True), r=[onesf, cum], w=[PSM])
                A(lambda e, st=st: e.activation(out=posall.ap[:, st, :], in_=PSM.ap[:, 0:NE], func=AF.Identity), r=[PSM], w=[posall[st]])
                V(lambda e, st=st: e.tensor_tensor(out=cum.ap, in0=cum.ap, in1=maskall.ap[:, st, :], op=ALU.add), r=[cum, maskall], w=[cum])
            T(lambda e: e.matmul(PSM.ap[:, 0:NE], lhsT=onesf.ap, rhs=cum.ap, start=True, stop=True), r=[onesf, cum], w=[PSM])
            V(lambda e: e.tensor_copy(out=cum.ap, in_=PSM.ap[:, 0:NE]), r=[PSM], w=[cum])
            V(lambda e: e.memset(nblk.ap, 0.0), w=[nblk])
            for jb in range(NTOK // BLK):
                V(lambda e, jb=jb: e.scalar_tensor_tensor(out=nblk.ap, in0=cum.ap, scalar=float(jb * BLK), in1=nblk.ap, op0=ALU.is_gt, op1=ALU.add),
                  r=[cum, nblk], w=[nblk])
            V(lambda e: e.tensor_scalar(out=nblk.ap, in0=nblk.ap, scalar1=float(BLK), scalar2=None, op0=ALU.mult), r=[nblk], w=[nblk])
            V(lambda e: e.tensor_tensor_scan(out=pend.ap, data0=onesf.ap[:, 0:NE], data1=nblk.ap, initial=0.0, op0=ALU.mult, op1=ALU.add),
              r=[onesf, nblk], w=[pend])
            V(lambda e: e.tensor_tensor(out=pstart.ap, in0=pend.ap, in1=nblk.ap, op=ALU.subtract), r=[pend, nblk], w=[pstart])
            V(lambda e: e.tensor_tensor(out=posall.ap, in0=posall.ap, in1=pstart.ap.unsqueeze(1).broadcast_to([P, NST, NE]), op=ALU.add),
              r=[posall, pstart], w=[posall])
            for k in range(4):
                V(lambda e, k=k: e.tensor_tensor(out=eqt.ap, in0=lgts.ap, in1=mx4.ap[:, :, k:k + 1].broadcast_to([P, NST, NE]), op=ALU.is_equal),
                  r=[lgts, mx4], w=[eqt])
                V(lambda e: e.tensor_tensor(out=eqt.ap, in0=eqt.ap, in1=posall.ap, op=ALU.mult), r=[eqt, posall], w=[eqt])
                V(lambda e, k=k: e.reduce_sum(out=destf.ap[:, :, k], in_=eqt.ap, axis=mybir.AxisListType.X), r=[eqt], w=[destf[k]])
            V(lambda e: e.tensor_copy(out=desti.ap, in_=destf.ap), r=[destf], w=[desti])
            V(lambda e: e.memset(bef.ap, 0.0), w=[bef])
            for ex in range(NE):
                V(lambda e, ex=ex: e.scalar_tensor_tensor(out=bef.ap, in0=iotab, scalar=pend.ap[:, ex:ex + 1], in1=bef.ap, op0=ALU.is_ge, op1=ALU.add),
                  r=[cst, pend, bef], w=[bef])
            V(lambda e: e.tensor_scalar(out=bef.ap, in0=bef.ap, scalar1=float(NE - 1), scalar2=None, op0=ALU.min), r=[bef], w=[bef])
            V(lambda e: e.scalar_tensor_tensor(out=widxf.ap, in0=bef.ap.unsqueeze(2).broadcast_to([P, NB, KC]), scalar=float(D),
                                               in1=basekp.unsqueeze(1).broadcast_to([P, NB, KC]), op0=ALU.mult, op1=ALU.add),
              r=[bef, cst], w=[widxf])
            V(lambda e: e.tensor_scalar(out=oobf.ap, in0=iotab, scalar1=pend.ap[:, NE - 1:NE], scalar2=65536.0, op0=ALU.is_ge, op1=ALU.mult),
              r=[cst, pend], w=[oobf])
            V(lambda e: e.tensor_tensor(out=widxf.ap, in0=widxf.ap, in1=oobf.ap.unsqueeze(2).broadcast_to([P, NB, KC]), op=ALU.add),
              r=[widxf, oobf], w=[widxf])
            V(lambda e: e.tensor_copy(out=widx.ap, in_=widxf.ap), r=[widxf], w=[widx])
            for st in range(nst):
                hr = hrow[st % 2]
                DMA("sp", lambda e, st=st, hr=hr: e.dma_start(out=hr.ap, in_=h2tm_d[st * P:(st + 1) * P, :]), w=[hr])
                for k in range(4):
                    DMA("pool", lambda e, st=st, k=k, hr=hr: e.indirect_dma_start(
                        out=xs_d[:, :], out_offset=bass.IndirectOffsetOnAxis(ap=desti.ap[:, st, k:k + 1], axis=0),
                        in_=hr.ap, in_offset=None), r=[hr, desti, XsB[4 * NST]], w=[XsB[st * 4 + k]])
            if dbg:
                dump("desti", desti)
                dump("bef", bef)
                dump("cnt", cum)
            barrier(None)

            bv = Carver(shared_end)
            W1b = [bv.get(BF16, [KC, 2 * D], "W1b%d" % i, KC) for i in range(2)]
            W2b = [bv.get(BF16, [KC, D], "W2b%d" % i, KC) for i in range(2)]
            xrows = bv.get(BF16, [4, D], "xrows", 4)
            xbTs = [bv.get(BF16, [KC, BLK], "xbT%d" % i, KC) for i in range(2)]
            actB = [bv.get(BF16, [KC, BLK], "actB%d" % i, KC) for i in range(2)]
            tgB = [bv.get(F32, [BLK], "tgB%d" % i) for i in range(2)]
            tsB = [bv.get(F32, [BLK], "tsB%d" % i) for i in range(2)]
            tlB = [bv.get(F32, [BLK], "tlB%d" % i) for i in range(2)]
            ysb = [bv.get(F32, [D], "ysb%d" % i, 2) for i in range(2)]
            oneh = bv.get(F32, [NE], "oneh")
            b1tmp = bv.get(F32, [16, NE], "b1tmp")
            b1sel = [bv.get(F32, [16], "b1sel%d" % i) for i in range(2)]
            print("arena bytes: blocks", bv.off)
            w1_flat = w1_2d
            w2_flat = w2_2d
            b1v = pv.ap[:, C_B1:C_B1 + NE * 16].rearrange("p (e i) -> p i e", i=16)
            ps6 = [0]

            def nps6():
                bk = PS[ps6[0] % 6]
                ps6[0] += 1
                return bk

            bc_cache = {}

            def bc_reg(e):
                if "r" not in bc_cache:
                    bc_cache["r"] = e.to_reg(NE * D - 1)
                return bc_cache["r"]

            def load_w1(b):
                wa = W1b[b % 2]
                for k in range(KC):
                    DMA("pool", lambda e, b=b, k=k, wa=wa: e.indirect_dma_start(
                        out=wa.ap[:, k, :], out_offset=None, in_=w1_flat[:, :],
                        in_offset=bass.IndirectOffsetOnAxis(ap=widx.ap[:, b, k:k + 1], axis=0),
                        bounds_check=bc_reg(e), oob_is_err=False), r=[widx], w=[wa[k]])

            def load_w2(b):
                wb2 = W2b[b % 2]
                for k in range(KC):
                    DMA("pool", lambda e, b=b, k=k, wb2=wb2: e.indirect_dma_start(
                        out=wb2.ap[:, k, :], out_offset=None, in_=w2_flat[:, :],
                        in_offset=bass.IndirectOffsetOnAxis(ap=widx.ap[:, b, k:k + 1], axis=0),
                        bounds_check=bc_reg(e), oob_is_err=False), r=[widx], w=[wb2[k]])

            def prep(b):
                xbT = xbTs[b % 2]
                DMA("sp", lambda e, b=b: e.dma_start(out=xrows.ap, in_=xs_d[b * BLK:(b + 1) * BLK, :].rearrange("(j p) d -> p j d", p=P)),
                    r=[XsB], w=[xrows])
                for k in range(KC):
                    pt = PT[k % 2]
                    for j in range(4):
                        T(lambda e, pt=pt, j=j, k=k: e.transpose(pt.ap[:, j * P:(j + 1) * P], xrows.ap[:, j, k * P:(k + 1) * P], identb.ap),
                          r=[xrows[j], identb], w=[pt])
                    if k % 2 == 0:
                        A(lambda e, pt=pt, k=k, xbT=xbT: e.activation(out=xbT.ap[:, k, :], in_=pt.ap[:, 0:BLK], func=AF.Identity), r=[pt], w=[xbT[k]])
                    else:
                        V(lambda e, pt=pt, k=k, xbT=xbT: e.tensor_copy(out=xbT.ap[:, k, :], in_=pt.ap[:, 0:BLK]), r=[pt], w=[xbT[k]])
                bs = b1sel[b % 2]
                V(lambda e, b=b: e.tensor_scalar(out=oneh.ap, in0=iotae, scalar1=bef.ap[:, b:b + 1], scalar2=None, op0=ALU.is_equal),
                  r=[cst, bef], w=[oneh])
                V(lambda e: e.tensor_tensor(out=b1tmp.ap, in0=b1v, in1=oneh.ap.unsqueeze(1).broadcast_to([P, 16, NE]), op=ALU.mult),
                  r=[pv, oneh], w=[b1tmp])
                V(lambda e, bs=bs: e.reduce_sum(out=bs.ap, in_=b1tmp.ap, axis=mybir.AxisListType.X), r=[b1tmp], w=[bs])
                V(lambda e, bs=bs: e.tensor_scalar(out=bs.ap[:, 8:16], in0=bs.ap[:, 8:16], scalar1=1.0, scalar2=None, op0=ALU.add), r=[bs], w=[bs])

            def w1_piece(b, i):
                wa, xbT, bs, aT = W1b[b % 2], xbTs[b % 2], b1sel[b % 2], actB[b % 2]
                psg = nps6()
                psl = nps6()
                for k in range(KC):
                    T(lambda e, k=k: e.matmul(psg.ap, lhsT=wa.ap[:, k, i * P:(i + 1) * P], rhs=xbT.ap[:, k, :],
                                              start=(k == 0), stop=(k == KC - 1)), r=[wa, xbT], w=[psg])
                for k in range(KC):
                    T(lambda e, k=k: e.matmul(psl.ap, lhsT=wa.ap[:, k, D + i * P:D + (i + 1) * P], rhs=xbT.ap[:, k, :],
                                              start=(k == 0), stop=(k == KC - 1)), r=[wa, xbT], w=[psl])
                a_, b_, c_ = tgB[i % 2], tsB[i % 2], tlB[i % 2]
                V(lambda e: e.tensor_scalar(out=a_.ap, in0=psg.ap, scalar1=bs.ap[:, i:i + 1], scalar2=7.0, op0=ALU.add, op1=ALU.min),
                  r=[psg, bs], w=[a_])
                A(lambda e: e.activation(out=b_.ap, in_=a_.ap, func=AF.Silu, scale=1.702), r=[a_], w=[b_])
                V(lambda e: e.tensor_scalar(out=c_.ap, in0=psl.ap, scalar1=bs.ap[:, 8 + i:9 + i], scalar2=8.0, op0=ALU.add, op1=ALU.min),
                  r=[psl, bs], w=[c_])
                V(lambda e: e.scalar_tensor_tensor(out=aT.ap[:, i, :], in0=c_.ap, scalar=-6.0, in1=b_.ap, op0=ALU.max, op1=ALU.mult),
                  r=[b_, c_], w=[aT[i]])

            def w2_piece(b, g):
                j4, half = g // 2, g % 2
                wb2, aT, yb = W2b[b % 2], actB[b % 2], ysb[j4 % 2]
                ps = nps6()
                for i in range(KC):
                    T(lambda e, i=i: e.matmul(ps.ap, lhsT=aT.ap[:, i, j4 * P:(j4 + 1) * P], rhs=wb2.ap[:, i, half * 512:(half + 1) * 512],
                                              start=(i == 0), stop=(i == KC - 1)), r=[aT, wb2], w=[ps])
                if half == 0:
                    A(lambda e: e.activation(out=yb.ap[:, 0:512], in_=ps.ap, func=AF.Identity, scale=1.0 / 1.702), r=[ps], w=[yb[0]])
                else:
                    A(lambda e: e.activation(out=yb.ap[:, 512:1024], in_=ps.ap, func=AF.Identity, scale=1.0 / 1.702), r=[ps], w=[yb[1]])
                    r0 = b * BLK + j4 * P
                    DMA("sp", lambda e: e.dma_start(out=ys_d[r0:r0 + P, :], in_=yb.ap), r=[yb], w=[YsB[b]])

            nblocks = NB if stage == 99 else int(os.environ.get("KNB", NB))
            if nblocks:
                load_w1(0)
                load_w2(0)
                prep(0)
                if nblocks > 1:
                    load_w1(1)
            for b in range(nblocks):
                for i in range(KC):
                    w1_piece(b, i)
                    if i == 1 and b + 1 < nblocks:
                        prep(b + 1)
                    if b > 0:
                        w2_piece(b - 1, i)
                if b + 2 < nblocks:
                    load_w1(b + 2)
                if b + 1 < nblocks:
                    load_w2(b + 1)
            if nblocks:
                for g in range(8):
                    w2_piece(nblocks - 1, g)
            barrier(None)


            cb = Carver(shared_end)
            Yk2 = [[cb.get(F32, [D], "Yk%d_%d" % (s_, i)) for i in range(4)] for s_ in range(2)]
            accs = cb.get(F32, [D], "accs", 2)
            cx1 = [cb.get(F32, [D], "cx1%d" % i) for i in range(2)]
            cxo = cb.get(F32, [D], "cxo")
            cjunk = cb.get(BF16, [D], "cjunk")
            cgT = cb.get(BF16, [P], "cgT")
            for st in range(nst):
                r0 = st * P
                xb = cx1[st % 2]
                Yk = Yk2[st % 2]
                DMA("sp", lambda e, xb=xb, r0=r0: e.dma_start(out=xb.ap, in_=x1_d[r0:r0 + P, :]), w=[xb])
                for k in range(4):
                    DMA("pool", lambda e, st=st, k=k, Yk=Yk: e.indirect_dma_start(
                        out=Yk[k].ap, out_offset=None, in_=ys_d[:, :],
                        in_offset=bass.IndirectOffsetOnAxis(ap=desti.ap[:, st, k:k + 1], axis=0)), r=[desti, YsB], w=[Yk[k]])
                T(lambda e, st=st: e.transpose(PSM.ap[0:NE, 0:P], gates.ap[:, st, :], ident_f), r=[gates[st], cst], w=[PSM])
                V(lambda e: e.tensor_copy(out=cgT.ap[0:NE, :], in_=PSM.ap[0:NE, 0:P]), r=[PSM], w=[cgT])
                for half in range(2):
                    ps = nps()
                    T(lambda e, ps=ps, half=half: e.matmul(ps.ap, lhsT=cgT.ap[0:NE, :], rhs=b2b.ap[0:NE, half * 512:(half + 1) * 512],
                                                          start=True, stop=True), r=[cgT, b2b], w=[ps])
                    A(lambda e, ps=ps, half=half: e.activation(out=accs.ap[:, half * 512:(half + 1) * 512], in_=ps.ap, func=AF.Identity),
                      r=[ps], w=[accs[half]])
                for k in range(4):
                    V(lambda e, st=st, k=k, Yk=Yk: e.scalar_tensor_tensor(out=accs.ap, in0=Yk[k].ap, scalar=gk.ap[:, st, k:k + 1], in1=accs.ap,
                                                                   op0=ALU.mult, op1=ALU.add), r=[Yk[k], gk[st], accs], w=[accs])
                V(lambda e: e.tensor_tensor(out=cxo.ap, in0=accs.ap, in1=ga2_bc.ap, op=ALU.mult), r=[accs, ga2_bc], w=[cxo])
                V(lambda e, xb=xb: e.tensor_tensor(out=cxo.ap, in0=cxo.ap, in1=xb.ap, op=ALU.add), r=[cxo, xb], w=[cxo])
                A(lambda e: e.activation(out=cjunk.ap, in_=cxo.ap, func=AF.Square, accum_out=small.ap[:, 32:33]), r=[cxo], w=[cjunk, small[32]])
                A(lambda e: e.activation(out=small.ap[:, 33:34], in_=small.ap[:, 32:33], func=AF.Sqrt, scale=1.0 / D, bias=EPS), r=[small[32]], w=[small[33]])
                V(lambda e: e.reciprocal(out=small.ap[:, 34:35], in_=small.ap[:, 33:34]), r=[small[33]], w=[small[34]])
                V(lambda e, xb=xb: e.scalar_tensor_tensor(out=xb.ap, in0=cxo.ap, scalar=small.ap[:, 34:35], in1=gfin_bc.ap, op0=ALU.mult, op1=ALU.mult),
                  r=[cxo, small[34], gfin_bc], w=[xb])
                DMA("sp", lambda e, xb=xb, r0=r0: e.dma_start(out=y_d[r0:r0 + P, :], in_=xb.ap), r=[xb])

        w1_v = w1_d.rearrange("e (k p) n -> e p k n", p=P)
        w2_v = w2_d.rearrange("e (k p) n -> e p k n", p=P)
        h2_v = h2_d.rearrange("p (k t) -> p k t", k=KC)

        def load_w1(e_, i):
            DMA("pool", lambda en: en.dma_start(out=W1[i].ap[:, :, 0:256], in_=w1_v[e_][:, :, i * 256:(i + 1) * 256]), w=[W1[i]])
            DMA("pool", lambda en: en.dma_start(out=W1[i].ap[:, :, 256:512], in_=w1_v[e_][:, :, D + i * 256:D + (i + 1) * 256]), w=[W1[i]])

        def load_w2(e_):
            DMA("pool", lambda en: en.dma_start(out=W2.ap, in_=w2_v[e_]), w=[W2])

        seq = [(q, e_) for q in range(NQ) for e_ in range(NE)]
        if stage <= 1 or SPARSE:
            seq = []
        if stage in (2, 3):
            seq = [(0, e_) for e_ in range(NE)]
        if SPARSE:
            seq = []
        if seq:
            for i in range(4):
                load_w1(0, i)
            load_w2(0)
        for si, (q, e_) in enumerate(seq):
            nxt = seq[si + 1][1] if si + 1 < len(seq) else None
            if e_ == 0:
                DMA("sp", lambda en, q=q: en.dma_start(out=h2q.ap, in_=h2_v[:, :, q * QT:(q + 1) * QT]), w=[h2q])
                for j in range(QT // P):
                    st = q * (QT // P) + j
                    T(lambda en, st=st: en.transpose(PSM.ap[0:NE, 0:P], gates.ap[:, st, :], ident_f), r=[gates[st], cst], w=[PSM])
                    V(lambda en: en.tensor_copy(out=gTb.ap[0:NE, :], in_=PSM.ap[0:NE, 0:P]), r=[PSM], w=[gTb])
                    for half in range(2):
                        ps = nps()
                        T(lambda en, ps=ps, half=half: en.matmul(ps.ap, lhsT=gTb.ap[0:NE, :], rhs=b2b.ap[0:NE, half * 512:(half + 1) * 512],
                                                                start=True, stop=True), r=[gTb, b2b], w=[ps])
                        A(lambda en, ps=ps, j=j, half=half: en.activation(out=acc.ap[:, j, half * 512:(half + 1) * 512], in_=ps.ap, func=AF.Identity),
                          r=[ps], w=[acc[j * 2 + half]])
            for blk in range(QT // 512):
                tsl = slice(blk * 512, (blk + 1) * 512)
                aT = actT[blk % 2]
                for i in range(KC):
                    g4, sub = i // 2, i % 2
                    wb = W1[g4]
                    psg = nps()
                    psl = nps()
                    for k in range(KC):
                        T(lambda en, psg=psg, k=k, wb=wb, sub=sub, tsl=tsl: en.matmul(psg.ap, lhsT=wb.ap[:, k, sub * P:(sub + 1) * P], rhs=h2q.ap[:, k, tsl],
                                                                                 start=(k == 0), stop=(k == KC - 1)), r=[wb, h2q], w=[psg])
                    for k in range(KC):
                        T(lambda en, psl=psl, k=k, wb=wb, sub=sub, tsl=tsl: en.matmul(psl.ap, lhsT=wb.ap[:, k, 256 + sub * P:256 + (sub + 1) * P], rhs=h2q.ap[:, k, tsl],
                                                                                 start=(k == 0), stop=(k == KC - 1)), r=[wb, h2q], w=[psl])
                    if blk == QT // 512 - 1 and sub == 1 and nxt is not None:
                        load_w1(nxt, g4)
                    cg = C_B1 + e_ * 16 + i
                    cl = C_B1 + e_ * 16 + 8 + i
                    a_, b_, c_ = tg[i % 2], tsg[i % 2], tl[i % 2]
                    V(lambda en, psg=psg, cg=cg, a_=a_: en.tensor_scalar(out=a_.ap, in0=psg.ap, scalar1=pv.ap[:, cg:cg + 1], scalar2=7.0, op0=ALU.add, op1=ALU.min),
                      r=[psg, pv], w=[a_])
                    A(lambda en, a_=a_, b_=b_: en.activation(out=b_.ap, in_=a_.ap, func=AF.Sigmoid, scale=1.702), r=[a_], w=[b_])
                    V(lambda en, psl=psl, cl=cl, c_=c_: en.tensor_scalar(out=c_.ap, in0=psl.ap, scalar1=pv.ap[:, cl:cl + 1], scalar2=7.0, op0=ALU.add, op1=ALU.min),
                      r=[psl, pv], w=[c_])
                    G(lambda en, c_=c_: en.tensor_scalar(out=c_.ap, in0=c_.ap, scalar1=-7.0, scalar2=1.0, op0=ALU.max, op1=ALU.add), r=[c_], w=[c_])
                    G(lambda en, a_=a_, b_=b_: en.tensor_tensor(out=a_.ap, in0=a_.ap, in1=b_.ap, op=ALU.mult), r=[a_, b_], w=[a_])
                    V(lambda en, a_=a_, c_=c_, aT=aT, i=i: en.tensor_tensor(out=aT.ap[:, i, :], in0=a_.ap, in1=c_.ap, op=ALU.mult), r=[a_, c_], w=[aT[i]])
                for j4 in range(4):
                    j = blk * 4 + j4
                    st = q * (QT // P) + j
                    for half in range(2):
                        ps = nps()
                        for i in range(KC):
                            T(lambda en, ps=ps, i=i, j4=j4, half=half, aT=aT: en.matmul(ps.ap, lhsT=aT.ap[:, i, j4 * P:(j4 + 1) * P],
                                                                                     rhs=W2.ap[:, i, half * 512:(half + 1) * 512],
                                                                                     start=(i == 0), stop=(i == KC - 1)), r=[aT, W2], w=[ps])
                        V(lambda en, ps=ps, j=j, half=half, st=st, e_=e_: en.scalar_tensor_tensor(
                            out=acc.ap[:, j, half * 512:(half + 1) * 512], in0=ps.ap, scalar=gates.ap[:, st, e_:e_ + 1],
                            in1=acc.ap[:, j, half * 512:(half + 1) * 512], op0=ALU.mult, op1=ALU.add), r=[ps, gates[st], acc[j * 2 + half]], w=[acc[j * 2 + half]])
            if nxt is not None:
                load_w2(nxt)
            if e_ == NE - 1:
                for j in range(QT // P):
                    r0 = q * QT + j * P
                    xb = x1t[j % 2]
                    DMA("sp", lambda en, xb=xb, r0=r0: en.dma_start(out=xb.ap, in_=x1_d[r0:r0 + P, :]), w=[xb])
                    V(lambda en, j=j: en.tensor_tensor(out=xo.ap, in0=acc.ap[:, j, :], in1=ga2_bc.ap, op=ALU.mult), r=[acc[slice(2 * j, 2 * j + 2)], ga2_bc], w=[xo])
                    G(lambda en, xb=xb: en.tensor_tensor(out=xo.ap, in0=xo.ap, in1=xb.ap, op=ALU.add), r=[xo, xb], w=[xo])
                    A(lambda en: en.activation(out=ejunk.ap, in_=xo.ap, func=AF.Square, accum_out=small.ap[:, 32:33]), r=[xo], w=[ejunk, small[32]])
                    A(lambda en: en.activation(out=small.ap[:, 33:34], in_=small.ap[:, 32:33], func=AF.Sqrt, scale=1.0 / D, bias=EPS), r=[small[32]], w=[small[33]])
                    V(lambda en: en.reciprocal(out=small.ap[:, 34:35], in_=small.ap[:, 33:34]), r=[small[33]], w=[small[34]])
                    V(lambda en, xb=xb: en.scalar_tensor_tensor(out=xb.ap, in0=xo.ap, scalar=small.ap[:, 34:35], in1=gfin_bc.ap, op0=ALU.mult, op1=ALU.mult),
                      r=[xo, small[34], gfin_bc], w=[xb])
                    DMA("sp", lambda en, xb=xb, r0=r0: en.dma_start(out=y_d[r0:r0 + P, :], in_=xb.ap), r=[xb])

        S.finish()
        print("ops:", {e: len(v) for e, v in S.ops.items()})
        with nc.Block() as block:
            @block.sync
            def _(e):
                S.run("sp", e)

            @block.scalar
            def _(e):
                S.run("act", e)

            @block.vector
            def _(e):
                S.run("dve", e)

            @block.gpsimd
            def _(e):
                S.run("pool", e)

            @block.tensor
            def _(e):
                S.run("pe", e)
    nc._dbg_names = DBG
    return nc


def _fm(v, n):
    return np.ascontiguousarray(np.asarray(v, np.float32).reshape(n, P).T)


def _consts():
    cst = np.zeros((P, CW), np.float32)
    cst[:, 0:128] = np.eye(P, dtype=np.float32)
    s = np.arange(P)[:, None]
    t = np.arange(P)[None, :]
    cst[:, 128:256] = ((s // 64 == t // 64) & (s <= t)).astype(np.float32)
    rm = np.ones((P, 256), np.float32)
    rm[:, ::64] = 0.0
    cst[:, 256:512] = rm
    cst[:, 512:640] = (s < t).astype(np.float32)
    cst[:, 640:704] = np.arange(NB, dtype=np.float32)[None, :] * BLK
    cst[:, 704:736] = np.arange(NE, dtype=np.float32)[None, :]
    cst[:, 736:744] = np.arange(KC, dtype=np.float32)[None, :] * P + np.arange(P, dtype=np.float32)[:, None]
    return cst


def make_in_maps(x, c, w_ada, b_ada, g_mix, w_in, conv_dw, conv_dw_bias, conv_ln_g, conv_ln_b,
                 w_conv_out, lb_param, hgrn_norm_g, w_hgrn_out, w_out, g_ffn, w_router, b_router,
                 w1, b1, w2, b2, g_final, cores=range(8)):
    f = lambda a: np.ascontiguousarray(np.asarray(a, np.float32))
    x = f(x)
    cst = _consts()
    dwT = np.ascontiguousarray(f(conv_dw)[0].T.reshape(KC, P, 31).transpose(1, 0, 2).reshape(P, KC * 31))
    b1T = np.ascontiguousarray(f(b1)[0].reshape(NE, 16, P).transpose(2, 0, 1).reshape(P, NE * 16))
    common = {
        "cst": cst,
        "w_ada": f(w_ada)[0], "b_ada": f(b_ada)[0:1], "w_in": f(w_in)[0],
        "w_conv_out": f(w_conv_out)[0], "w_hgrn_out": f(w_hgrn_out)[0], "w_out": f(w_out)[0],
        "w_router": f(w_router)[0], "b_router": f(b_router)[0:1],
        "w1": f(w1)[0].reshape(NE * D, 2 * D), "w2": f(w2)[0].reshape(NE * D, D), "b2": f(b2)[0], "g_final": f(g_final).reshape(1, D),
        "g_ffn": f(g_ffn)[0:1],
    }
    in_maps = []
    for core in cores:
        b, half = core // 2, core % 2
        pvec = np.zeros((P, RV), np.float32)
        pvec[:, C_C:C_C + 8] = _fm(np.asarray(c)[b], 8)
        pvec[:, C_BADA:C_BADA + 48] = _fm(np.asarray(b_ada)[0], 48)
        pvec[:, C_GMIX:C_GMIX + 8] = _fm(np.asarray(g_mix)[0], 8)
        pvec[:, C_DWB:C_DWB + 8] = _fm(np.asarray(conv_dw_bias)[0], 8)
        pvec[:, C_LNG:C_LNG + 8] = _fm(np.asarray(conv_ln_g)[0], 8)
        pvec[:, C_LNB:C_LNB + 8] = _fm(np.asarray(conv_ln_b)[0], 8)
        pvec[:, C_LB0:C_LB0 + 8] = _fm(np.asarray(lb_param)[0], 8)
        pvec[:, C_LB1:C_LB1 + 8] = _fm(np.asarray(lb_param)[1], 8)
        pvec[:, C_GFFN:C_GFFN + 8] = _fm(np.asarray(g_ffn)[0], 8)
        pvec[:, C_DW:C_DW + 248] = dwT
        pvec[:, C_B1:C_B1 + 512] = b1T
        pvec[:, C_NG] = np.asarray(hgrn_norm_g, np.float32)[0]
        m = dict(common)
        m["xm"] = np.ascontiguousarray(x[b, half * NTOK:(half + 1) * NTOK])
        m["xp"] = np.ascontiguousarray(x[b, 0:NTOK]) if half == 1 else np.zeros((NTOK, D), np.float32)
        m["flag"] = np.full((P, 1), float(half), np.float32)
        m["pvec"] = pvec
        in_maps.append(m)
    return in_maps


_NC = None


def kernel(**inputs):
    global _NC
    in_maps = make_in_maps(**inputs)
    if _NC is None:
        _NC = build_nc()
    res = run_bass_kernel_spmd(_NC, in_maps, core_ids=list(range(8)))
    out = np.zeros((4, 2 * NTOK, D), np.float32)
    for core in range(8):
        b, half = core // 2, core % 2
        out[b, half * NTOK:(half + 1) * NTOK] = np.asarray(res.results[core]["y"], np.float32)
    return out
```

```python
import os
import numpy as np
import concourse.bass as bass
import concourse.mybir as mybir
from concourse.bass_utils import run_bass_kernel_spmd

F32 = mybir.dt.float32
BF16 = mybir.dt.bfloat16
I32 = mybir.dt.int32
ALU = mybir.AluOpType
AF = mybir.ActivationFunctionType

P = 128
D = 1024
KC = 8
NTOK = 4096
TT = 256
NS = TT // P
NTILE = NTOK // TT
HALO = 30
UW = HALO + TT + 2
NE = 32
QT = 1024
NQ = NTOK // QT
EPS = 1e-6
BLK = 512
NB = 64
NR = NB * BLK
CW = 768
SPARSE = True
C_C, C_BADA, C_GMIX, C_DWB, C_LNG, C_LNB, C_LB0, C_LB1, C_GFFN, C_DW, C_B1, C_NG, RV = (
    0, 8, 56, 64, 72, 80, 88, 96, 104, 112, 360, 872, 876)
SAME_ENG_SYNC = True
CUT = int(os.environ.get('KCUT', '99'))
SUB = int(os.environ.get('KSUB', '99'))
EPOCH = 30000


class Op:
    __slots__ = ("eng", "fn", "deps", "marked", "sem", "val", "dma")


class Part:
    __slots__ = ("w", "r")

    def __init__(self):
        self.w = {}
        self.r = {}


class Buf:
    def __init__(self, ap, name="", nparts=1):
        self.ap = ap
        self.parts = [Part() for _ in range(nparts)]
        self.name = name

    def __getitem__(self, k):
        if len(self.parts) == 1:
            return self
        return (self, k)


def _expand(lst):
    out = {}
    for it in lst:
        if it is None:
            continue
        if isinstance(it, tuple):
            b, k = it
            if isinstance(k, int):
                ps = [b.parts[k]]
            elif isinstance(k, slice):
                ps = b.parts[k]
            else:
                ps = [b.parts[i] for i in k]
        else:
            ps = it.parts
        for p in ps:
            out[id(p)] = p
    return out


class Sched:
    ENG = ("sp", "act", "dve", "pool", "pe")

    def __init__(self, dma_sems, eng_sems):
        self.ops = {e: [] for e in self.ENG}
        self.pool = dma_sems
        self.esem = eng_sems
        self.dma_i = {q: 0 for q in dma_sems}
        self.dma_last = {}

    def op(self, eng, fn, r=(), w=(), dma=False):
        o = Op()
        o.eng, o.fn, o.dma, o.marked, o.deps, o.sem, o.val = eng, fn, dma, False, {}, None, 0
        rp = _expand(r)
        wp = _expand(w)
        for p in rp.values():
            for x in p.w.values():
                o.deps[id(x)] = x
        for p in wp.values():
            for x in p.r.values():
                o.deps[id(x)] = x
            for x in p.w.values():
                o.deps[id(x)] = x
        key = ("d", id(o)) if dma else eng
        for p in wp.values():
            p.w = {key: o}
            p.r = {}
        for k, p in rp.items():
            if k not in wp:
                p.r[key] = o
        if dma:
            pl = self.pool[eng]
            i = self.dma_i[eng] % len(pl)
            self.dma_i[eng] += 1
            prev = self.dma_last.get((eng, i))
            if prev is not None:
                o.deps[id(prev)] = prev
            o.sem = pl[i]
            o.val = (prev.val if prev is not None else 0) + 16
            self.dma_last[(eng, i)] = o
        for d in list(o.deps.values()):
            if (not d.dma) and d.eng == eng and (eng == "pe" or not SAME_ENG_SYNC):
                del o.deps[id(d)]
            else:
                d.marked = True
        self.ops[eng].append(o)
        return o

    def finish(self):
        o = Op()
        o.eng, o.fn, o.dma, o.marked, o.sem, o.val = "sp", (lambda e: e.nop()), False, False, None, 0
        o.deps = {id(x): x for x in self.dma_last.values()}
        self.ops["sp"].append(o)
        for eng in self.ENG:
            cnt = 0
            for q in self.ops[eng]:
                if (not q.dma) and q.marked:
                    q.sem = self.esem[eng][cnt // EPOCH]
                    q.val = cnt % EPOCH + 1
                    cnt += 1

    def run(self, eng, e):
        known = {}
        for o in self.ops[eng]:
            for d in o.deps.values():
                k = d.sem.num
                if known.get(k, 0) >= d.val:
                    continue
                e.wait_ge(d.sem, d.val)
                known[k] = d.val
            ins = o.fn(e)
            if o.dma:
                ins.then_inc(o.sem, 16)
            elif o.marked:
                ins.then_inc(o.sem, 1)


def build_nc(stage=99, dbg=False):
    DBG = []
    nc = bass.Bass("TRN2", target_bir_lowering=False)

    def dram(name, shape, dtype=F32, kind="ExternalInput"):
        return nc.dram_tensor(name, shape, dtype, kind=kind).ap()

    xm = dram("xm", [NTOK, D])
    xp = dram("xp", [NTOK, D])
    flag_d = dram("flag", [P, 1])
    pvec_d = dram("pvec", [P, RV])
    cst_d = dram("cst", [P, CW])
    gffn_d = dram("g_ffn", [1, D])
    w_ada = dram("w_ada", [D, 6 * D])
    b_ada = dram("b_ada", [1, 6 * D])
    w_in = dram("w_in", [D, 8 * D])
    wco_d = dram("w_conv_out", [D, D])
    who_d = dram("w_hgrn_out", [D, D])
    wout_d = dram("w_out", [D, D])
    wr_d = dram("w_router", [D, NE])
    br_d = dram("b_router", [1, NE])
    w1_2d = dram("w1", [NE * D, 2 * D])
    w2_2d = dram("w2", [NE * D, D])
    w1_d = w1_2d.rearrange("(e r) n -> e r n", e=NE)
    w2_d = w2_2d.rearrange("(e r) n -> e r n", e=NE)
    b2_d = dram("b2", [NE, D])
    gfin_d = dram("g_final", [1, D])
    y_d = dram("y", [NTOK, D], F32, "ExternalOutput")
    x1_d = dram("x1_scr", [NTOK, D], F32, "Internal")
    h2_d = dram("h2_scr", [P, KC * NTOK], BF16, "Internal")
    h2tm_d = dram("h2tm_scr", [NTOK, D], BF16, "Internal")
    dg_d = dram("dg_scr", [KC, P, 31, P], BF16, "Internal")
    wbf_d = dram("wbf_scr", [22, P, KC * 512], BF16, "Internal")
    xs_d = dram("xs_scr", [NR, D], BF16, "Internal")
    ys_d = dram("ys_scr", [NR, D], F32, "Internal")

    import contextlib
    es = contextlib.ExitStack()
    with es:
        AW = 52500
        arena = es.enter_context(nc.sbuf_tensor("arena", [P, AW], F32))
        psf = [es.enter_context(nc.psum_tensor("psf%d" % i, [P, 512], F32)) for i in range(6)]
        pst = [es.enter_context(nc.psum_tensor("pst%d" % i, [P, 1024], BF16)) for i in range(2)]
        dsems = {"sp": [es.enter_context(nc.semaphore("dmah%d" % i)) for i in range(20)],
                 "pool": [es.enter_context(nc.semaphore("dmas%d" % i)) for i in range(20)]}
        esems = {e: [es.enter_context(nc.semaphore("e_%s%d" % (e, i))) for i in range(3)]
                 for e in Sched.ENG}
        S = Sched(dsems, esems)
        PS = [Buf(t[:, :], "psf%d" % i, 1) for i, t in enumerate(psf)]
        PT = [Buf(t[:, :], "pst%d" % i, 1) for i, t in enumerate(pst)]
        psr = [0]

        def nps():
            b = PS[psr[0] % 4]
            psr[0] += 1
            return b
        PSS = PS[4]
        PSM = PS[5]

        class Carver:
            def __init__(self, start):
                self.off = start

            def get(self, dtype, free_shape, name="", nparts=1):
                n = int(np.prod(free_shape))
                nbytes = n * (2 if dtype == BF16 else 4)
                nbytes = (nbytes + 63) // 64 * 64
                w0 = self.off // 4
                w1 = (self.off + nbytes) // 4
                assert w1 <= AW, ("arena overflow", name, self.off + nbytes)
                ap = arena[:, w0:w1]
                if dtype != F32:
                    ap = ap.bitcast(dtype)
                ap = ap[:, 0:n]
                if len(free_shape) == 2:
                    ap = ap.rearrange("p (a b) -> p a b", a=free_shape[0])
                elif len(free_shape) == 3:
                    ap = ap.rearrange("p (a b c) -> p a b c", a=free_shape[0], b=free_shape[1])
                self.off += nbytes
                return Buf(ap, name, nparts)

        cv = Carver(0)
        pv = cv.get(F32, [RV], "pv")
        cst = cv.get(F32, [CW], "cst")
        flag = cv.get(F32, [1], "flag")
        identb = cv.get(BF16, [P], "identb")
        onesb = cv.get(BF16, [P], "onesb")
        onesf = cv.get(F32, [P], "onesf")
        mask01 = cv.get(BF16, [P], "mask01")
        modT = cv.get(F32, [48], "modT")
        gsc1 = cv.get(F32, [8], "gsc1")
        gsc2 = cv.get(F32, [8], "gsc2")
        lb = cv.get(F32, [8], "lb")
        oml = cv.get(F32, [8], "oml")
        sc = cv.get(F32, [8], "sc")
        ga1_bc = cv.get(F32, [D], "ga1_bc")
        ga2_bc = cv.get(F32, [D], "ga2_bc")
        gfin_bc = cv.get(F32, [D], "gfin_bc")
        br_bc = cv.get(F32, [NE], "br_bc")
        gates = cv.get(F32, [NTOK // P, NE], "gates", NTOK // P)
        wr = cv.get(BF16, [KC, NE], "wr")
        b2b = cv.get(BF16, [D], "b2b")
        small = cv.get(F32, [64], "small", 64)
        gsc2_bc = cv.get(F32, [D], "gsc2_bc")
        sh2_bc = cv.get(F32, [D], "sh2_bc")
        lgts = cv.get(F32, [NTOK // P, NE], "lgts", NTOK // P)
        mx4 = cv.get(F32, [NTOK // P, 4], "mx4", NTOK // P)
        gk = cv.get(F32, [NTOK // P, 4], "gk", NTOK // P)
        desti = cv.get(I32, [NTOK // P, 4], "desti")
        widx = cv.get(I32, [NB, KC], "widx")
        bef = cv.get(F32, [NB], "bef")
        shared_end = cv.off
        ident_f = cst.ap[:, 0:128]
        maskf = cst.ap[:, 128:256]
        resetm = cst.ap[:, 256:512]
        ustrict = cst.ap[:, 512:640]
        iotab = cst.ap[:, 640:704]
        iotae = cst.ap[:, 704:736]
        basekp = cst.ap[:, 736:744]

        mv = Carver(shared_end)
        xt = mv.get(F32, [NS, D], "xt", NS)
        junk = mv.get(BF16, [D], "junk")
        xn = mv.get(BF16, [NS, D], "xn", NS)
        hT = mv.get(BF16, [KC, TT], "hT", 8)
        setup_off = mv.off
        wslot = [mv.get(BF16, [KC, 512], "wslot%d" % i) for i in range(3)]
        uX = [mv.get(BF16, [KC, UW], "uX%d" % i, 8) for i in range(2)]
        FA = mv.get(F32, [KC, TT], "FA", 8)
        FB = mv.get(F32, [KC, TT], "FB", 8)
        FC = mv.get(F32, [KC, TT], "FC", 8)
        sgb = mv.get(BF16, [KC, TT], "sgb", 8)
        m1 = mv.get(BF16, [KC, TT], "m1", 8)
        QE = mv.get(BF16, [KC, TT], "QE", 8)
        QO = mv.get(BF16, [KC, TT], "QO", 8)
        kT = mv.get(BF16, [KC, TT], "kT", 8)
        sog = mv.get(BF16, [KC, TT], "sog", 8)
        sgc = mv.get(BF16, [KC, TT], "sgc", 8)
        sgh = mv.get(BF16, [KC, TT], "sgh", 8)
        osq = mv.get(BF16, [KC, TT], "osq", 8)
        mT = mv.get(BF16, [KC, TT], "mT", 8)
        h2t = mv.get(BF16, [KC, TT], "h2t", 8)
        vtm = mv.get(BF16, [NS, D], "vtm", NS)
        KE = mv.get(BF16, [NS, KC, P], "KE", NS)
        KO = mv.get(BF16, [NS, KC, P], "KO", NS)
        AT = [mv.get(BF16, [4, P], "AT%d" % i, 4) for i in range(2)]
        SA = mv.get(BF16, [KC, P], "SA", 8)
        SB = mv.get(BF16, [KC, P], "SB", 8)
        tmpa = mv.get(F32, [512], "tmpa")
        st_mean = mv.get(F32, [TT], "st_mean")
        st_var = mv.get(F32, [TT], "st_var")
        st_t = mv.get(F32, [TT], "st_t")
        lgt = mv.get(F32, [NE], "lgt")
        dgs = [mv.get(BF16, [31, P], "dgs%d" % i) for i in range(2)]
        mx8 = mv.get(F32, [8], "mx8")
        egt = mv.get(F32, [NE], "egt")
        mixer_end = mv.off
        sv = Carver(setup_off)
        scb = sv.get(F32, [KC, P], "scb")
        wada = [sv.get(F32, [KC, 512], "wada%d" % i) for i in range(2)]
        bada_bc = sv.get(F32, [D], "bada_bc")
        dgtmp = sv.get(BF16, [31, P], "dgtmp")
        DgB = Buf(None, "dg_dram")

        ev = Carver(shared_end)
        h2q = ev.get(BF16, [KC, QT], "h2q")
        acc = ev.get(F32, [QT // P, D], "acc", 2 * QT // P)
        W1 = [ev.get(BF16, [KC, 512], "W1_%d" % i) for i in range(4)]
        W2 = ev.get(BF16, [KC, D], "W2")
        actT = [ev.get(BF16, [KC, 512], "actT%d" % i, 8) for i in range(2)]
        tg = [ev.get(F32, [512], "tg%d" % i) for i in range(2)]
        tsg = [ev.get(F32, [512], "tsg%d" % i) for i in range(2)]
        tl = [ev.get(F32, [512], "tl%d" % i) for i in range(2)]
        x1t = [ev.get(F32, [D], "x1t%d" % i) for i in range(2)]
        xo = ev.get(F32, [D], "xo")
        ejunk = ev.get(BF16, [D], "ejunk")
        gTb = ev.get(BF16, [P], "gTb")
        moe_end = ev.off
        print("arena bytes: shared", shared_end, "mixer", mixer_end, "moe", moe_end)

        def V(fn, r=(), w=()):
            return S.op("dve", fn, r, w)

        def A(fn, r=(), w=()):
            return S.op("act", fn, r, w)

        def G(fn, r=(), w=()):
            return S.op("pool", fn, r, w)

        def T(fn, r=(), w=()):
            return S.op("pe", fn, r, w)

        def DMA(q, fn, r=(), w=()):
            return S.op(q, fn, r, w, dma=True)

        def dump(name, buf, ap=None):
            if not dbg:
                return
            ap = buf.ap if ap is None else ap
            shp = list(ap.shape)
            dtn = nc.dram_tensor("dbg_" + name, shp, ap.dtype, kind="ExternalOutput").ap()
            DMA("sp", lambda e: e.dma_start(out=dtn, in_=ap), r=[buf])
            DBG.append("dbg_" + name)

        def barrier(bufs):
            last = {e: S.ops[e][-1] for e in ("act", "dve", "pool", "pe") if S.ops[e]}
            bb = Buf(None, "barrier")
            for e, o in last.items():
                bb.parts[0].w[e] = o
            for x in S.dma_last.values():
                bb.parts[0].w[("d", id(x))] = x
            for e in ("act", "dve", "pool", "pe", "sp"):
                S.op(e, (lambda en: en.nop()), r=[bb])

        DMA("sp", lambda e: e.dma_start(out=pv.ap, in_=pvec_d), w=[pv])
        DMA("sp", lambda e: e.dma_start(out=cst.ap, in_=cst_d), w=[cst])
        DMA("sp", lambda e: e.dma_start(out=flag.ap, in_=flag_d), w=[flag])
        DMA("sp", lambda e: e.dma_start(out=gfin_bc.ap, in_=gfin_d.broadcast_to([P, D])), w=[gfin_bc])
        DMA("sp", lambda e: e.dma_start(out=br_bc.ap, in_=br_d.broadcast_to([P, NE])), w=[br_bc])
        DMA("pool", lambda e: e.dma_start(out=wr.ap, in_=wr_d.rearrange("(k p) n -> p k n", p=P)), w=[wr])
        DMA("pool", lambda e: e.dma_start(out=b2b.ap[0:NE, :], in_=b2_d), w=[b2b])
        V(lambda e: e.tensor_copy(out=identb.ap, in_=ident_f), r=[cst], w=[identb])
        V(lambda e: e.tensor_copy(out=mask01.ap, in_=maskf), r=[cst], w=[mask01])
        V(lambda e: e.memset(onesb.ap, 1.0), w=[onesb])
        V(lambda e: e.memset(onesf.ap, 1.0), w=[onesf])
        V(lambda e: e.memset(gates.ap, 0.0), w=[gates])
        V(lambda e: e.memset(lgts.ap, 0.0), w=[lgts])
        V(lambda e: e.memset(mx4.ap, 0.0), w=[mx4])
        V(lambda e: e.memset(gk.ap, 0.0), w=[gk])
        A(lambda e: e.activation(out=sc.ap, in_=pv.ap[:, C_C:C_C + 8], func=AF.Silu), r=[pv], w=[sc])
        V(lambda e: e.tensor_copy(out=scb.ap, in_=sc.ap.unsqueeze(2).broadcast_to([P, KC, P])), r=[sc], w=[scb])
        V(lambda e: e.tensor_tensor(out=small.ap[:, 0:8], in0=pv.ap[:, C_LB0:C_LB0 + 8],
                                    in1=pv.ap[:, C_LB1:C_LB1 + 8], op=ALU.subtract), r=[pv], w=[small[slice(0, 8)]])
        A(lambda e: e.activation(out=lb.ap, in_=small.ap[:, 0:8], func=AF.Sigmoid), r=[small[slice(0, 8)]], w=[lb])
        V(lambda e: e.tensor_scalar(out=oml.ap, in0=lb.ap, scalar1=-1.0, scalar2=1.0, op0=ALU.mult, op1=ALU.add),
          r=[lb], w=[oml])
        wada_v = w_ada.rearrange("(k p) n -> p k n", p=P)
        for g in range(12):
            wb = wada[g % 2]
            DMA("sp", lambda e, g=g, wb=wb: e.dma_start(out=wb.ap, in_=wada_v[:, :, g * 512:(g + 1) * 512]), w=[wb])
            for cc in range(4):
                j = g * 4 + cc
                for k in range(KC):
                    T(lambda e, j=j, k=k, cc=cc, wb=wb: e.matmul(PSM.ap[:, j:j + 1], lhsT=wb.ap[:, k, cc * 128:(cc + 1) * 128],
                                                             rhs=sc.ap[:, k:k + 1], start=(k == 0), stop=(k == KC - 1)),
                      r=[wb, sc], w=[PSM])
            if g in (4, 5, 6, 7, 8, 9, 10, 11):
                ps = nps()
                dst = {2: ga1_bc, 3: sh2_bc, 4: gsc2_bc, 5: ga2_bc}[g // 2]
                hh = g % 2
                for k in range(KC):
                    T(lambda e, k=k, wb=wb, ps=ps: e.matmul(ps.ap, lhsT=scb.ap[:, k, :], rhs=wb.ap[:, k, :],
                                                          start=(k == 0), stop=(k == KC - 1)), r=[wb, scb], w=[ps])
                DMA("sp", lambda e, g=g: e.dma_start(out=bada_bc.ap[:, 0:512],
                                                    in_=b_ada[:, g * 512:(g + 1) * 512].broadcast_to([P, 512])), w=[bada_bc])
                V(lambda e, ps=ps, dst=dst, hh=hh: e.tensor_tensor(out=dst.ap[:, hh * 512:(hh + 1) * 512], in0=ps.ap,
                                                                 in1=bada_bc.ap[:, 0:512], op=ALU.add),
                  r=[ps, bada_bc], w=[dst])
        V(lambda e: e.tensor_tensor(out=modT.ap, in0=PSM.ap[:, 0:48], in1=pv.ap[:, C_BADA:C_BADA + 48], op=ALU.add),
          r=[PSM, pv], w=[modT])
        V(lambda e: e.scalar_tensor_tensor(out=gsc1.ap, in0=modT.ap[:, 8:16], scalar=1.0, in1=pv.ap[:, C_GMIX:C_GMIX + 8],
                                           op0=ALU.add, op1=ALU.mult), r=[modT, pv], w=[gsc1])
        DMA("sp", lambda e: e.dma_start(out=bada_bc.ap, in_=gffn_d.broadcast_to([P, D])), w=[bada_bc])
        V(lambda e: e.scalar_tensor_tensor(out=gsc2_bc.ap, in0=gsc2_bc.ap, scalar=1.0, in1=bada_bc.ap, op0=ALU.add, op1=ALU.mult),
          r=[gsc2_bc, bada_bc], w=[gsc2_bc])
        V(lambda e: e.scalar_tensor_tensor(out=gsc2.ap, in0=modT.ap[:, 32:40], scalar=1.0, in1=pv.ap[:, C_GFFN:C_GFFN + 8],
                                           op0=ALU.add, op1=ALU.mult), r=[modT, pv], w=[gsc2])

        for c in range(KC):
            V(lambda e, c=c: e.tensor_tensor(out=dgtmp.ap, in0=identb.ap.unsqueeze(1).broadcast_to([P, 31, P]),
                                             in1=pv.ap[:, C_DW + c * 31:C_DW + (c + 1) * 31].unsqueeze(2).broadcast_to([P, 31, P]), op=ALU.mult),
              r=[identb, pv], w=[dgtmp])
            DMA("sp", lambda e, c=c: e.dma_start(out=dg_d[c], in_=dgtmp.ap), r=[dgtmp], w=[DgB])
        dump("modT", modT)
        dump("ga1", ga1_bc)
        dump("ga2", ga2_bc)
        dump("lb", lb)
        dump("gsc1", gsc1)
        barrier(None)
        V(lambda e: e.memset(QE.ap, 0.0), w=[QE])
        V(lambda e: e.memset(QO.ap, 0.0), w=[QO])
        V(lambda e: e.memset(KE.ap, 0.0), w=[KE])
        V(lambda e: e.memset(KO.ap, 0.0), w=[KO])
        V(lambda e: e.memset(SA.ap, 0.0), w=[SA])
        V(lambda e: e.memset(uX[0].ap, 0.0), w=[uX[0]])
        V(lambda e: e.memset(uX[1].ap, 0.0), w=[uX[1]])
        win_v = w_in.rearrange("(k p) n -> p k n", p=P)
        wco_v = wco_d.rearrange("(k p) n -> p k n", p=P)
        who_v = who_d.rearrange("(k p) n -> p k n", p=P)
        wout_v = wout_d.rearrange("(k p) n -> p k n", p=P)
        wq = {"n": 0, "pending": []}

        WbB = Buf(None, "wbf_dram", 22)

        def wsrc32(kind, g):
            if kind == "in":
                return win_v[:, :, g * 512:(g + 1) * 512]
            v = {"co": wco_v, "ho": who_v, "out": wout_v}[kind]
            return v[:, :, g * 512:(g + 1) * 512]

        def wsrc(kind, g):
            return {"in": 0, "co": 16, "ho": 18, "out": 20}[kind] + g

        gl_all = [("in", g) for g in (6, 7, 8, 9, 2, 3, 0, 1, 4, 5, 10, 11, 12, 13, 14, 15)] + \
                 [("co", 0), ("co", 1), ("ho", 0), ("ho", 1), ("out", 0), ("out", 1)]
        for n_, (k_, g_) in enumerate(gl_all):
            sl_ = wslot[n_ % 3]
            gid_ = wsrc(k_, g_)
            DMA("pool", lambda e, sl_=sl_, k_=k_, g_=g_: e.dma_start(out=sl_.ap, in_=wsrc32(k_, g_)), w=[sl_])
            DMA("sp", lambda e, sl_=sl_, gid_=gid_: e.dma_start(out=wbf_d[gid_].rearrange("p (k n) -> p k n", k=KC), in_=sl_.ap),
                r=[sl_], w=[WbB[gid_]])

        def wissue(src):
            slot = wslot[wq["n"] % 3]
            wq["n"] += 1
            if os.environ.get("KWMIX") == "none" and wq["n"] > 3:
                pass
            else:
                DMA("pool", lambda e, slot=slot, src=src: e.dma_start(out=slot.ap, in_=wbf_d[src].rearrange("p (k n) -> p k n", k=KC)),
                    r=[WbB[src]], w=[slot])
            wq["pending"].append(slot)

        def wnext():
            return wq["pending"].pop(0)

        def tile_groups(prefix, lastp):
            gl = []
            if (not prefix) or lastp:
                gl += [("in", 2), ("in", 3), ("in", 0), ("in", 1)]
            if CUT <= 3:
                return gl
            gl += [("in", 6), ("in", 7)]
            if CUT <= 4:
                return gl
            if not prefix:
                gl += [("in", 4), ("in", 5)]
            gl += [("in", 8), ("in", 9)]
            if not prefix:
                gl += [("in", 10), ("in", 11), ("in", 12), ("in", 13), ("in", 14), ("in", 15),
                       ("co", 0), ("co", 1)]
                if CUT > 6:
                    gl += [("ho", 0), ("ho", 1), ("out", 0), ("out", 1)]
            return gl

        tiles = [(True, t == NTILE - 1, t) for t in range(NTILE)] + [(False, False, t) for t in range(NTILE)]
        allg = []
        for (pf, lp, t) in tiles:
            allg += [wsrc(k, g) for (k, g) in tile_groups(pf, lp)]
        gi = {"i": 0}

        def wget():
            while gi["i"] < len(allg) and len(wq["pending"]) < 3:
                wissue(allg[gi["i"]])
                gi["i"] += 1
            return wnext()

        def fm_group(ws, cbase, evac):
            for cc in range(4):
                ps = nps()
                for k in range(KC):
                    T(lambda e, ps=ps, k=k, cc=cc: e.matmul(ps.ap[:, 0:TT], lhsT=ws.ap[:, k, cc * 128:(cc + 1) * 128],
                                                          rhs=hT.ap[:, k, :], start=(k == 0), stop=(k == KC - 1)),
                      r=[ws, hT], w=[ps])
                evac(ps, cbase + cc)

        def rms_to_T(src, dstT, scale_ap, bias_ap, scale_b, bias_b):
            for j in range(NS):
                A(lambda e, j=j: e.activation(out=junk.ap, in_=src.ap[:, j, :], func=AF.Square,
                                              accum_out=small.ap[:, 8 + j:9 + j]), r=[src[j]], w=[junk, small[8 + j]])
            A(lambda e: e.activation(out=small.ap[:, 12:12 + NS], in_=small.ap[:, 8:8 + NS], func=AF.Sqrt,
                                     scale=1.0 / D, bias=EPS), r=[small[slice(8, 8 + NS)]], w=[small[slice(12, 12 + NS)]])
            V(lambda e: e.reciprocal(out=small.ap[:, 16:16 + NS], in_=small.ap[:, 12:12 + NS]), r=[small[slice(12, 12 + NS)]], w=[small[slice(16, 16 + NS)]])
            for j in range(NS):
                V(lambda e, j=j: e.tensor_scalar(out=xn.ap[:, j, :], in0=src.ap[:, j, :], scalar1=small.ap[:, 16 + j:17 + j],
                                                 scalar2=None, op0=ALU.mult), r=[src[j], small[16 + j]], w=[xn[j]])
            for k in range(KC):
                pt = PT[k % 2]
                for j in range(NS):
                    T(lambda e, pt=pt, j=j, k=k: e.transpose(pt.ap[:, j * P:(j + 1) * P], xn.ap[:, j, k * P:(k + 1) * P],
                                                            identb.ap), r=[xn[j], identb], w=[pt[j]])
                A(lambda e, pt=pt, k=k: e.activation(out=dstT.ap[:, k, :], in_=pt.ap[:, 0:TT], func=AF.Identity,
                                                     scale=scale_ap[:, k:k + 1], bias=bias_ap[:, k:k + 1]),
                  r=[pt[slice(0, NS)], scale_b, bias_b], w=[dstT[k]])

        def mixer_tile(prefix, lastp, t):
            src = xp if prefix else xm
            tok0 = t * TT
            ucur = uX[t % 2]
            uprev = uX[(t + 1) % 2]
            do_conv_in = (not prefix) or lastp
            DMA("sp", lambda e: e.dma_start(out=xt.ap, in_=src[tok0:tok0 + TT, :].rearrange("(j p) d -> p j d", p=P)), w=[xt])
            rms_to_T(xt, hT, gsc1.ap, modT.ap[:, 0:8], gsc1, modT)
            if CUT <= 1:
                return
            dm = dbg and (not prefix) and t == 0
            if dm:
                dump("hT", hT)
            if do_conv_in:
                V(lambda e: e.tensor_copy(out=ucur.ap[:, :, 0:HALO], in_=uprev.ap[:, :, TT:TT + HALO]), r=[uprev], w=[ucur])
                for half in range(2):
                    ws = wget()
                    fm_group(ws, half * 4, lambda ps, c: A(
                        lambda e, ps=ps, c=c: e.activation(out=sgb.ap[:, c, :], in_=ps.ap[:, 0:TT], func=AF.Sigmoid),
                        r=[ps], w=[sgb[c]]))
                if dm:
                    dump("sgb0", sgb)
                for half in range(2):
                    ws = wget()
                    if dm:
                        dump("ws%d" % half, ws)
                    fm_group(ws, half * 4, lambda ps, c: V(
                        lambda e, ps=ps, c=c: e.tensor_tensor(out=ucur.ap[:, c, HALO:HALO + TT], in0=ps.ap[:, 0:TT],
                                                              in1=sgb.ap[:, c, :], op=ALU.mult), r=[ps, sgb[c]], w=[ucur[c]]))
                if prefix:
                    V(lambda e: e.tensor_scalar(out=ucur.ap[:, :, TT:TT + HALO], in0=ucur.ap[:, :, TT:TT + HALO],
                                                scalar1=flag.ap[:, 0:1], scalar2=None, op0=ALU.mult), r=[ucur, flag], w=[ucur])
            if CUT <= 2:
                return
            if not prefix:
                if os.environ.get("KCONV") == "dve":
                    for c in range(KC):
                        V(lambda e, c=c: e.tensor_scalar(out=FA.ap[:, c, :], in0=ucur.ap[:, c, 0:TT],
                                                         scalar1=pv.ap[:, C_DW + c * 31:C_DW + c * 31 + 1],
                                                         scalar2=pv.ap[:, C_DWB + c:C_DWB + c + 1], op0=ALU.mult, op1=ALU.add),
                          r=[ucur[c], pv], w=[FA[c]])
                    for j in range(1, 31):
                        for c in range(KC):
                            V(lambda e, c=c, j=j: e.scalar_tensor_tensor(out=FA.ap[:, c, :], in0=ucur.ap[:, c, j:j + TT],
                                                                         scalar=pv.ap[:, C_DW + c * 31 + j:C_DW + c * 31 + j + 1],
                                                                         in1=FA.ap[:, c, :], op0=ALU.mult, op1=ALU.add),
                              r=[ucur[c], pv, FA[c]], w=[FA[c]])
                else:
                    for c in range(KC):
                        dg = dgs[c % 2]
                        DMA("sp", lambda e, c=c, dg=dg: e.dma_start(out=dg.ap, in_=dg_d[c]), r=[DgB], w=[dg])
                        ps = nps()
                        for j in range(31):
                            T(lambda e, c=c, j=j, dg=dg, ps=ps: e.matmul(ps.ap[:, 0:TT], lhsT=dg.ap[:, j, :], rhs=ucur.ap[:, c, j:j + TT],
                                                                       start=(j == 0), stop=(j == 30)), r=[dg, ucur[c]], w=[ps])
                        V(lambda e, c=c, ps=ps: e.tensor_scalar(out=FA.ap[:, c, :], in0=ps.ap[:, 0:TT], scalar1=pv.ap[:, C_DWB + c:C_DWB + c + 1],
                                                                scalar2=None, op0=ALU.add), r=[ps, pv], w=[FA[c]])
                A(lambda e: e.activation(out=osq.ap, in_=FA.ap, func=AF.Identity), r=[FA], w=[osq])
                A(lambda e: e.activation(out=sog.ap, in_=FA.ap, func=AF.Square), r=[FA], w=[sog])
                for k in range(KC):
                    T(lambda e, k=k: e.matmul(PSS.ap[:, 0:TT], lhsT=onesb.ap, rhs=osq.ap[:, k, :], start=(k == 0), stop=(k == KC - 1)),
                      r=[onesb, osq[k]], w=[PSS[slice(0, 2)]])
                for k in range(KC):
                    T(lambda e, k=k: e.matmul(PSS.ap[:, TT:2 * TT], lhsT=onesb.ap, rhs=sog.ap[:, k, :], start=(k == 0), stop=(k == KC - 1)),
                      r=[onesb, sog[k]], w=[PSS[slice(2, 4)]])
                V(lambda e: e.tensor_scalar(out=st_mean.ap, in0=PSS.ap[:, 0:TT], scalar1=1.0 / D, scalar2=None, op0=ALU.mult),
                  r=[PSS], w=[st_mean])
                V(lambda e: e.tensor_tensor(out=st_t.ap, in0=st_mean.ap, in1=st_mean.ap, op=ALU.mult), r=[st_mean], w=[st_t])
                V(lambda e: e.scalar_tensor_tensor(out=st_var.ap, in0=PSS.ap[:, TT:2 * TT], scalar=1.0 / D, in1=st_t.ap,
                                                   op0=ALU.mult, op1=ALU.subtract), r=[PSS, st_t], w=[st_var])
                A(lambda e: e.activation(out=st_var.ap, in_=st_var.ap, func=AF.Sqrt, bias=EPS), r=[st_var], w=[st_var])
                V(lambda e: e.reciprocal(out=st_var.ap, in_=st_var.ap), r=[st_var], w=[st_var])
                V(lambda e: e.tensor_tensor(out=FA.ap, in0=FA.ap, in1=st_mean.ap.unsqueeze(1).broadcast_to([P, KC, TT]),
                                            op=ALU.subtract), r=[FA, st_mean], w=[FA])
                V(lambda e: e.tensor_tensor(out=FA.ap, in0=FA.ap, in1=st_var.ap.unsqueeze(1).broadcast_to([P, KC, TT]),
                                            op=ALU.mult), r=[FA, st_var], w=[FA])
                for c in range(KC):
                    A(lambda e, c=c: e.activation(out=sgb.ap[:, c, :], in_=FA.ap[:, c, :], func=AF.Silu,
                                                  scale=pv.ap[:, C_LNG + c:C_LNG + c + 1], bias=pv.ap[:, C_LNB + c:C_LNB + c + 1]),
                      r=[FA[c], pv], w=[sgb[c]])
            if dm:
                dump("ucur", ucur)
                dump("u2T", sgb)
            if CUT <= 3:
                return
            for half in range(2):
                ws = wget()
                fm_group(ws, half * 4, lambda ps, c: A(
                    lambda e, ps=ps, c=c: e.activation(out=FA.ap[:, c, :], in_=ps.ap[:, 0:TT], func=AF.Sigmoid), r=[ps], w=[FA[c]]))
            for c in range(KC):
                V(lambda e, c=c: e.tensor_scalar(out=FA.ap[:, c, :], in0=FA.ap[:, c, :], scalar1=oml.ap[:, c:c + 1],
                                                 scalar2=lb.ap[:, c:c + 1], op0=ALU.mult, op1=ALU.add), r=[FA[c], oml, lb], w=[FA[c]])
            A(lambda e: e.activation(out=FB.ap, in_=FA.ap, func=AF.Ln), r=[FA], w=[FB])
            for c in range(KC):
                V(lambda e, c=c: e.tensor_tensor_scan(out=FC.ap[:, c, :], data0=resetm, data1=FB.ap[:, c, :], initial=0.0,
                                                      op0=ALU.mult, op1=ALU.add), r=[FB[c], cst], w=[FC[c]])
            A(lambda e: e.activation(out=FB.ap, in_=FC.ap, func=AF.Exp), r=[FC], w=[FB])
            A(lambda e: e.activation(out=FC.ap, in_=FC.ap, func=AF.Exp, scale=-1.0), r=[FC], w=[FC])
            V(lambda e: e.scalar_tensor_tensor(out=kT.ap, in0=FA.ap, scalar=1.0, in1=FC.ap, op0=ALU.subtract, op1=ALU.mult),
              r=[FA, FC], w=[kT])
            if CUT <= 4:
                return
            if not prefix:
                for half in range(2):
                    ws = wget()

                    def evq(ps, c):
                        for par, dst in ((0, QE), (1, QO)):
                            V(lambda e, ps=ps, c=c, par=par, dst=dst: e.scalar_tensor_tensor(
                                out=dst.ap[:, c, :].rearrange("p (b two s) -> p b two s", two=2, s=64)[:, :, par, :],
                                in0=ps.ap[:, 0:TT].rearrange("p (b two s) -> p b two s", two=2, s=64)[:, :, par, :],
                                scalar=-(128.0 ** -0.5),
                                in1=FB.ap[:, c, :].rearrange("p (b two s) -> p b two s", two=2, s=64)[:, :, par, :],
                                op0=ALU.mult, op1=ALU.mult), r=[ps, FB[c]], w=[dst[c]])
                    fm_group(ws, half * 4, evq)
            for half in range(2):
                ws = wget()
                for j in range(NS):
                    ps = nps()
                    for k in range(KC):
                        T(lambda e, ps=ps, k=k, j=j, ws=ws: e.matmul(ps.ap, lhsT=hT.ap[:, k, j * P:(j + 1) * P], rhs=ws.ap[:, k, :],
                                                                   start=(k == 0), stop=(k == KC - 1)), r=[hT, ws], w=[ps])
                    A(lambda e, ps=ps, j=j, half=half: e.activation(out=vtm.ap[:, j, half * 512:(half + 1) * 512], in_=ps.ap,
                                                                    func=AF.Identity), r=[ps], w=[vtm[j]])
            if not prefix:
                ws = wget()
                fm_group(ws, 0, lambda ps, c: A(lambda e, ps=ps, c=c: e.activation(out=sog.ap[:, c, :], in_=ps.ap[:, 0:TT], func=AF.Silu), r=[ps], w=[sog[c]]))
                ws = wget()
                fm_group(ws, 4, lambda ps, c: A(lambda e, ps=ps, c=c: e.activation(out=sog.ap[:, c, :], in_=ps.ap[:, 0:TT], func=AF.Silu), r=[ps], w=[sog[c]]))
                for dst in (sgc, sgh):
                    for half in range(2):
                        ws = wget()
                        fm_group(ws, half * 4, lambda ps, c, dst=dst: A(
                            lambda e, ps=ps, c=c, dst=dst: e.activation(out=dst.ap[:, c, :], in_=ps.ap[:, 0:TT], func=AF.Sigmoid),
                            r=[ps], w=[dst[c]]))
                for half in range(2):
                    ws = wget()
                    for cc in range(4):
                        c = half * 4 + cc
                        ps = nps()
                        for k in range(KC):
                            T(lambda e, ps=ps, k=k, cc=cc, ws=ws: e.matmul(ps.ap[:, 0:TT], lhsT=ws.ap[:, k, cc * P:(cc + 1) * P],
                                                                         rhs=sgb.ap[:, k, :], start=(k == 0), stop=(k == KC - 1)),
                              r=[ws, sgb], w=[ps])
                        V(lambda e, ps=ps, c=c: e.tensor_tensor(out=m1.ap[:, c, :], in0=ps.ap[:, 0:TT], in1=sgc.ap[:, c, :], op=ALU.mult),
                          r=[ps, sgc[c]], w=[m1[c]])
            if dm:
                dump("f", FA)
                dump("eG", FB)
                dump("kT", kT)
                dump("QE", QE)
                dump("QO", QO)
                dump("vtm", vtm)
                dump("m1", m1)
                dump("sog", sog)
            if CUT <= 5:
                return
            for b in range(NS):
                blk = slice(b * P, (b + 1) * P)
                pt = PT[b % 2]
                for h in range(KC):
                    T(lambda e, pt=pt, h=h, blk=blk: e.transpose(pt.ap[:, h * P:(h + 1) * P], kT.ap[:, h, blk], identb.ap),
                      r=[kT[h], identb], w=[pt[h]])
                A(lambda e, pt=pt, b=b: e.activation(out=KE.ap[0:64, b, :, :], in_=pt.ap[0:64, :].rearrange("p (h k) -> p h k", h=KC),
                                                     func=AF.Identity), r=[pt], w=[KE[b]])
                V(lambda e, pt=pt, b=b: e.tensor_copy(out=KO.ap[64:128, b, :, :], in_=pt.ap[64:128, :].rearrange("p (h k) -> p h k", h=KC)),
                  r=[pt], w=[KO[b]])
                if not prefix:
                    for hq in range(2):
                        psa = nps()
                        at = AT[hq]
                        for hh in range(4):
                            h = hq * 4 + hh
                            T(lambda e, psa=psa, h=h, hh=hh, blk=blk: e.matmul(psa.ap[:, hh * P:(hh + 1) * P], lhsT=kT.ap[:, h, blk],
                                                                             rhs=QE.ap[:, h, blk], start=True, stop=False),
                              r=[kT[h], QE[h]], w=[psa[hh]])
                            T(lambda e, psa=psa, h=h, hh=hh, blk=blk: e.matmul(psa.ap[:, hh * P:(hh + 1) * P], lhsT=kT.ap[:, h, blk],
                                                                             rhs=QO.ap[:, h, blk], start=False, stop=True),
                              r=[kT[h], QO[h]], w=[psa[hh]])
                        V(lambda e, psa=psa, at=at: e.tensor_tensor(out=at.ap, in0=psa.ap.rearrange("p (h t) -> p h t", h=4),
                                                                    in1=mask01.ap.unsqueeze(1).broadcast_to([P, 4, P]), op=ALU.mult),
                          r=[psa, mask01], w=[at])
                for (Sin, Sout, Kx, par) in ((SA, SB, KE, 0), (SB, SA, KO, 1)):
                    for hq in range(2):
                        PSx = PSS if hq == 0 else PSM
                        at = AT[hq]
                        for hh in range(4):
                            h = hq * 4 + hh
                            T(lambda e, h=h, hh=hh, Sin=Sin, PSx=PSx: e.matmul(PSx.ap[:, hh * P:(hh + 1) * P], lhsT=identb.ap, rhs=Sin.ap[:, h, :],
                                                                               start=True, stop=False), r=[identb, Sin[h]], w=[PSx])
                            T(lambda e, h=h, hh=hh, Kx=Kx, b=b, PSx=PSx: e.matmul(PSx.ap[:, hh * P:(hh + 1) * P], lhsT=Kx.ap[:, b, h, :],
                                                                                  rhs=vtm.ap[:, b, h * P:(h + 1) * P], start=False, stop=True),
                              r=[Kx[b], vtm[b]], w=[PSx])
                        if par == 1 and (not prefix):
                            pso = nps()
                            for hh in range(4):
                                h = hq * 4 + hh
                                T(lambda e, pso=pso, h=h, hh=hh, b=b, at=at: e.matmul(pso.ap[:, hh * P:(hh + 1) * P], lhsT=vtm.ap[:, b, h * P:(h + 1) * P],
                                                                                     rhs=at.ap[:, hh, :], start=True, stop=False),
                                  r=[vtm[b], at[hh]], w=[pso[hh]])
                                T(lambda e, pso=pso, h=h, hh=hh, blk=blk: e.matmul(pso.ap[:, hh * P:(hh + 1) * P], lhsT=SA.ap[:, h, :],
                                                                                 rhs=QE.ap[:, h, blk], start=False, stop=False),
                                  r=[SA[h], QE[h]], w=[pso[hh]])
                                T(lambda e, pso=pso, h=h, hh=hh, blk=blk: e.matmul(pso.ap[:, hh * P:(hh + 1) * P], lhsT=SB.ap[:, h, :],
                                                                                 rhs=QO.ap[:, h, blk], start=False, stop=True),
                                  r=[SB[h], QO[h]], w=[pso[hh]])
                            A(lambda e, pso=pso, hq=hq, blk=blk: e.activation(out=FA.ap[:, hq * 4:hq * 4 + 4, blk],
                                                                              in_=pso.ap.rearrange("p (h t) -> p h t", h=4), func=AF.Identity),
                              r=[pso], w=[FA[slice(hq * 4, hq * 4 + 4)]])
                    for hq in range(2):
                        PSx = PSS if hq == 0 else PSM
                        h0 = hq * 4
                        col = b * P + par * 64 + 63
                        V(lambda e, h0=h0, col=col, Sout=Sout, PSx=PSx: e.tensor_tensor(
                            out=Sout.ap[:, h0:h0 + 4, :], in0=PSx.ap.rearrange("p (h v) -> p h v", h=4),
                            in1=FB.ap[:, h0:h0 + 4, col:col + 1].broadcast_to([P, 4, P]), op=ALU.mult),
                          r=[PSx, FB[slice(h0, h0 + 4)]], w=[Sout[slice(h0, h0 + 4)]])
            if CUT <= 6:
                return
            if prefix:
                if lastp:
                    V(lambda e: e.tensor_scalar(out=SA.ap, in0=SA.ap, scalar1=flag.ap[:, 0:1], scalar2=None, op0=ALU.mult),
                      r=[SA, flag], w=[SA])
                return
            if dm:
                dump("oT", FA)
                dump("SA", SA)
            A(lambda e: e.activation(out=osq.ap, in_=FA.ap, func=AF.Square), r=[FA], w=[osq])
            for hp in range(4):
                ps = nps()
                for i in range(2):
                    h = hp * 2 + i
                    T(lambda e, ps=ps, h=h, i=i: e.matmul(ps.ap[:, i * TT:(i + 1) * TT], lhsT=onesb.ap, rhs=osq.ap[:, h, :], start=True, stop=True),
                      r=[onesb, osq[h]], w=[ps[slice(i * 2, i * 2 + 2)]])
                A(lambda e, ps=ps, hp=hp: e.activation(out=FC.ap[:, hp * 2:hp * 2 + 2, :], in_=ps.ap.rearrange("p (h t) -> p h t", h=2),
                                                       func=AF.Sqrt, scale=1.0 / 128.0, bias=EPS), r=[ps], w=[FC[slice(hp * 2, hp * 2 + 2)]])
            V(lambda e: e.reciprocal(out=FC.ap, in_=FC.ap), r=[FC], w=[FC])
            V(lambda e: e.tensor_tensor(out=FA.ap, in0=FA.ap, in1=FC.ap, op=ALU.mult), r=[FA, FC], w=[FA])
            V(lambda e: e.scalar_tensor_tensor(out=kT.ap, in0=FA.ap, scalar=pv.ap[:, C_NG:C_NG + 1], in1=sog.ap,
                                               op0=ALU.mult, op1=ALU.mult), r=[FA, pv, sog], w=[kT])
            for half in range(2):
                ws = wget()
                for cc in range(4):
                    c = half * 4 + cc
                    ps = nps()
                    for k in range(KC):
                        T(lambda e, ps=ps, k=k, cc=cc, ws=ws: e.matmul(ps.ap[:, 0:TT], lhsT=ws.ap[:, k, cc * P:(cc + 1) * P], rhs=kT.ap[:, k, :],
                                                                     start=(k == 0), stop=(k == KC - 1)), r=[ws, kT], w=[ps])
                    V(lambda e, ps=ps, c=c: e.tensor_tensor(out=mT.ap[:, c, :], in0=ps.ap[:, 0:TT], in1=sgh.ap[:, c, :], op=ALU.mult),
                      r=[ps, sgh[c]], w=[mT[c]])
            V(lambda e: e.tensor_tensor(out=mT.ap, in0=mT.ap, in1=m1.ap, op=ALU.add), r=[mT, m1], w=[mT])
            for half in range(2):
                ws = wget()
                for j in range(NS):
                    ps = nps()
                    for k in range(KC):
                        T(lambda e, ps=ps, k=k, j=j, ws=ws: e.matmul(ps.ap, lhsT=mT.ap[:, k, j * P:(j + 1) * P], rhs=ws.ap[:, k, :],
                                                                   start=(k == 0), stop=(k == KC - 1)), r=[mT, ws], w=[ps])
                    V(lambda e, ps=ps, half=half: e.tensor_tensor(out=tmpa.ap, in0=ps.ap, in1=ga1_bc.ap[:, half * 512:(half + 1) * 512], op=ALU.mult),
                      r=[ps, ga1_bc], w=[tmpa])
                    V(lambda e, j=j, half=half: e.tensor_tensor(out=xt.ap[:, j, half * 512:(half + 1) * 512], in0=tmpa.ap,
                                                                in1=xt.ap[:, j, half * 512:(half + 1) * 512], op=ALU.add), r=[tmpa, xt[j]], w=[xt[j]])
            if dm:
                dump("ogT", kT)
                dump("mT", mT)
                dump("x1", xt)
            if CUT <= 7:
                return
            DMA("sp", lambda e: e.dma_start(out=x1_d[tok0:tok0 + TT, :].rearrange("(j p) d -> p j d", p=P), in_=xt.ap), r=[xt])
            rms_to_T(xt, h2t, gsc2.ap, modT.ap[:, 24:32], gsc2, modT)
            if not SPARSE:
                DMA("sp", lambda e: e.dma_start(out=h2_d.rearrange("p (k t) -> p k t", k=KC)[:, :, tok0:tok0 + TT], in_=h2t.ap), r=[h2t])
            else:
                for j in range(NS):
                    for half in range(2):
                        hs = slice(half * 512, (half + 1) * 512)
                        V(lambda e, j=j, hs=hs: e.scalar_tensor_tensor(out=tmpa.ap, in0=xt.ap[:, j, hs], scalar=small.ap[:, 16 + j:17 + j],
                                                                       in1=gsc2_bc.ap[:, hs], op0=ALU.mult, op1=ALU.mult),
                          r=[xt[j], small[16 + j], gsc2_bc], w=[tmpa])
                        V(lambda e, j=j, hs=hs: e.tensor_tensor(out=xn.ap[:, j, hs], in0=tmpa.ap, in1=sh2_bc.ap[:, hs], op=ALU.add),
                          r=[tmpa, sh2_bc], w=[xn[j]])
                DMA("sp", lambda e: e.dma_start(out=h2tm_d[tok0:tok0 + TT, :].rearrange("(j p) d -> p j d", p=P), in_=xn.ap), r=[xn])
            for j in range(NS):
                st = t * NS + j
                for k in range(KC):
                    T(lambda e, k=k, j=j: e.matmul(PSM.ap[:, 0:NE], lhsT=h2t.ap[:, k, j * P:(j + 1) * P], rhs=wr.ap[:, k, :],
                                                   start=(k == 0), stop=(k == KC - 1)), r=[h2t, wr], w=[PSM])
                V(lambda e: e.tensor_tensor(out=lgt.ap, in0=PSM.ap[:, 0:NE], in1=br_bc.ap, op=ALU.add), r=[PSM, br_bc], w=[lgt])
                V(lambda e, st=st: e.tensor_copy(out=lgts.ap[:, st, :], in_=lgt.ap), r=[lgt], w=[lgts[st]])
                V(lambda e: e.max(out=mx8.ap, in_=lgt.ap), r=[lgt], w=[mx8])
                V(lambda e: e.tensor_scalar(out=small.ap[:, 24:25], in0=mx8.ap[:, 0:1], scalar1=-1.0, scalar2=None, op0=ALU.mult),
                  r=[mx8], w=[small[24]])
                A(lambda e: e.activation(out=egt.ap, in_=lgt.ap, func=AF.Exp, bias=small.ap[:, 24:25]), r=[lgt, small[24]], w=[egt])
                V(lambda e: e.scalar_tensor_tensor(out=egt.ap, in0=lgt.ap, scalar=mx8.ap[:, 3:4], in1=egt.ap, op0=ALU.is_ge, op1=ALU.mult),
                  r=[lgt, mx8, egt], w=[egt])
                V(lambda e: e.reduce_sum(out=small.ap[:, 25:26], in_=egt.ap, axis=mybir.AxisListType.X), r=[egt], w=[small[25]])
                V(lambda e: e.reciprocal(out=small.ap[:, 26:27], in_=small.ap[:, 25:26]), r=[small[25]], w=[small[26]])
                V(lambda e, st=st: e.tensor_scalar(out=gates.ap[:, st, :], in0=egt.ap, scalar1=small.ap[:, 26:27], scalar2=None, op0=ALU.mult),
                  r=[egt, small[26]], w=[gates[st]])
                V(lambda e, st=st: e.tensor_copy(out=mx4.ap[:, st, :], in_=mx8.ap[:, 0:4]), r=[mx8], w=[mx4[st]])
                A(lambda e, st=st: e.activation(out=gk.ap[:, st, :], in_=mx8.ap[:, 0:4], func=AF.Exp, bias=small.ap[:, 24:25]),
                  r=[mx8, small[24]], w=[gk[st]])
                V(lambda e, st=st: e.tensor_scalar(out=gk.ap[:, st, :], in0=gk.ap[:, st, :], scalar1=small.ap[:, 26:27], scalar2=None, op0=ALU.mult),
                  r=[gk[st], small[26]], w=[gk[st]])

        if stage in (1, 3):
            tiles = [(True, True, NTILE - 1)] + [(False, False, t) for t in range(2 if stage == 1 else 4)]
            allg = []
            for (pf, lp, t) in tiles:
                allg += [wsrc(k, g) for (k, g) in tile_groups(pf, lp)]
        if stage == 0:
            tiles = []
        if os.environ.get("KPRE") == "0":
            tiles = [x for x in tiles if not x[0]]
            allg = []
            for (pf, lp, t) in tiles:
                allg += [wsrc(k, g) for (k, g) in tile_groups(pf, lp)]
        if os.environ.get("KPRE") == "only":
            tiles = [x for x in tiles if x[0]]
            allg = []
            for (pf, lp, t) in tiles:
                allg += [wsrc(k, g) for (k, g) in tile_groups(pf, lp)]
        for (pf, lp, t) in tiles:
            mixer_tile(pf, lp, t)
        if dbg and tiles:
            dump("h2t", h2t)
            dump("gates", gates)

        barrier(None)


        nst = len([1 for (pf, lp, t) in tiles if not pf]) * NS
        NST = NTOK // P
        XsB = Buf(None, "xs_dram", 4 * NST + 1)
        YsB = Buf(None, "ys_dram", NB)
        if stage == 6:
            nst = 0
        if SPARSE and nst > 0:
            rv = Carver(shared_end)
            maskall = rv.get(F32, [NST, NE], "maskall")
            posall = rv.get(F32, [NST, NE], "posall", NST)
            eqt = rv.get(F32, [NST, NE], "eqt")
            cum = rv.get(F32, [NE], "cum")
            nblk = rv.get(F32, [NE], "nblk")
            pend = rv.get(F32, [NE], "pend")
            pstart = rv.get(F32, [NE], "pstart")
            destf = rv.get(F32, [NST, 4], "destf", 4)
            widxf = rv.get(F32, [NB, KC], "widxf")
            oobf = rv.get(F32, [NB], "oobf")
            zt = rv.get(BF16, [4096], "zt")
            hrow = [rv.get(BF16, [D], "hrow%d" % i) for i in range(2)]
            V(lambda e: e.memset(zt.ap, 0.0), w=[zt])
            xs_fill = xs_d.rearrange("(c p q) d -> c p (q d)", p=P, q=4)
            for ci in range(NR // (P * 4)):
                DMA("sp", lambda e, ci=ci: e.dma_start(out=xs_fill[ci], in_=zt.ap), r=[zt], w=[XsB])
            V(lambda e: e.tensor_scalar(out=maskall.ap, in0=gates.ap, scalar1=0.0, scalar2=None, op0=ALU.is_gt), r=[gates], w=[maskall])
            V(lambda e: e.memset(cum.ap, 0.0), w=[cum])
            for st in range(NST):
                T(lambda e, st=st: e.matmul(PSM.ap[:, 0:NE], lhsT=ustrict, rhs=maskall.ap[:, st, :], start=True, stop=False), r=[cst, maskall], w=[PSM])
                T(lambda e: e.matmul(PSM.ap[:, 0:NE], lhsT=onesf.ap, rhs=cum.ap, start=False, stop=True), r=[onesf, cum], w=[PSM])
                A(lambda e, st=st: e.activation(out=posall.ap[:, st, :], in_=PSM.ap[:, 0:NE], func=AF.Identity), r=[PSM], w=[posall[st]])
                V(lambda e, st=st: e.tensor_tensor(out=cum.ap, in0=cum.ap, in1=maskall.ap[:, st, :], op=ALU.add), r=[cum, maskall], w=[cum])
            T(lambda e: e.matmul(PSM.ap[:, 0:NE], lhsT=onesf.ap, rhs=cum.ap, start=True, stop=True), r=[onesf, cum], w=[PSM])
            V(lambda e: e.tensor_copy(out=cum.ap, in_=PSM.ap[:, 0:NE]), r=[PSM], w=[cum])
            V(lambda e: e.memset(nblk.ap, 0.0), w=[nblk])
            for jb in range(NTOK // BLK):
                V(lambda e, jb=jb: e.scalar_tensor_tensor(out=nblk.ap, in0=cum.ap, scalar=float(jb * BLK), in1=nblk.ap, op0=ALU.is_gt, op1=ALU.add),
                  r=[cum, nblk], w=[nblk])
            V(lambda e: e.tensor_scalar(out=nblk.ap, in0=nblk.ap, scalar1=float(BLK), scalar2=None, op0=ALU.mult), r=[nblk], w=[nblk])
            V(lambda e: e.tensor_tensor_scan(out=pend.ap, data0=onesf.ap[:, 0:NE], data1=nblk.ap, initial=0.0, op0=ALU.mult, op1=ALU.add),
              r=[onesf, nblk], w=[pend])
            V(lambda e: e.tensor_tensor(out=pstart.ap, in0=pend.ap, in1=nblk.ap, op=ALU.subtract), r=[pend, nblk], w=[pstart])
            V(lambda e: e.tensor_tensor(out=posall.ap, in0=posall.ap, in1=pstart.ap.unsqueeze(1).broadcast_to([P, NST, NE]), op=ALU.add),
              r=[posall, pstart], w=[posall])
            for k in range(4):
                V(lambda e, k=k: e.tensor_tensor(out=eqt.ap, in0=lgts.ap, in1=mx4.ap[:, :, k:k + 1].broadcast_to([P, NST, NE]), op=ALU.is_equal),
                  r=[lgts, mx4], w=[eqt])
                V(lambda e: e.tensor_tensor(out=eqt.ap, in0=eqt.ap, in1=posall.ap, op=ALU.mult), r=[eqt, posall], w=[eqt])
                V(lambda e, k=k: e.reduce_sum(out=destf.ap[:, :, k], in_=eqt.ap, axis=mybir.AxisListType.X), r=[eqt], w=[destf[k]])
            V(lambda e: e.tensor_copy(out=desti.ap, in_=destf.ap), r=[destf], w=[desti])
            V(lambda e: e.memset(bef.ap, 0.0), w=[bef])
            for ex in range(NE):
                V(lambda e, ex=ex: e.scalar_tensor_tensor(out=bef.ap, in0=iotab, scalar=pend.ap[:, ex:ex + 1], in1=bef.ap, op0=ALU.is_ge, op1=ALU.add),
                  r=[cst, pend, bef], w=[bef])
            V(lambda e: e.tensor_scalar(out=bef.ap, in0=bef.ap, scalar1=float(NE - 1), scalar2=None, op0=ALU.min), r=[bef], w=[bef])
            V(lambda e: e.scalar_tensor_tensor(out=widxf.ap, in0=bef.ap.unsqueeze(2).broadcast_to([P, NB, KC]), scalar=float(D),
                                               in1=basekp.unsqueeze(1).broadcast_to([P, NB, KC]), op0=ALU.mult, op1=ALU.add),
              r=[bef, cst], w=[widxf])
            V(lambda e: e.tensor_scalar(out=oobf.ap, in0=iotab, scalar1=pend.ap[:, NE - 1:NE], scalar2=65536.0, op0=ALU.is_ge, op1=ALU.mult),
              r=[cst, pend], w=[oobf])
            V(lambda e: e.tensor_tensor(out=widxf.ap, in0=widxf.ap, in1=oobf.ap.unsqueeze(2).broadcast_to([P, NB, KC]), op=ALU.add),
              r=[widxf, oobf], w=[widxf])
            V(lambda e: e.tensor_copy(out=widx.ap, in_=widxf.ap), r=[widxf], w=[widx])
            for st in range(nst):
                hr = hrow[st % 2]
                DMA("sp", lambda e, st=st, hr=hr: e.dma_start(out=hr.ap, in_=h2tm_d[st * P:(st + 1) * P, :]), w=[hr])
                for k in range(4):
                    DMA("pool", lambda e, st=st, k=k, hr=hr: e.indirect_dma_start(
                        out=xs_d[:, :], out_offset=bass.IndirectOffsetOnAxis(ap=desti.ap[:, st, k:k + 1], axis=0),
                        in_=hr.ap, in_offset=None), r=[hr, desti, XsB[4 * NST]], w=[XsB[st * 4 + k]])
            if dbg:
                dump("desti", desti)
                dump("bef", bef)
                dump("cnt", cum)
            barrier(None)

            bv = Carver(shared_end)
            W1b = [bv.get(BF16, [KC, 2 * D], "W1b%d" % i, KC) for i in range(2)]
            W2b = [bv.get(BF16, [KC, D], "W2b%d" % i, KC) for i in range(2)]
            xrows = bv.get(BF16, [4, D], "xrows", 4)
            xbTs = [bv.get(BF16, [KC, BLK], "xbT%d" % i, KC) for i in range(2)]
            actB = [bv.get(BF16, [KC, BLK], "actB%d" % i, KC) for i in range(2)]
            tgB = [bv.get(F32, [BLK], "tgB%d" % i) for i in range(2)]
            tsB = [bv.get(F32, [BLK], "tsB%d" % i) for i in range(2)]
            tlB = [bv.get(F32, [BLK], "tlB%d" % i) for i in range(2)]
            ysb = [bv.get(F32, [D], "ysb%d" % i, 2) for i in range(2)]
            oneh = bv.get(F32, [NE], "oneh")
            b1tmp = bv.get(F32, [16, NE], "b1tmp")
            b1sel = [bv.get(F32, [16], "b1sel%d" % i) for i in range(2)]
            print("arena bytes: blocks", bv.off)
            w1_flat = w1_2d
            w2_flat = w2_2d
            b1v = pv.ap[:, C_B1:C_B1 + NE * 16].rearrange("p (e i) -> p i e", i=16)
            ps6 = [0]

            def nps6():
                bk = PS[ps6[0] % 6]
                ps6[0] += 1
                return bk

            bc_cache = {}

            def bc_reg(e):
                if "r" not in bc_cache:
                    bc_cache["r"] = e.to_reg(NE * D - 1)
                return bc_cache["r"]

            def load_w1(b):
                wa = W1b[b % 2]
                for k in range(KC):
                    DMA("pool", lambda e, b=b, k=k, wa=wa: e.indirect_dma_start(
                        out=wa.ap[:, k, :], out_offset=None, in_=w1_flat[:, :],
                        in_offset=bass.IndirectOffsetOnAxis(ap=widx.ap[:, b, k:k + 1], axis=0),
                        bounds_check=bc_reg(e), oob_is_err=False), r=[widx], w=[wa[k]])

            def load_w2(b):
                wb2 = W2b[b % 2]
                for k in range(KC):
                    DMA("pool", lambda e, b=b, k=k, wb2=wb2: e.indirect_dma_start(
                        out=wb2.ap[:, k, :], out_offset=None, in_=w2_flat[:, :],
                        in_offset=bass.IndirectOffsetOnAxis(ap=widx.ap[:, b, k:k + 1], axis=0),
                        bounds_check=bc_reg(e), oob_is_err=False), r=[widx], w=[wb2[k]])

            def prep(b):
                xbT = xbTs[b % 2]
                DMA("sp", lambda e, b=b: e.dma_start(out=xrows.ap, in_=xs_d[b * BLK:(b + 1) * BLK, :].rearrange("(j p) d -> p j d", p=P)),
                    r=[XsB], w=[xrows])
                for k in range(KC):
                    pt = PT[k % 2]
                    for j in range(4):
                        T(lambda e, pt=pt, j=j, k=k: e.transpose(pt.ap[:, j * P:(j + 1) * P], xrows.ap[:, j, k * P:(k + 1) * P], identb.ap),
                          r=[xrows[j], identb], w=[pt])
                    if k % 2 == 0:
                        A(lambda e, pt=pt, k=k, xbT=xbT: e.activation(out=xbT.ap[:, k, :], in_=pt.ap[:, 0:BLK], func=AF.Identity), r=[pt], w=[xbT[k]])
                    else:
                        V(lambda e, pt=pt, k=k, xbT=xbT: e.tensor_copy(out=xbT.ap[:, k, :], in_=pt.ap[:, 0:BLK]), r=[pt], w=[xbT[k]])
                bs = b1sel[b % 2]
                V(lambda e, b=b: e.tensor_scalar(out=oneh.ap, in0=iotae, scalar1=bef.ap[:, b:b + 1], scalar2=None, op0=ALU.is_equal),
                  r=[cst, bef], w=[oneh])
                V(lambda e: e.tensor_tensor(out=b1tmp.ap, in0=b1v, in1=oneh.ap.unsqueeze(1).broadcast_to([P, 16, NE]), op=ALU.mult),
                  r=[pv, oneh], w=[b1tmp])
                V(lambda e, bs=bs: e.reduce_sum(out=bs.ap, in_=b1tmp.ap, axis=mybir.AxisListType.X), r=[b1tmp], w=[bs])
                V(lambda e, bs=bs: e.tensor_scalar(out=bs.ap[:, 8:16], in0=bs.ap[:, 8:16], scalar1=1.0, scalar2=None, op0=ALU.add), r=[bs], w=[bs])

            def w1_piece(b, i):
                wa, xbT, bs, aT = W1b[b % 2], xbTs[b % 2], b1sel[b % 2], actB[b % 2]
                psg = nps6()
                psl = nps6()
                for k in range(KC):
                    T(lambda e, k=k: e.matmul(psg.ap, lhsT=wa.ap[:, k, i * P:(i + 1) * P], rhs=xbT.ap[:, k, :],
                                              start=(k == 0), stop=(k == KC - 1)), r=[wa, xbT], w=[psg])
                for k in range(KC):
                    T(lambda e, k=k: e.matmul(psl.ap, lhsT=wa.ap[:, k, D + i * P:D + (i + 1) * P], rhs=xbT.ap[:, k, :],
                                              start=(k == 0), stop=(k == KC - 1)), r=[wa, xbT], w=[psl])
                a_, b_, c_ = tgB[i % 2], tsB[i % 2], tlB[i % 2]
                V(lambda e: e.tensor_scalar(out=a_.ap, in0=psg.ap, scalar1=bs.ap[:, i:i + 1], scalar2=7.0, op0=ALU.add, op1=ALU.min),
                  r=[psg, bs], w=[a_])
                A(lambda e: e.activation(out=b_.ap, in_=a_.ap, func=AF.Silu, scale=1.702), r=[a_], w=[b_])
                V(lambda e: e.tensor_scalar(out=c_.ap, in0=psl.ap, scalar1=bs.ap[:, 8 + i:9 + i], scalar2=8.0, op0=ALU.add, op1=ALU.min),
                  r=[psl, bs], w=[c_])
                V(lambda e: e.scalar_tensor_tensor(out=aT.ap[:, i, :], in0=c_.ap, scalar=-6.0, in1=b_.ap, op0=ALU.max, op1=ALU.mult),
                  r=[b_, c_], w=[aT[i]])

            def w2_piece(b, g):
                j4, half = g // 2, g % 2
                wb2, aT, yb = W2b[b % 2], actB[b % 2], ysb[j4 % 2]
                ps = nps6()
                for i in range(KC):
                    T(lambda e, i=i: e.matmul(ps.ap, lhsT=aT.ap[:, i, j4 * P:(j4 + 1) * P], rhs=wb2.ap[:, i, half * 512:(half + 1) * 512],
                                              start=(i == 0), stop=(i == KC - 1)), r=[aT, wb2], w=[ps])
                if half == 0:
                    A(lambda e: e.activation(out=yb.ap[:, 0:512], in_=ps.ap, func=AF.Identity, scale=1.0 / 1.702), r=[ps], w=[yb[0]])
                else:
                    A(lambda e: e.activation(out=yb.ap[:, 512:1024], in_=ps.ap, func=AF.Identity, scale=1.0 / 1.702), r=[ps], w=[yb[1]])
                    r0 = b * BLK + j4 * P
                    DMA("sp", lambda e: e.dma_start(out=ys_d[r0:r0 + P, :], in_=yb.ap), r=[yb], w=[YsB[b]])

            nblocks = NB if stage == 99 else int(os.environ.get("KNB", NB))
            if nblocks:
                load_w1(0)
                load_w2(0)
                prep(0)
                if nblocks > 1:
                    load_w1(1)
            for b in range(nblocks):
                for i in range(KC):
                    w1_piece(b, i)
                    if i == 1 and b + 1 < nblocks:
                        prep(b + 1)
                    if b > 0:
                        w2_piece(b - 1, i)
                if b + 2 < nblocks:
                    load_w1(b + 2)
                if b + 1 < nblocks:
                    load_w2(b + 1)
            if nblocks:
                for g in range(8):
                    w2_piece(nblocks - 1, g)
            barrier(None)


            cb = Carver(shared_end)
            Yk2 = [[cb.get(F32, [D], "Yk%d_%d" % (s_, i)) for i in range(4)] for s_ in range(2)]
            accs = cb.get(F32, [D], "accs", 2)
            cx1 = [cb.get(F32, [D], "cx1%d" % i) for i in range(2)]
            cxo = cb.get(F32, [D], "cxo")
            cjunk = cb.get(BF16, [D], "cjunk")
            cgT = cb.get(BF16, [P], "cgT")
            for st in range(nst):
                r0 = st * P
                xb = cx1[st % 2]
                Yk = Yk2[st % 2]
                DMA("sp", lambda e, xb=xb, r0=r0: e.dma_start(out=xb.ap, in_=x1_d[r0:r0 + P, :]), w=[xb])
                for k in range(4):
                    DMA("pool", lambda e, st=st, k=k, Yk=Yk: e.indirect_dma_start(
                        out=Yk[k].ap, out_offset=None, in_=ys_d[:, :],
                        in_offset=bass.IndirectOffsetOnAxis(ap=desti.ap[:, st, k:k + 1], axis=0)), r=[desti, YsB], w=[Yk[k]])
                T(lambda e, st=st: e.transpose(PSM.ap[0:NE, 0:P], gates.ap[:, st, :], ident_f), r=[gates[st], cst], w=[PSM])
                V(lambda e: e.tensor_copy(out=cgT.ap[0:NE, :], in_=PSM.ap[0:NE, 0:P]), r=[PSM], w=[cgT])
                for half in range(2):
                    ps = nps()
                    T(lambda e, ps=ps, half=half: e.matmul(ps.ap, lhsT=cgT.ap[0:NE, :], rhs=b2b.ap[0:NE, half * 512:(half + 1) * 512],
                                                          start=True, stop=True), r=[cgT, b2b], w=[ps])
                    A(lambda e, ps=ps, half=half: e.activation(out=accs.ap[:, half * 512:(half + 1) * 512], in_=ps.ap, func=AF.Identity),
                      r=[ps], w=[accs[half]])
                for k in range(4):
                    V(lambda e, st=st, k=k, Yk=Yk: e.scalar_tensor_tensor(out=accs.ap, in0=Yk[k].ap, scalar=gk.ap[:, st, k:k + 1], in1=accs.ap,
                                                                   op0=ALU.mult, op1=ALU.add), r=[Yk[k], gk[st], accs], w=[accs])
                V(lambda e: e.tensor_tensor(out=cxo.ap, in0=accs.ap, in1=ga2_bc.ap, op=ALU.mult), r=[accs, ga2_bc], w=[cxo])
                V(lambda e, xb=xb: e.tensor_tensor(out=cxo.ap, in0=cxo.ap, in1=xb.ap, op=ALU.add), r=[cxo, xb], w=[cxo])
                A(lambda e: e.activation(out=cjunk.ap, in_=cxo.ap, func=AF.Square, accum_out=small.ap[:, 32:33]), r=[cxo], w=[cjunk, small[32]])
                A(lambda e: e.activation(out=small.ap[:, 33:34], in_=small.ap[:, 32:33], func=AF.Sqrt, scale=1.0 / D, bias=EPS), r=[small[32]], w=[small[33]])
                V(lambda e: e.reciprocal(out=small.ap[:, 34:35], in_=small.ap[:, 33:34]), r=[small[33]], w=[small[34]])
                V(lambda e, xb=xb: e.scalar_tensor_tensor(out=xb.ap, in0=cxo.ap, scalar=small.ap[:, 34:35], in1=gfin_bc.ap, op0=ALU.mult, op1=ALU.mult),
                  r=[cxo, small[34], gfin_bc], w=[xb])
                DMA("sp", lambda e, xb=xb, r0=r0: e.dma_start(out=y_d[r0:r0 + P, :], in_=xb.ap), r=[xb])

        w1_v = w1_d.rearrange("e (k p) n -> e p k n", p=P)
        w2_v = w2_d.rearrange("e (k p) n -> e p k n", p=P)
        h2_v = h2_d.rearrange("p (k t) -> p k t", k=KC)

        def load_w1(e_, i):
            DMA("pool", lambda en: en.dma_start(out=W1[i].ap[:, :, 0:256], in_=w1_v[e_][:, :, i * 256:(i + 1) * 256]), w=[W1[i]])
            DMA("pool", lambda en: en.dma_start(out=W1[i].ap[:, :, 256:512], in_=w1_v[e_][:, :, D + i * 256:D + (i + 1) * 256]), w=[W1[i]])

        def load_w2(e_):
            DMA("pool", lambda en: en.dma_start(out=W2.ap, in_=w2_v[e_]), w=[W2])

        seq = [(q, e_) for q in range(NQ) for e_ in range(NE)]
        if stage <= 1 or SPARSE:
            seq = []
        if stage in (2, 3):
            seq = [(0, e_) for e_ in range(NE)]
        if SPARSE:
            seq = []
        if seq:
            for i in range(4):
                load_w1(0, i)
            load_w2(0)
        for si, (q, e_) in enumerate(seq):
            nxt = seq[si + 1][1] if si + 1 < len(seq) else None
            if e_ == 0:
                DMA("sp", lambda en, q=q: en.dma_start(out=h2q.ap, in_=h2_v[:, :, q * QT:(q + 1) * QT]), w=[h2q])
                for j in range(QT // P):
                    st = q * (QT // P) + j
                    T(lambda en, st=st: en.transpose(PSM.ap[0:NE, 0:P], gates.ap[:, st, :], ident_f), r=[gates[st], cst], w=[PSM])
                    V(lambda en: en.tensor_copy(out=gTb.ap[0:NE, :], in_=PSM.ap[0:NE, 0:P]), r=[PSM], w=[gTb])
                    for half in range(2):
                        ps = nps()
                        T(lambda en, ps=ps, half=half: en.matmul(ps.ap, lhsT=gTb.ap[0:NE, :], rhs=b2b.ap[0:NE, half * 512:(half + 1) * 512],
                                                                start=True, stop=True), r=[gTb, b2b], w=[ps])
                        A(lambda en, ps=ps, j=j, half=half: en.activation(out=acc.ap[:, j, half * 512:(half + 1) * 512], in_=ps.ap, func=AF.Identity),
                          r=[ps], w=[acc[j * 2 + half]])
            for blk in range(QT // 512):
                tsl = slice(blk * 512, (blk + 1) * 512)
                aT = actT[blk % 2]
                for i in range(KC):
                    g4, sub = i // 2, i % 2
                    wb = W1[g4]
                    psg = nps()
                    psl = nps()
                    for k in range(KC):
                        T(lambda en, psg=psg, k=k, wb=wb, sub=sub, tsl=tsl: en.matmul(psg.ap, lhsT=wb.ap[:, k, sub * P:(sub + 1) * P], rhs=h2q.ap[:, k, tsl],
                                                                                 start=(k == 0), stop=(k == KC - 1)), r=[wb, h2q], w=[psg])
                    for k in range(KC):
                        T(lambda en, psl=psl, k=k, wb=wb, sub=sub, tsl=tsl: en.matmul(psl.ap, lhsT=wb.ap[:, k, 256 + sub * P:256 + (sub + 1) * P], rhs=h2q.ap[:, k, tsl],
                                                                                 start=(k == 0), stop=(k == KC - 1)), r=[wb, h2q], w=[psl])
                    if blk == QT // 512 - 1 and sub == 1 and nxt is not None:
                        load_w1(nxt, g4)
                    cg = C_B1 + e_ * 16 + i
                    cl = C_B1 + e_ * 16 + 8 + i
                    a_, b_, c_ = tg[i % 2], tsg[i % 2], tl[i % 2]
                    V(lambda en, psg=psg, cg=cg, a_=a_: en.tensor_scalar(out=a_.ap, in0=psg.ap, scalar1=pv.ap[:, cg:cg + 1], scalar2=7.0, op0=ALU.add, op1=ALU.min),
                      r=[psg, pv], w=[a_])
                    A(lambda en, a_=a_, b_=b_: en.activation(out=b_.ap, in_=a_.ap, func=AF.Sigmoid, scale=1.702), r=[a_], w=[b_])
                    V(lambda en, psl=psl, cl=cl, c_=c_: en.tensor_scalar(out=c_.ap, in0=psl.ap, scalar1=pv.ap[:, cl:cl + 1], scalar2=7.0, op0=ALU.add, op1=ALU.min),
                      r=[psl, pv], w=[c_])
                    G(lambda en, c_=c_: en.tensor_scalar(out=c_.ap, in0=c_.ap, scalar1=-7.0, scalar2=1.0, op0=ALU.max, op1=ALU.add), r=[c_], w=[c_])
                    G(lambda en, a_=a_, b_=b_: en.tensor_tensor(out=a_.ap, in0=a_.ap, in1=b_.ap, op=ALU.mult), r=[a_, b_], w=[a_])
                    V(lambda en, a_=a_, c_=c_, aT=aT, i=i: en.tensor_tensor(out=aT.ap[:, i, :], in0=a_.ap, in1=c_.ap, op=ALU.mult), r=[a_, c_], w=[aT[i]])
                for j4 in range(4):
                    j = blk * 4 + j4
                    st = q * (QT // P) + j
                    for half in range(2):
                        ps = nps()
                        for i in range(KC):
                            T(lambda en, ps=ps, i=i, j4=j4, half=half, aT=aT: en.matmul(ps.ap, lhsT=aT.ap[:, i, j4 * P:(j4 + 1) * P],
                                                                                     rhs=W2.ap[:, i, half * 512:(half + 1) * 512],
                                                                                     start=(i == 0), stop=(i == KC - 1)), r=[aT, W2], w=[ps])
                        V(lambda en, ps=ps, j=j, half=half, st=st, e_=e_: en.scalar_tensor_tensor(
                            out=acc.ap[:, j, half * 512:(half + 1) * 512], in0=ps.ap, scalar=gates.ap[:, st, e_:e_ + 1],
                            in1=acc.ap[:, j, half * 512:(half + 1) * 512], op0=ALU.mult, op1=ALU.add), r=[ps, gates[st], acc[j * 2 + half]], w=[acc[j * 2 + half]])
            if nxt is not None:
                load_w2(nxt)
            if e_ == NE - 1:
                for j in range(QT // P):
                    r0 = q * QT + j * P
                    xb = x1t[j % 2]
                    DMA("sp", lambda en, xb=xb, r0=r0: en.dma_start(out=xb.ap, in_=x1_d[r0:r0 + P, :]), w=[xb])
                    V(lambda en, j=j: en.tensor_tensor(out=xo.ap, in0=acc.ap[:, j, :], in1=ga2_bc.ap, op=ALU.mult), r=[acc[slice(2 * j, 2 * j + 2)], ga2_bc], w=[xo])
                    G(lambda en, xb=xb: en.tensor_tensor(out=xo.ap, in0=xo.ap, in1=xb.ap, op=ALU.add), r=[xo, xb], w=[xo])
                    A(lambda en: en.activation(out=ejunk.ap, in_=xo.ap, func=AF.Square, accum_out=small.ap[:, 32:33]), r=[xo], w=[ejunk, small[32]])
                    A(lambda en: en.activation(out=small.ap[:, 33:34], in_=small.ap[:, 32:33], func=AF.Sqrt, scale=1.0 / D, bias=EPS), r=[small[32]], w=[small[33]])
                    V(lambda en: en.reciprocal(out=small.ap[:, 34:35], in_=small.ap[:, 33:34]), r=[small[33]], w=[small[34]])
                    V(lambda en, xb=xb: en.scalar_tensor_tensor(out=xb.ap, in0=xo.ap, scalar=small.ap[:, 34:35], in1=gfin_bc.ap, op0=ALU.mult, op1=ALU.mult),
                      r=[xo, small[34], gfin_bc], w=[xb])
                    DMA("sp", lambda en, xb=xb, r0=r0: en.dma_start(out=y_d[r0:r0 + P, :], in_=xb.ap), r=[xb])

        S.finish()
        print("ops:", {e: len(v) for e, v in S.ops.items()})
        with nc.Block() as block:
            @block.sync
            def _(e):
                S.run("sp", e)

            @block.scalar
            def _(e):
                S.run("act", e)

            @block.vector
            def _(e):
                S.run("dve", e)

            @block.gpsimd
            def _(e):
                S.run("pool", e)

            @block.tensor
            def _(e):
                S.run("pe", e)
    nc._dbg_names = DBG
    return nc


def _fm(v, n):
    return np.ascontiguousarray(np.asarray(v, np.float32).reshape(n, P).T)


def _consts():
    cst = np.zeros((P, CW), np.float32)
    cst[:, 0:128] = np.eye(P, dtype=np.float32)
    s = np.arange(P)[:, None]
    t = np.arange(P)[None, :]
    cst[:, 128:256] = ((s // 64 == t // 64) & (s <= t)).astype(np.float32)
    rm = np.ones((P, 256), np.float32)
    rm[:, ::64] = 0.0
    cst[:, 256:512] = rm
    cst[:, 512:640] = (s < t).astype(np.float32)
    cst[:, 640:704] = np.arange(NB, dtype=np.float32)[None, :] * BLK
    cst[:, 704:736] = np.arange(NE, dtype=np.float32)[None, :]
    cst[:, 736:744] = np.arange(KC, dtype=np.float32)[None, :] * P + np.arange(P, dtype=np.float32)[:, None]
    return cst


def make_in_maps(x, c, w_ada, b_ada, g_mix, w_in, conv_dw, conv_dw_bias, conv_ln_g, conv_ln_b,
                 w_conv_out, lb_param, hgrn_norm_g, w_hgrn_out, w_out, g_ffn, w_router, b_router,
                 w1, b1, w2, b2, g_final, cores=range(8)):
    f = lambda a: np.ascontiguousarray(np.asarray(a, np.float32))
    x = f(x)
    cst = _consts()
    dwT = np.ascontiguousarray(f(conv_dw)[0].T.reshape(KC, P, 31).transpose(1, 0, 2).reshape(P, KC * 31))
    b1T = np.ascontiguousarray(f(b1)[0].reshape(NE, 16, P).transpose(2, 0, 1).reshape(P, NE * 16))
    common = {
        "cst": cst,
        "w_ada": f(w_ada)[0], "b_ada": f(b_ada)[0:1], "w_in": f(w_in)[0],
        "w_conv_out": f(w_conv_out)[0], "w_hgrn_out": f(w_hgrn_out)[0], "w_out": f(w_out)[0],
        "w_router": f(w_router)[0], "b_router": f(b_router)[0:1],
        "w1": f(w1)[0].reshape(NE * D, 2 * D), "w2": f(w2)[0].reshape(NE * D, D), "b2": f(b2)[0], "g_final": f(g_final).reshape(1, D),
        "g_ffn": f(g_ffn)[0:1],
    }
    in_maps = []
    for core in cores:
        b, half = core // 2, core % 2
        pvec = np.zeros((P, RV), np.float32)
        pvec[:, C_C:C_C + 8] = _fm(np.asarray(c)[b], 8)
        pvec[:, C_BADA:C_BADA + 48] = _fm(np.asarray(b_ada)[0], 48)
        pvec[:, C_GMIX:C_GMIX + 8] = _fm(np.asarray(g_mix)[0], 8)
        pvec[:, C_DWB:C_DWB + 8] = _fm(np.asarray(conv_dw_bias)[0], 8)
        pvec[:, C_LNG:C_LNG + 8] = _fm(np.asarray(conv_ln_g)[0], 8)
        pvec[:, C_LNB:C_LNB + 8] = _fm(np.asarray(conv_ln_b)[0], 8)
        pvec[:, C_LB0:C_LB0 + 8] = _fm(np.asarray(lb_param)[0], 8)
        pvec[:, C_LB1:C_LB1 + 8] = _fm(np.asarray(lb_param)[1], 8)
        pvec[:, C_GFFN:C_GFFN + 8] = _fm(np.asarray(g_ffn)[0], 8)
        pvec[:, C_DW:C_DW + 248] = dwT
        pvec[:, C_B1:C_B1 + 512] = b1T
        pvec[:, C_NG] = np.asarray(hgrn_norm_g, np.float32)[0]
        m = dict(common)
        m["xm"] = np.ascontiguousarray(x[b, half * NTOK:(half + 1) * NTOK])
        m["xp"] = np.ascontiguousarray(x[b, 0:NTOK]) if half == 1 else np.zeros((NTOK, D), np.float32)
        m["flag"] = np.full((P, 1), float(half), np.float32)
        m["pvec"] = pvec
        in_maps.append(m)
    return in_maps


_NC = None


def kernel(**inputs):
    global _NC
    in_maps = make_in_maps(**inputs)
    if _NC is None:
        _NC = build_nc()
    res = run_bass_kernel_spmd(_NC, in_maps, core_ids=list(range(8)))
    out = np.zeros((4, 2 * NTOK, D), np.float32)
    for core in range(8):
        b, half = core // 2, core % 2
        out[b, half * NTOK:(half + 1) * NTOK] = np.asarray(res.results[core]["y"], np.float32)
    return out
```

```python
import os
import numpy as np
import concourse.bass as bass
import concourse.mybir as mybir
from concourse.bass_utils import run_bass_kernel_spmd

F32 = mybir.dt.float32
BF16 = mybir.dt.bfloat16
I32 = mybir.dt.int32
ALU = mybir.AluOpType
AF = mybir.ActivationFunctionType

P = 128
D = 1024
KC = 8
NTOK = 4096
TT = 256
NS = TT // P
NTILE = NTOK // TT
HALO = 30
UW = HALO + TT + 2
NE = 32
QT = 1024
NQ = NTOK // QT
EPS = 1e-6
BLK = 512
NB = 64
NR = NB * BLK
CW = 768
SPARSE = True
C_C, C_BADA, C_GMIX, C_DWB, C_LNG, C_LNB, C_LB0, C_LB1, C_GFFN, C_DW, C_B1, C_NG, RV = (
    0, 8, 56, 64, 72, 80, 88, 96, 104, 112, 360, 872, 876)
SAME_ENG_SYNC = True
CUT = int(os.environ.get('KCUT', '99'))
SUB = int(os.environ.get('KSUB', '99'))
EPOCH = 30000


class Op:
    __slots__ = ("eng", "fn", "deps", "marked", "sem", "val", "dma")


class Part:
    __slots__ = ("w", "r")

    def __init__(self):
        self.w = {}
        self.r = {}


class Buf:
    def __init__(self, ap, name="", nparts=1):
        self.ap = ap
        self.parts = [Part() for _ in range(nparts)]
        self.name = name

    def __getitem__(self, k):
        if len(self.parts) == 1:
            return self
        return (self, k)


def _expand(lst):
    out = {}
    for it in lst:
        if it is None:
            continue
        if isinstance(it, tuple):
            b, k = it
            if isinstance(k, int):
                ps = [b.parts[k]]
            elif isinstance(k, slice):
                ps = b.parts[k]
            else:
                ps = [b.parts[i] for i in k]
        else:
            ps = it.parts
        for p in ps:
            out[id(p)] = p
    return out


class Sched:
    ENG = ("sp", "act", "dve", "pool", "pe")

    def __init__(self, dma_sems, eng_sems):
        self.ops = {e: [] for e in self.ENG}
        self.pool = dma_sems
        self.esem = eng_sems
        self.dma_i = {q: 0 for q in dma_sems}
        self.dma_last = {}

    def op(self, eng, fn, r=(), w=(), dma=False):
        o = Op()
        o.eng, o.fn, o.dma, o.marked, o.deps, o.sem, o.val = eng, fn, dma, False, {}, None, 0
        rp = _expand(r)
        wp = _expand(w)
        for p in rp.values():
            for x in p.w.values():
                o.deps[id(x)] = x
        for p in wp.values():
            for x in p.r.values():
                o.deps[id(x)] = x
            for x in p.w.values():
                o.deps[id(x)] = x
        key = ("d", id(o)) if dma else eng
        for p in wp.values():
            p.w = {key: o}
            p.r = {}
        for k, p in rp.items():
            if k not in wp:
                p.r[key] = o
        if dma:
            pl = self.pool[eng]
            i = self.dma_i[eng] % len(pl)
            self.dma_i[eng] += 1
            prev = self.dma_last.get((eng, i))
            if prev is not None:
                o.deps[id(prev)] = prev
            o.sem = pl[i]
            o.val = (prev.val if prev is not None else 0) + 16
            self.dma_last[(eng, i)] = o
        for d in list(o.deps.values()):
            if (not d.dma) and d.eng == eng and (eng == "pe" or not SAME_ENG_SYNC):
                del o.deps[id(d)]
            else:
                d.marked = True
        self.ops[eng].append(o)
        return o

    def finish(self):
        o = Op()
        o.eng, o.fn, o.dma, o.marked, o.sem, o.val = "sp", (lambda e: e.nop()), False, False, None, 0
        o.deps = {id(x): x for x in self.dma_last.values()}
        self.ops["sp"].append(o)
        for eng in self.ENG:
            cnt = 0
            for q in self.ops[eng]:
                if (not q.dma) and q.marked:
                    q.sem = self.esem[eng][cnt // EPOCH]
                    q.val = cnt % EPOCH + 1
                    cnt += 1

    def run(self, eng, e):
        known = {}
        for o in self.ops[eng]:
            for d in o.deps.values():
                k = d.sem.num
                if known.get(k, 0) >= d.val:
                    continue
                e.wait_ge(d.sem, d.val)
                known[k] = d.val
            ins = o.fn(e)
            if o.dma:
                ins.then_inc(o.sem, 16)
            elif o.marked:
                ins.then_inc(o.sem, 1)


def build_nc(stage=99, dbg=False):
    DBG = []
    nc = bass.Bass("TRN2", target_bir_lowering=False)

    def dram(name, shape, dtype=F32, kind="ExternalInput"):
        return nc.dram_tensor(name, shape, dtype, kind=kind).ap()

    xm = dram("xm", [NTOK, D])
    xp = dram("xp", [NTOK, D])
    flag_d = dram("flag", [P, 1])
    pvec_d = dram("pvec", [P, RV])
    cst_d = dram("cst", [P, CW])
    gffn_d = dram("g_ffn", [1, D])
    w_ada = dram("w_ada", [D, 6 * D])
    b_ada = dram("b_ada", [1, 6 * D])
    w_in = dram("w_in", [D, 8 * D])
    wco_d = dram("w_conv_out", [D, D])
    who_d = dram("w_hgrn_out", [D, D])
    wout_d = dram("w_out", [D, D])
    wr_d = dram("w_router", [D, NE])
    br_d = dram("b_router", [1, NE])
    w1_2d = dram("w1", [NE * D, 2 * D])
    w2_2d = dram("w2", [NE * D, D])
    w1_d = w1_2d.rearrange("(e r) n -> e r n", e=NE)
    w2_d = w2_2d.rearrange("(e r) n -> e r n", e=NE)
    b2_d = dram("b2", [NE, D])
    gfin_d = dram("g_final", [1, D])
    y_d = dram("y", [NTOK, D], F32, "ExternalOutput")
    x1_d = dram("x1_scr", [NTOK, D], F32, "Internal")
    h2_d = dram("h2_scr", [P, KC * NTOK], BF16, "Internal")
    h2tm_d = dram("h2tm_scr", [NTOK, D], BF16, "Internal")
    dg_d = dram("dg_scr", [KC, P, 31, P], BF16, "Internal")
    wbf_d = dram("wbf_scr", [22, P, KC * 512], BF16, "Internal")
    xs_d = dram("xs_scr", [NR, D], BF16, "Internal")
    ys_d = dram("ys_scr", [NR, D], F32, "Internal")

    import contextlib
    es = contextlib.ExitStack()
    with es:
        AW = 52500
        arena = es.enter_context(nc.sbuf_tensor("arena", [P, AW], F32))
        psf = [es.enter_context(nc.psum_tensor("psf%d" % i, [P, 512], F32)) for i in range(6)]
        pst = [es.enter_context(nc.psum_tensor("pst%d" % i, [P, 1024], BF16)) for i in range(2)]
        dsems = {"sp": [es.enter_context(nc.semaphore("dmah%d" % i)) for i in range(20)],
                 "pool": [es.enter_context(nc.semaphore("dmas%d" % i)) for i in range(20)]}
        esems = {e: [es.enter_context(nc.semaphore("e_%s%d" % (e, i))) for i in range(3)]
                 for e in Sched.ENG}
        S = Sched(dsems, esems)
        PS = [Buf(t[:, :], "psf%d" % i, 1) for i, t in enumerate(psf)]
        PT = [Buf(t[:, :], "pst%d" % i, 1) for i, t in enumerate(pst)]
        psr = [0]

        def nps():
            b = PS[psr[0] % 4]
            psr[0] += 1
            return b
        PSS = PS[4]
        PSM = PS[5]

        class Carver:
            def __init__(self, start):
                self.off = start

            def get(self, dtype, free_shape, name="", nparts=1):
                n = int(np.prod(free_shape))
                nbytes = n * (2 if dtype == BF16 else 4)
                nbytes = (nbytes + 63) // 64 * 64
                w0 = self.off // 4
                w1 = (self.off + nbytes) // 4
                assert w1 <= AW, ("arena overflow", name, self.off + nbytes)
                ap = arena[:, w0:w1]
                if dtype != F32:
                    ap = ap.bitcast(dtype)
                ap = ap[:, 0:n]
                if len(free_shape) == 2:
                    ap = ap.rearrange("p (a b) -> p a b", a=free_shape[0])
                elif len(free_shape) == 3:
                    ap = ap.rearrange("p (a b c) -> p a b c", a=free_shape[0], b=free_shape[1])
                self.off += nbytes
                return Buf(ap, name, nparts)

        cv = Carver(0)
        pv = cv.get(F32, [RV], "pv")
        cst = cv.get(F32, [CW], "cst")
        flag = cv.get(F32, [1], "flag")
        identb = cv.get(BF16, [P], "identb")
        onesb = cv.get(BF16, [P], "onesb")
        onesf = cv.get(F32, [P], "onesf")
        mask01 = cv.get(BF16, [P], "mask01")
        modT = cv.get(F32, [48], "modT")
        gsc1 = cv.get(F32, [8], "gsc1")
        gsc2 = cv.get(F32, [8], "gsc2")
        lb = cv.get(F32, [8], "lb")
        oml = cv.get(F32, [8], "oml")
        sc = cv.get(F32, [8], "sc")
        ga1_bc = cv.get(F32, [D], "ga1_bc")
        ga2_bc = cv.get(F32, [D], "ga2_bc")
        gfin_bc = cv.get(F32, [D], "gfin_bc")
        br_bc = cv.get(F32, [NE], "br_bc")
        gates = cv.get(F32, [NTOK // P, NE], "gates", NTOK // P)
        wr = cv.get(BF16, [KC, NE], "wr")
        b2b = cv.get(BF16, [D], "b2b")
        small = cv.get(F32, [64], "small", 64)
        gsc2_bc = cv.get(F32, [D], "gsc2_bc")
        sh2_bc = cv.get(F32, [D], "sh2_bc")
        lgts = cv.get(F32, [NTOK // P, NE], "lgts", NTOK // P)
        mx4 = cv.get(F32, [NTOK // P, 4], "mx4", NTOK // P)
        gk = cv.get(F32, [NTOK // P, 4], "gk", NTOK // P)
        desti = cv.get(I32, [NTOK // P, 4], "desti")
        widx = cv.get(I32, [NB, KC], "widx")
        bef = cv.get(F32, [NB], "bef")
        shared_end = cv.off
        ident_f = cst.ap[:, 0:128]
        maskf = cst.ap[:, 128:256]
        resetm = cst.ap[:, 256:512]
        ustrict = cst.ap[:, 512:640]
        iotab = cst.ap[:, 640:704]
        iotae = cst.ap[:, 704:736]
        basekp = cst.ap[:, 736:744]

        mv = Carver(shared_end)
        xt = mv.get(F32, [NS, D], "xt", NS)
        junk = mv.get(BF16, [D], "junk")
        xn = mv.get(BF16, [NS, D], "xn", NS)
        hT = mv.get(BF16, [KC, TT], "hT", 8)
        setup_off = mv.off
        wslot = [mv.get(BF16, [KC, 512], "wslot%d" % i) for i in range(3)]
        uX = [mv.get(BF16, [KC, UW], "uX%d" % i, 8) for i in range(2)]
        FA = mv.get(F32, [KC, TT], "FA", 8)
        FB = mv.get(F32, [KC, TT], "FB", 8)
        FC = mv.get(F32, [KC, TT], "FC", 8)
        sgb = mv.get(BF16, [KC, TT], "sgb", 8)
        m1 = mv.get(BF16, [KC, TT], "m1", 8)
        QE = mv.get(BF16, [KC, TT], "QE", 8)
        QO = mv.get(BF16, [KC, TT], "QO", 8)
        kT = mv.get(BF16, [KC, TT], "kT", 8)
        sog = mv.get(BF16, [KC, TT], "sog", 8)
        sgc = mv.get(BF16, [KC, TT], "sgc", 8)
        sgh = mv.get(BF16, [KC, TT], "sgh", 8)
        osq = mv.get(BF16, [KC, TT], "osq", 8)
        mT = mv.get(BF16, [KC, TT], "mT", 8)
        h2t = mv.get(BF16, [KC, TT], "h2t", 8)
        vtm = mv.get(BF16, [NS, D], "vtm", NS)
        KE = mv.get(BF16, [NS, KC, P], "KE", NS)
        KO = mv.get(BF16, [NS, KC, P], "KO", NS)
        AT = [mv.get(BF16, [4, P], "AT%d" % i, 4) for i in range(2)]
        SA = mv.get(BF16, [KC, P], "SA", 8)
        SB = mv.get(BF16, [KC, P], "SB", 8)
        tmpa = mv.get(F32, [512], "tmpa")
        st_mean = mv.get(F32, [TT], "st_mean")
        st_var = mv.get(F32, [TT], "st_var")
        st_t = mv.get(F32, [TT], "st_t")
        lgt = mv.get(F32, [NE], "lgt")
        dgs = [mv.get(BF16, [31, P], "dgs%d" % i) for i in range(2)]
        mx8 = mv.get(F32, [8], "mx8")
        egt = mv.get(F32, [NE], "egt")
        mixer_end = mv.off
        sv = Carver(setup_off)
        scb = sv.get(F32, [KC, P], "scb")
        wada = [sv.get(F32, [KC, 512], "wada%d" % i) for i in range(2)]
        bada_bc = sv.get(F32, [D], "bada_bc")
        dgtmp = sv.get(BF16, [31, P], "dgtmp")
        DgB = Buf(None, "dg_dram")

        ev = Carver(shared_end)
        h2q = ev.get(BF16, [KC, QT], "h2q")
        acc = ev.get(F32, [QT // P, D], "acc", 2 * QT // P)
        W1 = [ev.get(BF16, [KC, 512], "W1_%d" % i) for i in range(4)]
        W2 = ev.get(BF16, [KC, D], "W2")
        actT = [ev.get(BF16, [KC, 512], "actT%d" % i, 8) for i in range(2)]
        tg = [ev.get(F32, [512], "tg%d" % i) for i in range(2)]
        tsg = [ev.get(F32, [512], "tsg%d" % i) for i in range(2)]
        tl = [ev.get(F32, [512], "tl%d" % i) for i in range(2)]
        x1t = [ev.get(F32, [D], "x1t%d" % i) for i in range(2)]
        xo = ev.get(F32, [D], "xo")
        ejunk = ev.get(BF16, [D], "ejunk")
        gTb = ev.get(BF16, [P], "gTb")
        moe_end = ev.off
        print("arena bytes: shared", shared_end, "mixer", mixer_end, "moe", moe_end)

        def V(fn, r=(), w=()):
            return S.op("dve", fn, r, w)

        def A(fn, r=(), w=()):
            return S.op("act", fn, r, w)

        def G(fn, r=(), w=()):
            return S.op("pool", fn, r, w)

        def T(fn, r=(), w=()):
            return S.op("pe", fn, r, w)

        def DMA(q, fn, r=(), w=()):
            return S.op(q, fn, r, w, dma=True)

        def dump(name, buf, ap=None):
            if not dbg:
                return
            ap = buf.ap if ap is None else ap
            shp = list(ap.shape)
            dtn = nc.dram_tensor("dbg_" + name, shp, ap.dtype, kind="ExternalOutput").ap()
            DMA("sp", lambda e: e.dma_start(out=dtn, in_=ap), r=[buf])
            DBG.append("dbg_" + name)

        def barrier(bufs):
            last = {e: S.ops[e][-1] for e in ("act", "dve", "pool", "pe") if S.ops[e]}
            bb = Buf(None, "barrier")
            for e, o in last.items():
                bb.parts[0].w[e] = o
            for x in S.dma_last.values():
                bb.parts[0].w[("d", id(x))] = x
            for e in ("act", "dve", "pool", "pe", "sp"):
                S.op(e, (lambda en: en.nop()), r=[bb])

        DMA("sp", lambda e: e.dma_start(out=pv.ap, in_=pvec_d), w=[pv])
        DMA("sp", lambda e: e.dma_start(out=cst.ap, in_=cst_d), w=[cst])
        DMA("sp", lambda e: e.dma_start(out=flag.ap, in_=flag_d), w=[flag])
        DMA("sp", lambda e: e.dma_start(out=gfin_bc.ap, in_=gfin_d.broadcast_to([P, D])), w=[gfin_bc])
        DMA("sp", lambda e: e.dma_start(out=br_bc.ap, in_=br_d.broadcast_to([P, NE])), w=[br_bc])
        DMA("pool", lambda e: e.dma_start(out=wr.ap, in_=wr_d.rearrange("(k p) n -> p k n", p=P)), w=[wr])
        DMA("pool", lambda e: e.dma_start(out=b2b.ap[0:NE, :], in_=b2_d), w=[b2b])
        V(lambda e: e.tensor_copy(out=identb.ap, in_=ident_f), r=[cst], w=[identb])
        V(lambda e: e.tensor_copy(out=mask01.ap, in_=maskf), r=[cst], w=[mask01])
        V(lambda e: e.memset(onesb.ap, 1.0), w=[onesb])
        V(lambda e: e.memset(onesf.ap, 1.0), w=[onesf])
        V(lambda e: e.memset(gates.ap, 0.0), w=[gates])
        V(lambda e: e.memset(lgts.ap, 0.0), w=[lgts])
        V(lambda e: e.memset(mx4.ap, 0.0), w=[mx4])
        V(lambda e: e.memset(gk.ap, 0.0), w=[gk])
        A(lambda e: e.activation(out=sc.ap, in_=pv.ap[:, C_C:C_C + 8], func=AF.Silu), r=[pv], w=[sc])
        V(lambda e: e.tensor_copy(out=scb.ap, in_=sc.ap.unsqueeze(2).broadcast_to([P, KC, P])), r=[sc], w=[scb])
        V(lambda e: e.tensor_tensor(out=small.ap[:, 0:8], in0=pv.ap[:, C_LB0:C_LB0 + 8],
                                    in1=pv.ap[:, C_LB1:C_LB1 + 8], op=ALU.subtract), r=[pv], w=[small[slice(0, 8)]])
        A(lambda e: e.activation(out=lb.ap, in_=small.ap[:, 0:8], func=AF.Sigmoid), r=[small[slice(0, 8)]], w=[lb])
        V(lambda e: e.tensor_scalar(out=oml.ap, in0=lb.ap, scalar1=-1.0, scalar2=1.0, op0=ALU.mult, op1=ALU.add),
          r=[lb], w=[oml])
        wada_v = w_ada.rearrange("(k p) n -> p k n", p=P)
        for g in range(12):
            wb = wada[g % 2]
            DMA("sp", lambda e, g=g, wb=wb: e.dma_start(out=wb.ap, in_=wada_v[:, :, g * 512:(g + 1) * 512]), w=[wb])
            for cc in range(4):
                j = g * 4 + cc
                for k in range(KC):
                    T(lambda e, j=j, k=k, cc=cc, wb=wb: e.matmul(PSM.ap[:, j:j + 1], lhsT=wb.ap[:, k, cc * 128:(cc + 1) * 128],
                                                             rhs=sc.ap[:, k:k + 1], start=(k == 0), stop=(k == KC - 1)),
                      r=[wb, sc], w=[PSM])
            if g in (4, 5, 6, 7, 8, 9, 10, 11):
                ps = nps()
                dst = {2: ga1_bc, 3: sh2_bc, 4: gsc2_bc, 5: ga2_bc}[g // 2]
                hh = g % 2
                for k in range(KC):
                    T(lambda e, k=k, wb=wb, ps=ps: e.matmul(ps.ap, lhsT=scb.ap[:, k, :], rhs=wb.ap[:, k, :],
                                                          start=(k == 0), stop=(k == KC - 1)), r=[wb, scb], w=[ps])
                DMA("sp", lambda e, g=g: e.dma_start(out=bada_bc.ap[:, 0:512],
                                                    in_=b_ada[:, g * 512:(g + 1) * 512].broadcast_to([P, 512])), w=[bada_bc])
                V(lambda e, ps=ps, dst=dst, hh=hh: e.tensor_tensor(out=dst.ap[:, hh * 512:(hh + 1) * 512], in0=ps.ap,
                                                                 in1=bada_bc.ap[:, 0:512], op=ALU.add),
                  r=[ps, bada_bc], w=[dst])
        V(lambda e: e.tensor_tensor(out=modT.ap, in0=PSM.ap[:, 0:48], in1=pv.ap[:, C_BADA:C_BADA + 48], op=ALU.add),
          r=[PSM, pv], w=[modT])
        V(lambda e: e.scalar_tensor_tensor(out=gsc1.ap, in0=modT.ap[:, 8:16], scalar=1.0, in1=pv.ap[:, C_GMIX:C_GMIX + 8],
                                           op0=ALU.add, op1=ALU.mult), r=[modT, pv], w=[gsc1])
        DMA("sp", lambda e: e.dma_start(out=bada_bc.ap, in_=gffn_d.broadcast_to([P, D])), w=[bada_bc])
        V(lambda e: e.scalar_tensor_tensor(out=gsc2_bc.ap, in0=gsc2_bc.ap, scalar=1.0, in1=bada_bc.ap, op0=ALU.add, op1=ALU.mult),
          r=[gsc2_bc, bada_bc], w=[gsc2_bc])
        V(lambda e: e.scalar_tensor_tensor(out=gsc2.ap, in0=modT.ap[:, 32:40], scalar=1.0, in1=pv.ap[:, C_GFFN:C_GFFN + 8],
                                           op0=ALU.add, op1=ALU.mult), r=[modT, pv], w=[gsc2])

        for c in range(KC):
            V(lambda e, c=c: e.tensor_tensor(out=dgtmp.ap, in0=identb.ap.unsqueeze(1).broadcast_to([P, 31, P]),
                                             in1=pv.ap[:, C_DW + c * 31:C_DW + (c + 1) * 31].unsqueeze(2).broadcast_to([P, 31, P]), op=ALU.mult),
              r=[identb, pv], w=[dgtmp])
            DMA("sp", lambda e, c=c: e.dma_start(out=dg_d[c], in_=dgtmp.ap), r=[dgtmp], w=[DgB])
        dump("modT", modT)
        dump("ga1", ga1_bc)
        dump("ga2", ga2_bc)
        dump("lb", lb)
        dump("gsc1", gsc1)
        barrier(None)
        V(lambda e: e.memset(QE.ap, 0.0), w=[QE])
        V(lambda e: e.memset(QO.ap, 0.0), w=[QO])
        V(lambda e: e.memset(KE.ap, 0.0), w=[KE])
        V(lambda e: e.memset(KO.ap, 0.0), w=[KO])
        V(lambda e: e.memset(SA.ap, 0.0), w=[SA])
        V(lambda e: e.memset(uX[0].ap, 0.0), w=[uX[0]])
        V(lambda e: e.memset(uX[1].ap, 0.0), w=[uX[1]])
        win_v = w_in.rearrange("(k p) n -> p k n", p=P)
        wco_v = wco_d.rearrange("(k p) n -> p k n", p=P)
        who_v = who_d.rearrange("(k p) n -> p k n", p=P)
        wout_v = wout_d.rearrange("(k p) n -> p k n", p=P)
        wq = {"n": 0, "pending": []}

        WbB = Buf(None, "wbf_dram", 22)

        def wsrc32(kind, g):
            if kind == "in":
                return win_v[:, :, g * 512:(g + 1) * 512]
            v = {"co": wco_v, "ho": who_v, "out": wout_v}[kind]
            return v[:, :, g * 512:(g + 1) * 512]

        def wsrc(kind, g):
            return {"in": 0, "co": 16, "ho": 18, "out": 20}[kind] + g

        gl_all = [("in", g) for g in range(16)] + [("co", 0), ("co", 1), ("ho", 0), ("ho", 1), ("out", 0), ("out", 1)]
        for n_, (k_, g_) in enumerate(gl_all):
            sl_ = wslot[n_ % 3]
            DMA("pool", lambda e, sl_=sl_, k_=k_, g_=g_: e.dma_start(out=sl_.ap, in_=wsrc32(k_, g_)), w=[sl_])
            DMA("sp", lambda e, sl_=sl_, n_=n_: e.dma_start(out=wbf_d[n_].rearrange("p (k n) -> p k n", k=KC), in_=sl_.ap),
                r=[sl_], w=[WbB[n_]])

        def wissue(src):
            slot = wslot[wq["n"] % 3]
            wq["n"] += 1
            if os.environ.get("KWMIX") == "none" and wq["n"] > 3:
                pass
            else:
                DMA("pool", lambda e, slot=slot, src=src: e.dma_start(out=slot.ap, in_=wbf_d[src].rearrange("p (k n) -> p k n", k=KC)),
                    r=[WbB[src]], w=[slot])
            wq["pending"].append(slot)

        def wnext():
            return wq["pending"].pop(0)

        def tile_groups(prefix, lastp):
            gl = []
            if (not prefix) or lastp:
                gl += [("in", 2), ("in", 3), ("in", 0), ("in", 1)]
            if CUT <= 3:
                return gl
            gl += [("in", 6), ("in", 7)]
            if CUT <= 4:
                return gl
            if not prefix:
                gl += [("in", 4), ("in", 5)]
            gl += [("in", 8), ("in", 9)]
            if not prefix:
                gl += [("in", 10), ("in", 11), ("in", 12), ("in", 13), ("in", 14), ("in", 15),
                       ("co", 0), ("co", 1)]
                if CUT > 6:
                    gl += [("ho", 0), ("ho", 1), ("out", 0), ("out", 1)]
            return gl

        tiles = [(True, t == NTILE - 1, t) for t in range(NTILE)] + [(False, False, t) for t in range(NTILE)]
        allg = []
        for (pf, lp, t) in tiles:
            allg += [wsrc(k, g) for (k, g) in tile_groups(pf, lp)]
        gi = {"i": 0}

        def wget():
            while gi["i"] < len(allg) and len(wq["pending"]) < 3:
                wissue(allg[gi["i"]])
                gi["i"] += 1
            return wnext()

        def fm_group(ws, cbase, evac):
            for cc in range(4):
                ps = nps()
                for k in range(KC):
                    T(lambda e, ps=ps, k=k, cc=cc: e.matmul(ps.ap[:, 0:TT], lhsT=ws.ap[:, k, cc * 128:(cc + 1) * 128],
                                                          rhs=hT.ap[:, k, :], start=(k == 0), stop=(k == KC - 1)),
                      r=[ws, hT], w=[ps])
                evac(ps, cbase + cc)

        def rms_to_T(src, dstT, scale_ap, bias_ap, scale_b, bias_b):
            for j in range(NS):
                A(lambda e, j=j: e.activation(out=junk.ap, in_=src.ap[:, j, :], func=AF.Square,
                                              accum_out=small.ap[:, 8 + j:9 + j]), r=[src[j]], w=[junk, small[8 + j]])
            A(lambda e: e.activation(out=small.ap[:, 12:12 + NS], in_=small.ap[:, 8:8 + NS], func=AF.Sqrt,
                                     scale=1.0 / D, bias=EPS), r=[small[slice(8, 8 + NS)]], w=[small[slice(12, 12 + NS)]])
            V(lambda e: e.reciprocal(out=small.ap[:, 16:16 + NS], in_=small.ap[:, 12:12 + NS]), r=[small[slice(12, 12 + NS)]], w=[small[slice(16, 16 + NS)]])
            for j in range(NS):
                V(lambda e, j=j: e.tensor_scalar(out=xn.ap[:, j, :], in0=src.ap[:, j, :], scalar1=small.ap[:, 16 + j:17 + j],
                                                 scalar2=None, op0=ALU.mult), r=[src[j], small[16 + j]], w=[xn[j]])
            for k in range(KC):
                pt = PT[k % 2]
                for j in range(NS):
                    T(lambda e, pt=pt, j=j, k=k: e.transpose(pt.ap[:, j * P:(j + 1) * P], xn.ap[:, j, k * P:(k + 1) * P],
                                                            identb.ap), r=[xn[j], identb], w=[pt[j]])
                A(lambda e, pt=pt, k=k: e.activation(out=dstT.ap[:, k, :], in_=pt.ap[:, 0:TT], func=AF.Identity,
                                                     scale=scale_ap[:, k:k + 1], bias=bias_ap[:, k:k + 1]),
                  r=[pt[slice(0, NS)], scale_b, bias_b], w=[dstT[k]])

        def mixer_tile(prefix, lastp, t):
            src = xp if prefix else xm
            tok0 = t * TT
            ucur = uX[t % 2]
            uprev = uX[(t + 1) % 2]
            do_conv_in = (not prefix) or lastp
            DMA("sp", lambda e: e.dma_start(out=xt.ap, in_=src[tok0:tok0 + TT, :].rearrange("(j p) d -> p j d", p=P)), w=[xt])
            rms_to_T(xt, hT, gsc1.ap, modT.ap[:, 0:8], gsc1, modT)
            if CUT <= 1:
                return
            dm = dbg and (not prefix) and t == 0
            if dm:
                dump("hT", hT)
            if do_conv_in:
                V(lambda e: e.tensor_copy(out=ucur.ap[:, :, 0:HALO], in_=uprev.ap[:, :, TT:TT + HALO]), r=[uprev], w=[ucur])
                for half in range(2):
                    ws = wget()
                    fm_group(ws, half * 4, lambda ps, c: A(
                        lambda e, ps=ps, c=c: e.activation(out=sgb.ap[:, c, :], in_=ps.ap[:, 0:TT], func=AF.Sigmoid),
                        r=[ps], w=[sgb[c]]))
                if dm:
                    dump("sgb0", sgb)
                for half in range(2):
                    ws = wget()
                    if dm:
                        dump("ws%d" % half, ws)
                    fm_group(ws, half * 4, lambda ps, c: V(
                        lambda e, ps=ps, c=c: e.tensor_tensor(out=ucur.ap[:, c, HALO:HALO + TT], in0=ps.ap[:, 0:TT],
                                                              in1=sgb.ap[:, c, :], op=ALU.mult), r=[ps, sgb[c]], w=[ucur[c]]))
                if prefix:
                    V(lambda e: e.tensor_scalar(out=ucur.ap[:, :, TT:TT + HALO], in0=ucur.ap[:, :, TT:TT + HALO],
                                                scalar1=flag.ap[:, 0:1], scalar2=None, op0=ALU.mult), r=[ucur, flag], w=[ucur])
            if CUT <= 2:
                return
            if not prefix:
                if os.environ.get("KCONV") == "dve":
                    for c in range(KC):
                        V(lambda e, c=c: e.tensor_scalar(out=FA.ap[:, c, :], in0=ucur.ap[:, c, 0:TT],
                                                         scalar1=pv.ap[:, C_DW + c * 31:C_DW + c * 31 + 1],
                                                         scalar2=pv.ap[:, C_DWB + c:C_DWB + c + 1], op0=ALU.mult, op1=ALU.add),
                          r=[ucur[c], pv], w=[FA[c]])
                    for j in range(1, 31):
                        for c in range(KC):
                            V(lambda e, c=c, j=j: e.scalar_tensor_tensor(out=FA.ap[:, c, :], in0=ucur.ap[:, c, j:j + TT],
                                                                         scalar=pv.ap[:, C_DW + c * 31 + j:C_DW + c * 31 + j + 1],
                                                                         in1=FA.ap[:, c, :], op0=ALU.mult, op1=ALU.add),
                              r=[ucur[c], pv, FA[c]], w=[FA[c]])
                else:
                    for c in range(KC):
                        dg = dgs[c % 2]
                        DMA("sp", lambda e, c=c, dg=dg: e.dma_start(out=dg.ap, in_=dg_d[c]), r=[DgB], w=[dg])
                        ps = nps()
                        for j in range(31):
                            T(lambda e, c=c, j=j, dg=dg, ps=ps: e.matmul(ps.ap[:, 0:TT], lhsT=dg.ap[:, j, :], rhs=ucur.ap[:, c, j:j + TT],
                                                                       start=(j == 0), stop=(j == 30)), r=[dg, ucur[c]], w=[ps])
                        V(lambda e, c=c, ps=ps: e.tensor_scalar(out=FA.ap[:, c, :], in0=ps.ap[:, 0:TT], scalar1=pv.ap[:, C_DWB + c:C_DWB + c + 1],
                                                                scalar2=None, op0=ALU.add), r=[ps, pv], w=[FA[c]])
                A(lambda e: e.activation(out=FB.ap, in_=FA.ap, func=AF.Square), r=[FA], w=[FB])
                for k in range(KC):
                    T(lambda e, k=k: e.matmul(PSS.ap[:, 0:TT], lhsT=onesf.ap, rhs=FA.ap[:, k, :], start=(k == 0), stop=(k == KC - 1)),
                      r=[onesf, FA[k]], w=[PSS[slice(0, 2)]])
                for k in range(KC):
                    T(lambda e, k=k: e.matmul(PSS.ap[:, TT:2 * TT], lhsT=onesf.ap, rhs=FB.ap[:, k, :], start=(k == 0), stop=(k == KC - 1)),
                      r=[onesf, FB[k]], w=[PSS[slice(2, 4)]])
                V(lambda e: e.tensor_scalar(out=st_mean.ap, in0=PSS.ap[:, 0:TT], scalar1=1.0 / D, scalar2=None, op0=ALU.mult),
                  r=[PSS], w=[st_mean])
                V(lambda e: e.tensor_tensor(out=st_t.ap, in0=st_mean.ap, in1=st_mean.ap, op=ALU.mult), r=[st_mean], w=[st_t])
                V(lambda e: e.scalar_tensor_tensor(out=st_var.ap, in0=PSS.ap[:, TT:2 * TT], scalar=1.0 / D, in1=st_t.ap,
                                                   op0=ALU.mult, op1=ALU.subtract), r=[PSS, st_t], w=[st_var])
                A(lambda e: e.activation(out=st_var.ap, in_=st_var.ap, func=AF.Sqrt, bias=EPS), r=[st_var], w=[st_var])
                V(lambda e: e.reciprocal(out=st_var.ap, in_=st_var.ap), r=[st_var], w=[st_var])
                V(lambda e: e.tensor_tensor(out=FA.ap, in0=FA.ap, in1=st_mean.ap.unsqueeze(1).broadcast_to([P, KC, TT]),
                                            op=ALU.subtract), r=[FA, st_mean], w=[FA])
                V(lambda e: e.tensor_tensor(out=FA.ap, in0=FA.ap, in1=st_var.ap.unsqueeze(1).broadcast_to([P, KC, TT]),
                                            op=ALU.mult), r=[FA, st_var], w=[FA])
                for c in range(KC):
                    A(lambda e, c=c: e.activation(out=sgb.ap[:, c, :], in_=FA.ap[:, c, :], func=AF.Silu,
                                                  scale=pv.ap[:, C_LNG + c:C_LNG + c + 1], bias=pv.ap[:, C_LNB + c:C_LNB + c + 1]),
                      r=[FA[c], pv], w=[sgb[c]])
            if dm:
                dump("ucur", ucur)
                dump("u2T", sgb)
            if CUT <= 3:
                return
            for half in range(2):
                ws = wget()
                fm_group(ws, half * 4, lambda ps, c: A(
                    lambda e, ps=ps, c=c: e.activation(out=FA.ap[:, c, :], in_=ps.ap[:, 0:TT], func=AF.Sigmoid), r=[ps], w=[FA[c]]))
            for c in range(KC):
                V(lambda e, c=c: e.tensor_scalar(out=FA.ap[:, c, :], in0=FA.ap[:, c, :], scalar1=oml.ap[:, c:c + 1],
                                                 scalar2=lb.ap[:, c:c + 1], op0=ALU.mult, op1=ALU.add), r=[FA[c], oml, lb], w=[FA[c]])
            A(lambda e: e.activation(out=FB.ap, in_=FA.ap, func=AF.Ln), r=[FA], w=[FB])
            for c in range(KC):
                V(lambda e, c=c: e.tensor_tensor_scan(out=FC.ap[:, c, :], data0=resetm, data1=FB.ap[:, c, :], initial=0.0,
                                                      op0=ALU.mult, op1=ALU.add), r=[FB[c], cst], w=[FC[c]])
            A(lambda e: e.activation(out=FB.ap, in_=FC.ap, func=AF.Exp), r=[FC], w=[FB])
            A(lambda e: e.activation(out=FC.ap, in_=FC.ap, func=AF.Exp, scale=-1.0), r=[FC], w=[FC])
            V(lambda e: e.scalar_tensor_tensor(out=kT.ap, in0=FA.ap, scalar=1.0, in1=FC.ap, op0=ALU.subtract, op1=ALU.mult),
              r=[FA, FC], w=[kT])
            if CUT <= 4:
                return
            if not prefix:
                for half in range(2):
                    ws = wget()

                    def evq(ps, c):
                        for par, dst in ((0, QE), (1, QO)):
                            V(lambda e, ps=ps, c=c, par=par, dst=dst: e.scalar_tensor_tensor(
                                out=dst.ap[:, c, :].rearrange("p (b two s) -> p b two s", two=2, s=64)[:, :, par, :],
                                in0=ps.ap[:, 0:TT].rearrange("p (b two s) -> p b two s", two=2, s=64)[:, :, par, :],
                                scalar=-(128.0 ** -0.5),
                                in1=FB.ap[:, c, :].rearrange("p (b two s) -> p b two s", two=2, s=64)[:, :, par, :],
                                op0=ALU.mult, op1=ALU.mult), r=[ps, FB[c]], w=[dst[c]])
                    fm_group(ws, half * 4, evq)
            for half in range(2):
                ws = wget()
                for j in range(NS):
                    ps = nps()
                    for k in range(KC):
                        T(lambda e, ps=ps, k=k, j=j, ws=ws: e.matmul(ps.ap, lhsT=hT.ap[:, k, j * P:(j + 1) * P], rhs=ws.ap[:, k, :],
                                                                   start=(k == 0), stop=(k == KC - 1)), r=[hT, ws], w=[ps])
                    A(lambda e, ps=ps, j=j, half=half: e.activation(out=vtm.ap[:, j, half * 512:(half + 1) * 512], in_=ps.ap,
                                                                    func=AF.Identity), r=[ps], w=[vtm[j]])
            if not prefix:
                ws = wget()
                fm_group(ws, 0, lambda ps, c: A(lambda e, ps=ps, c=c: e.activation(out=sog.ap[:, c, :], in_=ps.ap[:, 0:TT], func=AF.Silu), r=[ps], w=[sog[c]]))
                ws = wget()
                fm_group(ws, 4, lambda ps, c: A(lambda e, ps=ps, c=c: e.activation(out=sog.ap[:, c, :], in_=ps.ap[:, 0:TT], func=AF.Silu), r=[ps], w=[sog[c]]))
                for dst in (sgc, sgh):
                    for half in range(2):
                        ws = wget()
                        fm_group(ws, half * 4, lambda ps, c, dst=dst: A(
                            lambda e, ps=ps, c=c, dst=dst: e.activation(out=dst.ap[:, c, :], in_=ps.ap[:, 0:TT], func=AF.Sigmoid),
                            r=[ps], w=[dst[c]]))
                for half in range(2):
                    ws = wget()
                    for cc in range(4):
                        c = half * 4 + cc
                        ps = nps()
                        for k in range(KC):
                            T(lambda e, ps=ps, k=k, cc=cc, ws=ws: e.matmul(ps.ap[:, 0:TT], lhsT=ws.ap[:, k, cc * P:(cc + 1) * P],
                                                                         rhs=sgb.ap[:, k, :], start=(k == 0), stop=(k == KC - 1)),
                              r=[ws, sgb], w=[ps])
                        V(lambda e, ps=ps, c=c: e.tensor_tensor(out=m1.ap[:, c, :], in0=ps.ap[:, 0:TT], in1=sgc.ap[:, c, :], op=ALU.mult),
                          r=[ps, sgc[c]], w=[m1[c]])
            if dm:
                dump("f", FA)
                dump("eG", FB)
                dump("kT", kT)
                dump("QE", QE)
                dump("QO", QO)
                dump("vtm", vtm)
                dump("m1", m1)
                dump("sog", sog)
            if CUT <= 5:
                return
            for b in range(NS):
                blk = slice(b * P, (b + 1) * P)
                pt = PT[b % 2]
                for h in range(KC):
                    T(lambda e, pt=pt, h=h, blk=blk: e.transpose(pt.ap[:, h * P:(h + 1) * P], kT.ap[:, h, blk], identb.ap),
                      r=[kT[h], identb], w=[pt[h]])
                A(lambda e, pt=pt, b=b: e.activation(out=KE.ap[0:64, b, :, :], in_=pt.ap[0:64, :].rearrange("p (h k) -> p h k", h=KC),
                                                     func=AF.Identity), r=[pt], w=[KE[b]])
                V(lambda e, pt=pt, b=b: e.tensor_copy(out=KO.ap[64:128, b, :, :], in_=pt.ap[64:128, :].rearrange("p (h k) -> p h k", h=KC)),
                  r=[pt], w=[KO[b]])
                if not prefix:
                    for hq in range(2):
                        psa = nps()
                        at = AT[hq]
                        for hh in range(4):
                            h = hq * 4 + hh
                            T(lambda e, psa=psa, h=h, hh=hh, blk=blk: e.matmul(psa.ap[:, hh * P:(hh + 1) * P], lhsT=kT.ap[:, h, blk],
                                                                             rhs=QE.ap[:, h, blk], start=True, stop=False),
                              r=[kT[h], QE[h]], w=[psa[hh]])
                            T(lambda e, psa=psa, h=h, hh=hh, blk=blk: e.matmul(psa.ap[:, hh * P:(hh + 1) * P], lhsT=kT.ap[:, h, blk],
                                                                             rhs=QO.ap[:, h, blk], start=False, stop=True),
                              r=[kT[h], QO[h]], w=[psa[hh]])
                        V(lambda e, psa=psa, at=at: e.tensor_tensor(out=at.ap, in0=psa.ap.rearrange("p (h t) -> p h t", h=4),
                                                                    in1=mask01.ap.unsqueeze(1).broadcast_to([P, 4, P]), op=ALU.mult),
                          r=[psa, mask01], w=[at])
                for (Sin, Sout, Kx, par) in ((SA, SB, KE, 0), (SB, SA, KO, 1)):
                    for hq in range(2):
                        PSx = PSS if hq == 0 else PSM
                        at = AT[hq]
                        for hh in range(4):
                            h = hq * 4 + hh
                            T(lambda e, h=h, hh=hh, Sin=Sin, PSx=PSx: e.matmul(PSx.ap[:, hh * P:(hh + 1) * P], lhsT=identb.ap, rhs=Sin.ap[:, h, :],
                                                                               start=True, stop=False), r=[identb, Sin[h]], w=[PSx])
                            T(lambda e, h=h, hh=hh, Kx=Kx, b=b, PSx=PSx: e.matmul(PSx.ap[:, hh * P:(hh + 1) * P], lhsT=Kx.ap[:, b, h, :],
                                                                                  rhs=vtm.ap[:, b, h * P:(h + 1) * P], start=False, stop=True),
                              r=[Kx[b], vtm[b]], w=[PSx])
                        if par == 1 and (not prefix):
                            pso = nps()
                            for hh in range(4):
                                h = hq * 4 + hh
                                T(lambda e, pso=pso, h=h, hh=hh, b=b, at=at: e.matmul(pso.ap[:, hh * P:(hh + 1) * P], lhsT=vtm.ap[:, b, h * P:(h + 1) * P],
                                                                                     rhs=at.ap[:, hh, :], start=True, stop=False),
                                  r=[vtm[b], at[hh]], w=[pso[hh]])
                                T(lambda e, pso=pso, h=h, hh=hh, blk=blk: e.matmul(pso.ap[:, hh * P:(hh + 1) * P], lhsT=SA.ap[:, h, :],
                                                                                 rhs=QE.ap[:, h, blk], start=False, stop=False),
                                  r=[SA[h], QE[h]], w=[pso[hh]])
                                T(lambda e, pso=pso, h=h, hh=hh, blk=blk: e.matmul(pso.ap[:, hh * P:(hh + 1) * P], lhsT=SB.ap[:, h, :],
                                                                                 rhs=QO.ap[:, h, blk], start=False, stop=True),
                                  r=[SB[h], QO[h]], w=[pso[hh]])
                            A(lambda e, pso=pso, hq=hq, blk=blk: e.activation(out=FA.ap[:, hq * 4:hq * 4 + 4, blk],
                                                                              in_=pso.ap.rearrange("p (h t) -> p h t", h=4), func=AF.Identity),
                              r=[pso], w=[FA[slice(hq * 4, hq * 4 + 4)]])
                    for hq in range(2):
                        PSx = PSS if hq == 0 else PSM
                        h0 = hq * 4
                        col = b * P + par * 64 + 63
                        V(lambda e, h0=h0, col=col, Sout=Sout, PSx=PSx: e.tensor_tensor(
                            out=Sout.ap[:, h0:h0 + 4, :], in0=PSx.ap.rearrange("p (h v) -> p h v", h=4),
                            in1=FB.ap[:, h0:h0 + 4, col:col + 1].broadcast_to([P, 4, P]), op=ALU.mult),
                          r=[PSx, FB[slice(h0, h0 + 4)]], w=[Sout[slice(h0, h0 + 4)]])
            if CUT <= 6:
                return
            if prefix:
                if lastp:
                    V(lambda e: e.tensor_scalar(out=SA.ap, in0=SA.ap, scalar1=flag.ap[:, 0:1], scalar2=None, op0=ALU.mult),
                      r=[SA, flag], w=[SA])
                return
            if dm:
                dump("oT", FA)
                dump("SA", SA)
            A(lambda e: e.activation(out=osq.ap, in_=FA.ap, func=AF.Square), r=[FA], w=[osq])
            for hp in range(4):
                ps = nps()
                for i in range(2):
                    h = hp * 2 + i
                    T(lambda e, ps=ps, h=h, i=i: e.matmul(ps.ap[:, i * TT:(i + 1) * TT], lhsT=onesb.ap, rhs=osq.ap[:, h, :], start=True, stop=True),
                      r=[onesb, osq[h]], w=[ps[slice(i * 2, i * 2 + 2)]])
                A(lambda e, ps=ps, hp=hp: e.activation(out=FC.ap[:, hp * 2:hp * 2 + 2, :], in_=ps.ap.rearrange("p (h t) -> p h t", h=2),
                                                       func=AF.Sqrt, scale=1.0 / 128.0, bias=EPS), r=[ps], w=[FC[slice(hp * 2, hp * 2 + 2)]])
            V(lambda e: e.reciprocal(out=FC.ap, in_=FC.ap), r=[FC], w=[FC])
            V(lambda e: e.tensor_tensor(out=FA.ap, in0=FA.ap, in1=FC.ap, op=ALU.mult), r=[FA, FC], w=[FA])
            V(lambda e: e.scalar_tensor_tensor(out=kT.ap, in0=FA.ap, scalar=pv.ap[:, C_NG:C_NG + 1], in1=sog.ap,
                                               op0=ALU.mult, op1=ALU.mult), r=[FA, pv, sog], w=[kT])
            for half in range(2):
                ws = wget()
                for cc in range(4):
                    c = half * 4 + cc
                    ps = nps()
                    for k in range(KC):
                        T(lambda e, ps=ps, k=k, cc=cc, ws=ws: e.matmul(ps.ap[:, 0:TT], lhsT=ws.ap[:, k, cc * P:(cc + 1) * P], rhs=kT.ap[:, k, :],
                                                                     start=(k == 0), stop=(k == KC - 1)), r=[ws, kT], w=[ps])
                    V(lambda e, ps=ps, c=c: e.tensor_tensor(out=mT.ap[:, c, :], in0=ps.ap[:, 0:TT], in1=sgh.ap[:, c, :], op=ALU.mult),
                      r=[ps, sgh[c]], w=[mT[c]])
            V(lambda e: e.tensor_tensor(out=mT.ap, in0=mT.ap, in1=m1.ap, op=ALU.add), r=[mT, m1], w=[mT])
            for half in range(2):
                ws = wget()
                for j in range(NS):
                    ps = nps()
                    for k in range(KC):
                        T(lambda e, ps=ps, k=k, j=j, ws=ws: e.matmul(ps.ap, lhsT=mT.ap[:, k, j * P:(j + 1) * P], rhs=ws.ap[:, k, :],
                                                                   start=(k == 0), stop=(k == KC - 1)), r=[mT, ws], w=[ps])
                    V(lambda e, ps=ps, half=half: e.tensor_tensor(out=tmpa.ap, in0=ps.ap, in1=ga1_bc.ap[:, half * 512:(half + 1) * 512], op=ALU.mult),
                      r=[ps, ga1_bc], w=[tmpa])
                    V(lambda e, j=j, half=half: e.tensor_tensor(out=xt.ap[:, j, half * 512:(half + 1) * 512], in0=tmpa.ap,
                                                                in1=xt.ap[:, j, half * 512:(half + 1) * 512], op=ALU.add), r=[tmpa, xt[j]], w=[xt[j]])
            if dm:
                dump("ogT", kT)
                dump("mT", mT)
                dump("x1", xt)
            if CUT <= 7:
                return
            DMA("sp", lambda e: e.dma_start(out=x1_d[tok0:tok0 + TT, :].rearrange("(j p) d -> p j d", p=P), in_=xt.ap), r=[xt])
            rms_to_T(xt, h2t, gsc2.ap, modT.ap[:, 24:32], gsc2, modT)
            if not SPARSE:
                DMA("sp", lambda e: e.dma_start(out=h2_d.rearrange("p (k t) -> p k t", k=KC)[:, :, tok0:tok0 + TT], in_=h2t.ap), r=[h2t])
            else:
                for j in range(NS):
                    for half in range(2):
                        hs = slice(half * 512, (half + 1) * 512)
                        V(lambda e, j=j, hs=hs: e.scalar_tensor_tensor(out=tmpa.ap, in0=xt.ap[:, j, hs], scalar=small.ap[:, 16 + j:17 + j],
                                                                       in1=gsc2_bc.ap[:, hs], op0=ALU.mult, op1=ALU.mult),
                          r=[xt[j], small[16 + j], gsc2_bc], w=[tmpa])
                        V(lambda e, j=j, hs=hs: e.tensor_tensor(out=xn.ap[:, j, hs], in0=tmpa.ap, in1=sh2_bc.ap[:, hs], op=ALU.add),
                          r=[tmpa, sh2_bc], w=[xn[j]])
                DMA("sp", lambda e: e.dma_start(out=h2tm_d[tok0:tok0 + TT, :].rearrange("(j p) d -> p j d", p=P), in_=xn.ap), r=[xn])
            for j in range(NS):
                st = t * NS + j
                for k in range(KC):
                    T(lambda e, k=k, j=j: e.matmul(PSM.ap[:, 0:NE], lhsT=h2t.ap[:, k, j * P:(j + 1) * P], rhs=wr.ap[:, k, :],
                                                   start=(k == 0), stop=(k == KC - 1)), r=[h2t, wr], w=[PSM])
                V(lambda e: e.tensor_tensor(out=lgt.ap, in0=PSM.ap[:, 0:NE], in1=br_bc.ap, op=ALU.add), r=[PSM, br_bc], w=[lgt])
                V(lambda e, st=st: e.tensor_copy(out=lgts.ap[:, st, :], in_=lgt.ap), r=[lgt], w=[lgts[st]])
                V(lambda e: e.max(out=mx8.ap, in_=lgt.ap), r=[lgt], w=[mx8])
                V(lambda e: e.tensor_scalar(out=small.ap[:, 24:25], in0=mx8.ap[:, 0:1], scalar1=-1.0, scalar2=None, op0=ALU.mult),
                  r=[mx8], w=[small[24]])
                A(lambda e: e.activation(out=egt.ap, in_=lgt.ap, func=AF.Exp, bias=small.ap[:, 24:25]), r=[lgt, small[24]], w=[egt])
                V(lambda e: e.scalar_tensor_tensor(out=egt.ap, in0=lgt.ap, scalar=mx8.ap[:, 3:4], in1=egt.ap, op0=ALU.is_ge, op1=ALU.mult),
                  r=[lgt, mx8, egt], w=[egt])
                V(lambda e: e.reduce_sum(out=small.ap[:, 25:26], in_=egt.ap, axis=mybir.AxisListType.X), r=[egt], w=[small[25]])
                V(lambda e: e.reciprocal(out=small.ap[:, 26:27], in_=small.ap[:, 25:26]), r=[small[25]], w=[small[26]])
                V(lambda e, st=st: e.tensor_scalar(out=gates.ap[:, st, :], in0=egt.ap, scalar1=small.ap[:, 26:27], scalar2=None, op0=ALU.mult),
                  r=[egt, small[26]], w=[gates[st]])
                V(lambda e, st=st: e.tensor_copy(out=mx4.ap[:, st, :], in_=mx8.ap[:, 0:4]), r=[mx8], w=[mx4[st]])
                A(lambda e, st=st: e.activation(out=gk.ap[:, st, :], in_=mx8.ap[:, 0:4], func=AF.Exp, bias=small.ap[:, 24:25]),
                  r=[mx8, small[24]], w=[gk[st]])
                V(lambda e, st=st: e.tensor_scalar(out=gk.ap[:, st, :], in0=gk.ap[:, st, :], scalar1=small.ap[:, 26:27], scalar2=None, op0=ALU.mult),
                  r=[gk[st], small[26]], w=[gk[st]])

        if stage in (1, 3):
            tiles = [(True, True, NTILE - 1)] + [(False, False, t) for t in range(2 if stage == 1 else 4)]
            allg = []
            for (pf, lp, t) in tiles:
                allg += [wsrc(k, g) for (k, g) in tile_groups(pf, lp)]
        if stage == 0:
            tiles = []
        if os.environ.get("KPRE") == "0":
            tiles = [x for x in tiles if not x[0]]
            allg = []
            for (pf, lp, t) in tiles:
                allg += [wsrc(k, g) for (k, g) in tile_groups(pf, lp)]
        if os.environ.get("KPRE") == "only":
            tiles = [x for x in tiles if x[0]]
            allg = []
            for (pf, lp, t) in tiles:
                allg += [wsrc(k, g) for (k, g) in tile_groups(pf, lp)]
        for (pf, lp, t) in tiles:
            mixer_tile(pf, lp, t)
        if dbg and tiles:
            dump("h2t", h2t)
            dump("gates", gates)

        barrier(None)


        nst = len([1 for (pf, lp, t) in tiles if not pf]) * NS
        NST = NTOK // P
        XsB = Buf(None, "xs_dram", 4 * NST + 1)
        YsB = Buf(None, "ys_dram", NB)
        if stage == 6:
            nst = 0
        if SPARSE and nst > 0:
            rv = Carver(shared_end)
            maskall = rv.get(F32, [NST, NE], "maskall")
            posall = rv.get(F32, [NST, NE], "posall", NST)
            eqt = rv.get(F32, [NST, NE], "eqt")
            cum = rv.get(F32, [NE], "cum")
            nblk = rv.get(F32, [NE], "nblk")
            pend = rv.get(F32, [NE], "pend")
            pstart = rv.get(F32, [NE], "pstart")
            destf = rv.get(F32, [NST, 4], "destf", 4)
            widxf = rv.get(F32, [NB, KC], "widxf")
            oobf = rv.get(F32, [NB], "oobf")
            oob2 = rv.get(F32, [NB], "oob2")
            zt = rv.get(BF16, [4096], "zt")
            hrow = [rv.get(BF16, [D], "hrow%d" % i) for i in range(2)]
            V(lambda e: e.memset(zt.ap, 0.0), w=[zt])
            xs_fill = xs_d.rearrange("(c p q) d -> c p (q d)", p=P, q=4)
            for ci in range(NR // (P * 4)):
                DMA("sp", lambda e, ci=ci: e.dma_start(out=xs_fill[ci], in_=zt.ap), r=[zt], w=[XsB])
            V(lambda e: e.tensor_scalar(out=maskall.ap, in0=gates.ap, scalar1=0.0, scalar2=None, op0=ALU.is_gt), r=[gates], w=[maskall])
            V(lambda e: e.memset(cum.ap, 0.0), w=[cum])
            for st in range(NST):
                T(lambda e, st=st: e.matmul(PSM.ap[:, 0:NE], lhsT=ustrict, rhs=maskall.ap[:, st, :], start=True, stop=False), r=[cst, maskall], w=[PSM])
                T(lambda e: e.matmul(PSM.ap[:, 0:NE], lhsT=onesf.ap, rhs=cum.ap, start=False, stop=True), r=[onesf, cum], w=[PSM])
                A(lambda e, st=st: e.activation(out=posall.ap[:, st, :], in_=PSM.ap[:, 0:NE], func=AF.Identity), r=[PSM], w=[posall[st]])
                V(lambda e, st=st: e.tensor_tensor(out=cum.ap, in0=cum.ap, in1=maskall.ap[:, st, :], op=ALU.add), r=[cum, maskall], w=[cum])
            T(lambda e: e.matmul(PSM.ap[:, 0:NE], lhsT=onesf.ap, rhs=cum.ap, start=True, stop=True), r=[onesf, cum], w=[PSM])
            V(lambda e: e.tensor_copy(out=cum.ap, in_=PSM.ap[:, 0:NE]), r=[PSM], w=[cum])
            V(lambda e: e.memset(nblk.ap, 0.0), w=[nblk])
            for jb in range(NTOK // BLK):
                V(lambda e, jb=jb: e.scalar_tensor_tensor(out=nblk.ap, in0=cum.ap, scalar=float(jb * BLK), in1=nblk.ap, op0=ALU.is_gt, op1=ALU.add),
                  r=[cum, nblk], w=[nblk])
            V(lambda e: e.tensor_scalar(out=nblk.ap, in0=nblk.ap, scalar1=float(BLK), scalar2=None, op0=ALU.mult), r=[nblk], w=[nblk])
            V(lambda e: e.tensor_tensor_scan(out=pend.ap, data0=onesf.ap[:, 0:NE], data1=nblk.ap, initial=0.0, op0=ALU.mult, op1=ALU.add),
              r=[onesf, nblk], w=[pend])
            V(lambda e: e.tensor_tensor(out=pstart.ap, in0=pend.ap, in1=nblk.ap, op=ALU.subtract), r=[pend, nblk], w=[pstart])
            V(lambda e: e.tensor_tensor(out=posall.ap, in0=posall.ap, in1=pstart.ap.unsqueeze(1).broadcast_to([P, NST, NE]), op=ALU.add),
              r=[posall, pstart], w=[posall])
            for k in range(4):
                V(lambda e, k=k: e.tensor_tensor(out=eqt.ap, in0=lgts.ap, in1=mx4.ap[:, :, k:k + 1].broadcast_to([P, NST, NE]), op=ALU.is_equal),
                  r=[lgts, mx4], w=[eqt])
                V(lambda e: e.tensor_tensor(out=eqt.ap, in0=eqt.ap, in1=posall.ap, op=ALU.mult), r=[eqt, posall], w=[eqt])
                V(lambda e, k=k: e.reduce_sum(out=destf.ap[:, :, k], in_=eqt.ap, axis=mybir.AxisListType.X), r=[eqt], w=[destf[k]])
            V(lambda e: e.tensor_copy(out=desti.ap, in_=destf.ap), r=[destf], w=[desti])
            V(lambda e: e.memset(bef.ap, 0.0), w=[bef])
            for ex in range(NE):
                V(lambda e, ex=ex: e.scalar_tensor_tensor(out=bef.ap, in0=iotab, scalar=pend.ap[:, ex:ex + 1], in1=bef.ap, op0=ALU.is_ge, op1=ALU.add),
                  r=[cst, pend, bef], w=[bef])
            V(lambda e: e.tensor_scalar(out=bef.ap, in0=bef.ap, scalar1=float(NE - 1), scalar2=None, op0=ALU.min), r=[bef], w=[bef])
            V(lambda e: e.scalar_tensor_tensor(out=widxf.ap, in0=bef.ap.unsqueeze(2).broadcast_to([P, NB, KC]), scalar=float(D),
                                               in1=basekp.unsqueeze(1).broadcast_to([P, NB, KC]), op0=ALU.mult, op1=ALU.add),
              r=[bef, cst], w=[widxf])
            V(lambda e: e.tensor_scalar(out=oobf.ap, in0=iotab, scalar1=pend.ap[:, NE - 1:NE], scalar2=65536.0, op0=ALU.is_ge, op1=ALU.mult),
              r=[cst, pend], w=[oobf])
            V(lambda e: e.tensor_tensor(out=oob2.ap[:, 2:NB], in0=bef.ap[:, 2:NB], in1=bef.ap[:, 0:NB - 2], op=ALU.is_equal), r=[bef], w=[oob2])
            V(lambda e: e.scalar_tensor_tensor(out=oobf.ap[:, 2:NB], in0=oob2.ap[:, 2:NB], scalar=65536.0, in1=oobf.ap[:, 2:NB],
                                               op0=ALU.mult, op1=ALU.add), r=[oob2, oobf], w=[oobf])
            V(lambda e: e.tensor_tensor(out=widxf.ap, in0=widxf.ap, in1=oobf.ap.unsqueeze(2).broadcast_to([P, NB, KC]), op=ALU.add),
              r=[widxf, oobf], w=[widxf])
            V(lambda e: e.tensor_copy(out=widx.ap, in_=widxf.ap), r=[widxf], w=[widx])
            for st in range(nst):
                hr = hrow[st % 2]
                DMA("sp", lambda e, st=st, hr=hr: e.dma_start(out=hr.ap, in_=h2tm_d[st * P:(st + 1) * P, :]), w=[hr])
                for k in range(4):
                    DMA("pool", lambda e, st=st, k=k, hr=hr: e.indirect_dma_start(
                        out=xs_d[:, :], out_offset=bass.IndirectOffsetOnAxis(ap=desti.ap[:, st, k:k + 1], axis=0),
                        in_=hr.ap, in_offset=None), r=[hr, desti, XsB[4 * NST]], w=[XsB[st * 4 + k]])
            if dbg:
                dump("desti", desti)
                dump("bef", bef)
                dump("cnt", cum)
            barrier(None)

            bv = Carver(shared_end)
            W1b = [bv.get(BF16, [KC, 2 * D], "W1b%d" % i, KC) for i in range(2)]
            W2b = [bv.get(BF16, [KC, D], "W2b%d" % i, KC) for i in range(2)]
            xrows = bv.get(BF16, [4, D], "xrows", 4)
            xbTs = [bv.get(BF16, [KC, BLK], "xbT%d" % i, KC) for i in range(2)]
            actB = [bv.get(BF16, [KC, BLK], "actB%d" % i, KC) for i in range(2)]
            tgB = [bv.get(F32, [BLK], "tgB%d" % i) for i in range(2)]
            tsB = [bv.get(F32, [BLK], "tsB%d" % i) for i in range(2)]
            tlB = [bv.get(F32, [BLK], "tlB%d" % i) for i in range(2)]
            ysb = [bv.get(F32, [D], "ysb%d" % i, 2) for i in range(2)]
            oneh = bv.get(F32, [NE], "oneh")
            b1tmp = bv.get(F32, [16, NE], "b1tmp")
            b1sel = [bv.get(F32, [16], "b1sel%d" % i) for i in range(2)]
            print("arena bytes: blocks", bv.off)
            w1_flat = w1_2d
            w2_flat = w2_2d
            b1v = pv.ap[:, C_B1:C_B1 + NE * 16].rearrange("p (e i) -> p i e", i=16)
            ps6 = [0]

            def nps6():
                bk = PS[ps6[0] % 6]
                ps6[0] += 1
                return bk

            bc_cache = {}

            def bc_reg(e):
                if "r" not in bc_cache:
                    bc_cache["r"] = e.to_reg(NE * D - 1)
                return bc_cache["r"]

            def load_w1(b):
                wa = W1b[b % 2]
                for k in range(KC):
                    DMA("pool", lambda e, b=b, k=k, wa=wa: e.indirect_dma_start(
                        out=wa.ap[:, k, :], out_offset=None, in_=w1_flat[:, :],
                        in_offset=bass.IndirectOffsetOnAxis(ap=widx.ap[:, b, k:k + 1], axis=0),
                        bounds_check=bc_reg(e), oob_is_err=False), r=[widx], w=[wa[k]])

            def load_w2(b):
                wb2 = W2b[b % 2]
                for k in range(KC):
                    DMA("pool", lambda e, b=b, k=k, wb2=wb2: e.indirect_dma_start(
                        out=wb2.ap[:, k, :], out_offset=None, in_=w2_flat[:, :],
                        in_offset=bass.IndirectOffsetOnAxis(ap=widx.ap[:, b, k:k + 1], axis=0),
                        bounds_check=bc_reg(e), oob_is_err=False), r=[widx], w=[wb2[k]])

            def prep(b):
                xbT = xbTs[b % 2]
                DMA("sp", lambda e, b=b: e.dma_start(out=xrows.ap, in_=xs_d[b * BLK:(b + 1) * BLK, :].rearrange("(j p) d -> p j d", p=P)),
                    r=[XsB], w=[xrows])
                for k in range(KC):
                    pt = PT[k % 2]
                    for j in range(4):
                        T(lambda e, pt=pt, j=j, k=k: e.transpose(pt.ap[:, j * P:(j + 1) * P], xrows.ap[:, j, k * P:(k + 1) * P], identb.ap),
                          r=[xrows[j], identb], w=[pt])
                    if k % 2 == 0:
                        A(lambda e, pt=pt, k=k, xbT=xbT: e.activation(out=xbT.ap[:, k, :], in_=pt.ap[:, 0:BLK], func=AF.Identity), r=[pt], w=[xbT[k]])
                    else:
                        V(lambda e, pt=pt, k=k, xbT=xbT: e.tensor_copy(out=xbT.ap[:, k, :], in_=pt.ap[:, 0:BLK]), r=[pt], w=[xbT[k]])
                bs = b1sel[b % 2]
                V(lambda e, b=b: e.tensor_scalar(out=oneh.ap, in0=iotae, scalar1=bef.ap[:, b:b + 1], scalar2=None, op0=ALU.is_equal),
                  r=[cst, bef], w=[oneh])
                V(lambda e: e.tensor_tensor(out=b1tmp.ap, in0=b1v, in1=oneh.ap.unsqueeze(1).broadcast_to([P, 16, NE]), op=ALU.mult),
                  r=[pv, oneh], w=[b1tmp])
                V(lambda e, bs=bs: e.reduce_sum(out=bs.ap, in_=b1tmp.ap, axis=mybir.AxisListType.X), r=[b1tmp], w=[bs])
                V(lambda e, bs=bs: e.tensor_scalar(out=bs.ap[:, 8:16], in0=bs.ap[:, 8:16], scalar1=1.0, scalar2=None, op0=ALU.add), r=[bs], w=[bs])

            def w1_piece(b, i):
                wa, xbT, bs, aT = W1b[b % 2], xbTs[b % 2], b1sel[b % 2], actB[b % 2]
                psg = nps6()
                psl = nps6()
                for k in range(KC):
                    T(lambda e, k=k: e.matmul(psg.ap, lhsT=wa.ap[:, k, i * P:(i + 1) * P], rhs=xbT.ap[:, k, :],
                                              start=(k == 0), stop=(k == KC - 1)), r=[wa, xbT], w=[psg])
                for k in range(KC):
                    T(lambda e, k=k: e.matmul(psl.ap, lhsT=wa.ap[:, k, D + i * P:D + (i + 1) * P], rhs=xbT.ap[:, k, :],
                                              start=(k == 0), stop=(k == KC - 1)), r=[wa, xbT], w=[psl])
                a_, b_, c_ = tgB[i % 2], tsB[i % 2], tlB[i % 2]
                V(lambda e: e.tensor_scalar(out=a_.ap, in0=psg.ap, scalar1=bs.ap[:, i:i + 1], scalar2=7.0, op0=ALU.add, op1=ALU.min),
                  r=[psg, bs], w=[a_])
                A(lambda e: e.activation(out=b_.ap, in_=a_.ap, func=AF.Silu, scale=1.702), r=[a_], w=[b_])
                V(lambda e: e.tensor_scalar(out=c_.ap, in0=psl.ap, scalar1=bs.ap[:, 8 + i:9 + i], scalar2=8.0, op0=ALU.add, op1=ALU.min),
                  r=[psl, bs], w=[c_])
                V(lambda e: e.scalar_tensor_tensor(out=aT.ap[:, i, :], in0=c_.ap, scalar=-6.0, in1=b_.ap, op0=ALU.max, op1=ALU.mult),
                  r=[b_, c_], w=[aT[i]])

            def w2_piece(b, g):
                j4, half = g // 2, g % 2
                wb2, aT, yb = W2b[b % 2], actB[b % 2], ysb[j4 % 2]
                ps = nps6()
                for i in range(KC):
                    T(lambda e, i=i: e.matmul(ps.ap, lhsT=aT.ap[:, i, j4 * P:(j4 + 1) * P], rhs=wb2.ap[:, i, half * 512:(half + 1) * 512],
                                              start=(i == 0), stop=(i == KC - 1)), r=[aT, wb2], w=[ps])
                if half == 0:
                    A(lambda e: e.activation(out=yb.ap[:, 0:512], in_=ps.ap, func=AF.Identity, scale=1.0 / 1.702), r=[ps], w=[yb[0]])
                else:
                    A(lambda e: e.activation(out=yb.ap[:, 512:1024], in_=ps.ap, func=AF.Identity, scale=1.0 / 1.702), r=[ps], w=[yb[1]])
                    r0 = b * BLK + j4 * P
                    DMA("sp", lambda e: e.dma_start(out=ys_d[r0:r0 + P, :], in_=yb.ap), r=[yb], w=[YsB[b]])

            nblocks = NB if stage == 99 else int(os.environ.get("KNB", NB))
            if nblocks:
                load_w1(0)
                load_w2(0)
                prep(0)
                if nblocks > 1:
                    load_w1(1)
            for b in range(nblocks):
                for i in range(KC):
                    w1_piece(b, i)
                    if i == 1 and b + 1 < nblocks:
                        prep(b + 1)
                    if b > 0:
                        w2_piece(b - 1, i)
                if b + 2 < nblocks:
                    load_w1(b + 2)
                if b + 1 < nblocks:
                    load_w2(b + 1)
            if nblocks:
                for g in range(8):
                    w2_piece(nblocks - 1, g)
            barrier(None)


            cb = Carver(shared_end)
            Yk2 = [[cb.get(F32, [D], "Yk%d_%d" % (s_, i)) for i in range(4)] for s_ in range(2)]
            accs = cb.get(F32, [D], "accs", 2)
            cx1 = [cb.get(F32, [D], "cx1%d" % i) for i in range(2)]
            cxo = cb.get(F32, [D], "cxo")
            cjunk = cb.get(BF16, [D], "cjunk")
            cgT = cb.get(BF16, [P], "cgT")
            for st in range(nst):
                r0 = st * P
                xb = cx1[st % 2]
                Yk = Yk2[st % 2]
                DMA("sp", lambda e, xb=xb, r0=r0: e.dma_start(out=xb.ap, in_=x1_d[r0:r0 + P, :]), w=[xb])
                for k in range(4):
                    DMA("pool", lambda e, st=st, k=k, Yk=Yk: e.indirect_dma_start(
                        out=Yk[k].ap, out_offset=None, in_=ys_d[:, :],
                        in_offset=bass.IndirectOffsetOnAxis(ap=desti.ap[:, st, k:k + 1], axis=0)), r=[desti, YsB], w=[Yk[k]])
                T(lambda e, st=st: e.transpose(PSM.ap[0:NE, 0:P], gates.ap[:, st, :], ident_f), r=[gates[st], cst], w=[PSM])
                V(lambda e: e.tensor_copy(out=cgT.ap[0:NE, :], in_=PSM.ap[0:NE, 0:P]), r=[PSM], w=[cgT])
                for half in range(2):
                    ps = nps()
                    T(lambda e, ps=ps, half=half: e.matmul(ps.ap, lhsT=cgT.ap[0:NE, :], rhs=b2b.ap[0:NE, half * 512:(half + 1) * 512],
                                                          start=True, stop=True), r=[cgT, b2b], w=[ps])
                    A(lambda e, ps=ps, half=half: e.activation(out=accs.ap[:, half * 512:(half + 1) * 512], in_=ps.ap, func=AF.Identity),
                      r=[ps], w=[accs[half]])
                for k in range(4):
                    V(lambda e, st=st, k=k, Yk=Yk: e.scalar_tensor_tensor(out=accs.ap, in0=Yk[k].ap, scalar=gk.ap[:, st, k:k + 1], in1=accs.ap,
                                                                   op0=ALU.mult, op1=ALU.add), r=[Yk[k], gk[st], accs], w=[accs])
                V(lambda e: e.tensor_tensor(out=cxo.ap, in0=accs.ap, in1=ga2_bc.ap, op=ALU.mult), r=[accs, ga2_bc], w=[cxo])
                V(lambda e, xb=xb: e.tensor_tensor(out=cxo.ap, in0=cxo.ap, in1=xb.ap, op=ALU.add), r=[cxo, xb], w=[cxo])
                A(lambda e: e.activation(out=cjunk.ap, in_=cxo.ap, func=AF.Square, accum_out=small.ap[:, 32:33]), r=[cxo], w=[cjunk, small[32]])
                A(lambda e: e.activation(out=small.ap[:, 33:34], in_=small.ap[:, 32:33], func=AF.Sqrt, scale=1.0 / D, bias=EPS), r=[small[32]], w=[small[33]])
                V(lambda e: e.reciprocal(out=small.ap[:, 34:35], in_=small.ap[:, 33:34]), r=[small[33]], w=[small[34]])
                V(lambda e, xb=xb: e.scalar_tensor_tensor(out=xb.ap, in0=cxo.ap, scalar=small.ap[:, 34:35], in1=gfin_bc.ap, op0=ALU.mult, op1=ALU.mult),
                  r=[cxo, small[34], gfin_bc], w=[xb])
                DMA("sp", lambda e, xb=xb, r0=r0: e.dma_start(out=y_d[r0:r0 + P, :], in_=xb.ap), r=[xb])

        w1_v = w1_d.rearrange("e (k p) n -> e p k n", p=P)
        w2_v = w2_d.rearrange("e (k p) n -> e p k n", p=P)
        h2_v = h2_d.rearrange("p (k t) -> p k t", k=KC)

        def load_w1(e_, i):
            DMA("pool", lambda en: en.dma_start(out=W1[i].ap[:, :, 0:256], in_=w1_v[e_][:, :, i * 256:(i + 1) * 256]), w=[W1[i]])
            DMA("pool", lambda en: en.dma_start(out=W1[i].ap[:, :, 256:512], in_=w1_v[e_][:, :, D + i * 256:D + (i + 1) * 256]), w=[W1[i]])

        def load_w2(e_):
            DMA("pool", lambda en: en.dma_start(out=W2.ap, in_=w2_v[e_]), w=[W2])

        seq = [(q, e_) for q in range(NQ) for e_ in range(NE)]
        if stage <= 1 or SPARSE:
            seq = []
        if stage in (2, 3):
            seq = [(0, e_) for e_ in range(NE)]
        if SPARSE:
            seq = []
        if seq:
            for i in range(4):
                load_w1(0, i)
            load_w2(0)
        for si, (q, e_) in enumerate(seq):
            nxt = seq[si + 1][1] if si + 1 < len(seq) else None
            if e_ == 0:
                DMA("sp", lambda en, q=q: en.dma_start(out=h2q.ap, in_=h2_v[:, :, q * QT:(q + 1) * QT]), w=[h2q])
                for j in range(QT // P):
                    st = q * (QT // P) + j
                    T(lambda en, st=st: en.transpose(PSM.ap[0:NE, 0:P], gates.ap[:, st, :], ident_f), r=[gates[st], cst], w=[PSM])
                    V(lambda en: en.tensor_copy(out=gTb.ap[0:NE, :], in_=PSM.ap[0:NE, 0:P]), r=[PSM], w=[gTb])
                    for half in range(2):
                        ps = nps()
                        T(lambda en, ps=ps, half=half: en.matmul(ps.ap, lhsT=gTb.ap[0:NE, :], rhs=b2b.ap[0:NE, half * 512:(half + 1) * 512],
                                                                start=True, stop=True), r=[gTb, b2b], w=[ps])
                        A(lambda en, ps=ps, j=j, half=half: en.activation(out=acc.ap[:, j, half * 512:(half + 1) * 512], in_=ps.ap, func=AF.Identity),
                          r=[ps], w=[acc[j * 2 + half]])
            for blk in range(QT // 512):
                tsl = slice(blk * 512, (blk + 1) * 512)
                aT = actT[blk % 2]
                for i in range(KC):
                    g4, sub = i // 2, i % 2
                    wb = W1[g4]
                    psg = nps()
                    psl = nps()
                    for k in range(KC):
                        T(lambda en, psg=psg, k=k, wb=wb, sub=sub, tsl=tsl: en.matmul(psg.ap, lhsT=wb.ap[:, k, sub * P:(sub + 1) * P], rhs=h2q.ap[:, k, tsl],
                                                                                 start=(k == 0), stop=(k == KC - 1)), r=[wb, h2q], w=[psg])
                    for k in range(KC):
                        T(lambda en, psl=psl, k=k, wb=wb, sub=sub, tsl=tsl: en.matmul(psl.ap, lhsT=wb.ap[:, k, 256 + sub * P:256 + (sub + 1) * P], rhs=h2q.ap[:, k, tsl],
                                                                                 start=(k == 0), stop=(k == KC - 1)), r=[wb, h2q], w=[psl])
                    if blk == QT // 512 - 1 and sub == 1 and nxt is not None:
                        load_w1(nxt, g4)
                    cg = C_B1 + e_ * 16 + i
                    cl = C_B1 + e_ * 16 + 8 + i
                    a_, b_, c_ = tg[i % 2], tsg[i % 2], tl[i % 2]
                    V(lambda en, psg=psg, cg=cg, a_=a_: en.tensor_scalar(out=a_.ap, in0=psg.ap, scalar1=pv.ap[:, cg:cg + 1], scalar2=7.0, op0=ALU.add, op1=ALU.min),
                      r=[psg, pv], w=[a_])
                    A(lambda en, a_=a_, b_=b_: en.activation(out=b_.ap, in_=a_.ap, func=AF.Sigmoid, scale=1.702), r=[a_], w=[b_])
                    V(lambda en, psl=psl, cl=cl, c_=c_: en.tensor_scalar(out=c_.ap, in0=psl.ap, scalar1=pv.ap[:, cl:cl + 1], scalar2=7.0, op0=ALU.add, op1=ALU.min),
                      r=[psl, pv], w=[c_])
                    G(lambda en, c_=c_: en.tensor_scalar(out=c_.ap, in0=c_.ap, scalar1=-7.0, scalar2=1.0, op0=ALU.max, op1=ALU.add), r=[c_], w=[c_])
                    G(lambda en, a_=a_, b_=b_: en.tensor_tensor(out=a_.ap, in0=a_.ap, in1=b_.ap, op=ALU.mult), r=[a_, b_], w=[a_])
                    V(lambda en, a_=a_, c_=c_, aT=aT, i=i: en.tensor_tensor(out=aT.ap[:, i, :], in0=a_.ap, in1=c_.ap, op=ALU.mult), r=[a_, c_], w=[aT[i]])
                for j4 in range(4):
                    j = blk * 4 + j4
                    st = q * (QT // P) + j
                    for half in range(2):
                        ps = nps()
                        for i in range(KC):
                            T(lambda en, ps=ps, i=i, j4=j4, half=half, aT=aT: en.matmul(ps.ap, lhsT=aT.ap[:, i, j4 * P:(j4 + 1) * P],
                                                                                     rhs=W2.ap[:, i, half * 512:(half + 1) * 512],
                                                                                     start=(i == 0), stop=(i == KC - 1)), r=[aT, W2], w=[ps])
                        V(lambda en, ps=ps, j=j, half=half, st=st, e_=e_: en.scalar_tensor_tensor(
                            out=acc.ap[:, j, half * 512:(half + 1) * 512], in0=ps.ap, scalar=gates.ap[:, st, e_:e_ + 1],
                            in1=acc.ap[:, j, half * 512:(half + 1) * 512], op0=ALU.mult, op1=ALU.add), r=[ps, gates[st], acc[j * 2 + half]], w=[acc[j * 2 + half]])
            if nxt is not None:
                load_w2(nxt)
            if e_ == NE - 1:
                for j in range(QT // P):
                    r0 = q * QT + j * P
                    xb = x1t[j % 2]
                    DMA("sp", lambda en, xb=xb, r0=r0: en.dma_start(out=xb.ap, in_=x1_d[r0:r0 + P, :]), w=[xb])
                    V(lambda en, j=j: en.tensor_tensor(out=xo.ap, in0=acc.ap[:, j, :], in1=ga2_bc.ap, op=ALU.mult), r=[acc[slice(2 * j, 2 * j + 2)], ga2_bc], w=[xo])
                    G(lambda en, xb=xb: en.tensor_tensor(out=xo.ap, in0=xo.ap, in1=xb.ap, op=ALU.add), r=[xo, xb], w=[xo])
                    A(lambda en: en.activation(out=ejunk.ap, in_=xo.ap, func=AF.Square, accum_out=small.ap[:, 32:33]), r=[xo], w=[ejunk, small[32]])
                    A(lambda en: en.activation(out=small.ap[:, 33:34], in_=small.ap[:, 32:33], func=AF.Sqrt, scale=1.0 / D, bias=EPS), r=[small[32]], w=[small[33]])
                    V(lambda en: en.reciprocal(out=small.ap[:, 34:35], in_=small.ap[:, 33:34]), r=[small[33]], w=[small[34]])
                    V(lambda en, xb=xb: en.scalar_tensor_tensor(out=xb.ap, in0=xo.ap, scalar=small.ap[:, 34:35], in1=gfin_bc.ap, op0=ALU.mult, op1=ALU.mult),
                      r=[xo, small[34], gfin_bc], w=[xb])
                    DMA("sp", lambda en, xb=xb, r0=r0: en.dma_start(out=y_d[r0:r0 + P, :], in_=xb.ap), r=[xb])

        S.finish()
        print("ops:", {e: len(v) for e, v in S.ops.items()})
        with nc.Block() as block:
            @block.sync
            def _(e):
                S.run("sp", e)

            @block.scalar
            def _(e):
                S.run("act", e)

            @block.vector
            def _(e):
                S.run("dve", e)

            @block.gpsimd
            def _(e):
                S.run("pool", e)

            @block.tensor
            def _(e):
                S.run("pe", e)
    nc._dbg_names = DBG
    return nc


def _fm(v, n):
    return np.ascontiguousarray(np.asarray(v, np.float32).reshape(n, P).T)


def _consts():
    cst = np.zeros((P, CW), np.float32)
    cst[:, 0:128] = np.eye(P, dtype=np.float32)
    s = np.arange(P)[:, None]
    t = np.arange(P)[None, :]
    cst[:, 128:256] = ((s // 64 == t // 64) & (s <= t)).astype(np.float32)
    rm = np.ones((P, 256), np.float32)
    rm[:, ::64] = 0.0
    cst[:, 256:512] = rm
    cst[:, 512:640] = (s < t).astype(np.float32)
    cst[:, 640:704] = np.arange(NB, dtype=np.float32)[None, :] * BLK
    cst[:, 704:736] = np.arange(NE, dtype=np.float32)[None, :]
    cst[:, 736:744] = np.arange(KC, dtype=np.float32)[None, :] * P + np.arange(P, dtype=np.float32)[:, None]
    return cst


def make_in_maps(x, c, w_ada, b_ada, g_mix, w_in, conv_dw, conv_dw_bias, conv_ln_g, conv_ln_b,
                 w_conv_out, lb_param, hgrn_norm_g, w_hgrn_out, w_out, g_ffn, w_router, b_router,
                 w1, b1, w2, b2, g_final, cores=range(8)):
    f = lambda a: np.ascontiguousarray(np.asarray(a, np.float32))
    x = f(x)
    cst = _consts()
    dwT = np.ascontiguousarray(f(conv_dw)[0].T.reshape(KC, P, 31).transpose(1, 0, 2).reshape(P, KC * 31))
    b1T = np.ascontiguousarray(f(b1)[0].reshape(NE, 16, P).transpose(2, 0, 1).reshape(P, NE * 16))
    common = {
        "cst": cst,
        "w_ada": f(w_ada)[0], "b_ada": f(b_ada)[0:1], "w_in": f(w_in)[0],
        "w_conv_out": f(w_conv_out)[0], "w_hgrn_out": f(w_hgrn_out)[0], "w_out": f(w_out)[0],
        "w_router": f(w_router)[0], "b_router": f(b_router)[0:1],
        "w1": f(w1)[0].reshape(NE * D, 2 * D), "w2": f(w2)[0].reshape(NE * D, D), "b2": f(b2)[0], "g_final": f(g_final).reshape(1, D),
        "g_ffn": f(g_ffn)[0:1],
    }
    in_maps = []
    for core in cores:
        b, half = core // 2, core % 2
        pvec = np.zeros((P, RV), np.float32)
        pvec[:, C_C:C_C + 8] = _fm(np.asarray(c)[b], 8)
        pvec[:, C_BADA:C_BADA + 48] = _fm(np.asarray(b_ada)[0], 48)
        pvec[:, C_GMIX:C_GMIX + 8] = _fm(np.asarray(g_mix)[0], 8)
        pvec[:, C_DWB:C_DWB + 8] = _fm(np.asarray(conv_dw_bias)[0], 8)
        pvec[:, C_LNG:C_LNG + 8] = _fm(np.asarray(conv_ln_g)[0], 8)
        pvec[:, C_LNB:C_LNB + 8] = _fm(np.asarray(conv_ln_b)[0], 8)
        pvec[:, C_LB0:C_LB0 + 8] = _fm(np.asarray(lb_param)[0], 8)
        pvec[:, C_LB1:C_LB1 + 8] = _fm(np.asarray(lb_param)[1], 8)
        pvec[:, C_GFFN:C_GFFN + 8] = _fm(np.asarray(g_ffn)[0], 8)
        pvec[:, C_DW:C_DW + 248] = dwT
        pvec[:, C_B1:C_B1 + 512] = b1T
        pvec[:, C_NG] = np.asarray(hgrn_norm_g, np.float32)[0]
        m = dict(common)
        m["xm"] = np.ascontiguousarray(x[b, half * NTOK:(half + 1) * NTOK])
        m["xp"] = np.ascontiguousarray(x[b, 0:NTOK]) if half == 1 else np.zeros((NTOK, D), np.float32)
        m["flag"] = np.full((P, 1), float(half), np.float32)
        m["pvec"] = pvec
        in_maps.append(m)
    return in_maps


_NC = None


def kernel(**inputs):
    global _NC
    in_maps = make_in_maps(**inputs)
    if _NC is None:
        _NC = build_nc()
    res = run_bass_kernel_spmd(_NC, in_maps, core_ids=list(range(8)))
    out = np.zeros((4, 2 * NTOK, D), np.float32)
    for core in range(8):
        b, half = core // 2, core % 2
        out[b, half * NTOK:(half + 1) * NTOK] = np.asarray(res.results[core]["y"], np.float32)
    return out
```

```python
import os
import numpy as np
import concourse.bass as bass
import concourse.mybir as mybir
from concourse.bass_utils import run_bass_kernel_spmd

F32 = mybir.dt.float32
BF16 = mybir.dt.bfloat16
I32 = mybir.dt.int32
ALU = mybir.AluOpType
AF = mybir.ActivationFunctionType

P = 128
D = 1024
KC = 8
NTOK = 4096
TT = 256
NS = TT // P
NTILE = NTOK // TT
HALO = 30
UW = HALO + TT + 2
NE = 32
QT = 1024
NQ = NTOK // QT
EPS = 1e-6
BLK = 512
NB = 64
NR = NB * BLK
CW = 768
SPARSE = True
C_C, C_BADA, C_GMIX, C_DWB, C_LNG, C_LNB, C_LB0, C_LB1, C_GFFN, C_DW, C_B1, C_NG, RV = (
    0, 8, 56, 64, 72, 80, 88, 96, 104, 112, 360, 872, 876)
SAME_ENG_SYNC = True
CUT = int(os.environ.get('KCUT', '99'))
SUB = int(os.environ.get('KSUB', '99'))
EPOCH = 30000


class Op:
    __slots__ = ("eng", "fn", "deps", "marked", "sem", "val", "dma")


class Part:
    __slots__ = ("w", "r")

    def __init__(self):
        self.w = {}
        self.r = {}


class Buf:
    def __init__(self, ap, name="", nparts=1):
        self.ap = ap
        self.parts = [Part() for _ in range(nparts)]
        self.name = name

    def __getitem__(self, k):
        if len(self.parts) == 1:
            return self
        return (self, k)


def _expand(lst):
    out = {}
    for it in lst:
        if it is None:
            continue
        if isinstance(it, tuple):
            b, k = it
            if isinstance(k, int):
                ps = [b.parts[k]]
            elif isinstance(k, slice):
                ps = b.parts[k]
            else:
                ps = [b.parts[i] for i in k]
        else:
            ps = it.parts
        for p in ps:
            out[id(p)] = p
    return out


class Sched:
    ENG = ("sp", "act", "dve", "pool", "pe")

    def __init__(self, dma_sems, eng_sems):
        self.ops = {e: [] for e in self.ENG}
        self.pool = dma_sems
        self.esem = eng_sems
        self.dma_i = {q: 0 for q in dma_sems}
        self.dma_last = {}

    def op(self, eng, fn, r=(), w=(), dma=False):
        o = Op()
        o.eng, o.fn, o.dma, o.marked, o.deps, o.sem, o.val = eng, fn, dma, False, {}, None, 0
        rp = _expand(r)
        wp = _expand(w)
        for p in rp.values():
            for x in p.w.values():
                o.deps[id(x)] = x
        for p in wp.values():
            for x in p.r.values():
                o.deps[id(x)] = x
            for x in p.w.values():
                o.deps[id(x)] = x
        key = ("d", id(o)) if dma else eng
        for p in wp.values():
            p.w = {key: o}
            p.r = {}
        for k, p in rp.items():
            if k not in wp:
                p.r[key] = o
        if dma:
            pl = self.pool[eng]
            i = self.dma_i[eng] % len(pl)
            self.dma_i[eng] += 1
            prev = self.dma_last.get((eng, i))
            if prev is not None:
                o.deps[id(prev)] = prev
            o.sem = pl[i]
            o.val = (prev.val if prev is not None else 0) + 16
            self.dma_last[(eng, i)] = o
        for d in list(o.deps.values()):
            if (not d.dma) and d.eng == eng and (eng == "pe" or not SAME_ENG_SYNC):
                del o.deps[id(d)]
            else:
                d.marked = True
        self.ops[eng].append(o)
        return o

    def finish(self):
        o = Op()
        o.eng, o.fn, o.dma, o.marked, o.sem, o.val = "sp", (lambda e: e.nop()), False, False, None, 0
        o.deps = {id(x): x for x in self.dma_last.values()}
        self.ops["sp"].append(o)
        for eng in self.ENG:
            cnt = 0
            for q in self.ops[eng]:
                if (not q.dma) and q.marked:
                    q.sem = self.esem[eng][cnt // EPOCH]
                    q.val = cnt % EPOCH + 1
                    cnt += 1

    def run(self, eng, e):
        known = {}
        for o in self.ops[eng]:
            for d in o.deps.values():
                k = d.sem.num
                if known.get(k, 0) >= d.val:
                    continue
                e.wait_ge(d.sem, d.val)
                known[k] = d.val
            ins = o.fn(e)
            if o.dma:
                ins.then_inc(o.sem, 16)
            elif o.marked:
                ins.then_inc(o.sem, 1)


def build_nc(stage=99, dbg=False):
    DBG = []
    nc = bass.Bass("TRN2", target_bir_lowering=False)

    def dram(name, shape, dtype=F32, kind="ExternalInput"):
        return nc.dram_tensor(name, shape, dtype, kind=kind).ap()

    xm = dram("xm", [NTOK, D])
    xp = dram("xp", [NTOK, D])
    flag_d = dram("flag", [P, 1])
    pvec_d = dram("pvec", [P, RV])
    cst_d = dram("cst", [P, CW])
    gffn_d = dram("g_ffn", [1, D])
    w_ada = dram("w_ada", [D, 6 * D])
    b_ada = dram("b_ada", [1, 6 * D])
    w_in = dram("w_in", [D, 8 * D])
    wco_d = dram("w_conv_out", [D, D])
    who_d = dram("w_hgrn_out", [D, D])
    wout_d = dram("w_out", [D, D])
    wr_d = dram("w_router", [D, NE])
    br_d = dram("b_router", [1, NE])
    w1_2d = dram("w1", [NE * D, 2 * D])
    w2_2d = dram("w2", [NE * D, D])
    w1_d = w1_2d.rearrange("(e r) n -> e r n", e=NE)
    w2_d = w2_2d.rearrange("(e r) n -> e r n", e=NE)
    b2_d = dram("b2", [NE, D])
    gfin_d = dram("g_final", [1, D])
    y_d = dram("y", [NTOK, D], F32, "ExternalOutput")
    x1_d = dram("x1_scr", [NTOK, D], F32, "Internal")
    h2_d = dram("h2_scr", [P, KC * NTOK], BF16, "Internal")
    h2tm_d = dram("h2tm_scr", [NTOK, D], BF16, "Internal")
    dg_d = dram("dg_scr", [KC, P, 31, P], BF16, "Internal")
    wbf_d = dram("wbf_scr", [22, P, KC * 512], BF16, "Internal")
    xs_d = dram("xs_scr", [NR, D], BF16, "Internal")
    ys_d = dram("ys_scr", [NR, D], F32, "Internal")

    import contextlib
    es = contextlib.ExitStack()
    with es:
        AW = 52500
        arena = es.enter_context(nc.sbuf_tensor("arena", [P, AW], F32))
        psf = [es.enter_context(nc.psum_tensor("psf%d" % i, [P, 512], F32)) for i in range(6)]
        pst = [es.enter_context(nc.psum_tensor("pst%d" % i, [P, 1024], BF16)) for i in range(2)]
        dsems = {"sp": [es.enter_context(nc.semaphore("dmah%d" % i)) for i in range(20)],
                 "pool": [es.enter_context(nc.semaphore("dmas%d" % i)) for i in range(20)]}
        esems = {e: [es.enter_context(nc.semaphore("e_%s%d" % (e, i))) for i in range(3)]
                 for e in Sched.ENG}
        S = Sched(dsems, esems)
        PS = [Buf(t[:, :], "psf%d" % i, 1) for i, t in enumerate(psf)]
        PT = [Buf(t[:, :], "pst%d" % i, 1) for i, t in enumerate(pst)]
        psr = [0]

        def nps():
            b = PS[psr[0] % 4]
            psr[0] += 1
            return b
        PSS = PS[4]
        PSM = PS[5]

        class Carver:
            def __init__(self, start):
                self.off = start

            def get(self, dtype, free_shape, name="", nparts=1):
                n = int(np.prod(free_shape))
                nbytes = n * (2 if dtype == BF16 else 4)
                nbytes = (nbytes + 63) // 64 * 64
                w0 = self.off // 4
                w1 = (self.off + nbytes) // 4
                assert w1 <= AW, ("arena overflow", name, self.off + nbytes)
                ap = arena[:, w0:w1]
                if dtype != F32:
                    ap = ap.bitcast(dtype)
                ap = ap[:, 0:n]
                if len(free_shape) == 2:
                    ap = ap.rearrange("p (a b) -> p a b", a=free_shape[0])
                elif len(free_shape) == 3:
                    ap = ap.rearrange("p (a b c) -> p a b c", a=free_shape[0], b=free_shape[1])
                self.off += nbytes
                return Buf(ap, name, nparts)

        cv = Carver(0)
        pv = cv.get(F32, [RV], "pv")
        cst = cv.get(F32, [CW], "cst")
        flag = cv.get(F32, [1], "flag")
        identb = cv.get(BF16, [P], "identb")
        onesb = cv.get(BF16, [P], "onesb")
        onesf = cv.get(F32, [P], "onesf")
        mask01 = cv.get(BF16, [P], "mask01")
        modT = cv.get(F32, [48], "modT")
        gsc1 = cv.get(F32, [8], "gsc1")
        gsc2 = cv.get(F32, [8], "gsc2")
        lb = cv.get(F32, [8], "lb")
        oml = cv.get(F32, [8], "oml")
        sc = cv.get(F32, [8], "sc")
        ga1_bc = cv.get(F32, [D], "ga1_bc")
        ga2_bc = cv.get(F32, [D], "ga2_bc")
        gfin_bc = cv.get(F32, [D], "gfin_bc")
        br_bc = cv.get(F32, [NE], "br_bc")
        gates = cv.get(F32, [NTOK // P, NE], "gates", NTOK // P)
        wr = cv.get(BF16, [KC, NE], "wr")
        b2b = cv.get(BF16, [D], "b2b")
        small = cv.get(F32, [64], "small", 64)
        gsc2_bc = cv.get(F32, [D], "gsc2_bc")
        sh2_bc = cv.get(F32, [D], "sh2_bc")
        lgts = cv.get(F32, [NTOK // P, NE], "lgts", NTOK // P)
        mx4 = cv.get(F32, [NTOK // P, 4], "mx4", NTOK // P)
        gk = cv.get(F32, [NTOK // P, 4], "gk", NTOK // P)
        desti = cv.get(I32, [NTOK // P, 4], "desti")
        widx = cv.get(I32, [NB, KC], "widx")
        bef = cv.get(F32, [NB], "bef")
        shared_end = cv.off
        ident_f = cst.ap[:, 0:128]
        maskf = cst.ap[:, 128:256]
        resetm = cst.ap[:, 256:512]
        ustrict = cst.ap[:, 512:640]
        iotab = cst.ap[:, 640:704]
        iotae = cst.ap[:, 704:736]
        basekp = cst.ap[:, 736:744]

        mv = Carver(shared_end)
        xt = mv.get(F32, [NS, D], "xt", NS)
        junk = mv.get(BF16, [D], "junk")
        xn = mv.get(BF16, [NS, D], "xn", NS)
        hT = mv.get(BF16, [KC, TT], "hT", 8)
        setup_off = mv.off
        wslot = [mv.get(BF16, [KC, 512], "wslot%d" % i) for i in range(3)]
        uX = [mv.get(BF16, [KC, UW], "uX%d" % i, 8) for i in range(2)]
        FA = mv.get(F32, [KC, TT], "FA", 8)
        FB = mv.get(F32, [KC, TT], "FB", 8)
        FC = mv.get(F32, [KC, TT], "FC", 8)
        sgb = mv.get(BF16, [KC, TT], "sgb", 8)
        m1 = mv.get(BF16, [KC, TT], "m1", 8)
        QE = mv.get(BF16, [KC, TT], "QE", 8)
        QO = mv.get(BF16, [KC, TT], "QO", 8)
        kT = mv.get(BF16, [KC, TT], "kT", 8)
        sog = mv.get(BF16, [KC, TT], "sog", 8)
        sgc = mv.get(BF16, [KC, TT], "sgc", 8)
        sgh = mv.get(BF16, [KC, TT], "sgh", 8)
        osq = mv.get(BF16, [KC, TT], "osq", 8)
        mT = mv.get(BF16, [KC, TT], "mT", 8)
        h2t = mv.get(BF16, [KC, TT], "h2t", 8)
        vtm = mv.get(BF16, [NS, D], "vtm", NS)
        KE = mv.get(BF16, [NS, KC, P], "KE", NS)
        KO = mv.get(BF16, [NS, KC, P], "KO", NS)
        AT = [mv.get(BF16, [4, P], "AT%d" % i, 4) for i in range(2)]
        SA = mv.get(BF16, [KC, P], "SA", 8)
        SB = mv.get(BF16, [KC, P], "SB", 8)
        tmpa = mv.get(F32, [512], "tmpa")
        st_mean = mv.get(F32, [TT], "st_mean")
        st_var = mv.get(F32, [TT], "st_var")
        st_t = mv.get(F32, [TT], "st_t")
        lgt = mv.get(F32, [NE], "lgt")
        dgs = [mv.get(BF16, [31, P], "dgs%d" % i) for i in range(2)]
        zt1 = mv.get(BF16, [D], "zt1")
        mx8 = mv.get(F32, [8], "mx8")
        egt = mv.get(F32, [NE], "egt")
        mixer_end = mv.off
        sv = Carver(setup_off)
        scb = sv.get(F32, [KC, P], "scb")
        wada = [sv.get(F32, [KC, 512], "wada%d" % i) for i in range(2)]
        bada_bc = sv.get(F32, [D], "bada_bc")
        dgtmp = sv.get(BF16, [31, P], "dgtmp")
        DgB = Buf(None, "dg_dram")

        ev = Carver(shared_end)
        h2q = ev.get(BF16, [KC, QT], "h2q")
        acc = ev.get(F32, [QT // P, D], "acc", 2 * QT // P)
        W1 = [ev.get(BF16, [KC, 512], "W1_%d" % i) for i in range(4)]
        W2 = ev.get(BF16, [KC, D], "W2")
        actT = [ev.get(BF16, [KC, 512], "actT%d" % i, 8) for i in range(2)]
        tg = [ev.get(F32, [512], "tg%d" % i) for i in range(2)]
        tsg = [ev.get(F32, [512], "tsg%d" % i) for i in range(2)]
        tl = [ev.get(F32, [512], "tl%d" % i) for i in range(2)]
        x1t = [ev.get(F32, [D], "x1t%d" % i) for i in range(2)]
        xo = ev.get(F32, [D], "xo")
        ejunk = ev.get(BF16, [D], "ejunk")
        gTb = ev.get(BF16, [P], "gTb")
        moe_end = ev.off
        print("arena bytes: shared", shared_end, "mixer", mixer_end, "moe", moe_end)

        def V(fn, r=(), w=()):
            return S.op("dve", fn, r, w)

        def A(fn, r=(), w=()):
            return S.op("act", fn, r, w)

        def G(fn, r=(), w=()):
            return S.op("pool", fn, r, w)

        def T(fn, r=(), w=()):
            return S.op("pe", fn, r, w)

        def DMA(q, fn, r=(), w=()):
            return S.op(q, fn, r, w, dma=True)

        def dump(name, buf, ap=None):
            if not dbg:
                return
            ap = buf.ap if ap is None else ap
            shp = list(ap.shape)
            dtn = nc.dram_tensor("dbg_" + name, shp, ap.dtype, kind="ExternalOutput").ap()
            DMA("sp", lambda e: e.dma_start(out=dtn, in_=ap), r=[buf])
            DBG.append("dbg_" + name)

        def barrier(bufs):
            last = {e: S.ops[e][-1] for e in ("act", "dve", "pool", "pe") if S.ops[e]}
            bb = Buf(None, "barrier")
            for e, o in last.items():
                bb.parts[0].w[e] = o
            for x in S.dma_last.values():
                bb.parts[0].w[("d", id(x))] = x
            for e in ("act", "dve", "pool", "pe", "sp"):
                S.op(e, (lambda en: en.nop()), r=[bb])

        XsB = Buf(None, "xs_dram", 4 * (NTOK // P) + 1)
        V(lambda e: e.memset(zt1.ap, 0.0), w=[zt1])
        xs_fill = xs_d.rearrange("(c p q) d -> c p q d", p=P, q=16)
        for ci in range(NR // (P * 16)):
            DMA("sp", lambda e, ci=ci: e.dma_start(out=xs_fill[ci], in_=zt1.ap.unsqueeze(1).broadcast_to([P, 16, D])), r=[zt1], w=[XsB])
        DMA("sp", lambda e: e.dma_start(out=pv.ap, in_=pvec_d), w=[pv])
        DMA("sp", lambda e: e.dma_start(out=cst.ap, in_=cst_d), w=[cst])
        DMA("sp", lambda e: e.dma_start(out=flag.ap, in_=flag_d), w=[flag])
        DMA("sp", lambda e: e.dma_start(out=gfin_bc.ap, in_=gfin_d.broadcast_to([P, D])), w=[gfin_bc])
        DMA("sp", lambda e: e.dma_start(out=br_bc.ap, in_=br_d.broadcast_to([P, NE])), w=[br_bc])
        DMA("pool", lambda e: e.dma_start(out=wr.ap, in_=wr_d.rearrange("(k p) n -> p k n", p=P)), w=[wr])
        DMA("pool", lambda e: e.dma_start(out=b2b.ap[0:NE, :], in_=b2_d), w=[b2b])
        V(lambda e: e.tensor_copy(out=identb.ap, in_=ident_f), r=[cst], w=[identb])
        V(lambda e: e.tensor_copy(out=mask01.ap, in_=maskf), r=[cst], w=[mask01])
        V(lambda e: e.memset(onesb.ap, 1.0), w=[onesb])
        V(lambda e: e.memset(onesf.ap, 1.0), w=[onesf])
        V(lambda e: e.memset(gates.ap, 0.0), w=[gates])
        V(lambda e: e.memset(lgts.ap, 0.0), w=[lgts])
        V(lambda e: e.memset(mx4.ap, 0.0), w=[mx4])
        V(lambda e: e.memset(gk.ap, 0.0), w=[gk])
        A(lambda e: e.activation(out=sc.ap, in_=pv.ap[:, C_C:C_C + 8], func=AF.Silu), r=[pv], w=[sc])
        V(lambda e: e.tensor_copy(out=scb.ap, in_=sc.ap.unsqueeze(2).broadcast_to([P, KC, P])), r=[sc], w=[scb])
        V(lambda e: e.tensor_tensor(out=small.ap[:, 0:8], in0=pv.ap[:, C_LB0:C_LB0 + 8],
                                    in1=pv.ap[:, C_LB1:C_LB1 + 8], op=ALU.subtract), r=[pv], w=[small[slice(0, 8)]])
        A(lambda e: e.activation(out=lb.ap, in_=small.ap[:, 0:8], func=AF.Sigmoid), r=[small[slice(0, 8)]], w=[lb])
        V(lambda e: e.tensor_scalar(out=oml.ap, in0=lb.ap, scalar1=-1.0, scalar2=1.0, op0=ALU.mult, op1=ALU.add),
          r=[lb], w=[oml])
        wada_v = w_ada.rearrange("(k p) n -> p k n", p=P)
        for g in range(12):
            wb = wada[g % 2]
            DMA("sp", lambda e, g=g, wb=wb: e.dma_start(out=wb.ap, in_=wada_v[:, :, g * 512:(g + 1) * 512]), w=[wb])
            for cc in range(4):
                j = g * 4 + cc
                for k in range(KC):
                    T(lambda e, j=j, k=k, cc=cc, wb=wb: e.matmul(PSM.ap[:, j:j + 1], lhsT=wb.ap[:, k, cc * 128:(cc + 1) * 128],
                                                             rhs=sc.ap[:, k:k + 1], start=(k == 0), stop=(k == KC - 1)),
                      r=[wb, sc], w=[PSM])
            if g in (4, 5, 6, 7, 8, 9, 10, 11):
                ps = nps()
                dst = {2: ga1_bc, 3: sh2_bc, 4: gsc2_bc, 5: ga2_bc}[g // 2]
                hh = g % 2
                for k in range(KC):
                    T(lambda e, k=k, wb=wb, ps=ps: e.matmul(ps.ap, lhsT=scb.ap[:, k, :], rhs=wb.ap[:, k, :],
                                                          start=(k == 0), stop=(k == KC - 1)), r=[wb, scb], w=[ps])
                DMA("sp", lambda e, g=g: e.dma_start(out=bada_bc.ap[:, 0:512],
                                                    in_=b_ada[:, g * 512:(g + 1) * 512].broadcast_to([P, 512])), w=[bada_bc])
                V(lambda e, ps=ps, dst=dst, hh=hh: e.tensor_tensor(out=dst.ap[:, hh * 512:(hh + 1) * 512], in0=ps.ap,
                                                                 in1=bada_bc.ap[:, 0:512], op=ALU.add),
                  r=[ps, bada_bc], w=[dst])
        V(lambda e: e.tensor_tensor(out=modT.ap, in0=PSM.ap[:, 0:48], in1=pv.ap[:, C_BADA:C_BADA + 48], op=ALU.add),
          r=[PSM, pv], w=[modT])
        V(lambda e: e.scalar_tensor_tensor(out=gsc1.ap, in0=modT.ap[:, 8:16], scalar=1.0, in1=pv.ap[:, C_GMIX:C_GMIX + 8],
                                           op0=ALU.add, op1=ALU.mult), r=[modT, pv], w=[gsc1])
        DMA("sp", lambda e: e.dma_start(out=bada_bc.ap, in_=gffn_d.broadcast_to([P, D])), w=[bada_bc])
        V(lambda e: e.scalar_tensor_tensor(out=gsc2_bc.ap, in0=gsc2_bc.ap, scalar=1.0, in1=bada_bc.ap, op0=ALU.add, op1=ALU.mult),
          r=[gsc2_bc, bada_bc], w=[gsc2_bc])
        V(lambda e: e.scalar_tensor_tensor(out=gsc2.ap, in0=modT.ap[:, 32:40], scalar=1.0, in1=pv.ap[:, C_GFFN:C_GFFN + 8],
                                           op0=ALU.add, op1=ALU.mult), r=[modT, pv], w=[gsc2])

        for c in range(KC):
            V(lambda e, c=c: e.tensor_tensor(out=dgtmp.ap, in0=identb.ap.unsqueeze(1).broadcast_to([P, 31, P]),
                                             in1=pv.ap[:, C_DW + c * 31:C_DW + (c + 1) * 31].unsqueeze(2).broadcast_to([P, 31, P]), op=ALU.mult),
              r=[identb, pv], w=[dgtmp])
            DMA("sp", lambda e, c=c: e.dma_start(out=dg_d[c], in_=dgtmp.ap), r=[dgtmp], w=[DgB])
        dump("modT", modT)
        dump("ga1", ga1_bc)
        dump("ga2", ga2_bc)
        dump("lb", lb)
        dump("gsc1", gsc1)
        barrier(None)
        V(lambda e: e.memset(QE.ap, 0.0), w=[QE])
        V(lambda e: e.memset(QO.ap, 0.0), w=[QO])
        V(lambda e: e.memset(KE.ap, 0.0), w=[KE])
        V(lambda e: e.memset(KO.ap, 0.0), w=[KO])
        V(lambda e: e.memset(SA.ap, 0.0), w=[SA])
        V(lambda e: e.memset(uX[0].ap, 0.0), w=[uX[0]])
        V(lambda e: e.memset(uX[1].ap, 0.0), w=[uX[1]])
        win_v = w_in.rearrange("(k p) n -> p k n", p=P)
        wco_v = wco_d.rearrange("(k p) n -> p k n", p=P)
        who_v = who_d.rearrange("(k p) n -> p k n", p=P)
        wout_v = wout_d.rearrange("(k p) n -> p k n", p=P)
        wq = {"n": 0, "pending": []}

        WbB = Buf(None, "wbf_dram", 22)

        def wsrc32(kind, g):
            if kind == "in":
                return win_v[:, :, g * 512:(g + 1) * 512]
            v = {"co": wco_v, "ho": who_v, "out": wout_v}[kind]
            return v[:, :, g * 512:(g + 1) * 512]

        def wsrc(kind, g):
            return {"in": 0, "co": 16, "ho": 18, "out": 20}[kind] + g

        gl_all = [("in", g) for g in range(16)] + [("co", 0), ("co", 1), ("ho", 0), ("ho", 1), ("out", 0), ("out", 1)]
        for n_, (k_, g_) in enumerate(gl_all):
            sl_ = wslot[n_ % 3]
            DMA("pool", lambda e, sl_=sl_, k_=k_, g_=g_: e.dma_start(out=sl_.ap, in_=wsrc32(k_, g_)), w=[sl_])
            DMA("sp", lambda e, sl_=sl_, n_=n_: e.dma_start(out=wbf_d[n_].rearrange("p (k n) -> p k n", k=KC), in_=sl_.ap),
                r=[sl_], w=[WbB[n_]])

        def wissue(src):
            slot = wslot[wq["n"] % 3]
            wq["n"] += 1
            if os.environ.get("KWMIX") == "none" and wq["n"] > 3:
                pass
            else:
                DMA("pool", lambda e, slot=slot, src=src: e.dma_start(out=slot.ap, in_=wbf_d[src].rearrange("p (k n) -> p k n", k=KC)),
                    r=[WbB[src]], w=[slot])
            wq["pending"].append(slot)

        def wnext():
            return wq["pending"].pop(0)

        def tile_groups(prefix, lastp):
            gl = []
            if (not prefix) or lastp:
                gl += [("in", 2), ("in", 3), ("in", 0), ("in", 1)]
            if CUT <= 3:
                return gl
            gl += [("in", 6), ("in", 7)]
            if CUT <= 4:
                return gl
            if not prefix:
                gl += [("in", 4), ("in", 5)]
            gl += [("in", 8), ("in", 9)]
            if not prefix:
                gl += [("in", 10), ("in", 11), ("in", 12), ("in", 13), ("in", 14), ("in", 15),
                       ("co", 0), ("co", 1)]
                if CUT > 6:
                    gl += [("ho", 0), ("ho", 1), ("out", 0), ("out", 1)]
            return gl

        tiles = [(True, t == NTILE - 1, t) for t in range(NTILE)] + [(False, False, t) for t in range(NTILE)]
        allg = []
        for (pf, lp, t) in tiles:
            allg += [wsrc(k, g) for (k, g) in tile_groups(pf, lp)]
        gi = {"i": 0}

        def wget():
            while gi["i"] < len(allg) and len(wq["pending"]) < 3:
                wissue(allg[gi["i"]])
                gi["i"] += 1
            return wnext()

        def fm_group(ws, cbase, evac):
            for cc in range(4):
                ps = nps()
                for k in range(KC):
                    T(lambda e, ps=ps, k=k, cc=cc: e.matmul(ps.ap[:, 0:TT], lhsT=ws.ap[:, k, cc * 128:(cc + 1) * 128],
                                                          rhs=hT.ap[:, k, :], start=(k == 0), stop=(k == KC - 1)),
                      r=[ws, hT], w=[ps])
                evac(ps, cbase + cc)

        def rms_to_T(src, dstT, scale_ap, bias_ap, scale_b, bias_b):
            for j in range(NS):
                A(lambda e, j=j: e.activation(out=junk.ap, in_=src.ap[:, j, :], func=AF.Square,
                                              accum_out=small.ap[:, 8 + j:9 + j]), r=[src[j]], w=[junk, small[8 + j]])
            A(lambda e: e.activation(out=small.ap[:, 12:12 + NS], in_=small.ap[:, 8:8 + NS], func=AF.Sqrt,
                                     scale=1.0 / D, bias=EPS), r=[small[slice(8, 8 + NS)]], w=[small[slice(12, 12 + NS)]])
            V(lambda e: e.reciprocal(out=small.ap[:, 16:16 + NS], in_=small.ap[:, 12:12 + NS]), r=[small[slice(12, 12 + NS)]], w=[small[slice(16, 16 + NS)]])
            for j in range(NS):
                V(lambda e, j=j: e.tensor_scalar(out=xn.ap[:, j, :], in0=src.ap[:, j, :], scalar1=small.ap[:, 16 + j:17 + j],
                                                 scalar2=None, op0=ALU.mult), r=[src[j], small[16 + j]], w=[xn[j]])
            for k in range(KC):
                pt = PT[k % 2]
                for j in range(NS):
                    T(lambda e, pt=pt, j=j, k=k: e.transpose(pt.ap[:, j * P:(j + 1) * P], xn.ap[:, j, k * P:(k + 1) * P],
                                                            identb.ap), r=[xn[j], identb], w=[pt[j]])
                A(lambda e, pt=pt, k=k: e.activation(out=dstT.ap[:, k, :], in_=pt.ap[:, 0:TT], func=AF.Identity,
                                                     scale=scale_ap[:, k:k + 1], bias=bias_ap[:, k:k + 1]),
                  r=[pt[slice(0, NS)], scale_b, bias_b], w=[dstT[k]])

        def mixer_tile(prefix, lastp, t):
            src = xp if prefix else xm
            tok0 = t * TT
            ucur = uX[t % 2]
            uprev = uX[(t + 1) % 2]
            do_conv_in = (not prefix) or lastp
            DMA("sp", lambda e: e.dma_start(out=xt.ap, in_=src[tok0:tok0 + TT, :].rearrange("(j p) d -> p j d", p=P)), w=[xt])
            rms_to_T(xt, hT, gsc1.ap, modT.ap[:, 0:8], gsc1, modT)
            if CUT <= 1:
                return
            dm = dbg and (not prefix) and t == 0
            if dm:
                dump("hT", hT)
            if do_conv_in:
                V(lambda e: e.tensor_copy(out=ucur.ap[:, :, 0:HALO], in_=uprev.ap[:, :, TT:TT + HALO]), r=[uprev], w=[ucur])
                for half in range(2):
                    ws = wget()
                    fm_group(ws, half * 4, lambda ps, c: A(
                        lambda e, ps=ps, c=c: e.activation(out=sgb.ap[:, c, :], in_=ps.ap[:, 0:TT], func=AF.Sigmoid),
                        r=[ps], w=[sgb[c]]))
                if dm:
                    dump("sgb0", sgb)
                for half in range(2):
                    ws = wget()
                    if dm:
                        dump("ws%d" % half, ws)
                    fm_group(ws, half * 4, lambda ps, c: V(
                        lambda e, ps=ps, c=c: e.tensor_tensor(out=ucur.ap[:, c, HALO:HALO + TT], in0=ps.ap[:, 0:TT],
                                                              in1=sgb.ap[:, c, :], op=ALU.mult), r=[ps, sgb[c]], w=[ucur[c]]))
                if prefix:
                    V(lambda e: e.tensor_scalar(out=ucur.ap[:, :, TT:TT + HALO], in0=ucur.ap[:, :, TT:TT + HALO],
                                                scalar1=flag.ap[:, 0:1], scalar2=None, op0=ALU.mult), r=[ucur, flag], w=[ucur])
            if CUT <= 2:
                return
            if not prefix:
                if os.environ.get("KCONV") == "dve":
                    for c in range(KC):
                        V(lambda e, c=c: e.tensor_scalar(out=FA.ap[:, c, :], in0=ucur.ap[:, c, 0:TT],
                                                         scalar1=pv.ap[:, C_DW + c * 31:C_DW + c * 31 + 1],
                                                         scalar2=pv.ap[:, C_DWB + c:C_DWB + c + 1], op0=ALU.mult, op1=ALU.add),
                          r=[ucur[c], pv], w=[FA[c]])
                    for j in range(1, 31):
                        for c in range(KC):
                            V(lambda e, c=c, j=j: e.scalar_tensor_tensor(out=FA.ap[:, c, :], in0=ucur.ap[:, c, j:j + TT],
                                                                         scalar=pv.ap[:, C_DW + c * 31 + j:C_DW + c * 31 + j + 1],
                                                                         in1=FA.ap[:, c, :], op0=ALU.mult, op1=ALU.add),
                              r=[ucur[c], pv, FA[c]], w=[FA[c]])
                else:
                    for c in range(KC):
                        dg = dgs[c % 2]
                        DMA("sp", lambda e, c=c, dg=dg: e.dma_start(out=dg.ap, in_=dg_d[c]), r=[DgB], w=[dg])
                        ps = nps()
                        for j in range(31):
                            T(lambda e, c=c, j=j, dg=dg, ps=ps: e.matmul(ps.ap[:, 0:TT], lhsT=dg.ap[:, j, :], rhs=ucur.ap[:, c, j:j + TT],
                                                                       start=(j == 0), stop=(j == 30)), r=[dg, ucur[c]], w=[ps])
                        V(lambda e, c=c, ps=ps: e.tensor_scalar(out=FA.ap[:, c, :], in0=ps.ap[:, 0:TT], scalar1=pv.ap[:, C_DWB + c:C_DWB + c + 1],
                                                                scalar2=None, op0=ALU.add), r=[ps, pv], w=[FA[c]])
                A(lambda e: e.activation(out=FB.ap, in_=FA.ap, func=AF.Square), r=[FA], w=[FB])
                for k in range(KC):
                    T(lambda e, k=k: e.matmul(PSS.ap[:, 0:TT], lhsT=onesf.ap, rhs=FA.ap[:, k, :], start=(k == 0), stop=(k == KC - 1)),
                      r=[onesf, FA[k]], w=[PSS[slice(0, 2)]])
                for k in range(KC):
                    T(lambda e, k=k: e.matmul(PSS.ap[:, TT:2 * TT], lhsT=onesf.ap, rhs=FB.ap[:, k, :], start=(k == 0), stop=(k == KC - 1)),
                      r=[onesf, FB[k]], w=[PSS[slice(2, 4)]])
                V(lambda e: e.tensor_scalar(out=st_mean.ap, in0=PSS.ap[:, 0:TT], scalar1=1.0 / D, scalar2=None, op0=ALU.mult),
                  r=[PSS], w=[st_mean])
                V(lambda e: e.tensor_tensor(out=st_t.ap, in0=st_mean.ap, in1=st_mean.ap, op=ALU.mult), r=[st_mean], w=[st_t])
                V(lambda e: e.scalar_tensor_tensor(out=st_var.ap, in0=PSS.ap[:, TT:2 * TT], scalar=1.0 / D, in1=st_t.ap,
                                                   op0=ALU.mult, op1=ALU.subtract), r=[PSS, st_t], w=[st_var])
                A(lambda e: e.activation(out=st_var.ap, in_=st_var.ap, func=AF.Sqrt, bias=EPS), r=[st_var], w=[st_var])
                V(lambda e: e.reciprocal(out=st_var.ap, in_=st_var.ap), r=[st_var], w=[st_var])
                V(lambda e: e.tensor_tensor(out=FA.ap, in0=FA.ap, in1=st_mean.ap.unsqueeze(1).broadcast_to([P, KC, TT]),
                                            op=ALU.subtract), r=[FA, st_mean], w=[FA])
                V(lambda e: e.tensor_tensor(out=FA.ap, in0=FA.ap, in1=st_var.ap.unsqueeze(1).broadcast_to([P, KC, TT]),
                                            op=ALU.mult), r=[FA, st_var], w=[FA])
                for c in range(KC):
                    A(lambda e, c=c: e.activation(out=sgb.ap[:, c, :], in_=FA.ap[:, c, :], func=AF.Silu,
                                                  scale=pv.ap[:, C_LNG + c:C_LNG + c + 1], bias=pv.ap[:, C_LNB + c:C_LNB + c + 1]),
                      r=[FA[c], pv], w=[sgb[c]])
            if dm:
                dump("ucur", ucur)
                dump("u2T", sgb)
            if CUT <= 3:
                return
            for half in range(2):
                ws = wget()
                fm_group(ws, half * 4, lambda ps, c: A(
                    lambda e, ps=ps, c=c: e.activation(out=FA.ap[:, c, :], in_=ps.ap[:, 0:TT], func=AF.Sigmoid), r=[ps], w=[FA[c]]))
            for c in range(KC):
                V(lambda e, c=c: e.tensor_scalar(out=FA.ap[:, c, :], in0=FA.ap[:, c, :], scalar1=oml.ap[:, c:c + 1],
                                                 scalar2=lb.ap[:, c:c + 1], op0=ALU.mult, op1=ALU.add), r=[FA[c], oml, lb], w=[FA[c]])
            A(lambda e: e.activation(out=FB.ap, in_=FA.ap, func=AF.Ln), r=[FA], w=[FB])
            for c in range(KC):
                V(lambda e, c=c: e.tensor_tensor_scan(out=FC.ap[:, c, :], data0=resetm, data1=FB.ap[:, c, :], initial=0.0,
                                                      op0=ALU.mult, op1=ALU.add), r=[FB[c], cst], w=[FC[c]])
            A(lambda e: e.activation(out=FB.ap, in_=FC.ap, func=AF.Exp), r=[FC], w=[FB])
            A(lambda e: e.activation(out=FC.ap, in_=FC.ap, func=AF.Exp, scale=-1.0), r=[FC], w=[FC])
            V(lambda e: e.scalar_tensor_tensor(out=kT.ap, in0=FA.ap, scalar=1.0, in1=FC.ap, op0=ALU.subtract, op1=ALU.mult),
              r=[FA, FC], w=[kT])
            if CUT <= 4:
                return
            if not prefix:
                for half in range(2):
                    ws = wget()

                    def evq(ps, c):
                        for par, dst in ((0, QE), (1, QO)):
                            V(lambda e, ps=ps, c=c, par=par, dst=dst: e.scalar_tensor_tensor(
                                out=dst.ap[:, c, :].rearrange("p (b two s) -> p b two s", two=2, s=64)[:, :, par, :],
                                in0=ps.ap[:, 0:TT].rearrange("p (b two s) -> p b two s", two=2, s=64)[:, :, par, :],
                                scalar=-(128.0 ** -0.5),
                                in1=FB.ap[:, c, :].rearrange("p (b two s) -> p b two s", two=2, s=64)[:, :, par, :],
                                op0=ALU.mult, op1=ALU.mult), r=[ps, FB[c]], w=[dst[c]])
                    fm_group(ws, half * 4, evq)
            for half in range(2):
                ws = wget()
                for j in range(NS):
                    ps = nps()
                    for k in range(KC):
                        T(lambda e, ps=ps, k=k, j=j, ws=ws: e.matmul(ps.ap, lhsT=hT.ap[:, k, j * P:(j + 1) * P], rhs=ws.ap[:, k, :],
                                                                   start=(k == 0), stop=(k == KC - 1)), r=[hT, ws], w=[ps])
                    A(lambda e, ps=ps, j=j, half=half: e.activation(out=vtm.ap[:, j, half * 512:(half + 1) * 512], in_=ps.ap,
                                                                    func=AF.Identity), r=[ps], w=[vtm[j]])
            if not prefix:
                ws = wget()
                fm_group(ws, 0, lambda ps, c: A(lambda e, ps=ps, c=c: e.activation(out=sog.ap[:, c, :], in_=ps.ap[:, 0:TT], func=AF.Silu), r=[ps], w=[sog[c]]))
                ws = wget()
                fm_group(ws, 4, lambda ps, c: A(lambda e, ps=ps, c=c: e.activation(out=sog.ap[:, c, :], in_=ps.ap[:, 0:TT], func=AF.Silu), r=[ps], w=[sog[c]]))
                for dst in (sgc, sgh):
                    for half in range(2):
                        ws = wget()
                        fm_group(ws, half * 4, lambda ps, c, dst=dst: A(
                            lambda e, ps=ps, c=c, dst=dst: e.activation(out=dst.ap[:, c, :], in_=ps.ap[:, 0:TT], func=AF.Sigmoid),
                            r=[ps], w=[dst[c]]))
                for half in range(2):
                    ws = wget()
                    for cc in range(4):
                        c = half * 4 + cc
                        ps = nps()
                        for k in range(KC):
                            T(lambda e, ps=ps, k=k, cc=cc, ws=ws: e.matmul(ps.ap[:, 0:TT], lhsT=ws.ap[:, k, cc * P:(cc + 1) * P],
                                                                         rhs=sgb.ap[:, k, :], start=(k == 0), stop=(k == KC - 1)),
                              r=[ws, sgb], w=[ps])
                        V(lambda e, ps=ps, c=c: e.tensor_tensor(out=m1.ap[:, c, :], in0=ps.ap[:, 0:TT], in1=sgc.ap[:, c, :], op=ALU.mult),
                          r=[ps, sgc[c]], w=[m1[c]])
            if dm:
                dump("f", FA)
                dump("eG", FB)
                dump("kT", kT)
                dump("QE", QE)
                dump("QO", QO)
                dump("vtm", vtm)
                dump("m1", m1)
                dump("sog", sog)
            if CUT <= 5:
                return
            for b in range(NS):
                blk = slice(b * P, (b + 1) * P)
                pt = PT[b % 2]
                for h in range(KC):
                    T(lambda e, pt=pt, h=h, blk=blk: e.transpose(pt.ap[:, h * P:(h + 1) * P], kT.ap[:, h, blk], identb.ap),
                      r=[kT[h], identb], w=[pt[h]])
                A(lambda e, pt=pt, b=b: e.activation(out=KE.ap[0:64, b, :, :], in_=pt.ap[0:64, :].rearrange("p (h k) -> p h k", h=KC),
                                                     func=AF.Identity), r=[pt], w=[KE[b]])
                V(lambda e, pt=pt, b=b: e.tensor_copy(out=KO.ap[64:128, b, :, :], in_=pt.ap[64:128, :].rearrange("p (h k) -> p h k", h=KC)),
                  r=[pt], w=[KO[b]])
                if not prefix:
                    for hq in range(2):
                        psa = nps()
                        at = AT[hq]
                        for hh in range(4):
                            h = hq * 4 + hh
                            T(lambda e, psa=psa, h=h, hh=hh, blk=blk: e.matmul(psa.ap[:, hh * P:(hh + 1) * P], lhsT=kT.ap[:, h, blk],
                                                                             rhs=QE.ap[:, h, blk], start=True, stop=False),
                              r=[kT[h], QE[h]], w=[psa[hh]])
                            T(lambda e, psa=psa, h=h, hh=hh, blk=blk: e.matmul(psa.ap[:, hh * P:(hh + 1) * P], lhsT=kT.ap[:, h, blk],
                                                                             rhs=QO.ap[:, h, blk], start=False, stop=True),
                              r=[kT[h], QO[h]], w=[psa[hh]])
                        V(lambda e, psa=psa, at=at: e.tensor_tensor(out=at.ap, in0=psa.ap.rearrange("p (h t) -> p h t", h=4),
                                                                    in1=mask01.ap.unsqueeze(1).broadcast_to([P, 4, P]), op=ALU.mult),
                          r=[psa, mask01], w=[at])
                for (Sin, Sout, Kx, par) in ((SA, SB, KE, 0), (SB, SA, KO, 1)):
                    for hq in range(2):
                        PSx = PSS if hq == 0 else PSM
                        at = AT[hq]
                        for hh in range(4):
                            h = hq * 4 + hh
                            T(lambda e, h=h, hh=hh, Sin=Sin, PSx=PSx: e.matmul(PSx.ap[:, hh * P:(hh + 1) * P], lhsT=identb.ap, rhs=Sin.ap[:, h, :],
                                                                               start=True, stop=False), r=[identb, Sin[h]], w=[PSx])
                            T(lambda e, h=h, hh=hh, Kx=Kx, b=b, PSx=PSx: e.matmul(PSx.ap[:, hh * P:(hh + 1) * P], lhsT=Kx.ap[:, b, h, :],
                                                                                  rhs=vtm.ap[:, b, h * P:(h + 1) * P], start=False, stop=True),
                              r=[Kx[b], vtm[b]], w=[PSx])
                        if par == 1 and (not prefix):
                            pso = nps()
                            for hh in range(4):
                                h = hq * 4 + hh
                                T(lambda e, pso=pso, h=h, hh=hh, b=b, at=at: e.matmul(pso.ap[:, hh * P:(hh + 1) * P], lhsT=vtm.ap[:, b, h * P:(h + 1) * P],
                                                                                     rhs=at.ap[:, hh, :], start=True, stop=False),
                                  r=[vtm[b], at[hh]], w=[pso[hh]])
                                T(lambda e, pso=pso, h=h, hh=hh, blk=blk: e.matmul(pso.ap[:, hh * P:(hh + 1) * P], lhsT=SA.ap[:, h, :],
                                                                                 rhs=QE.ap[:, h, blk], start=False, stop=False),
                                  r=[SA[h], QE[h]], w=[pso[hh]])
                                T(lambda e, pso=pso, h=h, hh=hh, blk=blk: e.matmul(pso.ap[:, hh * P:(hh + 1) * P], lhsT=SB.ap[:, h, :],
                                                                                 rhs=QO.ap[:, h, blk], start=False, stop=True),
                                  r=[SB[h], QO[h]], w=[pso[hh]])
                            A(lambda e, pso=pso, hq=hq, blk=blk: e.activation(out=FA.ap[:, hq * 4:hq * 4 + 4, blk],
                                                                              in_=pso.ap.rearrange("p (h t) -> p h t", h=4), func=AF.Identity),
                              r=[pso], w=[FA[slice(hq * 4, hq * 4 + 4)]])
                    for hq in range(2):
                        PSx = PSS if hq == 0 else PSM
                        h0 = hq * 4
                        col = b * P + par * 64 + 63
                        V(lambda e, h0=h0, col=col, Sout=Sout, PSx=PSx: e.tensor_tensor(
                            out=Sout.ap[:, h0:h0 + 4, :], in0=PSx.ap.rearrange("p (h v) -> p h v", h=4),
                            in1=FB.ap[:, h0:h0 + 4, col:col + 1].broadcast_to([P, 4, P]), op=ALU.mult),
                          r=[PSx, FB[slice(h0, h0 + 4)]], w=[Sout[slice(h0, h0 + 4)]])
            if CUT <= 6:
                return
            if prefix:
                if lastp:
                    V(lambda e: e.tensor_scalar(out=SA.ap, in0=SA.ap, scalar1=flag.ap[:, 0:1], scalar2=None, op0=ALU.mult),
                      r=[SA, flag], w=[SA])
                return
            if dm:
                dump("oT", FA)
                dump("SA", SA)
            A(lambda e: e.activation(out=osq.ap, in_=FA.ap, func=AF.Square), r=[FA], w=[osq])
            for hp in range(4):
                ps = nps()
                for i in range(2):
                    h = hp * 2 + i
                    T(lambda e, ps=ps, h=h, i=i: e.matmul(ps.ap[:, i * TT:(i + 1) * TT], lhsT=onesb.ap, rhs=osq.ap[:, h, :], start=True, stop=True),
                      r=[onesb, osq[h]], w=[ps[slice(i * 2, i * 2 + 2)]])
                A(lambda e, ps=ps, hp=hp: e.activation(out=FC.ap[:, hp * 2:hp * 2 + 2, :], in_=ps.ap.rearrange("p (h t) -> p h t", h=2),
                                                       func=AF.Sqrt, scale=1.0 / 128.0, bias=EPS), r=[ps], w=[FC[slice(hp * 2, hp * 2 + 2)]])
            V(lambda e: e.reciprocal(out=FC.ap, in_=FC.ap), r=[FC], w=[FC])
            V(lambda e: e.tensor_tensor(out=FA.ap, in0=FA.ap, in1=FC.ap, op=ALU.mult), r=[FA, FC], w=[FA])
            V(lambda e: e.scalar_tensor_tensor(out=kT.ap, in0=FA.ap, scalar=pv.ap[:, C_NG:C_NG + 1], in1=sog.ap,
                                               op0=ALU.mult, op1=ALU.mult), r=[FA, pv, sog], w=[kT])
            for half in range(2):
                ws = wget()
                for cc in range(4):
                    c = half * 4 + cc
                    ps = nps()
                    for k in range(KC):
                        T(lambda e, ps=ps, k=k, cc=cc, ws=ws: e.matmul(ps.ap[:, 0:TT], lhsT=ws.ap[:, k, cc * P:(cc + 1) * P], rhs=kT.ap[:, k, :],
                                                                     start=(k == 0), stop=(k == KC - 1)), r=[ws, kT], w=[ps])
                    V(lambda e, ps=ps, c=c: e.tensor_tensor(out=mT.ap[:, c, :], in0=ps.ap[:, 0:TT], in1=sgh.ap[:, c, :], op=ALU.mult),
                      r=[ps, sgh[c]], w=[mT[c]])
            V(lambda e: e.tensor_tensor(out=mT.ap, in0=mT.ap, in1=m1.ap, op=ALU.add), r=[mT, m1], w=[mT])
            for half in range(2):
                ws = wget()
                for j in range(NS):
                    ps = nps()
                    for k in range(KC):
                        T(lambda e, ps=ps, k=k, j=j, ws=ws: e.matmul(ps.ap, lhsT=mT.ap[:, k, j * P:(j + 1) * P], rhs=ws.ap[:, k, :],
                                                                   start=(k == 0), stop=(k == KC - 1)), r=[mT, ws], w=[ps])
                    V(lambda e, ps=ps, half=half: e.tensor_tensor(out=tmpa.ap, in0=ps.ap, in1=ga1_bc.ap[:, half * 512:(half + 1) * 512], op=ALU.mult),
                      r=[ps, ga1_bc], w=[tmpa])
                    V(lambda e, j=j, half=half: e.tensor_tensor(out=xt.ap[:, j, half * 512:(half + 1) * 512], in0=tmpa.ap,
                                                                in1=xt.ap[:, j, half * 512:(half + 1) * 512], op=ALU.add), r=[tmpa, xt[j]], w=[xt[j]])
            if dm:
                dump("ogT", kT)
                dump("mT", mT)
                dump("x1", xt)
            if CUT <= 7:
                return
            DMA("sp", lambda e: e.dma_start(out=x1_d[tok0:tok0 + TT, :].rearrange("(j p) d -> p j d", p=P), in_=xt.ap), r=[xt])
            rms_to_T(xt, h2t, gsc2.ap, modT.ap[:, 24:32], gsc2, modT)
            if not SPARSE:
                DMA("sp", lambda e: e.dma_start(out=h2_d.rearrange("p (k t) -> p k t", k=KC)[:, :, tok0:tok0 + TT], in_=h2t.ap), r=[h2t])
            else:
                for j in range(NS):
                    for half in range(2):
                        hs = slice(half * 512, (half + 1) * 512)
                        V(lambda e, j=j, hs=hs: e.scalar_tensor_tensor(out=tmpa.ap, in0=xt.ap[:, j, hs], scalar=small.ap[:, 16 + j:17 + j],
                                                                       in1=gsc2_bc.ap[:, hs], op0=ALU.mult, op1=ALU.mult),
                          r=[xt[j], small[16 + j], gsc2_bc], w=[tmpa])
                        V(lambda e, j=j, hs=hs: e.tensor_tensor(out=xn.ap[:, j, hs], in0=tmpa.ap, in1=sh2_bc.ap[:, hs], op=ALU.add),
                          r=[tmpa, sh2_bc], w=[xn[j]])
                DMA("sp", lambda e: e.dma_start(out=h2tm_d[tok0:tok0 + TT, :].rearrange("(j p) d -> p j d", p=P), in_=xn.ap), r=[xn])
            for j in range(NS):
                st = t * NS + j
                for k in range(KC):
                    T(lambda e, k=k, j=j: e.matmul(PSM.ap[:, 0:NE], lhsT=h2t.ap[:, k, j * P:(j + 1) * P], rhs=wr.ap[:, k, :],
                                                   start=(k == 0), stop=(k == KC - 1)), r=[h2t, wr], w=[PSM])
                V(lambda e: e.tensor_tensor(out=lgt.ap, in0=PSM.ap[:, 0:NE], in1=br_bc.ap, op=ALU.add), r=[PSM, br_bc], w=[lgt])
                V(lambda e, st=st: e.tensor_copy(out=lgts.ap[:, st, :], in_=lgt.ap), r=[lgt], w=[lgts[st]])
                V(lambda e: e.max(out=mx8.ap, in_=lgt.ap), r=[lgt], w=[mx8])
                V(lambda e: e.tensor_scalar(out=small.ap[:, 24:25], in0=mx8.ap[:, 0:1], scalar1=-1.0, scalar2=None, op0=ALU.mult),
                  r=[mx8], w=[small[24]])
                A(lambda e: e.activation(out=egt.ap, in_=lgt.ap, func=AF.Exp, bias=small.ap[:, 24:25]), r=[lgt, small[24]], w=[egt])
                V(lambda e: e.scalar_tensor_tensor(out=egt.ap, in0=lgt.ap, scalar=mx8.ap[:, 3:4], in1=egt.ap, op0=ALU.is_ge, op1=ALU.mult),
                  r=[lgt, mx8, egt], w=[egt])
                V(lambda e: e.reduce_sum(out=small.ap[:, 25:26], in_=egt.ap, axis=mybir.AxisListType.X), r=[egt], w=[small[25]])
                V(lambda e: e.reciprocal(out=small.ap[:, 26:27], in_=small.ap[:, 25:26]), r=[small[25]], w=[small[26]])
                V(lambda e, st=st: e.tensor_scalar(out=gates.ap[:, st, :], in0=egt.ap, scalar1=small.ap[:, 26:27], scalar2=None, op0=ALU.mult),
                  r=[egt, small[26]], w=[gates[st]])
                V(lambda e, st=st: e.tensor_copy(out=mx4.ap[:, st, :], in_=mx8.ap[:, 0:4]), r=[mx8], w=[mx4[st]])
                A(lambda e, st=st: e.activation(out=gk.ap[:, st, :], in_=mx8.ap[:, 0:4], func=AF.Exp, bias=small.ap[:, 24:25]),
                  r=[mx8, small[24]], w=[gk[st]])
                V(lambda e, st=st: e.tensor_scalar(out=gk.ap[:, st, :], in0=gk.ap[:, st, :], scalar1=small.ap[:, 26:27], scalar2=None, op0=ALU.mult),
                  r=[gk[st], small[26]], w=[gk[st]])

        if stage in (1, 3):
            tiles = [(True, True, NTILE - 1)] + [(False, False, t) for t in range(2 if stage == 1 else 4)]
            allg = []
            for (pf, lp, t) in tiles:
                allg += [wsrc(k, g) for (k, g) in tile_groups(pf, lp)]
        if stage == 0:
            tiles = []
        if os.environ.get("KPRE") == "0":
            tiles = [x for x in tiles if not x[0]]
            allg = []
            for (pf, lp, t) in tiles:
                allg += [wsrc(k, g) for (k, g) in tile_groups(pf, lp)]
        if os.environ.get("KPRE") == "only":
            tiles = [x for x in tiles if x[0]]
            allg = []
            for (pf, lp, t) in tiles:
                allg += [wsrc(k, g) for (k, g) in tile_groups(pf, lp)]
        for (pf, lp, t) in tiles:
            mixer_tile(pf, lp, t)
        if dbg and tiles:
            dump("h2t", h2t)
            dump("gates", gates)

        barrier(None)


        nst = len([1 for (pf, lp, t) in tiles if not pf]) * NS
        NST = NTOK // P
        YsB = Buf(None, "ys_dram", NB)
        if stage == 6:
            nst = 0
        if SPARSE and nst > 0:
            rv = Carver(shared_end)
            maskall = rv.get(F32, [NST, NE], "maskall")
            posall = rv.get(F32, [NST, NE], "posall", NST)
            eqt = rv.get(F32, [NST, NE], "eqt")
            cum = rv.get(F32, [NE], "cum")
            nblk = rv.get(F32, [NE], "nblk")
            pend = rv.get(F32, [NE], "pend")
            pstart = rv.get(F32, [NE], "pstart")
            destf = rv.get(F32, [NST, 4], "destf", 4)
            widxf = rv.get(F32, [NB, KC], "widxf")
            oobf = rv.get(F32, [NB], "oobf")
            zt = rv.get(BF16, [4096], "zt")
            hrow = [rv.get(BF16, [D], "hrow%d" % i) for i in range(2)]
            V(lambda e: e.tensor_scalar(out=maskall.ap, in0=gates.ap, scalar1=0.0, scalar2=None, op0=ALU.is_gt), r=[gates], w=[maskall])
            V(lambda e: e.memset(cum.ap, 0.0), w=[cum])
            for st in range(NST):
                T(lambda e, st=st: e.matmul(PSM.ap[:, 0:NE], lhsT=ustrict, rhs=maskall.ap[:, st, :], start=True, stop=False), r=[cst, maskall], w=[PSM])
                T(lambda e: e.matmul(PSM.ap[:, 0:NE], lhsT=onesf.ap, rhs=cum.ap, start=False, stop=True), r=[onesf, cum], w=[PSM])
                A(lambda e, st=st: e.activation(out=posall.ap[:, st, :], in_=PSM.ap[:, 0:NE], func=AF.Identity), r=[PSM], w=[posall[st]])
                V(lambda e, st=st: e.tensor_tensor(out=cum.ap, in0=cum.ap, in1=maskall.ap[:, st, :], op=ALU.add), r=[cum, maskall], w=[cum])
            T(lambda e: e.matmul(PSM.ap[:, 0:NE], lhsT=onesf.ap, rhs=cum.ap, start=True, stop=True), r=[onesf, cum], w=[PSM])
            V(lambda e: e.tensor_copy(out=cum.ap, in_=PSM.ap[:, 0:NE]), r=[PSM], w=[cum])
            V(lambda e: e.memset(nblk.ap, 0.0), w=[nblk])
            for jb in range(NTOK // BLK):
                V(lambda e, jb=jb: e.scalar_tensor_tensor(out=nblk.ap, in0=cum.ap, scalar=float(jb * BLK), in1=nblk.ap, op0=ALU.is_gt, op1=ALU.add),
                  r=[cum, nblk], w=[nblk])
            V(lambda e: e.tensor_scalar(out=nblk.ap, in0=nblk.ap, scalar1=float(BLK), scalar2=None, op0=ALU.mult), r=[nblk], w=[nblk])
            V(lambda e: e.tensor_tensor_scan(out=pend.ap, data0=onesf.ap[:, 0:NE], data1=nblk.ap, initial=0.0, op0=ALU.mult, op1=ALU.add),
              r=[onesf, nblk], w=[pend])
            V(lambda e: e.tensor_tensor(out=pstart.ap, in0=pend.ap, in1=nblk.ap, op=ALU.subtract), r=[pend, nblk], w=[pstart])
            V(lambda e: e.tensor_tensor(out=posall.ap, in0=posall.ap, in1=pstart.ap.unsqueeze(1).broadcast_to([P, NST, NE]), op=ALU.add),
              r=[posall, pstart], w=[posall])
            for k in range(4):
                V(lambda e, k=k: e.tensor_tensor(out=eqt.ap, in0=lgts.ap, in1=mx4.ap[:, :, k:k + 1].broadcast_to([P, NST, NE]), op=ALU.is_equal),
                  r=[lgts, mx4], w=[eqt])
                V(lambda e: e.tensor_tensor(out=eqt.ap, in0=eqt.ap, in1=posall.ap, op=ALU.mult), r=[eqt, posall], w=[eqt])
                V(lambda e, k=k: e.reduce_sum(out=destf.ap[:, :, k], in_=eqt.ap, axis=mybir.AxisListType.X), r=[eqt], w=[destf[k]])
            V(lambda e: e.tensor_copy(out=desti.ap, in_=destf.ap), r=[destf], w=[desti])
            V(lambda e: e.memset(bef.ap, 0.0), w=[bef])
            for ex in range(NE):
                V(lambda e, ex=ex: e.scalar_tensor_tensor(out=bef.ap, in0=iotab, scalar=pend.ap[:, ex:ex + 1], in1=bef.ap, op0=ALU.is_ge, op1=ALU.add),
                  r=[cst, pend, bef], w=[bef])
            V(lambda e: e.tensor_scalar(out=bef.ap, in0=bef.ap, scalar1=float(NE - 1), scalar2=None, op0=ALU.min), r=[bef], w=[bef])
            V(lambda e: e.scalar_tensor_tensor(out=widxf.ap, in0=bef.ap.unsqueeze(2).broadcast_to([P, NB, KC]), scalar=float(D),
                                               in1=basekp.unsqueeze(1).broadcast_to([P, NB, KC]), op0=ALU.mult, op1=ALU.add),
              r=[bef, cst], w=[widxf])
            V(lambda e: e.tensor_scalar(out=oobf.ap, in0=iotab, scalar1=pend.ap[:, NE - 1:NE], scalar2=65536.0, op0=ALU.is_ge, op1=ALU.mult),
              r=[cst, pend], w=[oobf])
            V(lambda e: e.tensor_tensor(out=widxf.ap, in0=widxf.ap, in1=oobf.ap.unsqueeze(2).broadcast_to([P, NB, KC]), op=ALU.add),
              r=[widxf, oobf], w=[widxf])
            V(lambda e: e.tensor_copy(out=widx.ap, in_=widxf.ap), r=[widxf], w=[widx])
            for st in range(nst):
                hr = hrow[st % 2]
                DMA("sp", lambda e, st=st, hr=hr: e.dma_start(out=hr.ap, in_=h2tm_d[st * P:(st + 1) * P, :]), w=[hr])
                for k in range(4):
                    DMA("pool", lambda e, st=st, k=k, hr=hr: e.indirect_dma_start(
                        out=xs_d[:, :], out_offset=bass.IndirectOffsetOnAxis(ap=desti.ap[:, st, k:k + 1], axis=0),
                        in_=hr.ap, in_offset=None), r=[hr, desti, XsB[4 * NST]], w=[XsB[st * 4 + k]])
            if dbg:
                dump("desti", desti)
                dump("bef", bef)
                dump("cnt", cum)
            barrier(None)

            bv = Carver(shared_end)
            W1b = [bv.get(BF16, [KC, 2 * D], "W1b%d" % i, KC) for i in range(2)]
            W2b = [bv.get(BF16, [KC, D], "W2b%d" % i, KC) for i in range(2)]
            xrows = bv.get(BF16, [4, D], "xrows", 4)
            xbTs = [bv.get(BF16, [KC, BLK], "xbT%d" % i, KC) for i in range(2)]
            actB = [bv.get(BF16, [KC, BLK], "actB%d" % i, KC) for i in range(2)]
            tgB = [bv.get(F32, [BLK], "tgB%d" % i) for i in range(2)]
            tsB = [bv.get(F32, [BLK], "tsB%d" % i) for i in range(2)]
            tlB = [bv.get(F32, [BLK], "tlB%d" % i) for i in range(2)]
            ysb = [bv.get(F32, [D], "ysb%d" % i, 2) for i in range(2)]
            oneh = bv.get(F32, [NE], "oneh")
            b1tmp = bv.get(F32, [16, NE], "b1tmp")
            b1sel = [bv.get(F32, [16], "b1sel%d" % i) for i in range(2)]
            print("arena bytes: blocks", bv.off)
            w1_flat = w1_2d
            w2_flat = w2_2d
            b1v = pv.ap[:, C_B1:C_B1 + NE * 16].rearrange("p (e i) -> p i e", i=16)
            ps6 = [0]

            def nps6():
                bk = PS[ps6[0] % 6]
                ps6[0] += 1
                return bk

            bc_cache = {}

            def bc_reg(e):
                if "r" not in bc_cache:
                    bc_cache["r"] = e.to_reg(NE * D - 1)
                return bc_cache["r"]

            def load_w1(b):
                wa = W1b[b % 2]
                for k in range(KC):
                    DMA("pool", lambda e, b=b, k=k, wa=wa: e.indirect_dma_start(
                        out=wa.ap[:, k, :], out_offset=None, in_=w1_flat[:, :],
                        in_offset=bass.IndirectOffsetOnAxis(ap=widx.ap[:, b, k:k + 1], axis=0),
                        bounds_check=bc_reg(e), oob_is_err=False), r=[widx], w=[wa[k]])

            def load_w2(b):
                wb2 = W2b[b % 2]
                for k in range(KC):
                    DMA("pool", lambda e, b=b, k=k, wb2=wb2: e.indirect_dma_start(
                        out=wb2.ap[:, k, :], out_offset=None, in_=w2_flat[:, :],
                        in_offset=bass.IndirectOffsetOnAxis(ap=widx.ap[:, b, k:k + 1], axis=0),
                        bounds_check=bc_reg(e), oob_is_err=False), r=[widx], w=[wb2[k]])

            def prep(b):
                xbT = xbTs[b % 2]
                DMA("sp", lambda e, b=b: e.dma_start(out=xrows.ap, in_=xs_d[b * BLK:(b + 1) * BLK, :].rearrange("(j p) d -> p j d", p=P)),
                    r=[XsB], w=[xrows])
                for k in range(KC):
                    pt = PT[k % 2]
                    for j in range(4):
                        T(lambda e, pt=pt, j=j, k=k: e.transpose(pt.ap[:, j * P:(j + 1) * P], xrows.ap[:, j, k * P:(k + 1) * P], identb.ap),
                          r=[xrows[j], identb], w=[pt])
                    if k % 2 == 0:
                        A(lambda e, pt=pt, k=k, xbT=xbT: e.activation(out=xbT.ap[:, k, :], in_=pt.ap[:, 0:BLK], func=AF.Identity), r=[pt], w=[xbT[k]])
                    else:
                        V(lambda e, pt=pt, k=k, xbT=xbT: e.tensor_copy(out=xbT.ap[:, k, :], in_=pt.ap[:, 0:BLK]), r=[pt], w=[xbT[k]])
                bs = b1sel[b % 2]
                V(lambda e, b=b: e.tensor_scalar(out=oneh.ap, in0=iotae, scalar1=bef.ap[:, b:b + 1], scalar2=None, op0=ALU.is_equal),
                  r=[cst, bef], w=[oneh])
                V(lambda e: e.tensor_tensor(out=b1tmp.ap, in0=b1v, in1=oneh.ap.unsqueeze(1).broadcast_to([P, 16, NE]), op=ALU.mult),
                  r=[pv, oneh], w=[b1tmp])
                V(lambda e, bs=bs: e.reduce_sum(out=bs.ap, in_=b1tmp.ap, axis=mybir.AxisListType.X), r=[b1tmp], w=[bs])
                V(lambda e, bs=bs: e.tensor_scalar(out=bs.ap[:, 8:16], in0=bs.ap[:, 8:16], scalar1=1.0, scalar2=None, op0=ALU.add), r=[bs], w=[bs])

            def w1_piece(b, i):
                wa, xbT, bs, aT = W1b[b % 2], xbTs[b % 2], b1sel[b % 2], actB[b % 2]
                psg = nps6()
                psl = nps6()
                for k in range(KC):
                    T(lambda e, k=k: e.matmul(psg.ap, lhsT=wa.ap[:, k, i * P:(i + 1) * P], rhs=xbT.ap[:, k, :],
                                              start=(k == 0), stop=(k == KC - 1)), r=[wa, xbT], w=[psg])
                for k in range(KC):
                    T(lambda e, k=k: e.matmul(psl.ap, lhsT=wa.ap[:, k, D + i * P:D + (i + 1) * P], rhs=xbT.ap[:, k, :],
                                              start=(k == 0), stop=(k == KC - 1)), r=[wa, xbT], w=[psl])
                a_, b_, c_ = tgB[i % 2], tsB[i % 2], tlB[i % 2]
                V(lambda e: e.tensor_scalar(out=a_.ap, in0=psg.ap, scalar1=bs.ap[:, i:i + 1], scalar2=7.0, op0=ALU.add, op1=ALU.min),
                  r=[psg, bs], w=[a_])
                A(lambda e: e.activation(out=b_.ap, in_=a_.ap, func=AF.Silu, scale=1.702), r=[a_], w=[b_])
                V(lambda e: e.tensor_scalar(out=c_.ap, in0=psl.ap, scalar1=bs.ap[:, 8 + i:9 + i], scalar2=8.0, op0=ALU.add, op1=ALU.min),
                  r=[psl, bs], w=[c_])
                V(lambda e: e.scalar_tensor_tensor(out=aT.ap[:, i, :], in0=c_.ap, scalar=-6.0, in1=b_.ap, op0=ALU.max, op1=ALU.mult),
                  r=[b_, c_], w=[aT[i]])

            def w2_piece(b, g):
                j4, half = g // 2, g % 2
                wb2, aT, yb = W2b[b % 2], actB[b % 2], ysb[j4 % 2]
                ps = nps6()
                for i in range(KC):
                    T(lambda e, i=i: e.matmul(ps.ap, lhsT=aT.ap[:, i, j4 * P:(j4 + 1) * P], rhs=wb2.ap[:, i, half * 512:(half + 1) * 512],
                                              start=(i == 0), stop=(i == KC - 1)), r=[aT, wb2], w=[ps])
                if half == 0:
                    A(lambda e: e.activation(out=yb.ap[:, 0:512], in_=ps.ap, func=AF.Identity, scale=1.0 / 1.702), r=[ps], w=[yb[0]])
                else:
                    A(lambda e: e.activation(out=yb.ap[:, 512:1024], in_=ps.ap, func=AF.Identity, scale=1.0 / 1.702), r=[ps], w=[yb[1]])
                    r0 = b * BLK + j4 * P
                    DMA("sp", lambda e: e.dma_start(out=ys_d[r0:r0 + P, :], in_=yb.ap), r=[yb], w=[YsB[b]])

            nblocks = NB if stage == 99 else int(os.environ.get("KNB", NB))
            if nblocks:
                load_w1(0)
                load_w2(0)
                prep(0)
                if nblocks > 1:
                    load_w1(1)
            for b in range(nblocks):
                for i in range(KC):
                    w1_piece(b, i)
                    if i == 1 and b + 1 < nblocks:
                        prep(b + 1)
                    if b > 0:
                        w2_piece(b - 1, i)
                if b + 2 < nblocks:
                    load_w1(b + 2)
                if b + 1 < nblocks:
                    load_w2(b + 1)
            if nblocks:
                for g in range(8):
                    w2_piece(nblocks - 1, g)
            barrier(None)


            cb = Carver(shared_end)
            Yk2 = [[cb.get(F32, [D], "Yk%d_%d" % (s_, i)) for i in range(4)] for s_ in range(2)]
            accs = cb.get(F32, [D], "accs", 2)
            cx1 = [cb.get(F32, [D], "cx1%d" % i) for i in range(2)]
            cxo = cb.get(F32, [D], "cxo")
            cjunk = cb.get(BF16, [D], "cjunk")
            cgT = cb.get(BF16, [P], "cgT")
            for st in range(nst):
                r0 = st * P
                xb = cx1[st % 2]
                Yk = Yk2[st % 2]
                DMA("sp", lambda e, xb=xb, r0=r0: e.dma_start(out=xb.ap, in_=x1_d[r0:r0 + P, :]), w=[xb])
                for k in range(4):
                    DMA("pool", lambda e, st=st, k=k, Yk=Yk: e.indirect_dma_start(
                        out=Yk[k].ap, out_offset=None, in_=ys_d[:, :],
                        in_offset=bass.IndirectOffsetOnAxis(ap=desti.ap[:, st, k:k + 1], axis=0)), r=[desti, YsB], w=[Yk[k]])
                T(lambda e, st=st: e.transpose(PSM.ap[0:NE, 0:P], gates.ap[:, st, :], ident_f), r=[gates[st], cst], w=[PSM])
                V(lambda e: e.tensor_copy(out=cgT.ap[0:NE, :], in_=PSM.ap[0:NE, 0:P]), r=[PSM], w=[cgT])
                for half in range(2):
                    ps = nps()
                    T(lambda e, ps=ps, half=half: e.matmul(ps.ap, lhsT=cgT.ap[0:NE, :], rhs=b2b.ap[0:NE, half * 512:(half + 1) * 512],
                                                          start=True, stop=True), r=[cgT, b2b], w=[ps])
                    A(lambda e, ps=ps, half=half: e.activation(out=accs.ap[:, half * 512:(half + 1) * 512], in_=ps.ap, func=AF.Identity),
                      r=[ps], w=[accs[half]])
                for k in range(4):
                    V(lambda e, st=st, k=k, Yk=Yk: e.scalar_tensor_tensor(out=accs.ap, in0=Yk[k].ap, scalar=gk.ap[:, st, k:k + 1], in1=accs.ap,
                                                                   op0=ALU.mult, op1=ALU.add), r=[Yk[k], gk[st], accs], w=[accs])
                V(lambda e: e.tensor_tensor(out=cxo.ap, in0=accs.ap, in1=ga2_bc.ap, op=ALU.mult), r=[accs, ga2_bc], w=[cxo])
                V(lambda e, xb=xb: e.tensor_tensor(out=cxo.ap, in0=cxo.ap, in1=xb.ap, op=ALU.add), r=[cxo, xb], w=[cxo])
                A(lambda e: e.activation(out=cjunk.ap, in_=cxo.ap, func=AF.Square, accum_out=small.ap[:, 32:33]), r=[cxo], w=[cjunk, small[32]])
                A(lambda e: e.activation(out=small.ap[:, 33:34], in_=small.ap[:, 32:33], func=AF.Sqrt, scale=1.0 / D, bias=EPS), r=[small[32]], w=[small[33]])
                V(lambda e: e.reciprocal(out=small.ap[:, 34:35], in_=small.ap[:, 33:34]), r=[small[33]], w=[small[34]])
                V(lambda e, xb=xb: e.scalar_tensor_tensor(out=xb.ap, in0=cxo.ap, scalar=small.ap[:, 34:35], in1=gfin_bc.ap, op0=ALU.mult, op1=ALU.mult),
                  r=[cxo, small[34], gfin_bc], w=[xb])
                DMA("sp", lambda e, xb=xb, r0=r0: e.dma_start(out=y_d[r0:r0 + P, :], in_=xb.ap), r=[xb])

        w1_v = w1_d.rearrange("e (k p) n -> e p k n", p=P)
        w2_v = w2_d.rearrange("e (k p) n -> e p k n", p=P)
        h2_v = h2_d.rearrange("p (k t) -> p k t", k=KC)

        def load_w1(e_, i):
            DMA("pool", lambda en: en.dma_start(out=W1[i].ap[:, :, 0:256], in_=w1_v[e_][:, :, i * 256:(i + 1) * 256]), w=[W1[i]])
            DMA("pool", lambda en: en.dma_start(out=W1[i].ap[:, :, 256:512], in_=w1_v[e_][:, :, D + i * 256:D + (i + 1) * 256]), w=[W1[i]])

        def load_w2(e_):
            DMA("pool", lambda en: en.dma_start(out=W2.ap, in_=w2_v[e_]), w=[W2])

        seq = [(q, e_) for q in range(NQ) for e_ in range(NE)]
        if stage <= 1 or SPARSE:
            seq = []
        if stage in (2, 3):
            seq = [(0, e_) for e_ in range(NE)]
        if SPARSE:
            seq = []
        if seq:
            for i in range(4):
                load_w1(0, i)
            load_w2(0)
        for si, (q, e_) in enumerate(seq):
            nxt = seq[si + 1][1] if si + 1 < len(seq) else None
            if e_ == 0:
                DMA("sp", lambda en, q=q: en.dma_start(out=h2q.ap, in_=h2_v[:, :, q * QT:(q + 1) * QT]), w=[h2q])
                for j in range(QT // P):
                    st = q * (QT // P) + j
                    T(lambda en, st=st: en.transpose(PSM.ap[0:NE, 0:P], gates.ap[:, st, :], ident_f), r=[gates[st], cst], w=[PSM])
                    V(lambda en: en.tensor_copy(out=gTb.ap[0:NE, :], in_=PSM.ap[0:NE, 0:P]), r=[PSM], w=[gTb])
                    for half in range(2):
                        ps = nps()
                        T(lambda en, ps=ps, half=half: en.matmul(ps.ap, lhsT=gTb.ap[0:NE, :], rhs=b2b.ap[0:NE, half * 512:(half + 1) * 512],
                                                                start=True, stop=True), r=[gTb, b2b], w=[ps])
                        A(lambda en, ps=ps, j=j, half=half: en.activation(out=acc.ap[:, j, half * 512:(half + 1) * 512], in_=ps.ap, func=AF.Identity),
                          r=[ps], w=[acc[j * 2 + half]])
            for blk in range(QT // 512):
                tsl = slice(blk * 512, (blk + 1) * 512)
                aT = actT[blk % 2]
                for i in range(KC):
                    g4, sub = i // 2, i % 2
                    wb = W1[g4]
                    psg = nps()
                    psl = nps()
                    for k in range(KC):
                        T(lambda en, psg=psg, k=k, wb=wb, sub=sub, tsl=tsl: en.matmul(psg.ap, lhsT=wb.ap[:, k, sub * P:(sub + 1) * P], rhs=h2q.ap[:, k, tsl],
                                                                                 start=(k == 0), stop=(k == KC - 1)), r=[wb, h2q], w=[psg])
                    for k in range(KC):
                        T(lambda en, psl=psl, k=k, wb=wb, sub=sub, tsl=tsl: en.matmul(psl.ap, lhsT=wb.ap[:, k, 256 + sub * P:256 + (sub + 1) * P], rhs=h2q.ap[:, k, tsl],
                                                                                 start=(k == 0), stop=(k == KC - 1)), r=[wb, h2q], w=[psl])
                    if blk == QT // 512 - 1 and sub == 1 and nxt is not None:
                        load_w1(nxt, g4)
                    cg = C_B1 + e_ * 16 + i
                    cl = C_B1 + e_ * 16 + 8 + i
                    a_, b_, c_ = tg[i % 2], tsg[i % 2], tl[i % 2]
                    V(lambda en, psg=psg, cg=cg, a_=a_: en.tensor_scalar(out=a_.ap, in0=psg.ap, scalar1=pv.ap[:, cg:cg + 1], scalar2=7.0, op0=ALU.add, op1=ALU.min),
                      r=[psg, pv], w=[a_])
                    A(lambda en, a_=a_, b_=b_: en.activation(out=b_.ap, in_=a_.ap, func=AF.Sigmoid, scale=1.702), r=[a_], w=[b_])
                    V(lambda en, psl=psl, cl=cl, c_=c_: en.tensor_scalar(out=c_.ap, in0=psl.ap, scalar1=pv.ap[:, cl:cl + 1], scalar2=7.0, op0=ALU.add, op1=ALU.min),
                      r=[psl, pv], w=[c_])
                    G(lambda en, c_=c_: en.tensor_scalar(out=c_.ap, in0=c_.ap, scalar1=-7.0, scalar2=1.0, op0=ALU.max, op1=ALU.add), r=[c_], w=[c_])
                    G(lambda en, a_=a_, b_=b_: en.tensor_tensor(out=a_.ap, in0=a_.ap, in1=b_.ap, op=ALU.mult), r=[a_, b_], w=[a_])
                    V(lambda en, a_=a_, c_=c_, aT=aT, i=i: en.tensor_tensor(out=aT.ap[:, i, :], in0=a_.ap, in1=c_.ap, op=ALU.mult), r=[a_, c_], w=[aT[i]])
                for j4 in range(4):
                    j = blk * 4 + j4
                    st = q * (QT // P) + j
                    for half in range(2):
                        ps = nps()
                        for i in range(KC):
                            T(lambda en, ps=ps, i=i, j4=j4, half=half, aT=aT: en.matmul(ps.ap, lhsT=aT.ap[:, i, j4 * P:(j4 + 1) * P],
                                                                                     rhs=W2.ap[:, i, half * 512:(half + 1) * 512],
                                                                                     start=(i == 0), stop=(i == KC - 1)), r=[aT, W2], w=[ps])
                        V(lambda en, ps=ps, j=j, half=half, st=st, e_=e_: en.scalar_tensor_tensor(
                            out=acc.ap[:, j, half * 512:(half + 1) * 512], in0=ps.ap, scalar=gates.ap[:, st, e_:e_ + 1],
                            in1=acc.ap[:, j, half * 512:(half + 1) * 512], op0=ALU.mult, op1=ALU.add), r=[ps, gates[st], acc[j * 2 + half]], w=[acc[j * 2 + half]])
            if nxt is not None:
                load_w2(nxt)
            if e_ == NE - 1:
                for j in range(QT // P):
                    r0 = q * QT + j * P
                    xb = x1t[j % 2]
                    DMA("sp", lambda en, xb=xb, r0=r0: en.dma_start(out=xb.ap, in_=x1_d[r0:r0 + P, :]), w=[xb])
                    V(lambda en, j=j: en.tensor_tensor(out=xo.ap, in0=acc.ap[:, j, :], in1=ga2_bc.ap, op=ALU.mult), r=[acc[slice(2 * j, 2 * j + 2)], ga2_bc], w=[xo])
                    G(lambda en, xb=xb: en.tensor_tensor(out=xo.ap, in0=xo.ap, in1=xb.ap, op=ALU.add), r=[xo, xb], w=[xo])
                    A(lambda en: en.activation(out=ejunk.ap, in_=xo.ap, func=AF.Square, accum_out=small.ap[:, 32:33]), r=[xo], w=[ejunk, small[32]])
                    A(lambda en: en.activation(out=small.ap[:, 33:34], in_=small.ap[:, 32:33], func=AF.Sqrt, scale=1.0 / D, bias=EPS), r=[small[32]], w=[small[33]])
                    V(lambda en: en.reciprocal(out=small.ap[:, 34:35], in_=small.ap[:, 33:34]), r=[small[33]], w=[small[34]])
                    V(lambda en, xb=xb: en.scalar_tensor_tensor(out=xb.ap, in0=xo.ap, scalar=small.ap[:, 34:35], in1=gfin_bc.ap, op0=ALU.mult, op1=ALU.mult),
                      r=[xo, small[34], gfin_bc], w=[xb])
                    DMA("sp", lambda en, xb=xb, r0=r0: en.dma_start(out=y_d[r0:r0 + P, :], in_=xb.ap), r=[xb])

        S.finish()
        print("ops:", {e: len(v) for e, v in S.ops.items()})
        with nc.Block() as block:
            @block.sync
            def _(e):
                S.run("sp", e)

            @block.scalar
            def _(e):
                S.run("act", e)

            @block.vector
            def _(e):
                S.run("dve", e)

            @block.gpsimd
            def _(e):
                S.run("pool", e)

            @block.tensor
            def _(e):
                S.run("pe", e)
    nc._dbg_names = DBG
    return nc


def _fm(v, n):
    return np.ascontiguousarray(np.asarray(v, np.float32).reshape(n, P).T)


def _consts():
    cst = np.zeros((P, CW), np.float32)
    cst[:, 0:128] = np.eye(P, dtype=np.float32)
    s = np.arange(P)[:, None]
    t = np.arange(P)[None, :]
    cst[:, 128:256] = ((s // 64 == t // 64) & (s <= t)).astype(np.float32)
    rm = np.ones((P, 256), np.float32)
    rm[:, ::64] = 0.0
    cst[:, 256:512] = rm
    cst[:, 512:640] = (s < t).astype(np.float32)
    cst[:, 640:704] = np.arange(NB, dtype=np.float32)[None, :] * BLK
    cst[:, 704:736] = np.arange(NE, dtype=np.float32)[None, :]
    cst[:, 736:744] = np.arange(KC, dtype=np.float32)[None, :] * P + np.arange(P, dtype=np.float32)[:, None]
    return cst


def make_in_maps(x, c, w_ada, b_ada, g_mix, w_in, conv_dw, conv_dw_bias, conv_ln_g, conv_ln_b,
                 w_conv_out, lb_param, hgrn_norm_g, w_hgrn_out, w_out, g_ffn, w_router, b_router,
                 w1, b1, w2, b2, g_final, cores=range(8)):
    f = lambda a: np.ascontiguousarray(np.asarray(a, np.float32))
    x = f(x)
    cst = _consts()
    dwT = np.ascontiguousarray(f(conv_dw)[0].T.reshape(KC, P, 31).transpose(1, 0, 2).reshape(P, KC * 31))
    b1T = np.ascontiguousarray(f(b1)[0].reshape(NE, 16, P).transpose(2, 0, 1).reshape(P, NE * 16))
    common = {
        "cst": cst,
        "w_ada": f(w_ada)[0], "b_ada": f(b_ada)[0:1], "w_in": f(w_in)[0],
        "w_conv_out": f(w_conv_out)[0], "w_hgrn_out": f(w_hgrn_out)[0], "w_out": f(w_out)[0],
        "w_router": f(w_router)[0], "b_router": f(b_router)[0:1],
        "w1": f(w1)[0].reshape(NE * D, 2 * D), "w2": f(w2)[0].reshape(NE * D, D), "b2": f(b2)[0], "g_final": f(g_final).reshape(1, D),
        "g_ffn": f(g_ffn)[0:1],
    }
    in_maps = []
    for core in cores:
        b, half = core // 2, core % 2
        pvec = np.zeros((P, RV), np.float32)
        pvec[:, C_C:C_C + 8] = _fm(np.asarray(c)[b], 8)
        pvec[:, C_BADA:C_BADA + 48] = _fm(np.asarray(b_ada)[0], 48)
        pvec[:, C_GMIX:C_GMIX + 8] = _fm(np.asarray(g_mix)[0], 8)
        pvec[:, C_DWB:C_DWB + 8] = _fm(np.asarray(conv_dw_bias)[0], 8)
        pvec[:, C_LNG:C_LNG + 8] = _fm(np.asarray(conv_ln_g)[0], 8)
        pvec[:, C_LNB:C_LNB + 8] = _fm(np.asarray(conv_ln_b)[0], 8)
        pvec[:, C_LB0:C_LB0 + 8] = _fm(np.asarray(lb_param)[0], 8)
        pvec[:, C_LB1:C_LB1 + 8] = _fm(np.asarray(lb_param)[1], 8)
        pvec[:, C_GFFN:C_GFFN + 8] = _fm(np.asarray(g_ffn)[0], 8)
        pvec[:, C_DW:C_DW + 248] = dwT
        pvec[:, C_B1:C_B1 + 512] = b1T
        pvec[:, C_NG] = np.asarray(hgrn_norm_g, np.float32)[0]
        m = dict(common)
        m["xm"] = np.ascontiguousarray(x[b, half * NTOK:(half + 1) * NTOK])
        m["xp"] = np.ascontiguousarray(x[b, 0:NTOK]) if half == 1 else np.zeros((NTOK, D), np.float32)
        m["flag"] = np.full((P, 1), float(half), np.float32)
        m["pvec"] = pvec
        in_maps.append(m)
    return in_maps


_NC = None


def kernel(**inputs):
    global _NC
    in_maps = make_in_maps(**inputs)
    if _NC is None:
        _NC = build_nc()
    res = run_bass_kernel_spmd(_NC, in_maps, core_ids=list(range(8)))
    out = np.zeros((4, 2 * NTOK, D), np.float32)
    for core in range(8):
        b, half = core // 2, core % 2
        out[b, half * NTOK:(half + 1) * NTOK] = np.asarray(res.results[core]["y"], np.float32)
    return out
```
